# Optimizing a Trainium2 kernel written in Bass

```python
import math
import jax, jax.numpy as jnp
from jax import lax
import numpy as np

D_MODEL = 1024
BATCH = 2
SEQ = 8192
DEPTH = 2

CHUNK = 64
WIDTH_A = D_MODEL // 2
HEAD_DIM_A = 128
N_HEADS_A = WIDTH_A // HEAD_DIM_A
CONV_K = 4
WIDTH_B = D_MODEL - WIDTH_A
HEAD_DIM_B = 64
N_HEADS_B = WIDTH_B // HEAD_DIM_B
IDX_HEADS = 8
IDX_DIM = 64
TOPK_KEYS_MAX = 256
Q_BLOCK = 128
REL_BUCKETS = 32
REL_MAX_DIST = 1024
N_EXPERTS = 32
TOP_K = 4
D_FF_EXPERT = 1024
SWIGLU_ALPHA = 1.702
SWIGLU_LIMIT = 7.0
MOE_BLOCK = 128
EPS = 1e-6
IN_SPLITS = (3 * WIDTH_A, WIDTH_A, N_HEADS_A, N_HEADS_A, 3 * WIDTH_B, IDX_HEADS * IDX_DIM, IDX_DIM, IDX_HEADS)
D_IN = 3 * WIDTH_A + WIDTH_A + 2 * N_HEADS_A + 3 * WIDTH_B + IDX_HEADS * IDX_DIM + IDX_DIM + IDX_HEADS

kernel_name = "hybrid_gdn_dsa_moe_block"


def rms_norm(x, w):
    xf = x.astype(jnp.float32)
    y = xf * lax.rsqrt(jnp.mean(xf * xf, axis=-1, keepdims=True) + EPS)
    return (y * w.astype(jnp.float32)).astype(x.dtype)


def l2_norm(x):
    return x * lax.rsqrt(jnp.sum(x * x, axis=-1, keepdims=True) + EPS)


def t5_bucket(rel):
    nb = REL_BUCKETS // 2
    max_exact = nb // 2
    side = jnp.where(rel > 0, nb, 0)
    n = jnp.abs(rel)
    nf = jnp.maximum(n, 1).astype(jnp.float32)
    large = max_exact + (jnp.log(nf / max_exact) / math.log(REL_MAX_DIST / max_exact) * (nb - max_exact)).astype(jnp.int32)
    large = jnp.minimum(large, nb - 1)
    return side + jnp.where(n < max_exact, n, large)


def causal_short_conv(u, w):
    S = u.shape[1]
    up = jnp.pad(u, ((0, 0), (CONV_K - 1, 0), (0, 0)))
    y = up[:, 0:S] * w[0]
    for j in range(1, CONV_K):
        y = y + up[:, j:j + S] * w[j]
    return jax.nn.silu(y)


def gated_deltanet(qkv, z, b_lin, a_lin, conv_w, a_log, dt_bias, norm_w):
    f32 = jnp.float32
    Bsz, S, _ = qkv.shape
    N = S // CHUNK
    u = causal_short_conv(qkv, conv_w).astype(f32)
    q, k, v = jnp.split(u, 3, axis=-1)

    def heads_chunks(t):
        return t.reshape(Bsz, N, CHUNK, N_HEADS_A, HEAD_DIM_A).transpose(0, 3, 1, 2, 4)

    def to_bhnc(t):
        return t.reshape(Bsz, N, CHUNK, N_HEADS_A).transpose(0, 3, 1, 2)

    q = l2_norm(heads_chunks(q)) * HEAD_DIM_A ** -0.5
    k = l2_norm(heads_chunks(k))
    v = heads_chunks(v)
    beta = to_bhnc(jax.nn.sigmoid(b_lin.astype(f32)))
    g = -jnp.exp(a_log.astype(f32)) * jax.nn.softplus(a_lin.astype(f32) + dt_bias.astype(f32))
    gc = jnp.cumsum(to_bhnc(g), axis=-1)
    causal = jnp.tril(jnp.ones((CHUNK, CHUNK), bool))
    strict = jnp.tril(jnp.ones((CHUNK, CHUNK), bool), -1)
    decay = jnp.exp(jnp.where(causal, gc[..., :, None] - gc[..., None, :], -jnp.inf))
    k_beta = k * beta[..., None]
    lmat = jnp.where(strict, jnp.einsum('bhncd,bhnsd->bhncs', k_beta, k) * decay, 0.0)
    rhs = jnp.concatenate([v * beta[..., None], k_beta * jnp.exp(gc)[..., None]], axis=-1)
    sol = lax.linalg.triangular_solve(lmat + jnp.eye(CHUNK, dtype=f32), rhs,
                                      left_side=True, lower=True, unit_diagonal=True)
    u_val, w_dec = sol[..., :HEAD_DIM_A], sol[..., HEAD_DIM_A:]
    attn = jnp.einsum('bhncd,bhnsd->bhncs', q, k) * decay

    def step(state, inp):
        q_n, k_n, u_n, w_n, gc_n, attn_n = inp
        v_new = u_n - jnp.einsum('bhck,bhkv->bhcv', w_n, state)
        o = (jnp.einsum('bhck,bhkv->bhcv', q_n * jnp.exp(gc_n)[..., None], state)
             + jnp.einsum('bhcs,bhsv->bhcv', attn_n, v_new))
        g_last = gc_n[..., -1]
        k_dec = k_n * jnp.exp(g_last[..., None] - gc_n)[..., None]
        state = state * jnp.exp(g_last)[..., None, None] + jnp.einsum('bhck,bhcv->bhkv', k_dec, v_new)
        return state, o

    xs = (jnp.moveaxis(q, 2, 0), jnp.moveaxis(k, 2, 0), jnp.moveaxis(u_val, 2, 0),
          jnp.moveaxis(w_dec, 2, 0), jnp.moveaxis(gc, 2, 0), jnp.moveaxis(attn, 2, 0))
    state0 = jnp.zeros((Bsz, N_HEADS_A, HEAD_DIM_A, HEAD_DIM_A), f32)
    _, o = lax.scan(step, state0, xs)
    o = o.transpose(1, 0, 3, 2, 4).reshape(Bsz, S, N_HEADS_A, HEAD_DIM_A)
    zh = z.reshape(Bsz, S, N_HEADS_A, HEAD_DIM_A).astype(f32)
    o = rms_norm(o, norm_w) * jax.nn.silu(zh)
    return o.reshape(Bsz, S, WIDTH_A).astype(qkv.dtype)


def dsa_attention(qkv_b, q_idx, k_idx, w_idx, q_norm_w, k_norm_w, rel_bias):
    f32 = jnp.float32
    Bsz, S, _ = qkv_b.shape
    k_sel = min(TOPK_KEYS_MAX, S // 4)
    nblk = S // Q_BLOCK
    q, k, v = jnp.split(qkv_b, 3, axis=-1)
    q = rms_norm(q.reshape(Bsz, S, N_HEADS_B, HEAD_DIM_B), q_norm_w)
    k = rms_norm(k.reshape(Bsz, S, N_HEADS_B, HEAD_DIM_B), k_norm_w)
    v = v.reshape(Bsz, S, N_HEADS_B, HEAD_DIM_B)
    q_idx = q_idx.reshape(Bsz, S, IDX_HEADS, IDX_DIM)
    w_idx = w_idx * (IDX_HEADS ** -0.5 * IDX_DIM ** -0.5)
    key_chunk = jnp.arange(S) // CHUNK
    bidx = jnp.arange(Bsz)[:, None, None]

    def blocks(t):
        return jnp.moveaxis(t.reshape((Bsz, nblk, Q_BLOCK) + t.shape[2:]), 1, 0)

    def one_block(args):
        blk, q_b, qi_b, w_b = args
        t_pos = blk * Q_BLOCK + jnp.arange(Q_BLOCK)
        q_chunk = t_pos // CHUNK
        dots = jnp.einsum('bqhd,bsd->bqhs', qi_b, k_idx)
        score = jnp.einsum('bqhs,bqh->bqs', jax.nn.relu(dots), w_b).astype(f32)
        admissible = key_chunk[None, :] <= q_chunk[:, None]
        score = jnp.where(admissible[None], score, -jnp.inf)
        _, sel = lax.top_k(score, k_sel)
        valid = (sel // CHUNK) <= q_chunk[None, :, None]
        k_g = k[bidx, sel]
        v_g = v[bidx, sel]
        bias = rel_bias[t5_bucket(sel - t_pos[None, :, None])]
        logits = (jnp.einsum('bqhd,bqkhd->bqhk', q_b, k_g).astype(f32) * HEAD_DIM_B ** -0.5
                  + jnp.moveaxis(bias, -1, 2).astype(f32))
        logits = jnp.where(valid[:, :, None, :], logits, -jnp.inf)
        p = jax.nn.softmax(logits, axis=-1).astype(v.dtype)
        return jnp.einsum('bqhk,bqkhd->bqhd', p, v_g)

    out = lax.map(one_block, (jnp.arange(nblk), blocks(q), blocks(q_idx), blocks(w_idx)))
    return jnp.moveaxis(out, 0, 1).reshape(Bsz, S, WIDTH_B)


def clamped_swiglu(u):
    u_glu, u_lin = u[..., ::2], u[..., 1::2]
    u_glu = jnp.minimum(u_glu, SWIGLU_LIMIT)
    u_lin = jnp.clip(u_lin, -SWIGLU_LIMIT, SWIGLU_LIMIT)
    return u_glu * jax.nn.sigmoid(SWIGLU_ALPHA * u_glu) * (u_lin + 1.0)


def moe_ffn(h, router_w, router_b, w1, b1, w2, b2):
    T, D = h.shape
    TK = T * TOP_K
    logits = (h @ router_w + router_b).astype(jnp.float32)
    top_val, top_idx = lax.top_k(logits, TOP_K)
    gate = jax.nn.softmax(top_val, axis=-1)
    flat_e = top_idx.reshape(-1)
    flat_tok = jnp.arange(TK) // TOP_K
    order = jnp.argsort(flat_e)
    sorted_e = flat_e[order]
    sorted_tok = flat_tok[order]
    gate_sorted = gate.reshape(-1)[order]
    counts = jnp.bincount(flat_e, length=N_EXPERTS)
    padded_counts = (counts + MOE_BLOCK - 1) // MOE_BLOCK * MOE_BLOCK
    group_start = jnp.cumsum(counts) - counts
    padded_end = jnp.cumsum(padded_counts)
    padded_start = padded_end - padded_counts
    dest = padded_start[sorted_e] + (jnp.arange(TK) - group_start[sorted_e])
    n_blocks = -(-TK // MOE_BLOCK) + N_EXPERTS
    P = n_blocks * MOE_BLOCK
    buf_tok = jnp.zeros((P,), jnp.int32).at[dest].set(sorted_tok)
    block_e = jnp.minimum(jnp.searchsorted(padded_end, jnp.arange(n_blocks) * MOE_BLOCK, side='right'), N_EXPERTS - 1)
    x_buf = h[buf_tok].reshape(n_blocks, MOE_BLOCK, D)

    def block_ffn(args):
        xb, e = args
        act = clamped_swiglu(xb @ w1[e] + b1[e])
        return act @ w2[e] + b2[e]

    y_buf = lax.map(block_ffn, (x_buf, block_e)).reshape(P, D)
    contrib = gate_sorted[:, None].astype(h.dtype) * y_buf[dest]
    return jnp.zeros((T, D), h.dtype).at[sorted_tok].add(contrib)


def setup_inputs(seed: int = 0) -> dict:
    key = jax.random.key(seed)
    ks = jax.random.split(key, 24)
    f32 = jnp.float32
    nrm = lambda k, shape, s: jax.random.normal(k, shape, f32) * s
    D = D_MODEL
    return {
        'x': nrm(ks[0], (BATCH, SEQ, D), 1.0),
        'c': nrm(ks[1], (BATCH, D), 1.0),
        'rel_bias': nrm(ks[2], (REL_BUCKETS, N_HEADS_B), 0.5),
        'mod_w': nrm(ks[3], (DEPTH, D, 6 * D), 0.5 * D ** -0.5),
        'mod_b': nrm(ks[4], (DEPTH, 6 * D), 0.02),
        'norm_mix_w': 1.0 + nrm(ks[5], (DEPTH, D), 0.02),
        'norm_ffn_w': 1.0 + nrm(ks[6], (DEPTH, D), 0.02),
        'w_in': nrm(ks[7], (DEPTH, D, D_IN), D ** -0.5),
        'conv_w': nrm(ks[8], (DEPTH, CONV_K, 3 * WIDTH_A), 0.5),
        'a_log': jnp.log(jax.random.uniform(ks[9], (DEPTH, N_HEADS_A), f32, 1.0, 16.0)),
        'dt_bias': nrm(ks[10], (DEPTH, N_HEADS_A), 0.1),
        'gdn_norm_w': 1.0 + nrm(ks[11], (DEPTH, HEAD_DIM_A), 0.02),
        'q_norm_w': 1.0 + nrm(ks[12], (DEPTH, HEAD_DIM_B), 0.02),
        'k_norm_w': 1.0 + nrm(ks[13], (DEPTH, HEAD_DIM_B), 0.02),
        'w_out': nrm(ks[14], (DEPTH, D, D), D ** -0.5),
        'router_w': nrm(ks[15], (DEPTH, D, N_EXPERTS), D ** -0.5),
        'router_b': nrm(ks[16], (DEPTH, N_EXPERTS), 0.01),
        'w1': nrm(ks[17], (DEPTH, N_EXPERTS, D, 2 * D_FF_EXPERT), D ** -0.5),
        'b1': nrm(ks[18], (DEPTH, N_EXPERTS, 2 * D_FF_EXPERT), 0.02),
        'w2': nrm(ks[19], (DEPTH, N_EXPERTS, D_FF_EXPERT, D), D_FF_EXPERT ** -0.5),
        'b2': nrm(ks[20], (DEPTH, N_EXPERTS, D), 0.02),
    }


def reference(x, c, rel_bias, mod_w, mod_b, norm_mix_w, norm_ffn_w, w_in, conv_w, a_log, dt_bias,
              gdn_norm_w, q_norm_w, k_norm_w, w_out, router_w, router_b, w1, b1, w2, b2):
    Bsz, S, D = x.shape
    split_pts = np.cumsum(IN_SPLITS)[:-1].tolist()
    for l in range(DEPTH):
        mod = (jax.nn.silu(c) @ mod_w[l] + mod_b[l])[:, None, :]
        sh1, sc1, g1, sh2, sc2, g2 = jnp.split(mod, 6, axis=-1)
        h = rms_norm(x, norm_mix_w[l]) * (1.0 + sc1) + sh1
        proj = h @ w_in[l]
        qkv_a, z_a, b_a, a_a, qkv_b, qi_b, ki_b, wi_b = jnp.split(proj, split_pts, axis=-1)
        y_a = gated_deltanet(qkv_a, z_a, b_a, a_a, conv_w[l], a_log[l], dt_bias[l], gdn_norm_w[l])
        y_b = dsa_attention(qkv_b, qi_b, ki_b, wi_b, q_norm_w[l], k_norm_w[l], rel_bias)
        x = x + g1 * (jnp.concatenate([y_a, y_b], axis=-1) @ w_out[l])
        h = rms_norm(x, norm_ffn_w[l]) * (1.0 + sc2) + sh2
        y = moe_ffn(h.reshape(Bsz * S, D), router_w[l], router_b[l], w1[l], b1[l], w2[l], b2[l])
        x = x + g2 * y.reshape(Bsz, S, D)
    return x
```

```python
import numpy as np
import concourse.bass as bass
import concourse.mybir as mybir
from concourse.bass_utils import run_bass_kernel_spmd

F32 = mybir.dt.float32
BF16 = mybir.dt.bfloat16
U32 = mybir.dt.uint32
AF = mybir.ActivationFunctionType
ALU = mybir.AluOpType
AX = mybir.AxisListType

D = 1024
S = 8192
NB = 2
D_IN = 4176
EPS = 1e-6
NCORES = 8
TPC = 2048
NT = 16


class Buf:
    __slots__ = ("name", "w", "r", "dsem", "dcnt")

    def __init__(self, name):
        self.name = name
        self.w = None
        self.r = {}
        self.dsem = None
        self.dcnt = 0


class KB:
    def __init__(self, nc, self_sync=True):
        self.nc = nc
        self.eng = {"pe": nc.tensor, "dve": nc.vector, "act": nc.scalar,
                    "pool": nc.gpsimd, "sp": nc.sync}
        self.sems = {}
        self.cnt = {}
        self.known = {k: {} for k in self.eng}
        for k in self.eng:
            self.sems[k] = nc.alloc_semaphore("sem_" + k)
            self.cnt[k] = 0
        self.self_sync = self_sync
        self.nsem = 0
        self.n_ops = 0
        self.n_waits = 0
        self.nbuf = 0

    def sb(self, shape, dt=F32, name=None):
        self.nbuf += 1
        name = (name or "t") + "_%d" % self.nbuf
        return self.nc.alloc_sbuf_tensor(name, list(shape), dt), Buf(name)

    def ps(self, shape, dt=F32, name=None):
        self.nbuf += 1
        name = (name or "p") + "_%d" % self.nbuf
        return self.nc.alloc_psum_tensor(name, list(shape), dt), Buf(name)

    def _deps(self, reads, writes):
        d = {}

        def add(kv):
            if kv is None:
                return
            k, v = kv
            if d.get(k, 0) < v:
                d[k] = v
        for b in reads:
            add(b.w)
        for b in writes:
            add(b.w)
            for k, v in b.r.items():
                add((k, v))
        return d

    def _wait(self, en, deps):
        e = self.eng[en]
        kn = self.known[en]
        for k, v in deps.items():
            if k == en and (en == "pe" or not self.self_sync):
                continue
            if kn.get(k, 0) >= v:
                continue
            e.wait_ge(self.sems[k], v)
            kn[k] = v
            self.n_waits += 1

    def op(self, en, fn, reads=(), writes=()):
        deps = self._deps(reads, writes)
        self._wait(en, deps)
        ins = fn(self.eng[en])
        self.cnt[en] += 1
        v = self.cnt[en]
        ins.then_inc(self.sems[en], 1)
        for b in reads:
            if b.r.get(en, 0) < v:
                b.r[en] = v
        for b in writes:
            b.w = (en, v)
            b.r = {}
        self.n_ops += 1
        return ins

    def dma(self, en, out, in_, reads, writes, **kw):
        assert len(writes) == 1
        dst = writes[0]
        if dst.dsem is None:
            self.nsem += 1
            key = "d%d_%s" % (self.nsem, dst.name)
            dst.dsem = key
            self.sems[key] = self.nc.alloc_semaphore(key[:40])
        deps = self._deps(reads, writes)
        self._wait(en, deps)
        ins = self.eng[en].dma_start(out=out, in_=in_, **kw)
        dst.dcnt += 16
        ins.then_inc(self.sems[dst.dsem], 16)
        for b in reads:
            if b.r.get(dst.dsem, 0) < dst.dcnt:
                b.r[dst.dsem] = dst.dcnt
        dst.w = (dst.dsem, dst.dcnt)
        dst.r = {}
        self.n_ops += 1
        return ins

    def finish(self, bufs, en="sp"):
        d = {}
        for b in bufs:
            if b.w is not None:
                k, v = b.w
                d[k] = max(d.get(k, 0), v)
        self._wait(en, d)


def rview(t, off_bytes, shape, dt):
    esz = 2 if dt == BF16 else 4
    n = 1
    for s in shape[1:]:
        n *= s
    flat = t[:, :] if dt == F32 else t[:, :].bitcast(dt)
    e0 = off_bytes // esz
    v = flat[0:shape[0], e0:e0 + n]
    if len(shape) == 3:
        v = v.rearrange("p (a b) -> p a b", a=shape[1])
    return v


def merge_deps(dst, srcs):
    for s in srcs:
        for kv in ([s.w] if s.w else []) + list(s.r.items()):
            k, v = kv
            if dst.r.get(k, 0) < v:
                dst.r[k] = v


def _din(nc, name, shape, dt=F32):
    return nc.dram_tensor(name, list(shape), dt, kind="ExternalInput").ap()


def _dout(nc, name, shape, dt=F32):
    return nc.dram_tensor(name, list(shape), dt, kind="ExternalOutput").ap()


def emit_mod(kb, cT, mod_w, mod_b, groups, wbufs=None, bbufs=None, pss=None, scb_t=None, outs=None):
    nc = kb.nc
    dr = Buf("mod_dram")
    ct, bct = kb.sb([128, 8], F32, "ct")
    kb.dma("sp", ct[:], cT[:, :], [dr], [bct])
    sc, bsc = kb.sb([128, 8], F32, "sc")
    kb.op("act", lambda e: e.activation(out=sc[:], in_=ct[:], func=AF.Silu), [bct], [bsc])
    scb, bscb = scb_t if scb_t is not None else kb.sb([128, 8, 128], F32, "scb")
    kb.op("dve", lambda e: e.tensor_copy(out=scb[:], in_=sc[:, :].unsqueeze(2).to_broadcast([128, 8, 128])),
          [bsc], [bscb])
    mw = mod_w.rearrange("(kc p) n -> p kc n", p=128)
    if wbufs is None:
        wbufs = [kb.sb([128, 8, 512], F32, "modw") for _ in range(2)]
    if bbufs is None:
        bbufs = [kb.sb([128, 512], F32, "modb") for _ in range(2)]
    if pss is None:
        pss = [kb.ps([128, 512], F32, "modps") for _ in range(2)]
    res = {}
    it = 0
    for g in groups:
        mt, bmt = outs[g] if outs is not None else kb.sb([128, 1024], F32, "modrow")
        res[g] = (mt, bmt)
        for half in range(2):
            c0 = g * 1024 + half * 512
            wt, bw = wbufs[it % 2]
            bt, bb = bbufs[it % 2]
            pt, bp = pss[it % 2]
            kb.dma("sp", wt[:], mw[:, :, c0:c0 + 512], [dr], [bw])
            kb.dma("sp", bt[:], mod_b[0:1, c0:c0 + 512].to_broadcast([128, 512]), [dr], [bb])
            for kc in range(8):
                kb.op("pe", lambda e, kc=kc: e.matmul(pt[:], scb[:, kc, :], wt[:, kc, :],
                                                      start=(kc == 0), stop=(kc == 7)),
                      [bscb, bw], [bp])
            kb.op("dve", lambda e: e.tensor_tensor(out=mt[:, half * 512:(half + 1) * 512], in0=pt[:],
                                                   in1=bt[:], op=ALU.add), [bp, bb], [bmt])
            it += 1
    return res


NF1 = 2064
NB1 = 2112


def build_l1():
    nc = bass.Bass("TRN2", target_bir_lowering=False)
    x = _din(nc, "x", [TPC, D])
    cT = _din(nc, "cT", [128, 8])
    mod_w = _din(nc, "mod_w", [D, 6 * D])
    mod_b = _din(nc, "mod_b", [1, 6 * D])
    normw = _din(nc, "normw", [1, D])
    w_in = _din(nc, "w_in", [D, D_IN])
    qkw = _din(nc, "qkw", [1, 128])
    ident = _din(nc, "ident", [128, 128])
    of = _dout(nc, "of", [TPC, NF1])
    ob = _dout(nc, "ob", [TPC, NB1], BF16)
    kb = KB(nc)
    dr = Buf("dram_in")
    bof, bob = Buf("of"), Buf("ob")

    idb, bidb = kb.sb([128, 128], BF16, "identb")
    kb.dma("pool", idb[:], ident[:, :], [dr], [bidb])
    epst, beps = kb.sb([128, 1], F32, "eps")
    kb.op("dve", lambda e: e.memset(epst[:], EPS), [], [beps])
    nwb, bnwb = kb.sb([128, D], F32, "nwb")
    kb.dma("sp", nwb[:], normw[0:1, :].to_broadcast([128, D]), [dr], [bnwb])
    qkb, bqkb = kb.sb([128, 128], F32, "qkb")
    kb.dma("sp", qkb[:], qkw[0:1, :].to_broadcast([128, 128]), [dr], [bqkb])
    kb.op("dve", lambda e: e.tensor_scalar(out=qkb[:, 0:64], in0=qkb[:, 0:64], scalar1=0.125, scalar2=None,
                                           op0=ALU.mult), [bqkb], [bqkb])

    wb, bwb = kb.sb([128, 8, D_IN], BF16, "w_in")
    for kc in range(8):
        for pc in range(3):
            c0 = pc * 1392
            kb.dma("pool", wb[:, kc, c0:c0 + 1392], w_in[kc * 128:(kc + 1) * 128, c0:c0 + 1392], [dr], [bwb])

    mods = emit_mod(kb, cT, mod_w, mod_b, [0, 1])
    sh1, bsh1 = mods[0]
    sc1, bsc1 = mods[1]
    A1, bA1 = kb.sb([128, D], F32, "A1")
    kb.op("dve", lambda e: e.scalar_tensor_tensor(out=A1[:], in0=sc1[:], scalar=1.0, in1=nwb[:],
                                                  op0=ALU.add, op1=ALU.mult), [bsc1, bnwb], [bA1])

    xts = [kb.sb([128, D], F32, "xt") for _ in range(2)]
    junk, bjunk = kb.sb([128, D], BF16, "junk")
    tmp, btmp = kb.sb([128, D], F32, "tmp")
    hbs = [kb.sb([128, D], BF16, "hb") for _ in range(2)]
    pT, bpT = kb.ps([128, D], BF16, "pT")
    hTs = [kb.sb([128, 8, 128], BF16, "hT") for _ in range(2)]
    pps = [kb.ps([128, 512], F32, "pp") for _ in range(4)]
    projs = [kb.sb([128, D_IN], F32, "proj") for _ in range(2)]
    obts = [kb.sb([128, NB1], BF16, "obt") for _ in range(2)]
    sq, bsq = kb.sb([128, 1024], F32, "sq")
    st, bst = kb.sb([128, 24], F32, "stats")

    QO = 2056
    npp = 0
    for ti in range(NT):
        xt, bxt = xts[ti % 2]
        hb, bhb = hbs[ti % 2]
        hT, bhT = hTs[ti % 2]
        pj, bpj = projs[ti % 2]
        obt, bobt = obts[ti % 2]
        kb.dma("sp", xt[:], x[ti * 128:(ti + 1) * 128, :], [dr], [bxt])
        kb.op("act", lambda e: e.activation(out=junk[:], in_=xt[:], func=AF.Square, accum_out=st[:, 0:1]),
              [bxt], [bjunk, bst])
        kb.op("act", lambda e: e.activation(out=st[:, 1:2], in_=st[:, 0:1], func=AF.Sqrt, scale=1.0 / D,
                                            bias=epst[:]), [bst, beps], [bst])
        kb.op("dve", lambda e: e.reciprocal(out=st[:, 2:3], in_=st[:, 1:2]), [bst], [bst])
        kb.op("dve", lambda e: e.scalar_tensor_tensor(out=tmp[:], in0=xt[:], scalar=st[:, 2:3], in1=A1[:],
                                                      op0=ALU.mult, op1=ALU.mult), [bxt, bst, bA1], [btmp])
        kb.op("dve", lambda e: e.tensor_tensor(out=hb[:], in0=tmp[:], in1=sh1[:], op=ALU.add),
              [btmp, bsh1], [bhb])
        for kc in range(8):
            kb.op("pe", lambda e, kc=kc: e.transpose(out=pT[:, kc * 128:(kc + 1) * 128],
                                                     in_=hb[:, kc * 128:(kc + 1) * 128], identity=idb[:]),
                  [bhb, bidb], [bpT])
        kb.op("act", lambda e: e.copy(out=hT[:].rearrange("p a b -> p (a b)"), in_=pT[:]), [bpT], [bhT])
        for cb in range(9):
            c0 = cb * 512
            n = min(512, D_IN - c0)
            pp, bpp = pps[npp % 4]
            for kc in range(8):
                kb.op("pe", lambda e, kc=kc: e.matmul(pp[:, 0:n], hT[:, kc, :], wb[:, kc, c0:c0 + n],
                                                      start=(kc == 0), stop=(kc == 7)), [bhT, bwb], [bpp])
            if npp % 2 == 0:
                kb.op("act", lambda e: e.copy(out=pj[:, c0:c0 + n], in_=pp[:, 0:n]), [bpp], [bpj])
            else:
                kb.op("dve", lambda e: e.tensor_copy(out=pj[:, c0:c0 + n], in_=pp[:, 0:n]), [bpp], [bpj])
            npp += 1
        qk = pj[:, QO:QO + 1024]
        kb.op("dve", lambda e: e.tensor_tensor(out=sq[:], in0=qk, in1=qk, op=ALU.mult), [bpj], [bsq])
        kb.op("dve", lambda e: e.tensor_reduce(out=st[:, 4:20], in_=sq[:].rearrange("p (g d) -> p g d", g=16),
                                               axis=AX.X, op=ALU.add), [bsq], [bst])
        kb.op("act", lambda e: e.activation(out=st[:, 4:20], in_=st[:, 4:20], func=AF.Sqrt, scale=1.0 / 64,
                                            bias=epst[:]), [bst, beps], [bst])
        kb.op("dve", lambda e: e.reciprocal(out=st[:, 4:20], in_=st[:, 4:20]), [bst], [bst])
        kb.op("dve", lambda e: e.tensor_tensor(out=sq[:].rearrange("p (g d) -> p g d", g=16),
                                               in0=qk.rearrange("p (g d) -> p g d", g=16),
                                               in1=st[:, 4:20].unsqueeze(2).to_broadcast([128, 16, 64]),
                                               op=ALU.mult), [bpj, bst], [bsq])
        for half in range(2):
            kb.op("dve", lambda e, half=half: e.tensor_tensor(
                out=obt[:, half * 512:(half + 1) * 512].rearrange("p (g d) -> p g d", g=8),
                in0=sq[:, half * 512:(half + 1) * 512].rearrange("p (g d) -> p g d", g=8),
                in1=qkb[:, half * 64:(half + 1) * 64].unsqueeze(1).to_broadcast([128, 8, 64]),
                op=ALU.mult), [bsq, bqkb], [bobt])
        kb.op("act", lambda e: e.copy(out=obt[:, 1024:NB1], in_=pj[:, QO + 1024:QO + 1024 + 1088]), [bpj], [bobt])
        kb.op("dve", lambda e: e.tensor_scalar(out=pj[:, 4168:4176], in0=pj[:, 4168:4176],
                                               scalar1=float(8 ** -0.5 * 64 ** -0.5), scalar2=None,
                                               op0=ALU.mult), [bpj], [bpj])
        kb.dma("sp", of[ti * 128:(ti + 1) * 128, 0:2056], pj[:, 0:2056], [bpj], [bof])
        kb.dma("sp", of[ti * 128:(ti + 1) * 128, 2056:2064], pj[:, 4168:4176], [bpj], [bof])
        kb.dma("sp", ob[ti * 128:(ti + 1) * 128, :], obt[:], [bobt], [bob])
    kb.finish([bof, bob])
    return nc


def core_tokens(a, c):
    b, r = c // 4, c % 4
    t = a[b].reshape((64, 128) + a.shape[2:])[r::4]
    return np.ascontiguousarray(t.reshape((TPC,) + a.shape[2:]))


def uncore_tokens(parts, tail):
    out = np.empty((NB, 64, 128) + tuple(tail), parts[0].dtype)
    for c in range(NCORES):
        b, r = c // 4, c % 4
        out[b, r::4] = parts[c].reshape((16, 128) + tuple(tail))
    return out.reshape((NB, S) + tuple(tail))


_NC_CACHE = {}
_TRACE = False
_TIMES = []


def _run(nc, maps):
    if _TRACE:
        res = run_bass_kernel_spmd(nc, maps, core_ids=list(range(NCORES)), trace=True)
        _TIMES.append(res.exec_time_ns)
        print('exec_time_ns', res.exec_time_ns)
        return res
    return run_bass_kernel_spmd(nc, maps, core_ids=list(range(NCORES)))


def _get(name, fn):
    if name not in _NC_CACHE:
        _NC_CACHE[name] = fn()
    return _NC_CACHE[name]


def cT_of(c, b):
    return np.ascontiguousarray(c[b].reshape(8, 128).T)


def run_l1(x, c, mod_w_l, mod_b_l, normw_l, w_in_l, qw_l, kw_l):
    nc = _get("l1", build_l1)
    ident = np.eye(128, dtype=np.float32)
    qkw = np.concatenate([qw_l, kw_l]).reshape(1, 128).astype(np.float32)
    maps = []
    for cid in range(NCORES):
        maps.append({"x": core_tokens(x, cid), "cT": cT_of(c, cid // 4), "mod_w": mod_w_l,
                     "mod_b": mod_b_l.reshape(1, -1), "normw": normw_l.reshape(1, -1), "w_in": w_in_l,
                     "qkw": qkw, "ident": ident})
    res = _run(nc, maps)
    of = uncore_tokens([r["of"] for r in res.results], (NF1,))
    ob = uncore_tokens([r["ob"] for r in res.results], (NB1,))
    return of, ob


NE = 32
ALPHA = 1.702
LIMIT = 7.0


def build_l4(e_lo=0, e_hi=NE, first=True):
    n_exp = e_hi - e_lo
    do_final = True
    nc = bass.Bass("TRN2", target_bir_lowering=False)
    x = _din(nc, "x", [TPC, D])
    ycat = _din(nc, "ycat", [TPC, D])
    cT = _din(nc, "cT", [128, 8])
    mod_w = _din(nc, "mod_w", [D, 6 * D])
    mod_b = _din(nc, "mod_b", [1, 6 * D])
    normw = _din(nc, "normw", [1, D])
    w_out = _din(nc, "w_out", [D, D])
    rw = _din(nc, "rw", [D, NE])
    rb = _din(nc, "rb", [1, NE])
    w1 = _din(nc, "w1", [max(n_exp, 1), 8, 128, 8 * 256])
    b1 = _din(nc, "b1", [128, NE * 16])
    w2 = _din(nc, "w2", [max(n_exp, 1), 8, 128, 8 * 128])
    b2 = _din(nc, "b2", [NE, D])
    ident = _din(nc, "ident", [128, 128])
    xprev = None if first else _din(nc, "xprev", [TPC, D])
    xo = _dout(nc, "xo", [TPC, D])
    kb = KB(nc)
    dr = Buf("dram_in")
    bxo = Buf("xo")

    PS = []
    for i in range(4):
        t = nc.alloc_psum_tensor("PS%d" % i, [128, 1024], F32)
        PS.append((t, Buf("ps%da" % i), Buf("ps%db" % i)))

    def bank(i):
        t, ba, bb = PS[i // 2]
        return (t[:, 0:512], ba) if i % 2 == 0 else (t[:, 512:1024], bb)

    R_hT, bhT = kb.sb([128, 8192], F32, "R_hT")
    hT = rview(R_hT, 0, [128, 8, TPC], BF16)
    R_acc, bacc = kb.sb([128, 16384], F32, "R_acc")
    acc = rview(R_acc, 0, [128, 8, TPC], F32)
    wo = rview(R_acc, 0, [128, 8, D], BF16)
    bwo = Buf("wo")
    R_act, bactT = kb.sb([128, 8192], F32, "R_act")
    actT = rview(R_act, 0, [128, 8, TPC], BF16)
    modw_bufs = [(rview(R_act, i * 16384, [128, 8, 512], F32), Buf("modw%d" % i)) for i in range(2)]
    gateT, bgateT = kb.sb([NE, TPC], F32, "gateT")
    R_gsb, bgsb = kb.sb([128, TPC], F32, "gsb")
    gsb = R_gsb
    scb_t = (rview(R_gsb, 0, [128, 8, 128], F32), Buf("scb"))
    modb_bufs = [(rview(R_gsb, 4096 + i * 2048, [128, 512], F32), Buf("modb%d" % i)) for i in range(2)]
    R_W, _ = kb.sb([128, 4608], F32, "R_W")
    W1R = [(rview(R_W, i * 4096, [128, 8, 256], BF16), Buf("w1r%d" % i)) for i in range(3)]
    W2R = [(rview(R_W, 12288 + i * 2048, [128, 8, 128], BF16), Buf("w2r%d" % i)) for i in range(3)]
    g1, bg1 = rview(R_W, 0, [128, D], F32), Buf("g1")
    sh2, bsh2 = rview(R_W, 4096, [128, D], F32), Buf("sh2")
    A2, bA2 = rview(R_W, 8192, [128, D], F32), Buf("A2")
    nwb, bnwb = rview(R_W, 12288, [128, D], F32), Buf("nwb")
    g2, bg2 = kb.sb([128, D], F32, "g2")
    xt, bxt = kb.sb([128, D], F32, "xt")
    yt, byt = kb.sb([128, D], F32, "yt")
    tmpA, btmpA = kb.sb([128, D], F32, "tmpA")
    tmpB, btmpB = kb.sb([128, D], F32, "tmpB")
    R1, bR1 = kb.sb([128, D], F32, "R1")
    ybt = rview(R1, 0, [128, D], BF16)
    yT = rview(R1, 2048, [128, 8, 128], BF16)
    h32T = rview(R1, 0, [128, 8, 128], F32)
    R2, bR2 = kb.sb([128, D], F32, "R2")
    hb = rview(R2, 0, [128, D], BF16)
    junk = rview(R2, 2048, [128, D], BF16)

    idf, bidf = kb.sb([128, 128], F32, "identf")
    kb.dma("sp", idf[:], ident[:, :], [dr], [bidf])
    idb, bidb = kb.sb([128, 128], BF16, "identb")
    kb.op("dve", lambda e: e.tensor_copy(out=idb[:], in_=idf[:]), [bidf], [bidb])
    epst, beps = kb.sb([128, 1], F32, "eps")
    kb.op("dve", lambda e: e.memset(epst[:], EPS), [], [beps])
    kb.dma("sp", nwb, normw[0:1, :].to_broadcast([128, D]), [dr], [bnwb])
    rbb, brbb = kb.sb([128, NE], F32, "rbb")
    kb.dma("sp", rbb[:], rb[0:1, :].to_broadcast([128, NE]), [dr], [brbb])
    rwt, brwt = kb.sb([128, 8, NE], F32, "rwt")
    kb.dma("sp", rwt[:], rw.rearrange("(kc p) e -> p kc e", p=128), [dr], [brwt])
    b1a, bb1a = kb.sb([128, NE * 16], F32, "b1a")
    kb.dma("sp", b1a[:], b1[:, :], [dr], [bb1a])
    b1av = b1a[:].rearrange("p (g two) -> p g two", two=2)
    kb.op("dve", lambda e: e.tensor_scalar(out=b1av[:, :, 0:1], in0=b1av[:, :, 0:1], scalar1=ALPHA, scalar2=None,
                                           op0=ALU.mult), [bb1a], [bb1a])
    kb.op("dve", lambda e: e.tensor_scalar(out=b1av[:, :, 1:2], in0=b1av[:, :, 1:2], scalar1=1.0, scalar2=None,
                                           op0=ALU.add), [bb1a], [bb1a])
    b2t, bb2t = kb.sb([NE, D], F32, "b2t")
    kb.dma("sp", b2t[:], b2[:, :], [dr], [bb2t])
    selbs = [kb.sb([NE, 128], F32, "selb") for _ in range(2)]
    st, bst = kb.sb([128, 64], F32, "st")
    lg, blg = kb.sb([128, NE], F32, "lg")

    sc2_out = (A2, bA2)
    mods = emit_mod(kb, cT, mod_w, mod_b, [2, 3, 4, 5], wbufs=modw_bufs, bbufs=modb_bufs,
                    pss=[bank(0), bank(1)], scb_t=scb_t,
                    outs={2: (g1, bg1), 3: (sh2, bsh2), 4: sc2_out, 5: (g2[:], bg2)})
    kb.op("dve", lambda e: e.scalar_tensor_tensor(out=A2, in0=A2, scalar=1.0, in1=nwb,
                                                  op0=ALU.add, op1=ALU.mult), [bA2, bnwb], [bA2])

    for kc in range(8):
        kb.dma("pool", wo[:, kc, :], w_out[kc * 128:(kc + 1) * 128, :], [dr], [bwo])
    for ti in range(NT):
        rows = slice(ti * 128, (ti + 1) * 128)
        kb.dma("sp", xt[:], x[rows, :], [dr], [bxt])
        kb.dma("sp", yt[:], ycat[rows, :], [dr], [byt])
        kb.op("act", lambda e: e.copy(out=ybt, in_=yt[:]), [byt], [bR1])
        pt, bpt = bank(0)
        ptb = pt.bitcast(BF16)
        for kc in range(8):
            kb.op("pe", lambda e, kc=kc: e.transpose(out=ptb[:, kc * 128:(kc + 1) * 128],
                                                     in_=ybt[:, kc * 128:(kc + 1) * 128], identity=idb[:]),
                  [bR1, bidb], [bpt])
        kb.op("act", lambda e: e.copy(out=yT.rearrange("p a b -> p (a b)"), in_=ptb), [bpt], [bR1])
        for half in range(2):
            pp, bpp = bank(2 + half)
            for kc in range(8):
                kb.op("pe", lambda e, kc=kc: e.matmul(pp, yT[:, kc, :], wo[:, kc, half * 512:(half + 1) * 512],
                                                      start=(kc == 0), stop=(kc == 7)), [bR1, bwo], [bpp])
            cs = slice(half * 512, (half + 1) * 512)
            kb.op("dve", lambda e: e.tensor_tensor(out=tmpA[:, cs], in0=pp, in1=g1[:, cs], op=ALU.mult),
                  [bpp, bg1], [btmpA])
        kb.op("dve", lambda e: e.tensor_tensor(out=xt[:], in0=tmpA[:], in1=xt[:], op=ALU.add), [btmpA, bxt], [bxt])
        kb.dma("sp", xo[rows, :], xt[:], [bxt], [bxo])
        kb.op("act", lambda e: e.activation(out=junk, in_=xt[:], func=AF.Square, accum_out=st[:, 0:1]),
              [bxt], [bR2, bst])
        kb.op("act", lambda e: e.activation(out=st[:, 1:2], in_=st[:, 0:1], func=AF.Sqrt, scale=1.0 / D,
                                            bias=epst[:]), [bst, beps], [bst])
        kb.op("dve", lambda e: e.reciprocal(out=st[:, 2:3], in_=st[:, 1:2]), [bst], [bst])
        kb.op("dve", lambda e: e.scalar_tensor_tensor(out=tmpA[:], in0=xt[:], scalar=st[:, 2:3], in1=A2,
                                                      op0=ALU.mult, op1=ALU.mult), [bxt, bst, bA2], [btmpA])
        kb.op("dve", lambda e: e.tensor_tensor(out=tmpB[:], in0=tmpA[:], in1=sh2, op=ALU.add),
              [btmpA, bsh2], [btmpB])
        kb.op("act", lambda e: e.copy(out=hb, in_=tmpB[:]), [btmpB], [bR2])
        pt, bpt = bank(1)
        ptb = pt.bitcast(BF16)
        for kc in range(8):
            kb.op("pe", lambda e, kc=kc: e.transpose(out=ptb[:, kc * 128:(kc + 1) * 128],
                                                     in_=hb[:, kc * 128:(kc + 1) * 128], identity=idb[:]),
                  [bR2, bidb], [bpt])
        kb.op("act", lambda e: e.copy(out=hT[:, :, rows], in_=ptb.rearrange("p (a b) -> p a b", a=8)),
              [bpt], [bhT])
        for half in range(2):
            pq, bpq = bank(4 + half)
            for k4 in range(4):
                kc = half * 4 + k4
                kb.op("pe", lambda e, kc=kc, k4=k4: e.transpose(out=pq[:, k4 * 128:(k4 + 1) * 128],
                                                                in_=tmpB[:, kc * 128:(kc + 1) * 128],
                                                                identity=idf[:]), [btmpB, bidf], [bpq])
            kb.op("dve", lambda e: e.tensor_copy(
                out=h32T[:, half * 4:(half + 1) * 4, :], in_=pq.rearrange("p (a b) -> p a b", a=4)),
                [bpq], [bR1])
        pr, bpr = bank(6)
        for kc in range(8):
            kb.op("pe", lambda e, kc=kc: e.matmul(pr[:, 0:NE], h32T[:, kc, :], rwt[:, kc, :],
                                                  start=(kc == 0), stop=(kc == 7)), [bR1, brwt], [bpr])
        kb.op("dve", lambda e: e.tensor_tensor(out=lg[:], in0=pr[:, 0:NE], in1=rbb[:], op=ALU.add),
              [bpr, brbb], [blg])
        kb.op("dve", lambda e: e.max(out=st[:, 8:16], in_=lg[:]), [blg], [bst])
        kb.op("dve", lambda e: e.tensor_scalar(out=st[:, 16:17], in0=st[:, 8:9], scalar1=-1.0, scalar2=None,
                                               op0=ALU.mult), [bst], [bst])
        kb.op("act", lambda e: e.activation(out=st[:, 20:24], in_=st[:, 8:12], func=AF.Exp, bias=st[:, 16:17],
                                            accum_out=st[:, 17:18]), [bst], [bst])
        kb.op("dve", lambda e: e.reciprocal(out=st[:, 18:19], in_=st[:, 17:18]), [bst], [bst])
        kb.op("act", lambda e: e.activation(out=st[:, 32:64], in_=lg[:], func=AF.Exp, bias=st[:, 16:17]),
              [blg, bst], [bst])
        kb.op("dve", lambda e: e.tensor_scalar(out=lg[:], in0=lg[:], scalar1=st[:, 11:12], scalar2=None,
                                               op0=ALU.is_ge), [blg, bst], [blg])
        kb.op("dve", lambda e: e.scalar_tensor_tensor(out=lg[:], in0=st[:, 32:64], scalar=st[:, 18:19], in1=lg[:],
                                                      op0=ALU.mult, op1=ALU.mult), [bst, blg], [blg])
        pg, bpg = bank(7)
        kb.op("pe", lambda e: e.transpose(out=pg[0:NE, 0:128], in_=lg[:], identity=idf[:]), [blg, bidf], [bpg])
        kb.op("act", lambda e: e.copy(out=gateT[:, rows], in_=pg[0:NE, 0:128]), [bpg], [bgateT])

    merge_deps(bacc, [bwo])
    merge_deps(bactT, [b for _, b in modw_bufs])
    merge_deps(bgsb, [scb_t[1]] + [b for _, b in modb_bufs])
    for _, b in W1R + W2R:
        merge_deps(b, [bg1, bsh2, bA2, bnwb])
    tts = [(tmpA[:, 0:512], Buf("tt0")), (tmpA[:, 512:1024], Buf("tt1"))]
    lps = [(tmpB[:, 0:512], Buf("lp0")), (tmpB[:, 512:1024], Buf("lp1"))]
    lgs = [(xt[:, 0:512], Buf("lg0")), (xt[:, 512:1024], Buf("lg1"))]
    for (_, b), src_ in zip(tts + lps + lgs, [btmpA, btmpA, btmpB, btmpB, bxt, bxt]):
        merge_deps(b, [src_])
    C0 = float(LIMIT * ALPHA / (1.0 + np.exp(-LIMIT * ALPHA)))

    w1_issued = [0]
    w2_issued = [0]

    def issue_w1(upto):
        while w1_issued[0] < min(upto, n_exp * 8):
            i = w1_issued[0]
            t, b = W1R[i % 3]
            kb.dma("pool", t.rearrange("p a b -> p (a b)"), w1[i // 8, i % 8, :, :], [dr], [b])
            w1_issued[0] += 1

    def issue_w2(upto):
        while w2_issued[0] < min(upto, n_exp * 8):
            i = w2_issued[0]
            t, b = W2R[i % 3]
            kb.dma("pool", t.rearrange("p a b -> p (a b)"), w2[i // 8, i % 8, :, :], [dr], [b])
            w2_issued[0] += 1

    nu = 0
    ny = 0
    for e in range(e_lo, e_hi):
        selb, bselb = selbs[e % 2]
        kb.op("dve", lambda en: en.tensor_copy(out=selb[:], in_=idf[0:NE, e:e + 1].to_broadcast([NE, 128])),
              [bidf], [bselb])
        for blk in range(4):
            pgb, bpgb = bank(6)
            kb.op("pe", lambda en, blk=blk: en.matmul(pgb, selb[:], gateT[:, blk * 512:(blk + 1) * 512],
                                                      start=True, stop=True), [bselb, bgateT], [bpgb])
            kb.op("act", lambda en, blk=blk: en.activation(out=gsb[:, blk * 512:(blk + 1) * 512], in_=pgb,
                                                           func=AF.Copy, scale=1.0 / ALPHA), [bpgb], [bgsb])
        for ft in range(8):
            gi = (e - e_lo) * 8 + ft
            issue_w1(gi + 2)
            wv, bwt = W1R[gi % 3]
            bcol = (e * 8 + ft) * 2
            for blk in range(4):
                ts_ = slice(blk * 512, (blk + 1) * 512)
                pgl, bpgl = bank(0 + 2 * (nu % 2))
                pli, bpli = bank(1 + 2 * (nu % 2))
                for kc in range(8):
                    kb.op("pe", lambda en, kc=kc: en.matmul(pgl, wv[:, kc, 0:128], hT[:, kc, ts_],
                                                            start=(kc == 0), stop=(kc == 7)), [bwt, bhT], [bpgl])
                for kc in range(8):
                    kb.op("pe", lambda en, kc=kc: en.matmul(pli, wv[:, kc, 128:256], hT[:, kc, ts_],
                                                            start=(kc == 0), stop=(kc == 7)), [bwt, bhT], [bpli])
                tt, btt = tts[nu % 2]
                lp, blp = lps[nu % 2]
                lgg, blgg = lgs[nu % 2]
                kb.op("act", lambda en: en.activation(out=tt, in_=pgl, func=AF.Silu, scale=ALPHA,
                                                      bias=b1a[:, bcol:bcol + 1]), [bpgl, bb1a], [btt])
                kb.op("dve", lambda en: en.tensor_scalar(out=lp, in0=pli, scalar1=b1a[:, bcol + 1:bcol + 2],
                                                         scalar2=LIMIT + 1.0, op0=ALU.add, op1=ALU.min),
                      [bpli, bb1a], [blp])
                kb.op("dve", lambda en: en.scalar_tensor_tensor(out=lgg, in0=lp, scalar=1.0 - LIMIT, in1=gsb[:, ts_],
                                                                op0=ALU.max, op1=ALU.mult), [blp, bgsb], [blgg])
                kb.op("dve", lambda en: en.scalar_tensor_tensor(out=actT[:, ft, ts_], in0=tt, scalar=C0, in1=lgg,
                                                                op0=ALU.min, op1=ALU.mult), [btt, blgg], [bactT])
                nu += 1
        for dt in range(8):
            gi = (e - e_lo) * 8 + dt
            issue_w2(gi + 2)
            wv, bwt = W2R[gi % 3]
            for blk in range(4):
                ts_ = slice(blk * 512, (blk + 1) * 512)
                py, bpy = bank(4 + (ny % 2))
                for fc in range(8):
                    kb.op("pe", lambda en, fc=fc: en.matmul(py, wv[:, fc, :], actT[:, fc, ts_],
                                                            start=(fc == 0), stop=(fc == 7)), [bwt, bactT], [bpy])
                if e == e_lo:
                    kb.op("dve", lambda en: en.tensor_copy(out=acc[:, dt, ts_], in_=py), [bpy], [bacc])
                else:
                    kb.op("dve", lambda en: en.tensor_tensor(out=acc[:, dt, ts_], in0=py, in1=acc[:, dt, ts_],
                                                             op=ALU.add), [bpy, bacc], [bacc])
                ny += 1

    tf, btf = tts[0]
    for ti in range(NT if do_final else 0):
        rows = slice(ti * 128, (ti + 1) * 128)
        if first:
            kb.dma("sp", yt[:], xo[rows, :], [bxo], [byt])
        else:
            kb.dma("sp", yt[:], xprev[rows, :], [dr], [byt])
        for half in range(2):
            po, bpo = bank(half)
            for d4 in range(4):
                dt = half * 4 + d4
                cs = slice(d4 * 128, (d4 + 1) * 128)
                if first:
                    kb.op("pe", lambda en, dt=dt, cs=cs: en.matmul(po[:, cs], gateT[:, rows],
                                                                  b2t[:, dt * 128:(dt + 1) * 128],
                                                                  start=True, stop=False), [bgateT, bb2t], [bpo])
                kb.op("pe", lambda en, dt=dt, cs=cs: en.matmul(po[:, cs], acc[:, dt, rows], idf[:],
                                                              start=(not first), stop=True), [bacc, bidf], [bpo])
            cs2 = slice(half * 512, (half + 1) * 512)
            kb.op("dve", lambda en: en.tensor_tensor(out=tf, in0=po, in1=g2[:, cs2], op=ALU.mult),
                  [bpo, bg2], [btf])
            kb.op("dve", lambda en: en.tensor_tensor(out=yt[:, cs2], in0=tf, in1=yt[:, cs2], op=ALU.add),
                  [btf, byt], [byt])
        kb.dma("sp", xo[rows, :], yt[:], [byt], [bxo])
    kb.finish([bxo])
    return nc


def prep_l4_weights(w1_l, b1_l, w2_l, b2_l):
    w1r = w1_l.reshape(NE, 8, 128, 8, 128, 2)
    w1r = w1r.transpose(0, 3, 2, 1, 5, 4)
    w1r = np.ascontiguousarray(w1r).reshape(NE, 8, 128, 8 * 256)
    b1r = b1_l.reshape(NE, 8, 128, 2).transpose(2, 0, 1, 3)
    b1r = np.ascontiguousarray(b1r).reshape(128, NE * 16)
    w2r = w2_l.reshape(NE, 8, 128, 8, 128).transpose(0, 3, 2, 1, 4)
    w2r = np.ascontiguousarray(w2r).reshape(NE, 8, 128, 8 * 128)
    return w1r, b1r, w2r, np.ascontiguousarray(b2_l)


def run_l4(x, ycat, c, mod_w_l, mod_b_l, normw_l, w_out_l, rw_l, rb_l, w1r, b1r, w2r, b2_l, splits=((0, 16), (16, 32))):
    ident = np.eye(128, dtype=np.float32)
    xprev = None
    for (lo, hi) in splits:
        first = (lo == 0)
        nc = _get("l4_%d_%d" % (lo, hi), lambda: build_l4(lo, hi, first))
        w1s = np.ascontiguousarray(w1r[lo:hi])
        w2s = np.ascontiguousarray(w2r[lo:hi])
        maps = []
        for cid in range(NCORES):
            m = {"x": core_tokens(x, cid), "ycat": core_tokens(ycat, cid), "cT": cT_of(c, cid // 4),
                 "mod_w": mod_w_l, "mod_b": mod_b_l.reshape(1, -1), "normw": normw_l.reshape(1, -1),
                 "w_out": w_out_l, "rw": rw_l, "rb": rb_l.reshape(1, -1), "w1": w1s, "b1": b1r, "w2": w2s,
                 "b2": b2_l, "ident": ident}
            if not first:
                m["xprev"] = xprev[cid]
            maps.append(m)
        res = _run(nc, maps)
        xprev = [r["xo"] for r in res.results]
    return uncore_tokens(xprev, (D,))


NIT = 16
KSEL = 256
NEGBIG = -30000.0
NREL = 1280


def build_l3(nj=16):
    nc = bass.Bass("TRN2", target_bir_lowering=False)
    qT = _din(nc, "qT", [128, 16, 4 * 128], BF16)
    kT = _din(nc, "kT", [128, 4 * S], BF16)
    v1 = _din(nc, "v1", [64, 128, 8 * 65], BF16)
    qiT = _din(nc, "qiT", [64, 16, 8 * 128], BF16)
    kiT = _din(nc, "kiT", [64, S], BF16)
    wi = _din(nc, "wi", [128, 128])
    pen = _din(nc, "pen", [128, 512])
    oh = _din(nc, "oh", [32, NREL])
    relb = _din(nc, "relb", [32, 8])
    negI4 = _din(nc, "negI4", [128, 512], BF16)
    ident = _din(nc, "ident", [128, 128])
    yb = _dout(nc, "yb", [TPC, 512])
    scr = nc.dram_tensor("scr", [8, NREL], BF16, kind="Internal").ap()
    kb = KB(nc)
    dr = Buf("dram_in")
    byb = Buf("yb")
    bscr = Buf("scr")

    PS = []
    for i in range(4):
        t = nc.alloc_psum_tensor("PS%d" % i, [128, 1024], F32)
        PS.append((t, Buf("ps%da" % i), Buf("ps%db" % i)))

    def bank(i):
        t, ba, bb = PS[i // 2]
        return (t[:, 0:512], ba) if i % 2 == 0 else (t[:, 512:1024], bb)

    kTt, bkT = kb.sb([128, 4 * S], BF16, "kT")
    for i in range(4):
        kb.dma("sp", kTt[:, i * S:(i + 1) * S], kT[:, i * S:(i + 1) * S], [dr], [bkT])
    kTv = kTt[:].rearrange("p (h s) -> p h s", h=4)
    kit, bki = kb.sb([64, S], BF16, "kiT")
    kb.dma("sp", kit[:], kiT[:, :], [dr], [bki])
    wit, bwi = kb.sb([128, 128], F32, "wi")
    kb.dma("sp", wit[:], wi[:, :], [dr], [bwi])
    pent, bpen = kb.sb([128, 512], F32, "pen")
    kb.dma("sp", pent[:], pen[:, :], [dr], [bpen])
    n4, bn4 = kb.sb([128, 512], BF16, "negI4")
    kb.dma("sp", n4[:], negI4[:, :], [dr], [bn4])
    idf, bidf = kb.sb([128, 128], F32, "identf")
    kb.dma("sp", idf[:], ident[:, :], [dr], [bidf])
    idb, bidb = kb.sb([128, 128], BF16, "identb")
    kb.op("dve", lambda e: e.tensor_copy(out=idb[:], in_=idf[:]), [bidf], [bidb])
    zb, bzb = kb.sb([128, 260], BF16, "zeros")
    kb.op("dve", lambda e: e.memset(zb[:], 0.0), [], [bzb])

    oht, boh = kb.sb([32, NREL], F32, "oh")
    kb.dma("sp", oht[:], oh[:, :], [dr], [boh])
    rbt, brb = kb.sb([32, 8], F32, "relb")
    kb.dma("sp", rbt[:], relb[:, :], [dr], [brb])
    bv, bbv = kb.sb([8, NREL], BF16, "bvec")
    for i, (c0, n) in enumerate([(0, 512), (512, 512), (1024, 256)]):
        pb, bpb = bank(7)
        kb.op("pe", lambda e: e.matmul(pb[0:8, 0:n], rbt[:], oht[:, c0:c0 + n], start=True, stop=True),
              [brb, boh], [bpb])
        kb.op("dve", lambda e: e.tensor_copy(out=bv[:, c0:c0 + n], in_=pb[0:8, 0:n]), [bpb], [bbv])
    kb.dma("sp", scr[:, :], bv[:], [bbv], [bscr])
    TU, bTU = kb.sb([128, 9, 8 * 128], BF16, "TU")
    for u in range(9):
        src = bass.AP(tensor=scr.tensor, offset=128 * u, ap=[[1, 128], [NREL, 8], [1, 128]])
        kb.dma("sp", TU[:, u, :].rearrange("p (h t) -> p h t", h=8), src, [bscr], [bTU])

    score, bscore = kb.sb([128, S], F32, "score")
    nots = [kb.sb([128, S], BF16, "notsel") for _ in range(2)]
    qts = [kb.sb([128, 512], BF16, "qTj") for _ in range(2)]
    qis = [kb.sb([64, 1024], BF16, "qiTj") for _ in range(2)]
    dgs = [kb.sb([128, 1024], BF16, "Dg") for _ in range(2)]
    rhs_ = [kb.sb([128, 512], BF16, "rh") for _ in range(4)]
    pts = [kb.sb([128, 512], BF16, "pt") for _ in range(2)]
    vts = [kb.sb([128, 520], BF16, "v1t") for _ in range(3)]
    outs = [kb.sb([128, 512], F32, "yo") for _ in range(2)]
    st, bst = kb.sb([128, 32], F32, "st")

    nr = [0]
    nd = [0]
    nv = [0]
    nsc = [0]
    LAG = 3
    rhs_.append(kb.sb([128, 512], BF16, "rh"))
    vts.append(kb.sb([128, 520], BF16, "v1t"))
    pts2 = [[pts[0], kb.sb([128, 512], BF16, "pt")], [pts[1], kb.sb([128, 512], BF16, "pt")]]

    def idx(j):
        qi_t, bqi = qis[j % 2]
        kb.dma("sp", qi_t[:], qiT[:, j, :], [dr], [bqi])
        dg, bdg = dgs[j % 2]
        for h in range(8):
            kb.op("dve", lambda e, h=h: e.tensor_scalar(out=dg[:, h * 128:(h + 1) * 128], in0=idf[:],
                                                        scalar1=wit[:, j * 8 + h:j * 8 + h + 1], scalar2=None,
                                                        op0=ALU.mult), [bidf, bwi], [bdg])
        steps = [(sb, h) for sb in range(j + 1) for h in range(8)]
        pend = []
        sbank = {}
        for s in range(len(steps) + LAG):
            if s < len(steps):
                sb, h = steps[s]
                pd, bpd = bank(nd[0] % 4)
                nd[0] += 1
                kb.op("pe", lambda e, h=h, sb=sb: e.matmul(pd, qi_t[:, h * 128:(h + 1) * 128],
                                                           kit[:, sb * 512:(sb + 1) * 512], start=True, stop=True),
                      [bqi, bki], [bpd])
                rh, brh = rhs_[nr[0] % 5]
                nr[0] += 1
                kb.op("act", lambda e: e.activation(out=rh[:], in_=pd, func=AF.Relu), [bpd], [brh])
                pend.append((sb, h, rh, brh))
            if s - LAG >= 0:
                sb, h, rh, brh = pend[s - LAG]
                if h == 0:
                    sbank[sb] = bank(6 + nsc[0] % 2)
                    nsc[0] += 1
                ps, bps = sbank[sb]
                kb.op("pe", lambda e, h=h: e.matmul(ps, dg[:, h * 128:(h + 1) * 128], rh[:],
                                                    start=(h == 0), stop=(h == 7)), [bdg, brh], [bps])
                if h == 7:
                    kb.op("dve", lambda e, sb=sb: e.tensor_copy(out=score[:, sb * 512:(sb + 1) * 512], in_=ps),
                          [bps], [bscore])

    def bis(j):
        n = 512 * (j + 1)
        ns, bns = nots[j % 2]
        sc = score[:, 0:n]
        kb.op("dve", lambda e: e.tensor_reduce(out=st[:, 0:1], in_=sc, axis=AX.X, op=ALU.min), [bscore], [bst])
        kb.op("dve", lambda e: e.tensor_tensor(out=score[:, n - 512:n], in0=score[:, n - 512:n], in1=pent[:],
                                               op=ALU.add), [bscore, bpen], [bscore])
        kb.op("dve", lambda e: e.tensor_reduce(out=st[:, 1:2], in_=sc, axis=AX.X, op=ALU.max), [bscore], [bst])
        kb.op("dve", lambda e: e.tensor_tensor(out=st[:, 2:3], in0=st[:, 1:2], in1=st[:, 0:1], op=ALU.subtract),
              [bst], [bst])
        kb.op("dve", lambda e: e.scalar_tensor_tensor(out=st[:, 3:4], in0=st[:, 2:3], scalar=-0.01, in1=st[:, 0:1],
                                                      op0=ALU.mult, op1=ALU.add), [bst], [bst])
        kb.op("dve", lambda e: e.tensor_scalar(out=st[:, 3:4], in0=st[:, 3:4], scalar1=-1e-6, scalar2=None,
                                               op0=ALU.add), [bst], [bst])
        kb.op("dve", lambda e: e.tensor_tensor(out=st[:, 4:5], in0=st[:, 1:2], in1=st[:, 3:4], op=ALU.subtract),
              [bst], [bst])
        kb.op("dve", lambda e: e.scalar_tensor_tensor(out=st[:, 5:6], in0=st[:, 4:5], scalar=0.5, in1=st[:, 3:4],
                                                      op0=ALU.mult, op1=ALU.add), [bst], [bst])
        kb.op("dve", lambda e: e.tensor_scalar(out=st[:, 6:7], in0=st[:, 4:5], scalar1=0.25, scalar2=None,
                                               op0=ALU.mult), [bst], [bst])
        for it in range(NIT):
            kb.op("dve", lambda e: e.tensor_scalar(out=ns[:, 0:n], in0=sc, scalar1=st[:, 5:6], scalar2=None,
                                                   op0=ALU.is_ge, op1=ALU.add, accum_out=st[:, 7:8]),
                  [bscore, bst], [bns, bst])
            kb.op("dve", lambda e: e.tensor_scalar(out=st[:, 8:9], in0=st[:, 7:8], scalar1=KSEL - 0.5, scalar2=2.0,
                                                   op0=ALU.is_ge, op1=ALU.mult), [bst], [bst])
            kb.op("dve", lambda e: e.scalar_tensor_tensor(out=st[:, 9:10], in0=st[:, 8:9], scalar=-1.0,
                                                          in1=st[:, 6:7], op0=ALU.add, op1=ALU.mult), [bst], [bst])
            kb.op("dve", lambda e: e.tensor_tensor(out=st[:, 5:6], in0=st[:, 5:6], in1=st[:, 9:10], op=ALU.add),
                  [bst], [bst])
            kb.op("dve", lambda e: e.tensor_scalar(out=st[:, 6:7], in0=st[:, 6:7], scalar1=0.5, scalar2=None,
                                                   op0=ALU.mult), [bst], [bst])
        kb.op("dve", lambda e: e.scalar_tensor_tensor(out=st[:, 10:11], in0=st[:, 6:7], scalar=-4.0, in1=st[:, 5:6],
                                                      op0=ALU.mult, op1=ALU.add), [bst], [bst])
        kb.op("dve", lambda e: e.tensor_scalar(out=ns[:, 0:n], in0=sc, scalar1=st[:, 10:11], scalar2=None,
                                               op0=ALU.is_lt), [bscore, bst], [bns])

    def att_main(j):
        ns, bns = nots[j % 2]
        qt, bqt = qts[j % 2]
        kb.dma("sp", qt[:], qT[:, j, :], [dr], [bqt])
        oacc = [bank(4), bank(5)]
        for g in range(2):
            oa, boa = oacc[g]
            kb.op("pe", lambda e: e.matmul(oa[:, 0:260], idb[:], zb[:], start=True, stop=False),
                  [bidb, bzb], [boa])
        ntile = 4 * j + 4
        vtl = {}
        for stl in range(ntile + 1):
            if stl < ntile:
                vt, bvt = vts[nv[0] % 4]
                nv[0] += 1
                vtl[stl] = (vt, bvt)
                kb.dma("sp", vt[:], v1[stl, :, :], [dr], [bvt])
                u = stl - 4 * j + 5
                for g in range(2):
                    lgp, blgp = bank(2 * g + stl % 2)
                    kb.op("pe", lambda e: e.matmul(lgp, ns[:, stl * 128:(stl + 1) * 128], n4[:], start=True,
                                                   stop=False), [bns, bn4], [blgp])
                    if u >= 0:
                        kb.op("pe", lambda e, u=u: e.matmul(lgp, idb[:], TU[:, u, g * 512:(g + 1) * 512],
                                                            start=False, stop=False), [bidb, bTU], [blgp])
                    for hq in range(4):
                        kb.op("pe", lambda e, hq=hq: e.matmul(lgp[:, hq * 128:(hq + 1) * 128],
                                                              kTv[g * 64:(g + 1) * 64, hq, stl * 128:(stl + 1) * 128],
                                                              qt[g * 64:(g + 1) * 64, hq * 128:(hq + 1) * 128],
                                                              start=False, stop=(hq == 3)), [bkT, bqt], [blgp])
                    pt, bpt = pts2[g][stl % 2]
                    kb.op("act", lambda e: e.activation(out=pt[:], in_=lgp, func=AF.Exp), [blgp], [bpt])
            if stl >= 1:
                sp_ = stl - 1
                vt, bvt = vtl[sp_]
                for g in range(2):
                    pt, bpt = pts2[g][sp_ % 2]
                    oa, boa = oacc[g]
                    for hq in range(4):
                        h = g * 4 + hq
                        kb.op("pe", lambda e, hq=hq, h=h: e.matmul(oa[:, hq * 65:(hq + 1) * 65],
                                                                   pt[:, hq * 128:(hq + 1) * 128],
                                                                   vt[:, h * 65:(h + 1) * 65],
                                                                   start=False, stop=(sp_ == ntile - 1)),
                              [bpt, bvt], [boa])

    def att_fin(j):
        oacc = [bank(4), bank(5)]
        yo, byo = outs[j % 2]
        for g in range(2):
            oa, boa = oacc[g]
            oav = oa[:, 0:260].rearrange("p (h c) -> p h c", h=4)
            kb.op("dve", lambda e: e.reciprocal(out=st[:, 16 + g * 4:20 + g * 4],
                                                in_=oav[:, :, 64:65].rearrange("p h c -> p (h c)")), [boa], [bst])
            kb.op("dve", lambda e: e.tensor_tensor(
                out=yo[:, g * 256:(g + 1) * 256].rearrange("p (h d) -> p h d", h=4), in0=oav[:, :, 0:64],
                in1=st[:, 16 + g * 4:20 + g * 4].unsqueeze(2).to_broadcast([128, 4, 64]), op=ALU.mult),
                [boa, bst], [byo])
        kb.dma("sp", yb[j * 128:(j + 1) * 128, :], yo[:], [byo], [byb])

    idx(0)
    bis(0)
    for j in range(nj):
        if j + 1 < nj:
            idx(j + 1)
        att_main(j)
        if j + 1 < nj:
            bis(j + 1)
        att_fin(j)
    kb.finish([byb])
    return nc


def t5_bucket_np(rel):
    nb = 16
    max_exact = 8
    side = np.where(rel > 0, nb, 0)
    n = np.abs(rel)
    nf = np.maximum(n, 1).astype(np.float32)
    large = max_exact + (np.log(nf / max_exact) / np.float32(np.log(1024 / max_exact)) * (nb - max_exact)).astype(np.int32)
    large = np.minimum(large, nb - 1)
    return side + np.where(n < max_exact, n, large)


def prep_l3(ob, of, cid):
    b, r = cid // 4, cid % 4
    bf = ob.dtype
    qsel = ob[b].reshape(64, 128, NB1)[r::4][:, ::-1]
    q = qsel[..., 0:512].reshape(16, 128, 2, 4, 64)
    qT = np.ascontiguousarray(q.transpose(2, 4, 0, 3, 1)).reshape(128, 16, 512)
    qi = qsel[..., 1536:2048].reshape(16, 128, 8, 64)
    qiT = np.ascontiguousarray(qi.transpose(3, 0, 2, 1)).reshape(64, 16, 1024)
    wsel = of[b].reshape(64, 128, NF1)[r::4][:, ::-1, 2056:2064]
    wi = np.ascontiguousarray(wsel.transpose(1, 0, 2)).reshape(128, 128).astype(np.float32)
    k = ob[b, :, 512:1024].reshape(S, 2, 4, 64)
    kT = np.ascontiguousarray(k.transpose(1, 3, 2, 0)).reshape(128, 4 * S)
    kiT = np.ascontiguousarray(ob[b, :, 2048:2112].T)
    v = ob[b, :, 1024:1536].reshape(64, 128, 8, 64)
    v1 = np.ones((64, 128, 8, 65), bf)
    v1[..., 0:64] = v
    v1 = v1.reshape(64, 128, 520)
    tq = np.arange(128)[:, None]
    sk = np.arange(512)[None, :]
    pen = np.where((sk // 64) <= 2 * r + (tq < 64), 0.0, -1e30).astype(np.float32)
    m = np.arange(NREL)
    rel = m - 767 - 128 * r
    bk = t5_bucket_np(rel)
    oh = np.zeros((32, NREL), np.float32)
    oh[bk, m] += 1.0
    oh[15, :] -= 1.0
    negI4 = np.tile(np.eye(128, dtype=np.float32) * NEGBIG, (1, 4)).astype(bf)
    return {"qT": qT, "kT": kT, "v1": v1, "qiT": qiT, "kiT": kiT, "wi": wi, "pen": pen, "oh": oh,
            "negI4": negI4, "ident": np.eye(128, dtype=np.float32)}


def run_l3(ob, of, rel_bias, nj=16):
    nc = _get("l3_%d" % nj, lambda: build_l3(nj))
    maps = []
    for cid in range(NCORES):
        m = prep_l3(ob, of, cid)
        m["relb"] = np.ascontiguousarray(rel_bias.astype(np.float32))
        maps.append(m)
    res = _run(nc, maps)
    parts = [r["yb"].reshape(16, 128, 512)[:, ::-1].reshape(TPC, 512) for r in res.results]
    return uncore_tokens(parts, (512,))


NCH = 64
PRE_STOP = 0
L2VAR = 0
POOL_ENG = "dve"


def build_l2(nch=NCH, stop=None):
    nc = bass.Bass("TRN2", target_bir_lowering=False)
    xin = _din(nc, "xin", [128, 3, S + 3])
    cw = _din(nc, "cw", [128, 12])
    zin = _din(nc, "zin", [128, NCH, 128])
    bcol = _din(nc, "bcol", [128, NCH])
    acol = _din(nc, "acol", [128, NCH])
    sc3 = _din(nc, "sc3", [1, 2])
    gnw = _din(nc, "gnw", [1, 128])
    cst = _din(nc, "cst", [128, 7, 128])
    ya = _dout(nc, "ya", [S, 128])
    kb = KB(nc)
    dr = Buf("dram_in")
    bya = Buf("ya")

    PSW = []
    for i in range(4):
        t = nc.alloc_psum_tensor("PS%d" % i, [128, 1024], F32)
        PSW.append(t)
    slots = []
    for bnk in range(6):
        t = PSW[bnk // 2]
        c0 = (bnk % 2) * 512
        slots.append((t[:, c0:c0 + 128], Buf("slot%d" % bnk)))
    wide = [(PSW[3][:, 0:512], Buf("wide0")), (PSW[3][:, 512:1024], Buf("wide1"))]
    nslot = [0]

    def slot():
        s = slots[nslot[0] % len(slots)]
        nslot[0] += 1
        return s

    ct, bct = kb.sb([128, 7, 128], F32, "cst")
    kb.dma("sp", ct[:], cst[:, :, :], [dr], [bct])
    ident, LT, ones, negones, penL, SM, sel127 = [ct[:, i, :] for i in range(7)]
    cwt, bcw = kb.sb([128, 12], F32, "cw")
    kb.dma("sp", cwt[:], cw[:, :], [dr], [bcw])
    gnb, bgnb = kb.sb([128, 128], F32, "gnw")
    kb.dma("sp", gnb[:], gnw[0:1, :].to_broadcast([128, 128]), [dr], [bgnb])
    s3, bs3 = kb.sb([128, 2], F32, "sc3")
    kb.dma("sp", s3[:], sc3[0:1, :].to_broadcast([128, 2]), [dr], [bs3])
    epst, beps = kb.sb([128, 3], F32, "eps")
    kb.op("dve", lambda e: e.memset(epst[:, 0:1], EPS), [], [beps])
    kb.op("dve", lambda e: e.memset(epst[:, 1:2], 128.0 * EPS), [], [beps])
    kb.op("dve", lambda e: e.memset(epst[:, 2:3], 1.0), [], [beps])

    cols, bcols = kb.sb([128, 10, NCH], F32, "cols")
    BETA, G, GC, EGC, BG, KD, EGL, NB_, TMP, TMP2 = range(10)
    kb.dma("sp", cols[:, BETA, :], bcol[:, :], [dr], [bcols])
    kb.dma("sp", cols[:, TMP, :], acol[:, :], [dr], [bcols])
    kb.op("act", lambda e: e.activation(out=cols[:, BETA, :], in_=cols[:, BETA, :], func=AF.Sigmoid), [bcols], [bcols])
    kb.op("act", lambda e: e.activation(out=cols[:, TMP, :], in_=cols[:, TMP, :], func=AF.Exp, bias=s3[:, 1:2]),
          [bcols, bs3], [bcols])
    kb.op("act", lambda e: e.activation(out=cols[:, TMP, :], in_=cols[:, TMP, :], func=AF.Ln, bias=epst[:, 2:3]),
          [bcols, beps], [bcols])
    kb.op("act", lambda e: e.activation(out=s3[:, 0:1], in_=s3[:, 0:1], func=AF.Exp), [bs3], [bs3])
    kb.op("dve", lambda e: e.tensor_scalar(out=cols[:, G, :], in0=cols[:, TMP, :], scalar1=s3[:, 0:1], scalar2=-1.0,
                                           op0=ALU.mult, op1=ALU.mult), [bcols, bs3], [bcols])
    pg, bpg = slot()
    kb.op("pe", lambda e: e.matmul(pg[:, 0:NCH], LT, cols[:, G, :], start=True, stop=True), [bct, bcols], [bpg])
    kb.op("dve", lambda e: e.tensor_copy(out=cols[:, GC, :], in_=pg[:, 0:NCH]), [bpg], [bcols])
    pg2, bpg2 = slot()
    kb.op("pe", lambda e: e.matmul(pg2[:, 0:NCH], sel127, cols[:, GC, :], start=True, stop=True), [bct, bcols], [bpg2])
    kb.op("dve", lambda e: e.tensor_copy(out=cols[:, TMP, :], in_=pg2[:, 0:NCH]), [bpg2], [bcols])
    kb.op("act", lambda e: e.activation(out=cols[:, EGL, :], in_=cols[:, TMP, :], func=AF.Exp), [bcols], [bcols])
    kb.op("dve", lambda e: e.tensor_tensor(out=cols[:, TMP2, :], in0=cols[:, TMP, :], in1=cols[:, GC, :], op=ALU.subtract),
          [bcols], [bcols])
    kb.op("act", lambda e: e.activation(out=cols[:, KD, :], in_=cols[:, TMP2, :], func=AF.Exp), [bcols], [bcols])
    kb.op("act", lambda e: e.activation(out=cols[:, EGC, :], in_=cols[:, GC, :], func=AF.Exp), [bcols], [bcols])
    kb.op("dve", lambda e: e.tensor_tensor(out=cols[:, BG, :], in0=cols[:, EGC, :], in1=cols[:, BETA, :], op=ALU.mult),
          [bcols], [bcols])
    kb.op("dve", lambda e: e.tensor_scalar(out=cols[:, NB_, :], in0=cols[:, BETA, :], scalar1=-1.0, scalar2=None,
                                           op0=ALU.mult), [bcols], [bcols])

    if stop == "cols":
        kb.dma("sp", ya[0:128, 0:NCH], cols[:, GC, :], [bcols], [bya])
        kb.finish([bya])
        return nc
    QT, bQT = kb.sb([128, S], F32, "QT")
    KT, bKT = kb.sb([128, S], F32, "KT")
    Vtok, bVtok = kb.sb([128, NCH, 128], F32, "Vtok")
    Ktok, bKtok = kb.sb([128, NCH, 128], F32, "Ktok")
    xbs = [kb.sb([128, 3, 515], F32, "xb") for _ in range(2)]
    u, bu = kb.sb([128, 3, 512], F32, "u")
    sqt, bsq = kb.sb([128, 512], F32, "sq")
    rs, brs = kb.sb([128, 512], F32, "rs")
    nblk = (nch * 128 + 511) // 512
    for blk in range(nblk):
        xb, bxb = xbs[blk % 2]
        kb.dma("sp", xb[:], xin[:, :, blk * 512:blk * 512 + 515], [dr], [bxb])
        for a in range(3):
            kb.op("dve", lambda e, a=a: e.tensor_scalar(out=u[:, a, :], in0=xb[:, a, 0:512],
                                                        scalar1=cwt[:, a * 4:a * 4 + 1], scalar2=None, op0=ALU.mult),
                  [bxb, bcw], [bu])
            for tap in range(1, 4):
                kb.op("dve", lambda e, a=a, tap=tap: e.scalar_tensor_tensor(
                    out=u[:, a, :], in0=xb[:, a, tap:tap + 512], scalar=cwt[:, a * 4 + tap:a * 4 + tap + 1],
                    in1=u[:, a, :], op0=ALU.mult, op1=ALU.add), [bxb, bcw, bu], [bu])
        kb.op("act", lambda e: e.activation(out=u[:].rearrange("p a n -> p (a n)"),
                                            in_=u[:].rearrange("p a n -> p (a n)"), func=AF.Silu), [bu], [bu])
        cs = slice(blk * 512, (blk + 1) * 512)
        for a, (dst, bdst, scl, epi) in enumerate([(QT, bQT, 128.0, 1), (KT, bKT, 1.0, 0)]):
            kb.op("dve", lambda e, a=a: e.tensor_tensor(out=sqt[:], in0=u[:, a, :], in1=u[:, a, :], op=ALU.mult),
                  [bu], [bsq])
            pw, bpw = wide[a]
            kb.op("pe", lambda e: e.matmul(pw, ones, sqt[:], start=True, stop=True), [bct, bsq], [bpw])
            kb.op("act", lambda e, scl=scl, epi=epi: e.activation(out=rs[:], in_=pw, func=AF.Sqrt, scale=scl,
                                                                  bias=epst[:, epi:epi + 1]), [bpw, beps], [brs])
            kb.op("dve", lambda e: e.reciprocal(out=rs[:], in_=rs[:]), [brs], [brs])
            kb.op("dve", lambda e, a=a, dst=dst: e.tensor_tensor(out=dst[:, cs], in0=u[:, a, :], in1=rs[:], op=ALU.mult),
                  [bu, brs], [bdst])
        for q4 in range(4):
            ch = blk * 4 + q4
            if ch >= nch:
                break
            pk, bpk = slot()
            kb.op("pe", lambda e, ch=ch: e.transpose(out=pk, in_=KT[:, ch * 128:(ch + 1) * 128], identity=ident),
                  [bKT, bct], [bpk])
            kb.op("act", lambda e, ch=ch: e.copy(out=Ktok[:, ch, :], in_=pk), [bpk], [bKtok])
            pv, bpv = slot()
            kb.op("pe", lambda e, q4=q4: e.transpose(out=pv, in_=u[:, 2, q4 * 128:(q4 + 1) * 128], identity=ident),
                  [bu, bct], [bpv])
            kb.op("act", lambda e, ch=ch: e.copy(out=Vtok[:, ch, :], in_=pv), [bpv], [bVtok])

    if stop == "prep":
        kb.dma("sp", ya[0:128, :], Ktok[:, 0, :], [bKtok], [bya])
        kb.dma("sp", ya[128:256, :], Vtok[:, 0, :], [bVtok], [bya])
        kb.dma("sp", ya[256:384, :], QT[:, 0:128], [bQT], [bya])
        kb.finish([bya])
        return nc
    RING = 4
    ring = [dict(wdT=kb.sb([128, 128], F32, "wdT"), uval=kb.sb([128, 128], F32, "uval"),
                 attnT=kb.sb([128, 128], F32, "attnT"), kdec=kb.sb([128, 128], F32, "kdec")) for _ in range(RING)]
    tmps = {}

    def tmp(name, k=2):
        if name not in tmps:
            tmps[name] = [kb.sb([128, 128], F32, name) for _ in range(k)]
            tmps[name + "_i"] = 0
        i = tmps[name + "_i"]
        tmps[name + "_i"] = i + 1
        return tmps[name][i % k]

    def pre(n):
        R = ring[n % RING]
        kt = KT[:, n * 128:(n + 1) * 128]
        qt = QT[:, n * 128:(n + 1) * 128]
        dg, bdg = tmp("diag")
        kb.op("dve", lambda e: e.tensor_scalar(out=dg[:], in0=ident, scalar1=cols[:, GC, n:n + 1], scalar2=None,
                                               op0=ALU.mult), [bct, bcols], [bdg])
        pD, bpD = slot()
        kb.op("pe", lambda e: e.matmul(pD, dg[:], ones, start=True, stop=False), [bdg, bct], [bpD])
        kb.op("pe", lambda e: e.matmul(pD, negones, dg[:], start=False, stop=True), [bdg, bct], [bpD])
        Dl, bDl = tmp("Dl")
        E, bE = tmp("E")
        if L2VAR == 1:
            kb.op("dve", lambda e: e.tensor_copy(out=E[:], in_=pD), [bpD], [bE])
        elif L2VAR == 2:
            kb.op("dve", lambda e: e.tensor_tensor(out=E[:], in0=pD, in1=penL, op=ALU.min), [bpD, bct], [bE])
        elif L2VAR == 3:
            kb.op("dve", lambda e: e.tensor_copy(out=Dl[:], in_=pD), [bpD], [bDl])
            kb.op("act", lambda e: e.activation(out=E[:], in_=Dl[:], func=AF.Exp), [bDl], [bE])
        else:
            kb.op("dve", lambda e: e.tensor_tensor(out=Dl[:], in0=pD, in1=penL, op=ALU.min), [bpD, bct], [bDl])
            kb.op("act", lambda e: e.activation(out=E[:], in_=Dl[:], func=AF.Exp), [bDl], [bE])
        if PRE_STOP == 1:
            kb.dma("sp", ya[0:128, :], E[:], [bE], [bya]); return
        Es, bEs = tmp("Es")
        kb.op(POOL_ENG, lambda e: e.tensor_tensor(out=Es[:], in0=E[:], in1=SM, op=ALU.mult), [bE, bct], [bEs])
        pA, bpA = slot()
        kb.op("pe", lambda e: e.matmul(pA, kt, kt, start=True, stop=True), [bKT], [bpA])
        Nm, bN = tmp("N", 3)
        kb.op("dve", lambda e: e.scalar_tensor_tensor(out=Nm[:], in0=pA, scalar=cols[:, NB_, n:n + 1], in1=Es[:],
                                                      op0=ALU.mult, op1=ALU.mult), [bpA, bcols, bEs], [bN])
        if PRE_STOP == 2:
            kb.dma("sp", ya[0:128, :], Nm[:], [bN], [bya]); return
        pM, bpM = slot()
        kb.op("pe", lambda e: e.transpose(out=pM, in_=Nm[:], identity=ident), [bN, bct], [bpM])
        Mm, bM = tmp("M", 3)
        kb.op("act", lambda e: e.copy(out=Mm[:], in_=pM), [bpM], [bM])
        P, bP = tmp("P", 3)
        kb.op("dve", lambda e: e.tensor_tensor(out=P[:], in0=Mm[:], in1=ident, op=ALU.add), [bM, bct], [bP])
        if PRE_STOP == 3:
            kb.dma("sp", ya[0:128, :], P[:], [bP], [bya]); return
        pQK, bpQK = slot()
        kb.op("pe", lambda e: e.matmul(pQK, qt, kt, start=True, stop=True), [bQT, bKT], [bpQK])
        at, bat = tmp("attn")
        kb.op("dve", lambda e: e.tensor_tensor(out=at[:], in0=pQK, in1=E[:], op=ALU.mult), [bpQK, bE], [bat])
        pAT, bpAT = slot()
        kb.op("pe", lambda e: e.transpose(out=pAT, in_=at[:], identity=ident), [bat, bct], [bpAT])
        aT, baT = R["attnT"]
        kb.op("act", lambda e: e.copy(out=aT[:], in_=pAT), [bpAT], [baT])
        if PRE_STOP == 4:
            kb.dma("sp", ya[0:128, :], aT[:], [baT], [bya]); return
        for lev in range(1, 7):
            if PRE_STOP == 4 + lev:
                kb.dma("sp", ya[0:128, :], P[:], [bP], [bya]); return
            pN2, bpN2 = slot()
            kb.op("pe", lambda e: e.matmul(pN2, Mm[:], Nm[:], start=True, stop=True), [bM, bN], [bpN2])
            N2, bN2 = tmp("N", 3)
            kb.op("act", lambda e: e.copy(out=N2[:], in_=pN2), [bpN2], [bN2])
            if lev < 6:
                pM2, bpM2 = slot()
                kb.op("pe", lambda e: e.matmul(pM2, Nm[:], Mm[:], start=True, stop=True), [bM, bN], [bpM2])
                M2, bM2 = tmp("M", 3)
                kb.op("act", lambda e: e.copy(out=M2[:], in_=pM2), [bpM2], [bM2])
            pP, bpP = slot()
            kb.op("pe", lambda e: e.matmul(pP, N2[:], P[:], start=True, stop=True), [bN2, bP], [bpP])
            P2, bP2 = tmp("P", 3)
            kb.op("dve", lambda e: e.tensor_tensor(out=P2[:], in0=pP, in1=P[:], op=ALU.add), [bpP, bP], [bP2])
            P, bP = P2, bP2
            Nm, bN = N2, bN2
            if lev < 6:
                Mm, bM = M2, bM2
        kbg, bkbg = tmp("kbg")
        kb.op(POOL_ENG, lambda e: e.tensor_scalar(out=kbg[:], in0=Ktok[:, n, :], scalar1=cols[:, BG, n:n + 1],
                                                scalar2=None, op0=ALU.mult), [bKtok, bcols], [bkbg])
        vb, bvb = tmp("vb")
        kb.op(POOL_ENG, lambda e: e.tensor_scalar(out=vb[:], in0=Vtok[:, n, :], scalar1=cols[:, BETA, n:n + 1],
                                                scalar2=None, op0=ALU.mult), [bVtok, bcols], [bvb])
        kd, bkd = R["kdec"]
        kb.op(POOL_ENG, lambda e: e.tensor_scalar(out=kd[:], in0=Ktok[:, n, :], scalar1=cols[:, KD, n:n + 1],
                                                scalar2=None, op0=ALU.mult), [bKtok, bcols], [bkd])
        pW, bpW = slot()
        kb.op("pe", lambda e: e.matmul(pW, kbg[:], P[:], start=True, stop=True), [bkbg, bP], [bpW])
        wd, bwd = R["wdT"]
        kb.op("act", lambda e: e.copy(out=wd[:], in_=pW), [bpW], [bwd])
        pU, bpU = slot()
        kb.op("pe", lambda e: e.matmul(pU, P[:], vb[:], start=True, stop=True), [bP, bvb], [bpU])
        uv, buv = R["uval"]
        kb.op("act", lambda e: e.copy(out=uv[:], in_=pU), [bpU], [buv])

    states = [kb.sb([128, 128], F32, "state") for _ in range(2)]
    kb.op("dve", lambda e: e.memset(states[0][0][:], 0.0), [], [states[0][1]])
    zts = [kb.sb([128, 128], F32, "zt") for _ in range(2)]
    st, bst = kb.sb([128, 8], F32, "st")
    junk, bjunk = kb.sb([128, 128], F32, "junk")

    def scan(n):
        R = ring[n % RING]
        wd, bwd = R["wdT"]
        uv, buv = R["uval"]
        aT, baT = R["attnT"]
        kd, bkd = R["kdec"]
        S0, bS0 = states[n % 2]
        S1, bS1 = states[(n + 1) % 2]
        zt, bzt = zts[n % 2]
        kb.dma("sp", zt[:], zin[:, n, :], [dr], [bzt])
        ppv, bppv = slot()
        kb.op("pe", lambda e: e.matmul(ppv, wd[:], S0[:], start=True, stop=True), [bwd, bS0], [bppv])
        po1, bpo1 = slot()
        kb.op("pe", lambda e: e.matmul(po1, QT[:, n * 128:(n + 1) * 128], S0[:], start=True, stop=True),
              [bQT, bS0], [bpo1])
        vn, bvn = tmp("vnew")
        kb.op("dve", lambda e: e.tensor_tensor(out=vn[:], in0=uv[:], in1=ppv, op=ALU.subtract), [buv, bppv], [bvn])
        psu, bpsu = slot()
        kb.op("pe", lambda e: e.matmul(psu, kd[:], vn[:], start=True, stop=True), [bkd, bvn], [bpsu])
        po2, bpo2 = slot()
        kb.op("pe", lambda e: e.matmul(po2, aT[:], vn[:], start=True, stop=True), [baT, bvn], [bpo2])
        kb.op("dve", lambda e: e.scalar_tensor_tensor(out=S1[:], in0=S0[:], scalar=cols[:, EGL, n:n + 1], in1=psu,
                                                      op0=ALU.mult, op1=ALU.add), [bS0, bcols, bpsu], [bS1])
        o2, bo2 = tmp("o2")
        kb.op("act", lambda e: e.copy(out=o2[:], in_=po2), [bpo2], [bo2])
        o, bo = tmp("o")
        kb.op("dve", lambda e: e.scalar_tensor_tensor(out=o[:], in0=po1, scalar=cols[:, EGC, n:n + 1], in1=o2[:],
                                                      op0=ALU.mult, op1=ALU.add), [bpo1, bcols, bo2], [bo])
        kb.op("act", lambda e: e.activation(out=junk[:], in_=o[:], func=AF.Square, accum_out=st[:, 0:1]),
              [bo], [bjunk, bst])
        kb.op("act", lambda e: e.activation(out=st[:, 1:2], in_=st[:, 0:1], func=AF.Ln, scale=1.0 / 128,
                                            bias=epst[:, 0:1]), [bst, beps], [bst])
        kb.op("act", lambda e: e.activation(out=st[:, 2:3], in_=st[:, 1:2], func=AF.Exp, scale=-0.5), [bst], [bst])
        sg, bsg = tmp("sg")
        kb.op("act", lambda e: e.activation(out=sg[:], in_=zt[:], func=AF.Exp, scale=-1.0), [bzt], [bsg])
        kb.op(POOL_ENG, lambda e: e.tensor_scalar(out=sg[:], in0=sg[:], scalar1=1.0, scalar2=None, op0=ALU.add),
              [bsg], [bsg])
        kb.op("dve", lambda e: e.reciprocal(out=sg[:], in_=sg[:]), [bsg], [bsg])
        kb.op(POOL_ENG, lambda e: e.tensor_tensor(out=sg[:], in0=sg[:], in1=zt[:], op=ALU.mult), [bsg, bzt], [bsg])
        t1, bt1 = tmp("t1")
        kb.op("dve", lambda e: e.scalar_tensor_tensor(out=t1[:], in0=o[:], scalar=st[:, 2:3], in1=gnb[:],
                                                      op0=ALU.mult, op1=ALU.mult), [bo, bst, bgnb], [bt1])
        yt_, byt_ = tmp("yout")
        kb.op("dve", lambda e: e.tensor_tensor(out=yt_[:], in0=t1[:], in1=sg[:], op=ALU.mult), [bt1, bsg], [byt_])
        kb.dma("sp", ya[n * 128:(n + 1) * 128, :], yt_[:], [byt_], [bya])

    LOOK = 2
    if stop == "alloc":
        kb.dma("sp", ya[0:128, :], states[0][0][:], [states[0][1]], [bya])
        kb.finish([bya])
        return nc
    if stop == "pre":
        pre(0)
        for i_, k_ in enumerate(["wdT", "uval", "attnT", "kdec"] if PRE_STOP == 0 else []):
            kb.dma("sp", ya[i_ * 128:(i_ + 1) * 128, :], ring[0][k_][0][:], [ring[0][k_][1]], [bya])
        kb.finish([bya])
        return nc
    for n in range(min(LOOK, nch)):
        pre(n)
    for n in range(nch):
        if n + LOOK < nch:
            pre(n + LOOK)
        scan(n)
    kb.finish([bya])
    return nc


def l2_consts():
    i = np.arange(128)
    ident = np.eye(128, dtype=np.float32)
    LT = (i[:, None] <= i[None, :]).astype(np.float32)
    ones = np.ones((128, 128), np.float32)
    penL = np.where(i[:, None] >= i[None, :], 0.0, -1e30).astype(np.float32)
    SM = (i[:, None] > i[None, :]).astype(np.float32)
    sel = np.zeros((128, 128), np.float32)
    sel[127, :] = 1.0
    return np.ascontiguousarray(np.stack([ident, LT, ones, -ones, penL, SM, sel], axis=1))


def run_l2(of, conv_w_l, a_log_l, dt_bias_l, gnw_l, nch=NCH, stop=None):
    nc = _get("l2_%d_%s" % (nch, stop), lambda: build_l2(nch, stop))
    cst = l2_consts()
    maps = []
    for cid in range(NCORES):
        b, g = cid // 4, cid % 4
        xs = []
        cws = []
        for a in range(3):
            cols_ = slice(a * 512 + g * 128, a * 512 + (g + 1) * 128)
            xa = np.zeros((128, S + 3), np.float32)
            xa[:, 3:] = of[b, :, cols_].T
            xs.append(xa)
            cws.append(conv_w_l[:, cols_].T)
        xin = np.ascontiguousarray(np.stack(xs, axis=1))
        cw = np.ascontiguousarray(np.concatenate(cws, axis=1)).astype(np.float32)
        z = of[b, :, 1536 + g * 128:1536 + (g + 1) * 128].reshape(NCH, 128, 128).transpose(1, 0, 2)
        bc = of[b, :, 2048 + g].reshape(NCH, 128).T
        ac = of[b, :, 2052 + g].reshape(NCH, 128).T
        maps.append({"xin": xin, "cw": cw, "zin": np.ascontiguousarray(z), "bcol": np.ascontiguousarray(bc),
                     "acol": np.ascontiguousarray(ac),
                     "sc3": np.array([[a_log_l[g], dt_bias_l[g]]], np.float32),
                     "gnw": gnw_l.reshape(1, 128).astype(np.float32), "cst": cst})
    res = _run(nc, maps)
    ya = np.zeros((NB, S, 512), np.float32)
    for cid in range(NCORES):
        b, g = cid // 4, cid % 4
        ya[b, :, g * 128:(g + 1) * 128] = res.results[cid]["ya"]
    return ya


def kernel(x, c, rel_bias, mod_w, mod_b, norm_mix_w, norm_ffn_w, w_in, conv_w, a_log, dt_bias,
           gdn_norm_w, q_norm_w, k_norm_w, w_out, router_w, router_b, w1, b1, w2, b2):
    f = lambda a: np.ascontiguousarray(np.asarray(a), dtype=np.float32)
    x = f(x)
    c = f(c)
    rel_bias = f(rel_bias)
    for l in range(2):
        of, ob = run_l1(x, c, f(mod_w[l]), f(mod_b[l]), f(norm_mix_w[l]), f(w_in[l]), f(q_norm_w[l]), f(k_norm_w[l]))
        ya = run_l2(of, f(conv_w[l]), f(a_log[l]), f(dt_bias[l]), f(gdn_norm_w[l]))
        yb = run_l3(ob, of, rel_bias)
        ycat = np.ascontiguousarray(np.concatenate([ya, yb], axis=-1))
        w1r, b1r, w2r, b2r = prep_l4_weights(f(w1[l]), f(b1[l]), f(w2[l]), f(b2[l]))
        x = run_l4(x, ycat, c, f(mod_w[l]), f(mod_b[l]), f(norm_ffn_w[l]), f(w_out[l]), f(router_w[l]),
                   f(router_b[l]), w1r, b1r, w2r, b2r)
    return x
```

```python
import numpy as np
import concourse.bass as bass
import concourse.mybir as mybir
from concourse.bass_utils import run_bass_kernel_spmd

F32 = mybir.dt.float32
BF16 = mybir.dt.bfloat16
U32 = mybir.dt.uint32
AF = mybir.ActivationFunctionType
ALU = mybir.AluOpType
AX = mybir.AxisListType

D = 1024
S = 8192
NB = 2
D_IN = 4176
EPS = 1e-6
NCORES = 8
TPC = 2048
NT = 16


class Buf:
    __slots__ = ("name", "w", "r", "dsem", "dcnt")

    def __init__(self, name):
        self.name = name
        self.w = None
        self.r = {}
        self.dsem = None
        self.dcnt = 0


class KB:
    def __init__(self, nc, self_sync=True):
        self.nc = nc
        self.eng = {"pe": nc.tensor, "dve": nc.vector, "act": nc.scalar,
                    "pool": nc.gpsimd, "sp": nc.sync}
        self.sems = {}
        self.cnt = {}
        self.known = {k: {} for k in self.eng}
        for k in self.eng:
            self.sems[k] = nc.alloc_semaphore("sem_" + k)
            self.cnt[k] = 0
        self.self_sync = self_sync
        self.nsem = 0
        self.n_ops = 0
        self.n_waits = 0
        self.nbuf = 0

    def sb(self, shape, dt=F32, name=None):
        self.nbuf += 1
        name = (name or "t") + "_%d" % self.nbuf
        return self.nc.alloc_sbuf_tensor(name, list(shape), dt), Buf(name)

    def ps(self, shape, dt=F32, name=None):
        self.nbuf += 1
        name = (name or "p") + "_%d" % self.nbuf
        return self.nc.alloc_psum_tensor(name, list(shape), dt), Buf(name)

    def _deps(self, reads, writes):
        d = {}

        def add(kv):
            if kv is None:
                return
            k, v = kv
            if d.get(k, 0) < v:
                d[k] = v
        for b in reads:
            add(b.w)
        for b in writes:
            add(b.w)
            for k, v in b.r.items():
                add((k, v))
        return d

    def _wait(self, en, deps):
        e = self.eng[en]
        kn = self.known[en]
        for k, v in deps.items():
            if k == en and (en == "pe" or not self.self_sync):
                continue
            if kn.get(k, 0) >= v:
                continue
            e.wait_ge(self.sems[k], v)
            kn[k] = v
            self.n_waits += 1

    def op(self, en, fn, reads=(), writes=()):
        deps = self._deps(reads, writes)
        self._wait(en, deps)
        ins = fn(self.eng[en])
        self.cnt[en] += 1
        v = self.cnt[en]
        ins.then_inc(self.sems[en], 1)
        for b in reads:
            if b.r.get(en, 0) < v:
                b.r[en] = v
        for b in writes:
            b.w = (en, v)
            b.r = {}
        self.n_ops += 1
        return ins

    def dma(self, en, out, in_, reads, writes, **kw):
        assert len(writes) == 1
        dst = writes[0]
        if dst.dsem is None:
            self.nsem += 1
            key = "d%d_%s" % (self.nsem, dst.name)
            dst.dsem = key
            self.sems[key] = self.nc.alloc_semaphore(key[:40])
        deps = self._deps(reads, writes)
        self._wait(en, deps)
        ins = self.eng[en].dma_start(out=out, in_=in_, **kw)
        dst.dcnt += 16
        ins.then_inc(self.sems[dst.dsem], 16)
        for b in reads:
            if b.r.get(dst.dsem, 0) < dst.dcnt:
                b.r[dst.dsem] = dst.dcnt
        dst.w = (dst.dsem, dst.dcnt)
        dst.r = {}
        self.n_ops += 1
        return ins

    def finish(self, bufs, en="sp"):
        d = {}
        for b in bufs:
            if b.w is not None:
                k, v = b.w
                d[k] = max(d.get(k, 0), v)
        self._wait(en, d)


def rview(t, off_bytes, shape, dt):
    esz = 2 if dt == BF16 else 4
    n = 1
    for s in shape[1:]:
        n *= s
    flat = t[:, :] if dt == F32 else t[:, :].bitcast(dt)
    e0 = off_bytes // esz
    v = flat[0:shape[0], e0:e0 + n]
    if len(shape) == 3:
        v = v.rearrange("p (a b) -> p a b", a=shape[1])
    return v


def merge_deps(dst, srcs):
    for s in srcs:
        for kv in ([s.w] if s.w else []) + list(s.r.items()):
            k, v = kv
            if dst.r.get(k, 0) < v:
                dst.r[k] = v


def _din(nc, name, shape, dt=F32):
    return nc.dram_tensor(name, list(shape), dt, kind="ExternalInput").ap()


def _dout(nc, name, shape, dt=F32):
    return nc.dram_tensor(name, list(shape), dt, kind="ExternalOutput").ap()


def emit_mod(kb, cT, mod_w, mod_b, groups, wbufs=None, bbufs=None, pss=None, scb_t=None, outs=None):
    nc = kb.nc
    dr = Buf("mod_dram")
    ct, bct = kb.sb([128, 8], F32, "ct")
    kb.dma("sp", ct[:], cT[:, :], [dr], [bct])
    sc, bsc = kb.sb([128, 8], F32, "sc")
    kb.op("act", lambda e: e.activation(out=sc[:], in_=ct[:], func=AF.Silu), [bct], [bsc])
    scb, bscb = scb_t if scb_t is not None else kb.sb([128, 8, 128], F32, "scb")
    kb.op("dve", lambda e: e.tensor_copy(out=scb[:], in_=sc[:, :].unsqueeze(2).to_broadcast([128, 8, 128])),
          [bsc], [bscb])
    mw = mod_w.rearrange("(kc p) n -> p kc n", p=128)
    if wbufs is None:
        wbufs = [kb.sb([128, 8, 512], F32, "modw") for _ in range(2)]
    if bbufs is None:
        bbufs = [kb.sb([128, 512], F32, "modb") for _ in range(2)]
    if pss is None:
        pss = [kb.ps([128, 512], F32, "modps") for _ in range(2)]
    res = {}
    it = 0
    for g in groups:
        mt, bmt = outs[g] if outs is not None else kb.sb([128, 1024], F32, "modrow")
        res[g] = (mt, bmt)
        for half in range(2):
            c0 = g * 1024 + half * 512
            wt, bw = wbufs[it % 2]
            bt, bb = bbufs[it % 2]
            pt, bp = pss[it % 2]
            kb.dma("sp", wt[:], mw[:, :, c0:c0 + 512], [dr], [bw])
            kb.dma("sp", bt[:], mod_b[0:1, c0:c0 + 512].to_broadcast([128, 512]), [dr], [bb])
            for kc in range(8):
                kb.op("pe", lambda e, kc=kc: e.matmul(pt[:], scb[:, kc, :], wt[:, kc, :],
                                                      start=(kc == 0), stop=(kc == 7)),
                      [bscb, bw], [bp])
            kb.op("dve", lambda e: e.tensor_tensor(out=mt[:, half * 512:(half + 1) * 512], in0=pt[:],
                                                   in1=bt[:], op=ALU.add), [bp, bb], [bmt])
            it += 1
    return res


NF1 = 2064
NB1 = 2112


def build_l1():
    nc = bass.Bass("TRN2", target_bir_lowering=False)
    x = _din(nc, "x", [TPC, D])
    cT = _din(nc, "cT", [128, 8])
    mod_w = _din(nc, "mod_w", [D, 6 * D])
    mod_b = _din(nc, "mod_b", [1, 6 * D])
    normw = _din(nc, "normw", [1, D])
    w_in = _din(nc, "w_in", [D, D_IN])
    qkw = _din(nc, "qkw", [1, 128])
    ident = _din(nc, "ident", [128, 128])
    of = _dout(nc, "of", [TPC, NF1])
    ob = _dout(nc, "ob", [TPC, NB1], BF16)
    kb = KB(nc)
    dr = Buf("dram_in")
    bof, bob = Buf("of"), Buf("ob")

    idb, bidb = kb.sb([128, 128], BF16, "identb")
    kb.dma("pool", idb[:], ident[:, :], [dr], [bidb])
    epst, beps = kb.sb([128, 1], F32, "eps")
    kb.op("dve", lambda e: e.memset(epst[:], EPS), [], [beps])
    nwb, bnwb = kb.sb([128, D], F32, "nwb")
    kb.dma("sp", nwb[:], normw[0:1, :].to_broadcast([128, D]), [dr], [bnwb])
    qkb, bqkb = kb.sb([128, 128], F32, "qkb")
    kb.dma("sp", qkb[:], qkw[0:1, :].to_broadcast([128, 128]), [dr], [bqkb])
    kb.op("dve", lambda e: e.tensor_scalar(out=qkb[:, 0:64], in0=qkb[:, 0:64], scalar1=0.125, scalar2=None,
                                           op0=ALU.mult), [bqkb], [bqkb])

    wb, bwb = kb.sb([128, 8, D_IN], BF16, "w_in")
    for kc in range(8):
        for pc in range(3):
            c0 = pc * 1392
            kb.dma("pool", wb[:, kc, c0:c0 + 1392], w_in[kc * 128:(kc + 1) * 128, c0:c0 + 1392], [dr], [bwb])

    mods = emit_mod(kb, cT, mod_w, mod_b, [0, 1])
    sh1, bsh1 = mods[0]
    sc1, bsc1 = mods[1]
    A1, bA1 = kb.sb([128, D], F32, "A1")
    kb.op("dve", lambda e: e.scalar_tensor_tensor(out=A1[:], in0=sc1[:], scalar=1.0, in1=nwb[:],
                                                  op0=ALU.add, op1=ALU.mult), [bsc1, bnwb], [bA1])

    xts = [kb.sb([128, D], F32, "xt") for _ in range(2)]
    junk, bjunk = kb.sb([128, D], BF16, "junk")
    tmp, btmp = kb.sb([128, D], F32, "tmp")
    hbs = [kb.sb([128, D], BF16, "hb") for _ in range(2)]
    pT, bpT = kb.ps([128, D], BF16, "pT")
    hTs = [kb.sb([128, 8, 128], BF16, "hT") for _ in range(2)]
    pps = [kb.ps([128, 512], F32, "pp") for _ in range(4)]
    projs = [kb.sb([128, D_IN], F32, "proj") for _ in range(2)]
    obts = [kb.sb([128, NB1], BF16, "obt") for _ in range(2)]
    sq, bsq = kb.sb([128, 1024], F32, "sq")
    st, bst = kb.sb([128, 24], F32, "stats")

    QO = 2056
    npp = 0
    for ti in range(NT):
        xt, bxt = xts[ti % 2]
        hb, bhb = hbs[ti % 2]
        hT, bhT = hTs[ti % 2]
        pj, bpj = projs[ti % 2]
        obt, bobt = obts[ti % 2]
        kb.dma("sp", xt[:], x[ti * 128:(ti + 1) * 128, :], [dr], [bxt])
        kb.op("act", lambda e: e.activation(out=junk[:], in_=xt[:], func=AF.Square, accum_out=st[:, 0:1]),
              [bxt], [bjunk, bst])
        kb.op("act", lambda e: e.activation(out=st[:, 1:2], in_=st[:, 0:1], func=AF.Sqrt, scale=1.0 / D,
                                            bias=epst[:]), [bst, beps], [bst])
        kb.op("dve", lambda e: e.reciprocal(out=st[:, 2:3], in_=st[:, 1:2]), [bst], [bst])
        kb.op("dve", lambda e: e.scalar_tensor_tensor(out=tmp[:], in0=xt[:], scalar=st[:, 2:3], in1=A1[:],
                                                      op0=ALU.mult, op1=ALU.mult), [bxt, bst, bA1], [btmp])
        kb.op("dve", lambda e: e.tensor_tensor(out=hb[:], in0=tmp[:], in1=sh1[:], op=ALU.add),
              [btmp, bsh1], [bhb])
        for kc in range(8):
            kb.op("pe", lambda e, kc=kc: e.transpose(out=pT[:, kc * 128:(kc + 1) * 128],
                                                     in_=hb[:, kc * 128:(kc + 1) * 128], identity=idb[:]),
                  [bhb, bidb], [bpT])
        kb.op("act", lambda e: e.copy(out=hT[:].rearrange("p a b -> p (a b)"), in_=pT[:]), [bpT], [bhT])
        for cb in range(9):
            c0 = cb * 512
            n = min(512, D_IN - c0)
            pp, bpp = pps[npp % 4]
            for kc in range(8):
                kb.op("pe", lambda e, kc=kc: e.matmul(pp[:, 0:n], hT[:, kc, :], wb[:, kc, c0:c0 + n],
                                                      start=(kc == 0), stop=(kc == 7)), [bhT, bwb], [bpp])
            if npp % 2 == 0:
                kb.op("act", lambda e: e.copy(out=pj[:, c0:c0 + n], in_=pp[:, 0:n]), [bpp], [bpj])
            else:
                kb.op("dve", lambda e: e.tensor_copy(out=pj[:, c0:c0 + n], in_=pp[:, 0:n]), [bpp], [bpj])
            npp += 1
        qk = pj[:, QO:QO + 1024]
        kb.op("dve", lambda e: e.tensor_tensor(out=sq[:], in0=qk, in1=qk, op=ALU.mult), [bpj], [bsq])
        kb.op("dve", lambda e: e.tensor_reduce(out=st[:, 4:20], in_=sq[:].rearrange("p (g d) -> p g d", g=16),
                                               axis=AX.X, op=ALU.add), [bsq], [bst])
        kb.op("act", lambda e: e.activation(out=st[:, 4:20], in_=st[:, 4:20], func=AF.Sqrt, scale=1.0 / 64,
                                            bias=epst[:]), [bst, beps], [bst])
        kb.op("dve", lambda e: e.reciprocal(out=st[:, 4:20], in_=st[:, 4:20]), [bst], [bst])
        kb.op("dve", lambda e: e.tensor_tensor(out=sq[:].rearrange("p (g d) -> p g d", g=16),
                                               in0=qk.rearrange("p (g d) -> p g d", g=16),
                                               in1=st[:, 4:20].unsqueeze(2).to_broadcast([128, 16, 64]),
                                               op=ALU.mult), [bpj, bst], [bsq])
        for half in range(2):
            kb.op("dve", lambda e, half=half: e.tensor_tensor(
                out=obt[:, half * 512:(half + 1) * 512].rearrange("p (g d) -> p g d", g=8),
                in0=sq[:, half * 512:(half + 1) * 512].rearrange("p (g d) -> p g d", g=8),
                in1=qkb[:, half * 64:(half + 1) * 64].unsqueeze(1).to_broadcast([128, 8, 64]),
                op=ALU.mult), [bsq, bqkb], [bobt])
        kb.op("act", lambda e: e.copy(out=obt[:, 1024:NB1], in_=pj[:, QO + 1024:QO + 1024 + 1088]), [bpj], [bobt])
        kb.op("dve", lambda e: e.tensor_scalar(out=pj[:, 4168:4176], in0=pj[:, 4168:4176],
                                               scalar1=float(8 ** -0.5 * 64 ** -0.5), scalar2=None,
                                               op0=ALU.mult), [bpj], [bpj])
        kb.dma("sp", of[ti * 128:(ti + 1) * 128, 0:2056], pj[:, 0:2056], [bpj], [bof])
        kb.dma("sp", of[ti * 128:(ti + 1) * 128, 2056:2064], pj[:, 4168:4176], [bpj], [bof])
        kb.dma("sp", ob[ti * 128:(ti + 1) * 128, :], obt[:], [bobt], [bob])
    kb.finish([bof, bob])
    return nc


def core_tokens(a, c):
    b, r = c // 4, c % 4
    t = a[b].reshape((64, 128) + a.shape[2:])[r::4]
    return np.ascontiguousarray(t.reshape((TPC,) + a.shape[2:]))


def uncore_tokens(parts, tail):
    out = np.empty((NB, 64, 128) + tuple(tail), parts[0].dtype)
    for c in range(NCORES):
        b, r = c // 4, c % 4
        out[b, r::4] = parts[c].reshape((16, 128) + tuple(tail))
    return out.reshape((NB, S) + tuple(tail))


_NC_CACHE = {}
_TRACE = False
_TIMES = []


def _run(nc, maps):
    if _TRACE:
        res = run_bass_kernel_spmd(nc, maps, core_ids=list(range(NCORES)), trace=True)
        _TIMES.append(res.exec_time_ns)
        print('exec_time_ns', res.exec_time_ns)
        return res
    return run_bass_kernel_spmd(nc, maps, core_ids=list(range(NCORES)))


def _get(name, fn):
    if name not in _NC_CACHE:
        _NC_CACHE[name] = fn()
    return _NC_CACHE[name]


def cT_of(c, b):
    return np.ascontiguousarray(c[b].reshape(8, 128).T)


def run_l1(x, c, mod_w_l, mod_b_l, normw_l, w_in_l, qw_l, kw_l):
    nc = _get("l1", build_l1)
    ident = np.eye(128, dtype=np.float32)
    qkw = np.concatenate([qw_l, kw_l]).reshape(1, 128).astype(np.float32)
    maps = []
    for cid in range(NCORES):
        maps.append({"x": core_tokens(x, cid), "cT": cT_of(c, cid // 4), "mod_w": mod_w_l,
                     "mod_b": mod_b_l.reshape(1, -1), "normw": normw_l.reshape(1, -1), "w_in": w_in_l,
                     "qkw": qkw, "ident": ident})
    res = _run(nc, maps)
    of = uncore_tokens([r["of"] for r in res.results], (NF1,))
    ob = uncore_tokens([r["ob"] for r in res.results], (NB1,))
    return of, ob


NE = 32
ALPHA = 1.702
LIMIT = 7.0


def build_l4(e_lo=0, e_hi=NE, first=True):
    n_exp = e_hi - e_lo
    do_final = True
    nc = bass.Bass("TRN2", target_bir_lowering=False)
    x = _din(nc, "x", [TPC, D])
    ycat = _din(nc, "ycat", [TPC, D])
    cT = _din(nc, "cT", [128, 8])
    mod_w = _din(nc, "mod_w", [D, 6 * D])
    mod_b = _din(nc, "mod_b", [1, 6 * D])
    normw = _din(nc, "normw", [1, D])
    w_out = _din(nc, "w_out", [D, D])
    rw = _din(nc, "rw", [D, NE])
    rb = _din(nc, "rb", [1, NE])
    w1 = _din(nc, "w1", [max(n_exp, 1), 8, 128, 8 * 256])
    b1 = _din(nc, "b1", [128, NE * 16])
    w2 = _din(nc, "w2", [max(n_exp, 1), 8, 128, 8 * 128])
    b2 = _din(nc, "b2", [NE, D])
    ident = _din(nc, "ident", [128, 128])
    xprev = None if first else _din(nc, "xprev", [TPC, D])
    xo = _dout(nc, "xo", [TPC, D])
    if first:
        hT_o = _dout(nc, "hT_o", [128, 8 * TPC], BF16)
        gT_o = _dout(nc, "gT_o", [NE, TPC])
        g2_o = _dout(nc, "g2_o", [128, D])
    else:
        hT_i = _din(nc, "hT_i", [128, 8 * TPC], BF16)
        gT_i = _din(nc, "gT_i", [NE, TPC])
        g2_i = _din(nc, "g2_i", [128, D])
    kb = KB(nc)
    dr = Buf("dram_in")
    bxo = Buf("xo")
    bho = Buf("handover")

    PS = []
    for i in range(4):
        t = nc.alloc_psum_tensor("PS%d" % i, [128, 1024], F32)
        PS.append((t, Buf("ps%da" % i), Buf("ps%db" % i)))

    def bank(i):
        t, ba, bb = PS[i // 2]
        return (t[:, 0:512], ba) if i % 2 == 0 else (t[:, 512:1024], bb)

    R_hT, bhT = kb.sb([128, 8192], F32, "R_hT")
    hT = rview(R_hT, 0, [128, 8, TPC], BF16)
    R_acc, bacc = kb.sb([128, 16384], F32, "R_acc")
    acc = rview(R_acc, 0, [128, 8, TPC], F32)
    wo = rview(R_acc, 0, [128, 8, D], BF16)
    bwo = Buf("wo")
    R_act, bactT = kb.sb([128, 8192], F32, "R_act")
    actT = rview(R_act, 0, [128, 8, TPC], BF16)
    modw_bufs = [(rview(R_act, i * 16384, [128, 8, 512], F32), Buf("modw%d" % i)) for i in range(2)]
    gateT, bgateT = kb.sb([NE, TPC], F32, "gateT")
    R_gsb, bgsb = kb.sb([128, TPC], F32, "gsb")
    gsb = R_gsb
    scb_t = (rview(R_gsb, 0, [128, 8, 128], F32), Buf("scb"))
    modb_bufs = [(rview(R_gsb, 4096 + i * 2048, [128, 512], F32), Buf("modb%d" % i)) for i in range(2)]
    R_W, _ = kb.sb([128, 4608], F32, "R_W")
    W1R = [(rview(R_W, i * 4096, [128, 8, 256], BF16), Buf("w1r%d" % i)) for i in range(3)]
    W2R = [(rview(R_W, 12288 + i * 2048, [128, 8, 128], BF16), Buf("w2r%d" % i)) for i in range(3)]
    g1, bg1 = rview(R_W, 0, [128, D], F32), Buf("g1")
    sh2, bsh2 = rview(R_W, 4096, [128, D], F32), Buf("sh2")
    A2, bA2 = rview(R_W, 8192, [128, D], F32), Buf("A2")
    nwb, bnwb = rview(R_W, 12288, [128, D], F32), Buf("nwb")
    g2, bg2 = kb.sb([128, D], F32, "g2")
    xt, bxt = kb.sb([128, D], F32, "xt")
    yt, byt = kb.sb([128, D], F32, "yt")
    tmpA, btmpA = kb.sb([128, D], F32, "tmpA")
    tmpB, btmpB = kb.sb([128, D], F32, "tmpB")
    R1, bR1 = kb.sb([128, D], F32, "R1")
    ybt = rview(R1, 0, [128, D], BF16)
    yT = rview(R1, 2048, [128, 8, 128], BF16)
    h32T = rview(R1, 0, [128, 8, 128], F32)
    R2, bR2 = kb.sb([128, D], F32, "R2")
    hb = rview(R2, 0, [128, D], BF16)
    junk = rview(R2, 2048, [128, D], BF16)

    idf, bidf = kb.sb([128, 128], F32, "identf")
    kb.dma("sp", idf[:], ident[:, :], [dr], [bidf])
    idb, bidb = kb.sb([128, 128], BF16, "identb")
    kb.op("dve", lambda e: e.tensor_copy(out=idb[:], in_=idf[:]), [bidf], [bidb])
    epst, beps = kb.sb([128, 1], F32, "eps")
    kb.op("dve", lambda e: e.memset(epst[:], EPS), [], [beps])
    kb.dma("sp", nwb, normw[0:1, :].to_broadcast([128, D]), [dr], [bnwb])
    rbb, brbb = kb.sb([128, NE], F32, "rbb")
    kb.dma("sp", rbb[:], rb[0:1, :].to_broadcast([128, NE]), [dr], [brbb])
    rwt, brwt = kb.sb([128, 8, NE], F32, "rwt")
    kb.dma("sp", rwt[:], rw.rearrange("(kc p) e -> p kc e", p=128), [dr], [brwt])
    b1a, bb1a = kb.sb([128, NE * 16], F32, "b1a")
    kb.dma("sp", b1a[:], b1[:, :], [dr], [bb1a])
    b1av = b1a[:].rearrange("p (g two) -> p g two", two=2)
    kb.op("dve", lambda e: e.tensor_scalar(out=b1av[:, :, 0:1], in0=b1av[:, :, 0:1], scalar1=ALPHA, scalar2=None,
                                           op0=ALU.mult), [bb1a], [bb1a])
    kb.op("dve", lambda e: e.tensor_scalar(out=b1av[:, :, 1:2], in0=b1av[:, :, 1:2], scalar1=1.0, scalar2=None,
                                           op0=ALU.add), [bb1a], [bb1a])
    b2t, bb2t = kb.sb([NE, D], F32, "b2t")
    kb.dma("sp", b2t[:], b2[:, :], [dr], [bb2t])
    selbs = [kb.sb([NE, 128], F32, "selb") for _ in range(2)]
    st, bst = kb.sb([128, 64], F32, "st")
    lg, blg = kb.sb([128, NE], F32, "lg")

    if first:
        sc2_out = (A2, bA2)
        mods = emit_mod(kb, cT, mod_w, mod_b, [2, 3, 4, 5], wbufs=modw_bufs, bbufs=modb_bufs,
                        pss=[bank(0), bank(1)], scb_t=scb_t,
                        outs={2: (g1, bg1), 3: (sh2, bsh2), 4: sc2_out, 5: (g2[:], bg2)})
        kb.op("dve", lambda e: e.scalar_tensor_tensor(out=A2, in0=A2, scalar=1.0, in1=nwb,
                                                      op0=ALU.add, op1=ALU.mult), [bA2, bnwb], [bA2])

        for kc in range(8):
            kb.dma("pool", wo[:, kc, :], w_out[kc * 128:(kc + 1) * 128, :], [dr], [bwo])
        for ti in range(NT):
            rows = slice(ti * 128, (ti + 1) * 128)
            kb.dma("sp", xt[:], x[rows, :], [dr], [bxt])
            kb.dma("sp", yt[:], ycat[rows, :], [dr], [byt])
            kb.op("act", lambda e: e.copy(out=ybt, in_=yt[:]), [byt], [bR1])
            pt, bpt = bank(0)
            ptb = pt.bitcast(BF16)
            for kc in range(8):
                kb.op("pe", lambda e, kc=kc: e.transpose(out=ptb[:, kc * 128:(kc + 1) * 128],
                                                         in_=ybt[:, kc * 128:(kc + 1) * 128], identity=idb[:]),
                      [bR1, bidb], [bpt])
            kb.op("act", lambda e: e.copy(out=yT.rearrange("p a b -> p (a b)"), in_=ptb), [bpt], [bR1])
            for half in range(2):
                pp, bpp = bank(2 + half)
                for kc in range(8):
                    kb.op("pe", lambda e, kc=kc: e.matmul(pp, yT[:, kc, :], wo[:, kc, half * 512:(half + 1) * 512],
                                                          start=(kc == 0), stop=(kc == 7)), [bR1, bwo], [bpp])
                cs = slice(half * 512, (half + 1) * 512)
                kb.op("dve", lambda e: e.tensor_tensor(out=tmpA[:, cs], in0=pp, in1=g1[:, cs], op=ALU.mult),
                      [bpp, bg1], [btmpA])
            kb.op("dve", lambda e: e.tensor_tensor(out=xt[:], in0=tmpA[:], in1=xt[:], op=ALU.add), [btmpA, bxt], [bxt])
            kb.dma("sp", xo[rows, :], xt[:], [bxt], [bxo])
            kb.op("act", lambda e: e.activation(out=junk, in_=xt[:], func=AF.Square, accum_out=st[:, 0:1]),
                  [bxt], [bR2, bst])
            kb.op("act", lambda e: e.activation(out=st[:, 1:2], in_=st[:, 0:1], func=AF.Sqrt, scale=1.0 / D,
                                                bias=epst[:]), [bst, beps], [bst])
            kb.op("dve", lambda e: e.reciprocal(out=st[:, 2:3], in_=st[:, 1:2]), [bst], [bst])
            kb.op("dve", lambda e: e.scalar_tensor_tensor(out=tmpA[:], in0=xt[:], scalar=st[:, 2:3], in1=A2,
                                                          op0=ALU.mult, op1=ALU.mult), [bxt, bst, bA2], [btmpA])
            kb.op("dve", lambda e: e.tensor_tensor(out=tmpB[:], in0=tmpA[:], in1=sh2, op=ALU.add),
                  [btmpA, bsh2], [btmpB])
            kb.op("act", lambda e: e.copy(out=hb, in_=tmpB[:]), [btmpB], [bR2])
            pt, bpt = bank(1)
            ptb = pt.bitcast(BF16)
            for kc in range(8):
                kb.op("pe", lambda e, kc=kc: e.transpose(out=ptb[:, kc * 128:(kc + 1) * 128],
                                                         in_=hb[:, kc * 128:(kc + 1) * 128], identity=idb[:]),
                      [bR2, bidb], [bpt])
            kb.op("act", lambda e: e.copy(out=hT[:, :, rows], in_=ptb.rearrange("p (a b) -> p a b", a=8)),
                  [bpt], [bhT])
            for half in range(2):
                pq, bpq = bank(4 + half)
                for k4 in range(4):
                    kc = half * 4 + k4
                    kb.op("pe", lambda e, kc=kc, k4=k4: e.transpose(out=pq[:, k4 * 128:(k4 + 1) * 128],
                                                                    in_=tmpB[:, kc * 128:(kc + 1) * 128],
                                                                    identity=idf[:]), [btmpB, bidf], [bpq])
                kb.op("dve", lambda e: e.tensor_copy(
                    out=h32T[:, half * 4:(half + 1) * 4, :], in_=pq.rearrange("p (a b) -> p a b", a=4)),
                    [bpq], [bR1])
            pr, bpr = bank(6)
            for kc in range(8):
                kb.op("pe", lambda e, kc=kc: e.matmul(pr[:, 0:NE], h32T[:, kc, :], rwt[:, kc, :],
                                                      start=(kc == 0), stop=(kc == 7)), [bR1, brwt], [bpr])
            kb.op("dve", lambda e: e.tensor_tensor(out=lg[:], in0=pr[:, 0:NE], in1=rbb[:], op=ALU.add),
                  [bpr, brbb], [blg])
            kb.op("dve", lambda e: e.max(out=st[:, 8:16], in_=lg[:]), [blg], [bst])
            kb.op("dve", lambda e: e.tensor_scalar(out=st[:, 16:17], in0=st[:, 8:9], scalar1=-1.0, scalar2=None,
                                                   op0=ALU.mult), [bst], [bst])
            kb.op("act", lambda e: e.activation(out=st[:, 20:24], in_=st[:, 8:12], func=AF.Exp, bias=st[:, 16:17],
                                                accum_out=st[:, 17:18]), [bst], [bst])
            kb.op("dve", lambda e: e.reciprocal(out=st[:, 18:19], in_=st[:, 17:18]), [bst], [bst])
            kb.op("act", lambda e: e.activation(out=st[:, 32:64], in_=lg[:], func=AF.Exp, bias=st[:, 16:17]),
                  [blg, bst], [bst])
            kb.op("dve", lambda e: e.tensor_scalar(out=lg[:], in0=lg[:], scalar1=st[:, 11:12], scalar2=None,
                                                   op0=ALU.is_ge), [blg, bst], [blg])
            kb.op("dve", lambda e: e.scalar_tensor_tensor(out=lg[:], in0=st[:, 32:64], scalar=st[:, 18:19], in1=lg[:],
                                                          op0=ALU.mult, op1=ALU.mult), [bst, blg], [blg])
            pg, bpg = bank(7)
            kb.op("pe", lambda e: e.transpose(out=pg[0:NE, 0:128], in_=lg[:], identity=idf[:]), [blg, bidf], [bpg])
            kb.op("act", lambda e: e.copy(out=gateT[:, rows], in_=pg[0:NE, 0:128]), [bpg], [bgateT])

        kb.dma("sp", hT_o[:, :], R_hT[:].bitcast(BF16), [bhT], [bho])
        kb.dma("sp", gT_o[:, :], gateT[:], [bgateT], [bho])
        kb.dma("sp", g2_o[:, :], g2[:], [bg2], [bho])
    else:
        kb.dma("sp", R_hT[:].bitcast(BF16), hT_i[:, :], [dr], [bhT])
        kb.dma("sp", gateT[:], gT_i[:, :], [dr], [bgateT])
        kb.dma("sp", g2[:], g2_i[:, :], [dr], [bg2])
    merge_deps(bacc, [bwo])
    merge_deps(bactT, [b for _, b in modw_bufs])
    merge_deps(bgsb, [scb_t[1]] + [b for _, b in modb_bufs])
    for _, b in W1R + W2R:
        merge_deps(b, [bg1, bsh2, bA2, bnwb])
    tts = [(tmpA[:, 0:512], Buf("tt0")), (tmpA[:, 512:1024], Buf("tt1"))]
    lps = [(tmpB[:, 0:512], Buf("lp0")), (tmpB[:, 512:1024], Buf("lp1"))]
    lgs = [(xt[:, 0:512], Buf("lg0")), (xt[:, 512:1024], Buf("lg1"))]
    for (_, b), src_ in zip(tts + lps + lgs, [btmpA, btmpA, btmpB, btmpB, bxt, bxt]):
        merge_deps(b, [src_])
    C0 = float(LIMIT * ALPHA / (1.0 + np.exp(-LIMIT * ALPHA)))

    w1_issued = [0]
    w2_issued = [0]

    def issue_w1(upto):
        while w1_issued[0] < min(upto, n_exp * 8):
            i = w1_issued[0]
            t, b = W1R[i % 3]
            kb.dma("pool", t.rearrange("p a b -> p (a b)"), w1[i // 8, i % 8, :, :], [dr], [b])
            w1_issued[0] += 1

    def issue_w2(upto):
        while w2_issued[0] < min(upto, n_exp * 8):
            i = w2_issued[0]
            t, b = W2R[i % 3]
            kb.dma("pool", t.rearrange("p a b -> p (a b)"), w2[i // 8, i % 8, :, :], [dr], [b])
            w2_issued[0] += 1

    nu = 0
    ny = 0
    for e in range(e_lo, e_hi):
        selb, bselb = selbs[e % 2]
        kb.op("dve", lambda en: en.tensor_copy(out=selb[:], in_=idf[0:NE, e:e + 1].to_broadcast([NE, 128])),
              [bidf], [bselb])
        for blk in range(4):
            pgb, bpgb = bank(6)
            kb.op("pe", lambda en, blk=blk: en.matmul(pgb, selb[:], gateT[:, blk * 512:(blk + 1) * 512],
                                                      start=True, stop=True), [bselb, bgateT], [bpgb])
            kb.op("act", lambda en, blk=blk: en.activation(out=gsb[:, blk * 512:(blk + 1) * 512], in_=pgb,
                                                           func=AF.Copy, scale=1.0 / ALPHA), [bpgb], [bgsb])
        for ft in range(8):
            gi = (e - e_lo) * 8 + ft
            issue_w1(gi + 2)
            wv, bwt = W1R[gi % 3]
            bcol = (e * 8 + ft) * 2
            for blk in range(4):
                ts_ = slice(blk * 512, (blk + 1) * 512)
                pgl, bpgl = bank(0 + 2 * (nu % 2))
                pli, bpli = bank(1 + 2 * (nu % 2))
                for kc in range(8):
                    kb.op("pe", lambda en, kc=kc: en.matmul(pgl, wv[:, kc, 0:128], hT[:, kc, ts_],
                                                            start=(kc == 0), stop=(kc == 7)), [bwt, bhT], [bpgl])
                for kc in range(8):
                    kb.op("pe", lambda en, kc=kc: en.matmul(pli, wv[:, kc, 128:256], hT[:, kc, ts_],
                                                            start=(kc == 0), stop=(kc == 7)), [bwt, bhT], [bpli])
                tt, btt = tts[nu % 2]
                lp, blp = lps[nu % 2]
                lgg, blgg = lgs[nu % 2]
                kb.op("act", lambda en: en.activation(out=tt, in_=pgl, func=AF.Silu, scale=ALPHA,
                                                      bias=b1a[:, bcol:bcol + 1]), [bpgl, bb1a], [btt])
                kb.op("dve", lambda en: en.tensor_scalar(out=lp, in0=pli, scalar1=b1a[:, bcol + 1:bcol + 2],
                                                         scalar2=LIMIT + 1.0, op0=ALU.add, op1=ALU.min),
                      [bpli, bb1a], [blp])
                kb.op("dve", lambda en: en.scalar_tensor_tensor(out=lgg, in0=lp, scalar=1.0 - LIMIT, in1=gsb[:, ts_],
                                                                op0=ALU.max, op1=ALU.mult), [blp, bgsb], [blgg])
                kb.op("dve", lambda en: en.scalar_tensor_tensor(out=actT[:, ft, ts_], in0=tt, scalar=C0, in1=lgg,
                                                                op0=ALU.min, op1=ALU.mult), [btt, blgg], [bactT])
                nu += 1
        for dt in range(8):
            gi = (e - e_lo) * 8 + dt
            issue_w2(gi + 2)
            wv, bwt = W2R[gi % 3]
            for blk in range(4):
                ts_ = slice(blk * 512, (blk + 1) * 512)
                py, bpy = bank(4 + (ny % 2))
                for fc in range(8):
                    kb.op("pe", lambda en, fc=fc: en.matmul(py, wv[:, fc, :], actT[:, fc, ts_],
                                                            start=(fc == 0), stop=(fc == 7)), [bwt, bactT], [bpy])
                if e == e_lo:
                    kb.op("dve", lambda en: en.tensor_copy(out=acc[:, dt, ts_], in_=py), [bpy], [bacc])
                else:
                    kb.op("dve", lambda en: en.tensor_tensor(out=acc[:, dt, ts_], in0=py, in1=acc[:, dt, ts_],
                                                             op=ALU.add), [bpy, bacc], [bacc])
                ny += 1

    tf, btf = tts[0]
    for ti in range(NT if do_final else 0):
        rows = slice(ti * 128, (ti + 1) * 128)
        if first:
            kb.dma("sp", yt[:], xo[rows, :], [bxo], [byt])
        else:
            kb.dma("sp", yt[:], xprev[rows, :], [dr], [byt])
        for half in range(2):
            po, bpo = bank(half)
            for d4 in range(4):
                dt = half * 4 + d4
                cs = slice(d4 * 128, (d4 + 1) * 128)
                if first:
                    kb.op("pe", lambda en, dt=dt, cs=cs: en.matmul(po[:, cs], gateT[:, rows],
                                                                  b2t[:, dt * 128:(dt + 1) * 128],
                                                                  start=True, stop=False), [bgateT, bb2t], [bpo])
                kb.op("pe", lambda en, dt=dt, cs=cs: en.matmul(po[:, cs], acc[:, dt, rows], idf[:],
                                                              start=(not first), stop=True), [bacc, bidf], [bpo])
            cs2 = slice(half * 512, (half + 1) * 512)
            kb.op("dve", lambda en: en.tensor_tensor(out=tf, in0=po, in1=g2[:, cs2], op=ALU.mult),
                  [bpo, bg2], [btf])
            kb.op("dve", lambda en: en.tensor_tensor(out=yt[:, cs2], in0=tf, in1=yt[:, cs2], op=ALU.add),
                  [btf, byt], [byt])
        kb.dma("sp", xo[rows, :], yt[:], [byt], [bxo])
    kb.finish([bxo, bho])
    return nc


def prep_l4_weights(w1_l, b1_l, w2_l, b2_l):
    w1r = w1_l.reshape(NE, 8, 128, 8, 128, 2)
    w1r = w1r.transpose(0, 3, 2, 1, 5, 4)
    w1r = np.ascontiguousarray(w1r).reshape(NE, 8, 128, 8 * 256)
    b1r = b1_l.reshape(NE, 8, 128, 2).transpose(2, 0, 1, 3)
    b1r = np.ascontiguousarray(b1r).reshape(128, NE * 16)
    w2r = w2_l.reshape(NE, 8, 128, 8, 128).transpose(0, 3, 2, 1, 4)
    w2r = np.ascontiguousarray(w2r).reshape(NE, 8, 128, 8 * 128)
    return w1r, b1r, w2r, np.ascontiguousarray(b2_l)


def run_l4(x, ycat, c, mod_w_l, mod_b_l, normw_l, w_out_l, rw_l, rb_l, w1r, b1r, w2r, b2_l, splits=((0, 16), (16, 32))):
    ident = np.eye(128, dtype=np.float32)
    xprev = None
    for (lo, hi) in splits:
        first = (lo == 0)
        nc = _get("l4_%d_%d" % (lo, hi), lambda: build_l4(lo, hi, first))
        w1s = np.ascontiguousarray(w1r[lo:hi])
        w2s = np.ascontiguousarray(w2r[lo:hi])
        maps = []
        for cid in range(NCORES):
            m = {"x": core_tokens(x, cid), "ycat": core_tokens(ycat, cid), "cT": cT_of(c, cid // 4),
                 "mod_w": mod_w_l, "mod_b": mod_b_l.reshape(1, -1), "normw": normw_l.reshape(1, -1),
                 "w_out": w_out_l, "rw": rw_l, "rb": rb_l.reshape(1, -1), "w1": w1s, "b1": b1r, "w2": w2s,
                 "b2": b2_l, "ident": ident}
            if not first:
                m["xprev"] = xprev[cid]
                m["hT_i"] = hand[cid]["hT_o"]
                m["gT_i"] = hand[cid]["gT_o"]
                m["g2_i"] = hand[cid]["g2_o"]
            maps.append(m)
        res = _run(nc, maps)
        xprev = [r["xo"] for r in res.results]
        if first:
            hand = res.results
    return uncore_tokens(xprev, (D,))


NIT = 16
KSEL = 256
NEGBIG = -30000.0
NREL = 1280


def build_l3(nj=16):
    nc = bass.Bass("TRN2", target_bir_lowering=False)
    qT = _din(nc, "qT", [128, 16, 4 * 128], BF16)
    kT = _din(nc, "kT", [128, 4 * S], BF16)
    v1 = _din(nc, "v1", [64, 128, 8 * 65], BF16)
    qiT = _din(nc, "qiT", [64, 16, 8 * 128], BF16)
    kiT = _din(nc, "kiT", [64, S], BF16)
    wi = _din(nc, "wi", [128, 128])
    pen = _din(nc, "pen", [128, 512])
    oh = _din(nc, "oh", [32, NREL])
    relb = _din(nc, "relb", [32, 8])
    negI4 = _din(nc, "negI4", [128, 512], BF16)
    ident = _din(nc, "ident", [128, 128])
    yb = _dout(nc, "yb", [TPC, 512])
    scr = nc.dram_tensor("scr", [8, NREL], BF16, kind="Internal").ap()
    kb = KB(nc)
    dr = Buf("dram_in")
    byb = Buf("yb")
    bscr = Buf("scr")

    PS = []
    for i in range(4):
        t = nc.alloc_psum_tensor("PS%d" % i, [128, 1024], F32)
        PS.append((t, Buf("ps%da" % i), Buf("ps%db" % i)))

    def bank(i):
        t, ba, bb = PS[i // 2]
        return (t[:, 0:512], ba) if i % 2 == 0 else (t[:, 512:1024], bb)

    kTt, bkT = kb.sb([128, 4 * S], BF16, "kT")
    for i in range(4):
        kb.dma("sp", kTt[:, i * S:(i + 1) * S], kT[:, i * S:(i + 1) * S], [dr], [bkT])
    kTv = kTt[:].rearrange("p (h s) -> p h s", h=4)
    kit, bki = kb.sb([64, S], BF16, "kiT")
    kb.dma("sp", kit[:], kiT[:, :], [dr], [bki])
    wit, bwi = kb.sb([128, 128], F32, "wi")
    kb.dma("sp", wit[:], wi[:, :], [dr], [bwi])
    pent, bpen = kb.sb([128, 512], F32, "pen")
    kb.dma("sp", pent[:], pen[:, :], [dr], [bpen])
    n4, bn4 = kb.sb([128, 512], BF16, "negI4")
    kb.dma("sp", n4[:], negI4[:, :], [dr], [bn4])
    idf, bidf = kb.sb([128, 128], F32, "identf")
    kb.dma("sp", idf[:], ident[:, :], [dr], [bidf])
    idb, bidb = kb.sb([128, 128], BF16, "identb")
    kb.op("dve", lambda e: e.tensor_copy(out=idb[:], in_=idf[:]), [bidf], [bidb])
    zb, bzb = kb.sb([128, 260], BF16, "zeros")
    kb.op("dve", lambda e: e.memset(zb[:], 0.0), [], [bzb])

    oht, boh = kb.sb([32, NREL], F32, "oh")
    kb.dma("sp", oht[:], oh[:, :], [dr], [boh])
    rbt, brb = kb.sb([32, 8], F32, "relb")
    kb.dma("sp", rbt[:], relb[:, :], [dr], [brb])
    bv, bbv = kb.sb([8, NREL], BF16, "bvec")
    for i, (c0, n) in enumerate([(0, 512), (512, 512), (1024, 256)]):
        pb, bpb = bank(7)
        kb.op("pe", lambda e: e.matmul(pb[0:8, 0:n], rbt[:], oht[:, c0:c0 + n], start=True, stop=True),
              [brb, boh], [bpb])
        kb.op("dve", lambda e: e.tensor_copy(out=bv[:, c0:c0 + n], in_=pb[0:8, 0:n]), [bpb], [bbv])
    kb.dma("sp", scr[:, :], bv[:], [bbv], [bscr])
    TU, bTU = kb.sb([128, 9, 8 * 128], BF16, "TU")
    for u in range(9):
        src = bass.AP(tensor=scr.tensor, offset=128 * u, ap=[[1, 128], [NREL, 8], [1, 128]])
        kb.dma("sp", TU[:, u, :].rearrange("p (h t) -> p h t", h=8), src, [bscr], [bTU])

    score, bscore = kb.sb([128, S], F32, "score")
    nots = [kb.sb([128, S], BF16, "notsel") for _ in range(2)]
    qts = [kb.sb([128, 512], BF16, "qTj") for _ in range(2)]
    qis = [kb.sb([64, 1024], BF16, "qiTj") for _ in range(2)]
    dgs = [kb.sb([128, 1024], BF16, "Dg") for _ in range(2)]
    rhs_ = [kb.sb([128, 512], BF16, "rh") for _ in range(4)]
    pts = [kb.sb([128, 512], BF16, "pt") for _ in range(2)]
    vts = [kb.sb([128, 520], BF16, "v1t") for _ in range(3)]
    outs = [kb.sb([128, 512], F32, "yo") for _ in range(2)]
    st, bst = kb.sb([128, 32], F32, "st")

    nr = [0]
    nd = [0]
    nv = [0]
    nsc = [0]
    LAG = 3
    rhs_.append(kb.sb([128, 512], BF16, "rh"))
    vts.append(kb.sb([128, 520], BF16, "v1t"))
    pts2 = [[pts[0], kb.sb([128, 512], BF16, "pt")], [pts[1], kb.sb([128, 512], BF16, "pt")]]

    def idx(j):
        qi_t, bqi = qis[j % 2]
        kb.dma("sp", qi_t[:], qiT[:, j, :], [dr], [bqi])
        dg, bdg = dgs[j % 2]
        for h in range(8):
            kb.op("dve", lambda e, h=h: e.tensor_scalar(out=dg[:, h * 128:(h + 1) * 128], in0=idf[:],
                                                        scalar1=wit[:, j * 8 + h:j * 8 + h + 1], scalar2=None,
                                                        op0=ALU.mult), [bidf, bwi], [bdg])
        steps = [(sb, h) for sb in range(j + 1) for h in range(8)]
        pend = []
        sbank = {}
        for s in range(len(steps) + LAG):
            if s < len(steps):
                sb, h = steps[s]
                pd, bpd = bank(nd[0] % 4)
                nd[0] += 1
                kb.op("pe", lambda e, h=h, sb=sb: e.matmul(pd, qi_t[:, h * 128:(h + 1) * 128],
                                                           kit[:, sb * 512:(sb + 1) * 512], start=True, stop=True),
                      [bqi, bki], [bpd])
                rh, brh = rhs_[nr[0] % 5]
                nr[0] += 1
                kb.op("act", lambda e: e.activation(out=rh[:], in_=pd, func=AF.Relu), [bpd], [brh])
                pend.append((sb, h, rh, brh))
            if s - LAG >= 0:
                sb, h, rh, brh = pend[s - LAG]
                if h == 0:
                    sbank[sb] = bank(6 + nsc[0] % 2)
                    nsc[0] += 1
                ps, bps = sbank[sb]
                kb.op("pe", lambda e, h=h: e.matmul(ps, dg[:, h * 128:(h + 1) * 128], rh[:],
                                                    start=(h == 0), stop=(h == 7)), [bdg, brh], [bps])
                if h == 7:
                    kb.op("dve", lambda e, sb=sb: e.tensor_copy(out=score[:, sb * 512:(sb + 1) * 512], in_=ps),
                          [bps], [bscore])

    def bis(j):
        n = 512 * (j + 1)
        ns, bns = nots[j % 2]
        sc = score[:, 0:n]
        kb.op("dve", lambda e: e.tensor_reduce(out=st[:, 0:1], in_=sc, axis=AX.X, op=ALU.min), [bscore], [bst])
        kb.op("dve", lambda e: e.tensor_tensor(out=score[:, n - 512:n], in0=score[:, n - 512:n], in1=pent[:],
                                               op=ALU.add), [bscore, bpen], [bscore])
        kb.op("dve", lambda e: e.tensor_reduce(out=st[:, 1:2], in_=sc, axis=AX.X, op=ALU.max), [bscore], [bst])
        kb.op("dve", lambda e: e.tensor_tensor(out=st[:, 2:3], in0=st[:, 1:2], in1=st[:, 0:1], op=ALU.subtract),
              [bst], [bst])
        kb.op("dve", lambda e: e.scalar_tensor_tensor(out=st[:, 3:4], in0=st[:, 2:3], scalar=-0.01, in1=st[:, 0:1],
                                                      op0=ALU.mult, op1=ALU.add), [bst], [bst])
        kb.op("dve", lambda e: e.tensor_scalar(out=st[:, 3:4], in0=st[:, 3:4], scalar1=-1e-6, scalar2=None,
                                               op0=ALU.add), [bst], [bst])
        kb.op("dve", lambda e: e.tensor_tensor(out=st[:, 4:5], in0=st[:, 1:2], in1=st[:, 3:4], op=ALU.subtract),
              [bst], [bst])
        kb.op("dve", lambda e: e.scalar_tensor_tensor(out=st[:, 5:6], in0=st[:, 4:5], scalar=0.5, in1=st[:, 3:4],
                                                      op0=ALU.mult, op1=ALU.add), [bst], [bst])
        kb.op("dve", lambda e: e.tensor_scalar(out=st[:, 6:7], in0=st[:, 4:5], scalar1=0.25, scalar2=None,
                                               op0=ALU.mult), [bst], [bst])
        for it in range(NIT):
            kb.op("dve", lambda e: e.tensor_scalar(out=ns[:, 0:n], in0=sc, scalar1=st[:, 5:6], scalar2=None,
                                                   op0=ALU.is_ge, op1=ALU.add, accum_out=st[:, 7:8]),
                  [bscore, bst], [bns, bst])
            kb.op("dve", lambda e: e.tensor_scalar(out=st[:, 8:9], in0=st[:, 7:8], scalar1=KSEL - 0.5, scalar2=2.0,
                                                   op0=ALU.is_ge, op1=ALU.mult), [bst], [bst])
            kb.op("dve", lambda e: e.scalar_tensor_tensor(out=st[:, 9:10], in0=st[:, 8:9], scalar=-1.0,
                                                          in1=st[:, 6:7], op0=ALU.add, op1=ALU.mult), [bst], [bst])
            kb.op("dve", lambda e: e.tensor_tensor(out=st[:, 5:6], in0=st[:, 5:6], in1=st[:, 9:10], op=ALU.add),
                  [bst], [bst])
            kb.op("dve", lambda e: e.tensor_scalar(out=st[:, 6:7], in0=st[:, 6:7], scalar1=0.5, scalar2=None,
                                                   op0=ALU.mult), [bst], [bst])
        kb.op("dve", lambda e: e.scalar_tensor_tensor(out=st[:, 10:11], in0=st[:, 6:7], scalar=-4.0, in1=st[:, 5:6],
                                                      op0=ALU.mult, op1=ALU.add), [bst], [bst])
        kb.op("dve", lambda e: e.tensor_scalar(out=ns[:, 0:n], in0=sc, scalar1=st[:, 10:11], scalar2=None,
                                               op0=ALU.is_lt), [bscore, bst], [bns])

    def att_main(j):
        ns, bns = nots[j % 2]
        qt, bqt = qts[j % 2]
        kb.dma("sp", qt[:], qT[:, j, :], [dr], [bqt])
        oacc = [bank(4), bank(5)]
        for g in range(2):
            oa, boa = oacc[g]
            kb.op("pe", lambda e: e.matmul(oa[:, 0:260], idb[:], zb[:], start=True, stop=False),
                  [bidb, bzb], [boa])
        ntile = 4 * j + 4
        vtl = {}
        for stl in range(ntile + 1):
            if stl < ntile:
                vt, bvt = vts[nv[0] % 4]
                nv[0] += 1
                vtl[stl] = (vt, bvt)
                kb.dma("sp", vt[:], v1[stl, :, :], [dr], [bvt])
                u = stl - 4 * j + 5
                for g in range(2):
                    lgp, blgp = bank(2 * g + stl % 2)
                    kb.op("pe", lambda e: e.matmul(lgp, ns[:, stl * 128:(stl + 1) * 128], n4[:], start=True,
                                                   stop=False), [bns, bn4], [blgp])
                    if u >= 0:
                        kb.op("pe", lambda e, u=u: e.matmul(lgp, idb[:], TU[:, u, g * 512:(g + 1) * 512],
                                                            start=False, stop=False), [bidb, bTU], [blgp])
                    for hq in range(4):
                        kb.op("pe", lambda e, hq=hq: e.matmul(lgp[:, hq * 128:(hq + 1) * 128],
                                                              kTv[g * 64:(g + 1) * 64, hq, stl * 128:(stl + 1) * 128],
                                                              qt[g * 64:(g + 1) * 64, hq * 128:(hq + 1) * 128],
                                                              start=False, stop=(hq == 3)), [bkT, bqt], [blgp])
                    pt, bpt = pts2[g][stl % 2]
                    kb.op("act", lambda e: e.activation(out=pt[:], in_=lgp, func=AF.Exp), [blgp], [bpt])
            if stl >= 1:
                sp_ = stl - 1
                vt, bvt = vtl[sp_]
                for g in range(2):
                    pt, bpt = pts2[g][sp_ % 2]
                    oa, boa = oacc[g]
                    for hq in range(4):
                        h = g * 4 + hq
                        kb.op("pe", lambda e, hq=hq, h=h: e.matmul(oa[:, hq * 65:(hq + 1) * 65],
                                                                   pt[:, hq * 128:(hq + 1) * 128],
                                                                   vt[:, h * 65:(h + 1) * 65],
                                                                   start=False, stop=(sp_ == ntile - 1)),
                              [bpt, bvt], [boa])

    def att_fin(j):
        oacc = [bank(4), bank(5)]
        yo, byo = outs[j % 2]
        for g in range(2):
            oa, boa = oacc[g]
            oav = oa[:, 0:260].rearrange("p (h c) -> p h c", h=4)
            kb.op("dve", lambda e: e.reciprocal(out=st[:, 16 + g * 4:20 + g * 4],
                                                in_=oav[:, :, 64:65].rearrange("p h c -> p (h c)")), [boa], [bst])
            kb.op("dve", lambda e: e.tensor_tensor(
                out=yo[:, g * 256:(g + 1) * 256].rearrange("p (h d) -> p h d", h=4), in0=oav[:, :, 0:64],
                in1=st[:, 16 + g * 4:20 + g * 4].unsqueeze(2).to_broadcast([128, 4, 64]), op=ALU.mult),
                [boa, bst], [byo])
        kb.dma("sp", yb[j * 128:(j + 1) * 128, :], yo[:], [byo], [byb])

    idx(0)
    bis(0)
    for j in range(nj):
        if j + 1 < nj:
            idx(j + 1)
        att_main(j)
        if j + 1 < nj:
            bis(j + 1)
        att_fin(j)
    kb.finish([byb])
    return nc


def t5_bucket_np(rel):
    nb = 16
    max_exact = 8
    side = np.where(rel > 0, nb, 0)
    n = np.abs(rel)
    nf = np.maximum(n, 1).astype(np.float32)
    large = max_exact + (np.log(nf / max_exact) / np.float32(np.log(1024 / max_exact)) * (nb - max_exact)).astype(np.int32)
    large = np.minimum(large, nb - 1)
    return side + np.where(n < max_exact, n, large)


def prep_l3(ob, of, cid):
    b, r = cid // 4, cid % 4
    bf = ob.dtype
    qsel = ob[b].reshape(64, 128, NB1)[r::4][:, ::-1]
    q = qsel[..., 0:512].reshape(16, 128, 2, 4, 64)
    qT = np.ascontiguousarray(q.transpose(2, 4, 0, 3, 1)).reshape(128, 16, 512)
    qi = qsel[..., 1536:2048].reshape(16, 128, 8, 64)
    qiT = np.ascontiguousarray(qi.transpose(3, 0, 2, 1)).reshape(64, 16, 1024)
    wsel = of[b].reshape(64, 128, NF1)[r::4][:, ::-1, 2056:2064]
    wi = np.ascontiguousarray(wsel.transpose(1, 0, 2)).reshape(128, 128).astype(np.float32)
    k = ob[b, :, 512:1024].reshape(S, 2, 4, 64)
    kT = np.ascontiguousarray(k.transpose(1, 3, 2, 0)).reshape(128, 4 * S)
    kiT = np.ascontiguousarray(ob[b, :, 2048:2112].T)
    v = ob[b, :, 1024:1536].reshape(64, 128, 8, 64)
    v1 = np.ones((64, 128, 8, 65), bf)
    v1[..., 0:64] = v
    v1 = v1.reshape(64, 128, 520)
    tq = np.arange(128)[:, None]
    sk = np.arange(512)[None, :]
    pen = np.where((sk // 64) <= 2 * r + (tq < 64), 0.0, -1e30).astype(np.float32)
    m = np.arange(NREL)
    rel = m - 767 - 128 * r
    bk = t5_bucket_np(rel)
    oh = np.zeros((32, NREL), np.float32)
    oh[bk, m] += 1.0
    oh[15, :] -= 1.0
    negI4 = np.tile(np.eye(128, dtype=np.float32) * NEGBIG, (1, 4)).astype(bf)
    return {"qT": qT, "kT": kT, "v1": v1, "qiT": qiT, "kiT": kiT, "wi": wi, "pen": pen, "oh": oh,
            "negI4": negI4, "ident": np.eye(128, dtype=np.float32)}


def run_l3(ob, of, rel_bias, nj=16):
    nc = _get("l3_%d" % nj, lambda: build_l3(nj))
    maps = []
    for cid in range(NCORES):
        m = prep_l3(ob, of, cid)
        m["relb"] = np.ascontiguousarray(rel_bias.astype(np.float32))
        maps.append(m)
    res = _run(nc, maps)
    parts = [r["yb"].reshape(16, 128, 512)[:, ::-1].reshape(TPC, 512) for r in res.results]
    return uncore_tokens(parts, (512,))


NCH = 64
PRE_STOP = 0
L2VAR = 0
POOL_ENG = "dve"


def build_l2(nch=NCH, stop=None):
    nc = bass.Bass("TRN2", target_bir_lowering=False)
    xin = _din(nc, "xin", [128, 3, S + 3])
    cw = _din(nc, "cw", [128, 12])
    zin = _din(nc, "zin", [128, NCH, 128])
    bcol = _din(nc, "bcol", [128, NCH])
    acol = _din(nc, "acol", [128, NCH])
    sc3 = _din(nc, "sc3", [1, 2])
    gnw = _din(nc, "gnw", [1, 128])
    cst = _din(nc, "cst", [128, 7, 128])
    ya = _dout(nc, "ya", [S, 128])
    kb = KB(nc)
    dr = Buf("dram_in")
    bya = Buf("ya")

    PSW = []
    for i in range(4):
        t = nc.alloc_psum_tensor("PS%d" % i, [128, 1024], F32)
        PSW.append(t)
    slots = []
    for bnk in range(6):
        t = PSW[bnk // 2]
        c0 = (bnk % 2) * 512
        slots.append((t[:, c0:c0 + 128], Buf("slot%d" % bnk)))
    wide = [(PSW[3][:, 0:512], Buf("wide0")), (PSW[3][:, 512:1024], Buf("wide1"))]
    nslot = [0]

    def slot():
        s = slots[nslot[0] % len(slots)]
        nslot[0] += 1
        return s

    ct, bct = kb.sb([128, 7, 128], F32, "cst")
    kb.dma("sp", ct[:], cst[:, :, :], [dr], [bct])
    ident, LT, ones, negones, penL, SM, sel127 = [ct[:, i, :] for i in range(7)]
    cwt, bcw = kb.sb([128, 12], F32, "cw")
    kb.dma("sp", cwt[:], cw[:, :], [dr], [bcw])
    gnb, bgnb = kb.sb([128, 128], F32, "gnw")
    kb.dma("sp", gnb[:], gnw[0:1, :].to_broadcast([128, 128]), [dr], [bgnb])
    s3, bs3 = kb.sb([128, 2], F32, "sc3")
    kb.dma("sp", s3[:], sc3[0:1, :].to_broadcast([128, 2]), [dr], [bs3])
    epst, beps = kb.sb([128, 3], F32, "eps")
    kb.op("dve", lambda e: e.memset(epst[:, 0:1], EPS), [], [beps])
    kb.op("dve", lambda e: e.memset(epst[:, 1:2], 128.0 * EPS), [], [beps])
    kb.op("dve", lambda e: e.memset(epst[:, 2:3], 1.0), [], [beps])

    cols, bcols = kb.sb([128, 10, NCH], F32, "cols")
    BETA, G, GC, EGC, BG, KD, EGL, NB_, TMP, TMP2 = range(10)
    kb.dma("sp", cols[:, BETA, :], bcol[:, :], [dr], [bcols])
    kb.dma("sp", cols[:, TMP, :], acol[:, :], [dr], [bcols])
    kb.op("act", lambda e: e.activation(out=cols[:, BETA, :], in_=cols[:, BETA, :], func=AF.Sigmoid), [bcols], [bcols])
    kb.op("act", lambda e: e.activation(out=cols[:, TMP, :], in_=cols[:, TMP, :], func=AF.Exp, bias=s3[:, 1:2]),
          [bcols, bs3], [bcols])
    kb.op("act", lambda e: e.activation(out=cols[:, TMP, :], in_=cols[:, TMP, :], func=AF.Ln, bias=epst[:, 2:3]),
          [bcols, beps], [bcols])
    kb.op("act", lambda e: e.activation(out=s3[:, 0:1], in_=s3[:, 0:1], func=AF.Exp), [bs3], [bs3])
    kb.op("dve", lambda e: e.tensor_scalar(out=cols[:, G, :], in0=cols[:, TMP, :], scalar1=s3[:, 0:1], scalar2=-1.0,
                                           op0=ALU.mult, op1=ALU.mult), [bcols, bs3], [bcols])
    pg, bpg = slot()
    kb.op("pe", lambda e: e.matmul(pg[:, 0:NCH], LT, cols[:, G, :], start=True, stop=True), [bct, bcols], [bpg])
    kb.op("dve", lambda e: e.tensor_copy(out=cols[:, GC, :], in_=pg[:, 0:NCH]), [bpg], [bcols])
    pg2, bpg2 = slot()
    kb.op("pe", lambda e: e.matmul(pg2[:, 0:NCH], sel127, cols[:, GC, :], start=True, stop=True), [bct, bcols], [bpg2])
    kb.op("dve", lambda e: e.tensor_copy(out=cols[:, TMP, :], in_=pg2[:, 0:NCH]), [bpg2], [bcols])
    kb.op("act", lambda e: e.activation(out=cols[:, EGL, :], in_=cols[:, TMP, :], func=AF.Exp), [bcols], [bcols])
    kb.op("dve", lambda e: e.tensor_tensor(out=cols[:, TMP2, :], in0=cols[:, TMP, :], in1=cols[:, GC, :], op=ALU.subtract),
          [bcols], [bcols])
    kb.op("act", lambda e: e.activation(out=cols[:, KD, :], in_=cols[:, TMP2, :], func=AF.Exp), [bcols], [bcols])
    kb.op("act", lambda e: e.activation(out=cols[:, EGC, :], in_=cols[:, GC, :], func=AF.Exp), [bcols], [bcols])
    kb.op("dve", lambda e: e.tensor_tensor(out=cols[:, BG, :], in0=cols[:, EGC, :], in1=cols[:, BETA, :], op=ALU.mult),
          [bcols], [bcols])
    kb.op("dve", lambda e: e.tensor_scalar(out=cols[:, NB_, :], in0=cols[:, BETA, :], scalar1=-1.0, scalar2=None,
                                           op0=ALU.mult), [bcols], [bcols])

    if stop == "cols":
        kb.dma("sp", ya[0:128, 0:NCH], cols[:, GC, :], [bcols], [bya])
        kb.finish([bya])
        return nc
    QT, bQT = kb.sb([128, S], F32, "QT")
    KT, bKT = kb.sb([128, S], F32, "KT")
    Vtok, bVtok = kb.sb([128, NCH, 128], F32, "Vtok")
    Ktok, bKtok = kb.sb([128, NCH, 128], F32, "Ktok")
    xbs = [kb.sb([128, 3, 515], F32, "xb") for _ in range(2)]
    u, bu = kb.sb([128, 3, 512], F32, "u")
    sqt, bsq = kb.sb([128, 512], F32, "sq")
    rs, brs = kb.sb([128, 512], F32, "rs")
    nblk = (nch * 128 + 511) // 512
    for blk in range(nblk):
        xb, bxb = xbs[blk % 2]
        kb.dma("sp", xb[:], xin[:, :, blk * 512:blk * 512 + 515], [dr], [bxb])
        for a in range(3):
            kb.op("dve", lambda e, a=a: e.tensor_scalar(out=u[:, a, :], in0=xb[:, a, 0:512],
                                                        scalar1=cwt[:, a * 4:a * 4 + 1], scalar2=None, op0=ALU.mult),
                  [bxb, bcw], [bu])
            for tap in range(1, 4):
                kb.op("dve", lambda e, a=a, tap=tap: e.scalar_tensor_tensor(
                    out=u[:, a, :], in0=xb[:, a, tap:tap + 512], scalar=cwt[:, a * 4 + tap:a * 4 + tap + 1],
                    in1=u[:, a, :], op0=ALU.mult, op1=ALU.add), [bxb, bcw, bu], [bu])
        kb.op("act", lambda e: e.activation(out=u[:].rearrange("p a n -> p (a n)"),
                                            in_=u[:].rearrange("p a n -> p (a n)"), func=AF.Silu), [bu], [bu])
        cs = slice(blk * 512, (blk + 1) * 512)
        for a, (dst, bdst, scl, epi) in enumerate([(QT, bQT, 128.0, 1), (KT, bKT, 1.0, 0)]):
            kb.op("dve", lambda e, a=a: e.tensor_tensor(out=sqt[:], in0=u[:, a, :], in1=u[:, a, :], op=ALU.mult),
                  [bu], [bsq])
            pw, bpw = wide[a]
            kb.op("pe", lambda e: e.matmul(pw, ones, sqt[:], start=True, stop=True), [bct, bsq], [bpw])
            kb.op("act", lambda e, scl=scl, epi=epi: e.activation(out=rs[:], in_=pw, func=AF.Sqrt, scale=scl,
                                                                  bias=epst[:, epi:epi + 1]), [bpw, beps], [brs])
            kb.op("dve", lambda e: e.reciprocal(out=rs[:], in_=rs[:]), [brs], [brs])
            kb.op("dve", lambda e, a=a, dst=dst: e.tensor_tensor(out=dst[:, cs], in0=u[:, a, :], in1=rs[:], op=ALU.mult),
                  [bu, brs], [bdst])
        for q4 in range(4):
            ch = blk * 4 + q4
            if ch >= nch:
                break
            pk, bpk = slot()
            kb.op("pe", lambda e, ch=ch: e.transpose(out=pk, in_=KT[:, ch * 128:(ch + 1) * 128], identity=ident),
                  [bKT, bct], [bpk])
            kb.op("act", lambda e, ch=ch: e.copy(out=Ktok[:, ch, :], in_=pk), [bpk], [bKtok])
            pv, bpv = slot()
            kb.op("pe", lambda e, q4=q4: e.transpose(out=pv, in_=u[:, 2, q4 * 128:(q4 + 1) * 128], identity=ident),
                  [bu, bct], [bpv])
            kb.op("act", lambda e, ch=ch: e.copy(out=Vtok[:, ch, :], in_=pv), [bpv], [bVtok])

    if stop == "prep":
        kb.dma("sp", ya[0:128, :], Ktok[:, 0, :], [bKtok], [bya])
        kb.dma("sp", ya[128:256, :], Vtok[:, 0, :], [bVtok], [bya])
        kb.dma("sp", ya[256:384, :], QT[:, 0:128], [bQT], [bya])
        kb.finish([bya])
        return nc
    RING = 4
    ring = [dict(wdT=kb.sb([128, 128], F32, "wdT"), uval=kb.sb([128, 128], F32, "uval"),
                 attnT=kb.sb([128, 128], F32, "attnT"), kdec=kb.sb([128, 128], F32, "kdec")) for _ in range(RING)]
    tmps = {}

    def tmp(name, k=2):
        if name not in tmps:
            tmps[name] = [kb.sb([128, 128], F32, name) for _ in range(k)]
            tmps[name + "_i"] = 0
        i = tmps[name + "_i"]
        tmps[name + "_i"] = i + 1
        return tmps[name][i % k]

    def pre(n):
        par = "_%d" % (n % 2)
        R = ring[n % RING]
        kt = KT[:, n * 128:(n + 1) * 128]
        qt = QT[:, n * 128:(n + 1) * 128]
        dg, bdg = tmp("diag" + par)
        kb.op("dve", lambda e: e.tensor_scalar(out=dg[:], in0=ident, scalar1=cols[:, GC, n:n + 1], scalar2=None,
                                               op0=ALU.mult), [bct, bcols], [bdg])
        pD, bpD = slot()
        kb.op("pe", lambda e: e.matmul(pD, dg[:], ones, start=True, stop=False), [bdg, bct], [bpD])
        kb.op("pe", lambda e: e.matmul(pD, negones, dg[:], start=False, stop=True), [bdg, bct], [bpD])
        Dl, bDl = tmp("Dl" + par)
        E, bE = tmp("E" + par)
        kb.op("dve", lambda e: e.tensor_tensor(out=Dl[:], in0=pD, in1=penL, op=ALU.min), [bpD, bct], [bDl])
        kb.op("act", lambda e: e.activation(out=E[:], in_=Dl[:], func=AF.Exp), [bDl], [bE])
        yield
        Es, bEs = tmp("Es" + par)
        kb.op(POOL_ENG, lambda e: e.tensor_tensor(out=Es[:], in0=E[:], in1=SM, op=ALU.mult), [bE, bct], [bEs])
        pA, bpA = slot()
        kb.op("pe", lambda e: e.matmul(pA, kt, kt, start=True, stop=True), [bKT], [bpA])
        Nm, bN = tmp("N" + par, 3)
        kb.op("dve", lambda e: e.scalar_tensor_tensor(out=Nm[:], in0=pA, scalar=cols[:, NB_, n:n + 1], in1=Es[:],
                                                      op0=ALU.mult, op1=ALU.mult), [bpA, bcols, bEs], [bN])
        yield
        pM, bpM = slot()
        kb.op("pe", lambda e: e.transpose(out=pM, in_=Nm[:], identity=ident), [bN, bct], [bpM])
        Mm, bM = tmp("M" + par, 3)
        kb.op("act", lambda e: e.copy(out=Mm[:], in_=pM), [bpM], [bM])
        P, bP = tmp("P" + par, 3)
        kb.op("dve", lambda e: e.tensor_tensor(out=P[:], in0=Mm[:], in1=ident, op=ALU.add), [bM, bct], [bP])
        yield
        pQK, bpQK = slot()
        kb.op("pe", lambda e: e.matmul(pQK, qt, kt, start=True, stop=True), [bQT, bKT], [bpQK])
        at, bat = tmp("attn" + par)
        kb.op("dve", lambda e: e.tensor_tensor(out=at[:], in0=pQK, in1=E[:], op=ALU.mult), [bpQK, bE], [bat])
        pAT, bpAT = slot()
        kb.op("pe", lambda e: e.transpose(out=pAT, in_=at[:], identity=ident), [bat, bct], [bpAT])
        aT, baT = R["attnT"]
        kb.op("act", lambda e: e.copy(out=aT[:], in_=pAT), [bpAT], [baT])
        yield
        for lev in range(1, 7):
            pN2, bpN2 = slot()
            kb.op("pe", lambda e: e.matmul(pN2, Mm[:], Nm[:], start=True, stop=True), [bM, bN], [bpN2])
            N2, bN2 = tmp("N" + par, 3)
            kb.op("act", lambda e: e.copy(out=N2[:], in_=pN2), [bpN2], [bN2])
            if lev < 6:
                pM2, bpM2 = slot()
                kb.op("pe", lambda e: e.matmul(pM2, Nm[:], Mm[:], start=True, stop=True), [bM, bN], [bpM2])
                M2, bM2 = tmp("M" + par, 3)
                kb.op("act", lambda e: e.copy(out=M2[:], in_=pM2), [bpM2], [bM2])
            yield
            pP, bpP = slot()
            kb.op("pe", lambda e: e.matmul(pP, N2[:], P[:], start=True, stop=True), [bN2, bP], [bpP])
            P2, bP2 = tmp("P" + par, 3)
            kb.op("dve", lambda e: e.tensor_tensor(out=P2[:], in0=pP, in1=P[:], op=ALU.add), [bpP, bP], [bP2])
            P, bP = P2, bP2
            yield
            Nm, bN = N2, bN2
            if lev < 6:
                Mm, bM = M2, bM2
        yield
        kbg, bkbg = tmp("kbg" + par)
        kb.op(POOL_ENG, lambda e: e.tensor_scalar(out=kbg[:], in0=Ktok[:, n, :], scalar1=cols[:, BG, n:n + 1],
                                                scalar2=None, op0=ALU.mult), [bKtok, bcols], [bkbg])
        vb, bvb = tmp("vb" + par)
        kb.op(POOL_ENG, lambda e: e.tensor_scalar(out=vb[:], in0=Vtok[:, n, :], scalar1=cols[:, BETA, n:n + 1],
                                                scalar2=None, op0=ALU.mult), [bVtok, bcols], [bvb])
        kd, bkd = R["kdec"]
        kb.op(POOL_ENG, lambda e: e.tensor_scalar(out=kd[:], in0=Ktok[:, n, :], scalar1=cols[:, KD, n:n + 1],
                                                scalar2=None, op0=ALU.mult), [bKtok, bcols], [bkd])
        pW, bpW = slot()
        kb.op("pe", lambda e: e.matmul(pW, kbg[:], P[:], start=True, stop=True), [bkbg, bP], [bpW])
        wd, bwd = R["wdT"]
        kb.op("act", lambda e: e.copy(out=wd[:], in_=pW), [bpW], [bwd])
        pU, bpU = slot()
        kb.op("pe", lambda e: e.matmul(pU, P[:], vb[:], start=True, stop=True), [bP, bvb], [bpU])
        uv, buv = R["uval"]
        kb.op("act", lambda e: e.copy(out=uv[:], in_=pU), [bpU], [buv])

    states = [kb.sb([128, 128], F32, "state") for _ in range(2)]
    kb.op("dve", lambda e: e.memset(states[0][0][:], 0.0), [], [states[0][1]])
    zts = [kb.sb([128, 128], F32, "zt") for _ in range(2)]
    st, bst = kb.sb([128, 8], F32, "st")
    junk, bjunk = kb.sb([128, 128], F32, "junk")

    def scan(n):
        R = ring[n % RING]
        wd, bwd = R["wdT"]
        uv, buv = R["uval"]
        aT, baT = R["attnT"]
        kd, bkd = R["kdec"]
        S0, bS0 = states[n % 2]
        S1, bS1 = states[(n + 1) % 2]
        zt, bzt = zts[n % 2]
        kb.dma("sp", zt[:], zin[:, n, :], [dr], [bzt])
        ppv, bppv = slot()
        kb.op("pe", lambda e: e.matmul(ppv, wd[:], S0[:], start=True, stop=True), [bwd, bS0], [bppv])
        po1, bpo1 = wide[0][0][:, 0:128], wide[0][1]
        kb.op("pe", lambda e: e.matmul(po1, QT[:, n * 128:(n + 1) * 128], S0[:], start=True, stop=True),
              [bQT, bS0], [bpo1])
        vn, bvn = tmp("vnew")
        kb.op("dve", lambda e: e.tensor_tensor(out=vn[:], in0=uv[:], in1=ppv, op=ALU.subtract), [buv, bppv], [bvn])
        yield
        psu, bpsu = slot()
        kb.op("pe", lambda e: e.matmul(psu, kd[:], vn[:], start=True, stop=True), [bkd, bvn], [bpsu])
        po2, bpo2 = slot()
        kb.op("pe", lambda e: e.matmul(po2, aT[:], vn[:], start=True, stop=True), [baT, bvn], [bpo2])
        kb.op("dve", lambda e: e.scalar_tensor_tensor(out=S1[:], in0=S0[:], scalar=cols[:, EGL, n:n + 1], in1=psu,
                                                      op0=ALU.mult, op1=ALU.add), [bS0, bcols, bpsu], [bS1])
        o2, bo2 = tmp("o2")
        kb.op("act", lambda e: e.copy(out=o2[:], in_=po2), [bpo2], [bo2])
        o, bo = tmp("o")
        kb.op("dve", lambda e: e.scalar_tensor_tensor(out=o[:], in0=po1, scalar=cols[:, EGC, n:n + 1], in1=o2[:],
                                                      op0=ALU.mult, op1=ALU.add), [bpo1, bcols, bo2], [bo])
        yield
        kb.op("act", lambda e: e.activation(out=junk[:], in_=o[:], func=AF.Square, accum_out=st[:, 0:1]),
              [bo], [bjunk, bst])
        kb.op("act", lambda e: e.activation(out=st[:, 1:2], in_=st[:, 0:1], func=AF.Ln, scale=1.0 / 128,
                                            bias=epst[:, 0:1]), [bst, beps], [bst])
        kb.op("act", lambda e: e.activation(out=st[:, 2:3], in_=st[:, 1:2], func=AF.Exp, scale=-0.5), [bst], [bst])
        sg, bsg = tmp("sg")
        kb.op("act", lambda e: e.activation(out=sg[:], in_=zt[:], func=AF.Exp, scale=-1.0), [bzt], [bsg])
        kb.op(POOL_ENG, lambda e: e.tensor_scalar(out=sg[:], in0=sg[:], scalar1=1.0, scalar2=None, op0=ALU.add),
              [bsg], [bsg])
        kb.op("dve", lambda e: e.reciprocal(out=sg[:], in_=sg[:]), [bsg], [bsg])
        kb.op(POOL_ENG, lambda e: e.tensor_tensor(out=sg[:], in0=sg[:], in1=zt[:], op=ALU.mult), [bsg, bzt], [bsg])
        yield
        t1, bt1 = tmp("t1")
        kb.op("dve", lambda e: e.scalar_tensor_tensor(out=t1[:], in0=o[:], scalar=st[:, 2:3], in1=gnb[:],
                                                      op0=ALU.mult, op1=ALU.mult), [bo, bst, bgnb], [bt1])
        yt_, byt_ = tmp("yout")
        kb.op("dve", lambda e: e.tensor_tensor(out=yt_[:], in0=t1[:], in1=sg[:], op=ALU.mult), [bt1, bsg], [byt_])
        kb.dma("sp", ya[n * 128:(n + 1) * 128, :], yt_[:], [byt_], [bya])

    def drive(gens):
        gens = list(gens)
        while gens:
            for g in list(gens):
                try:
                    next(g)
                except StopIteration:
                    gens.remove(g)

    def chain(*gs):
        for g in gs:
            yield from g

    if stop == "alloc":
        kb.dma("sp", ya[0:128, :], states[0][0][:], [states[0][1]], [bya])
        kb.finish([bya])
        return nc
    drive([pre(n) for n in range(min(2, nch))])
    for n in range(0, nch, 2):
        gens = [pre(m) for m in (n + 2, n + 3) if m < nch]
        gens.append(chain(*[scan(m) for m in (n, n + 1) if m < nch]))
        drive(gens)
    kb.finish([bya])
    return nc


def l2_consts():
    i = np.arange(128)
    ident = np.eye(128, dtype=np.float32)
    LT = (i[:, None] <= i[None, :]).astype(np.float32)
    ones = np.ones((128, 128), np.float32)
    penL = np.where(i[:, None] >= i[None, :], 0.0, -1e30).astype(np.float32)
    SM = (i[:, None] > i[None, :]).astype(np.float32)
    sel = np.zeros((128, 128), np.float32)
    sel[127, :] = 1.0
    return np.ascontiguousarray(np.stack([ident, LT, ones, -ones, penL, SM, sel], axis=1))


def run_l2(of, conv_w_l, a_log_l, dt_bias_l, gnw_l, nch=NCH, stop=None):
    nc = _get("l2_%d_%s" % (nch, stop), lambda: build_l2(nch, stop))
    cst = l2_consts()
    maps = []
    for cid in range(NCORES):
        b, g = cid // 4, cid % 4
        xs = []
        cws = []
        for a in range(3):
            cols_ = slice(a * 512 + g * 128, a * 512 + (g + 1) * 128)
            xa = np.zeros((128, S + 3), np.float32)
            xa[:, 3:] = of[b, :, cols_].T
            xs.append(xa)
            cws.append(conv_w_l[:, cols_].T)
        xin = np.ascontiguousarray(np.stack(xs, axis=1))
        cw = np.ascontiguousarray(np.concatenate(cws, axis=1)).astype(np.float32)
        z = of[b, :, 1536 + g * 128:1536 + (g + 1) * 128].reshape(NCH, 128, 128).transpose(1, 0, 2)
        bc = of[b, :, 2048 + g].reshape(NCH, 128).T
        ac = of[b, :, 2052 + g].reshape(NCH, 128).T
        maps.append({"xin": xin, "cw": cw, "zin": np.ascontiguousarray(z), "bcol": np.ascontiguousarray(bc),
                     "acol": np.ascontiguousarray(ac),
                     "sc3": np.array([[a_log_l[g], dt_bias_l[g]]], np.float32),
                     "gnw": gnw_l.reshape(1, 128).astype(np.float32), "cst": cst})
    res = _run(nc, maps)
    ya = np.zeros((NB, S, 512), np.float32)
    for cid in range(NCORES):
        b, g = cid // 4, cid % 4
        ya[b, :, g * 128:(g + 1) * 128] = res.results[cid]["ya"]
    return ya


def kernel(x, c, rel_bias, mod_w, mod_b, norm_mix_w, norm_ffn_w, w_in, conv_w, a_log, dt_bias,
           gdn_norm_w, q_norm_w, k_norm_w, w_out, router_w, router_b, w1, b1, w2, b2):
    f = lambda a: np.ascontiguousarray(np.asarray(a), dtype=np.float32)
    x = f(x)
    c = f(c)
    rel_bias = f(rel_bias)
    for l in range(2):
        of, ob = run_l1(x, c, f(mod_w[l]), f(mod_b[l]), f(norm_mix_w[l]), f(w_in[l]), f(q_norm_w[l]), f(k_norm_w[l]))
        ya = run_l2(of, f(conv_w[l]), f(a_log[l]), f(dt_bias[l]), f(gdn_norm_w[l]))
        yb = run_l3(ob, of, rel_bias)
        ycat = np.ascontiguousarray(np.concatenate([ya, yb], axis=-1))
        w1r, b1r, w2r, b2r = prep_l4_weights(f(w1[l]), f(b1[l]), f(w2[l]), f(b2[l]))
        x = run_l4(x, ycat, c, f(mod_w[l]), f(mod_b[l]), f(norm_ffn_w[l]), f(w_out[l]), f(router_w[l]),
                   f(router_b[l]), w1r, b1r, w2r, b2r)
    return x


CAP = 1024
ESTRIDE = CAP + 128
TRASH = NE * ESTRIDE


def _dma_ind(kb, out, out_off, in_, in_off, reads, writes):
    dst = writes[0]
    if dst.dsem is None:
        kb.nsem += 1
        key = "d%d_%s" % (kb.nsem, dst.name)
        dst.dsem = key
        kb.sems[key] = kb.nc.alloc_semaphore(key[:40])
    deps = kb._deps(reads, writes)
    kb._wait("pool", deps)
    ins = kb.nc.gpsimd.indirect_dma_start(out=out, out_offset=out_off, in_=in_, in_offset=in_off)
    dst.dcnt += 16
    ins.then_inc(kb.sems[dst.dsem], 16)
    for b in reads:
        if b.r.get(dst.dsem, 0) < dst.dcnt:
            b.r[dst.dsem] = dst.dcnt
    dst.w = (dst.dsem, dst.dcnt)
    dst.r = {}
    kb.n_ops += 1


def build_l4r(e_lo=0, e_hi=NE, first=True):
    n_exp = e_hi - e_lo
    nc = bass.Bass("TRN2", target_bir_lowering=False)
    x = _din(nc, "x", [TPC, D])
    ycat = _din(nc, "ycat", [TPC, D])
    cT = _din(nc, "cT", [128, 8])
    mod_w = _din(nc, "mod_w", [D, 6 * D])
    mod_b = _din(nc, "mod_b", [1, 6 * D])
    normw = _din(nc, "normw", [1, D])
    w_out = _din(nc, "w_out", [D, D])
    rw = _din(nc, "rw", [D, NE])
    rb = _din(nc, "rb", [1, NE])
    w1 = _din(nc, "w1", [n_exp, 8, 128, 8 * 256])
    b1 = _din(nc, "b1", [128, NE * 16])
    w2 = _din(nc, "w2", [n_exp, D, D])
    b2 = _din(nc, "b2", [NE, D])
    cst = _din(nc, "cst", [128, 3, 128])
    erow = _din(nc, "erow", [2, NE])
    xprev = None if first else _din(nc, "xprev", [TPC, D])
    xo = _dout(nc, "xo", [TPC, D])
    Xs = nc.dram_tensor("Xs", [TRASH + 128, D], BF16, kind="Internal").ap()
    Ys = nc.dram_tensor("Ys", [TRASH + 128, D], F32, kind="Internal").ap()
    kb = KB(nc)
    dr = Buf("dram_in")
    bxo = Buf("xo")
    bXs = Buf("Xs")
    bYs = Buf("Ys")

    PS = []
    for i in range(4):
        t = nc.alloc_psum_tensor("PS%d" % i, [128, 1024], F32)
        PS.append((t, Buf("ps%da" % i), Buf("ps%db" % i)))

    def bank(i):
        t, ba, bb = PS[i // 2]
        return (t[:, 0:512], ba) if i % 2 == 0 else (t[:, 512:1024], bb)

    R_x, _ = kb.sb([128, 8192], F32, "R_x")
    xrows = rview(R_x, 0, [128, 8, D], BF16)
    bxrows = Buf("xrows")
    xeT = rview(R_x, 16384, [128, 8, CAP], BF16)
    bxeT = Buf("xeT")
    modw_bufs = [(rview(R_x, i * 16384, [128, 8, 512], F32), Buf("modw%d" % i)) for i in range(2)]
    R_a, _ = kb.sb([128, 4096], F32, "R_a")
    actT = rview(R_a, 0, [128, 8, CAP], BF16)
    bactT = Buf("actT")
    g1, bg1 = rview(R_a, 0, [128, D], F32), Buf("g1")
    sh2, bsh2 = rview(R_a, 4096, [128, D], F32), Buf("sh2")
    A2, bA2 = rview(R_a, 8192, [128, D], F32), Buf("A2")
    nwb, bnwb = rview(R_a, 12288, [128, D], F32), Buf("nwb")
    R_w2, _ = kb.sb([128, 8192], F32, "R_w2")
    W2B = [(rview(R_w2, i * 16384, [128, 8, D], BF16), Buf("w2b%d" % i)) for i in range(2)]
    wo = rview(R_w2, 0, [128, 8, D], BF16)
    bwo = Buf("wo")
    scb_t = (rview(R_w2, 16384, [128, 8, 128], F32), Buf("scb"))
    modb_bufs = [(rview(R_w2, 20480 + i * 2048, [128, 512], F32), Buf("modb%d" % i)) for i in range(2)]
    R_W1, _ = kb.sb([128, 3072], F32, "R_W1")
    W1R = [(rview(R_W1, i * 4096, [128, 8, 256], BF16), Buf("w1r%d" % i)) for i in range(3)]
    g2, bg2 = kb.sb([128, D], F32, "g2")
    xt, bxt = kb.sb([128, D], F32, "xt")
    yt, byt = kb.sb([128, D], F32, "yt")
    tmpA, btmpA = kb.sb([128, D], F32, "tmpA")
    tmpB, btmpB = kb.sb([128, D], F32, "tmpB")
    R1, bR1 = kb.sb([128, D], F32, "R1")
    ybt = rview(R1, 0, [128, D], BF16)
    yT = rview(R1, 2048, [128, 8, 128], BF16)
    h32T = rview(R1, 0, [128, 8, 128], F32)
    R2, bR2 = kb.sb([128, D], F32, "R2")
    hb = rview(R2, 0, [128, D], BF16)
    junk = rview(R2, 2048, [128, D], BF16)
    yrows = [kb.sb([128, D], F32, "yrow") for _ in range(2)]
    b2bs = [kb.sb([128, D], F32, "b2bc") for _ in range(2)]
    grows = [kb.sb([128, D], F32, "grow") for _ in range(4)]

    ct, bct = kb.sb([128, 3, 128], F32, "cst")
    kb.dma("sp", ct[:], cst[:, :, :], [dr], [bct])
    idf, LTs, ones = ct[:, 0, :], ct[:, 1, :], ct[:, 2, :]
    bidf = bct
    idb, bidb = kb.sb([128, 128], BF16, "identb")
    kb.op("dve", lambda e: e.tensor_copy(out=idb[:], in_=idf), [bct], [bidb])
    er, ber = kb.sb([128, 2, NE], F32, "erow")
    kb.dma("sp", er[:, 0, :], erow[0:1, :].to_broadcast([128, NE]), [dr], [ber])
    kb.dma("sp", er[:, 1, :], erow[1:2, :].to_broadcast([128, NE]), [dr], [ber])
    epst, beps = kb.sb([128, 1], F32, "eps")
    kb.op("dve", lambda e: e.memset(epst[:], EPS), [], [beps])
    kb.dma("sp", nwb, normw[0:1, :].to_broadcast([128, D]), [dr], [bnwb])
    rbb, brbb = kb.sb([128, NE], F32, "rbb")
    kb.dma("sp", rbb[:], rb[0:1, :].to_broadcast([128, NE]), [dr], [brbb])
    rwt, brwt = kb.sb([128, 8, NE], F32, "rwt")
    kb.dma("sp", rwt[:], rw.rearrange("(kc p) e -> p kc e", p=128), [dr], [brwt])
    b1a, bb1a = kb.sb([128, NE * 16], F32, "b1a")
    kb.dma("sp", b1a[:], b1[:, :], [dr], [bb1a])
    b1av = b1a[:].rearrange("p (g two) -> p g two", two=2)
    kb.op("dve", lambda e: e.tensor_scalar(out=b1av[:, :, 0:1], in0=b1av[:, :, 0:1], scalar1=ALPHA, scalar2=None,
                                           op0=ALU.mult), [bb1a], [bb1a])
    kb.op("dve", lambda e: e.tensor_scalar(out=b1av[:, :, 1:2], in0=b1av[:, :, 1:2], scalar1=1.0, scalar2=None,
                                           op0=ALU.add), [bb1a], [bb1a])
    st, bst = kb.sb([128, 64], F32, "st")
    lg, blg = kb.sb([128, NE], F32, "lg")
    ohk, bohk = kb.sb([128, 4, NE], F32, "ohk")
    prod, bprod = kb.sb([128, 4, NE], F32, "prod")
    sel, bsel = kb.sb([128, NE], F32, "sel")
    pos, bpos = kb.sb([128, NE], F32, "pos")
    basebc, bbase = kb.sb([128, NE], F32, "basebc")
    kb.op("dve", lambda e: e.memset(basebc[:], 0.0), [], [bbase])
    gkall, bgk = kb.sb([128, NT, 4], F32, "gkall")
    dstf, bdstf = kb.sb([128, NT, 4], F32, "dstf")
    dsti, bdsti = kb.sb([128, NT, 4], U32, "dsti")

    kb.op("dve", lambda e: e.memset(tmpA[:], 0.0), [], [btmpA])
    zb16 = tmpA[:].bitcast(BF16)[:, 0:D]
    r0 = e_lo * ESTRIDE
    nz = (n_exp * ESTRIDE) // 128
    for i in range(nz):
        kb.dma("sp", Xs[r0 + i * 128:r0 + (i + 1) * 128, :], zb16, [btmpA], [bXs])
        kb.dma("sp", Ys[r0 + i * 128:r0 + (i + 1) * 128, :], tmpA[:], [btmpA], [bYs])
    kb.dma("sp", Ys[TRASH:TRASH + 128, :], tmpA[:], [btmpA], [bYs])
    kb.dma("sp", Xs[TRASH:TRASH + 128, :], zb16, [btmpA], [bXs])

    sc2_out = (A2, bA2)
    emit_mod(kb, cT, mod_w, mod_b, [2, 3, 4, 5], wbufs=modw_bufs, bbufs=modb_bufs,
             pss=[bank(0), bank(1)], scb_t=scb_t,
             outs={2: (g1, bg1), 3: (sh2, bsh2), 4: sc2_out, 5: (g2[:], bg2)})
    kb.op("dve", lambda e: e.scalar_tensor_tensor(out=A2, in0=A2, scalar=1.0, in1=nwb,
                                                  op0=ALU.add, op1=ALU.mult), [bA2, bnwb], [bA2])

    for kc in range(8):
        kb.dma("pool", wo[:, kc, :], w_out[kc * 128:(kc + 1) * 128, :], [dr], [bwo])
    for ti in range(NT):
        rows = slice(ti * 128, (ti + 1) * 128)
        kb.dma("sp", xt[:], x[rows, :], [dr], [bxt])
        kb.dma("sp", yt[:], ycat[rows, :], [dr], [byt])
        kb.op("act", lambda e: e.copy(out=ybt, in_=yt[:]), [byt], [bR1])
        pt, bpt = bank(0)
        ptb = pt.bitcast(BF16)
        for kc in range(8):
            kb.op("pe", lambda e, kc=kc: e.transpose(out=ptb[:, kc * 128:(kc + 1) * 128],
                                                     in_=ybt[:, kc * 128:(kc + 1) * 128], identity=idb[:]),
                  [bR1, bidb], [bpt])
        kb.op("act", lambda e: e.copy(out=yT.rearrange("p a b -> p (a b)"), in_=ptb), [bpt], [bR1])
        for half in range(2):
            pp, bpp = bank(2 + half)
            for kc in range(8):
                kb.op("pe", lambda e, kc=kc: e.matmul(pp, yT[:, kc, :], wo[:, kc, half * 512:(half + 1) * 512],
                                                      start=(kc == 0), stop=(kc == 7)), [bR1, bwo], [bpp])
            cs = slice(half * 512, (half + 1) * 512)
            kb.op("dve", lambda e: e.tensor_tensor(out=tmpA[:, cs], in0=pp, in1=g1[:, cs], op=ALU.mult),
                  [bpp, bg1], [btmpA])
        kb.op("dve", lambda e: e.tensor_tensor(out=xt[:], in0=tmpA[:], in1=xt[:], op=ALU.add), [btmpA, bxt], [bxt])
        kb.dma("sp", xo[rows, :], xt[:], [bxt], [bxo])
        kb.op("act", lambda e: e.activation(out=junk, in_=xt[:], func=AF.Square, accum_out=st[:, 0:1]),
              [bxt], [bR2, bst])
        kb.op("act", lambda e: e.activation(out=st[:, 1:2], in_=st[:, 0:1], func=AF.Sqrt, scale=1.0 / D,
                                            bias=epst[:]), [bst, beps], [bst])
        kb.op("dve", lambda e: e.reciprocal(out=st[:, 2:3], in_=st[:, 1:2]), [bst], [bst])
        kb.op("dve", lambda e: e.scalar_tensor_tensor(out=tmpA[:], in0=xt[:], scalar=st[:, 2:3], in1=A2,
                                                      op0=ALU.mult, op1=ALU.mult), [bxt, bst, bA2], [btmpA])
        kb.op("dve", lambda e: e.tensor_tensor(out=tmpB[:], in0=tmpA[:], in1=sh2, op=ALU.add),
              [btmpA, bsh2], [btmpB])
        kb.op("act", lambda e: e.copy(out=hb, in_=tmpB[:]), [btmpB], [bR2])
        for half in range(2):
            pq, bpq = bank(4 + half)
            for k4 in range(4):
                kc = half * 4 + k4
                kb.op("pe", lambda e, kc=kc, k4=k4: e.transpose(out=pq[:, k4 * 128:(k4 + 1) * 128],
                                                                in_=tmpB[:, kc * 128:(kc + 1) * 128],
                                                                identity=idf), [btmpB, bidf], [bpq])
            kb.op("dve", lambda e: e.tensor_copy(
                out=h32T[:, half * 4:(half + 1) * 4, :], in_=pq.rearrange("p (a b) -> p a b", a=4)),
                [bpq], [bR1])
        pr, bpr = bank(6)
        for kc in range(8):
            kb.op("pe", lambda e, kc=kc: e.matmul(pr[:, 0:NE], h32T[:, kc, :], rwt[:, kc, :],
                                                  start=(kc == 0), stop=(kc == 7)), [bR1, brwt], [bpr])
        kb.op("dve", lambda e: e.tensor_tensor(out=lg[:], in0=pr[:, 0:NE], in1=rbb[:], op=ALU.add),
              [bpr, brbb], [blg])
        kb.op("dve", lambda e: e.max(out=st[:, 8:16], in_=lg[:]), [blg], [bst])
        kb.op("dve", lambda e: e.tensor_scalar(out=st[:, 16:17], in0=st[:, 8:9], scalar1=-1.0, scalar2=None,
                                               op0=ALU.mult), [bst], [bst])
        kb.op("act", lambda e: e.activation(out=st[:, 20:24], in_=st[:, 8:12], func=AF.Exp, bias=st[:, 16:17],
                                            accum_out=st[:, 17:18]), [bst], [bst])
        kb.op("dve", lambda e: e.reciprocal(out=st[:, 18:19], in_=st[:, 17:18]), [bst], [bst])
        for k in range(4):
            kb.op("dve", lambda e, k=k: e.tensor_scalar(out=ohk[:, k, :], in0=lg[:], scalar1=st[:, 8 + k:9 + k],
                                                        scalar2=None, op0=ALU.is_equal), [blg, bst], [bohk])
        kb.op("dve", lambda e: e.tensor_scalar(out=sel[:], in0=lg[:], scalar1=st[:, 11:12], scalar2=None,
                                               op0=ALU.is_ge), [blg, bst], [bsel])
        ppf, bppf = bank(7)
        kb.op("pe", lambda e: e.matmul(ppf[:, 0:NE], LTs, sel[:], start=True, stop=True), [bct, bsel], [bppf])
        kb.op("dve", lambda e: e.tensor_tensor(out=pos[:], in0=ppf[:, 0:NE], in1=basebc[:], op=ALU.add),
              [bppf, bbase], [bpos])
        ppb, bppb = bank(1)
        kb.op("pe", lambda e: e.matmul(ppb[:, 0:NE], ones, sel[:], start=True, stop=True), [bct, bsel], [bppb])
        kb.op("dve", lambda e: e.tensor_tensor(out=basebc[:], in0=ppb[:, 0:NE], in1=basebc[:], op=ALU.add),
              [bppb, bbase], [bbase])
        kb.op("dve", lambda e: e.scalar_tensor_tensor(out=pos[:], in0=pos[:], scalar=float(CAP), in1=er[:, 0, :],
                                                      op0=ALU.min, op1=ALU.add), [bpos, ber], [bpos])
        kb.op("dve", lambda e: e.tensor_tensor(out=ohk[:], in0=ohk[:],
                                               in1=er[:, 1, :].unsqueeze(1).to_broadcast([128, 4, NE]),
                                               op=ALU.mult), [bohk, ber], [bohk])
        kb.op("dve", lambda e: e.tensor_tensor(out=prod[:], in0=ohk[:],
                                               in1=pos[:].unsqueeze(1).to_broadcast([128, 4, NE]),
                                               op=ALU.mult), [bohk, bpos], [bprod])
        kb.op("dve", lambda e: e.tensor_reduce(out=dstf[:, ti, :], in_=prod[:], axis=AX.X, op=ALU.add),
              [bprod], [bdstf])
        kb.op("dve", lambda e: e.tensor_reduce(out=st[:, 24:28], in_=ohk[:], axis=AX.X, op=ALU.add),
              [bohk], [bst])
        kb.op("dve", lambda e: e.tensor_scalar(out=st[:, 28:32], in0=st[:, 24:28], scalar1=-float(TRASH),
                                               scalar2=float(TRASH), op0=ALU.mult, op1=ALU.add), [bst], [bst])
        kb.op("dve", lambda e: e.tensor_tensor(out=dstf[:, ti, :], in0=dstf[:, ti, :], in1=st[:, 28:32], op=ALU.add),
              [bdstf, bst], [bdstf])
        kb.op("dve", lambda e: e.scalar_tensor_tensor(out=gkall[:, ti, :], in0=st[:, 20:24], scalar=st[:, 18:19],
                                                      in1=st[:, 24:28], op0=ALU.mult, op1=ALU.mult), [bst], [bgk])
        kb.op("dve", lambda e: e.tensor_copy(out=dsti[:, ti, :], in_=dstf[:, ti, :]), [bdstf], [bdsti])
        for k in range(4):
            _dma_ind(kb, Xs[:, :], bass.IndirectOffsetOnAxis(ap=dsti[:, ti, k:k + 1], axis=0), hb, None,
                     [bR2, bdsti], [bXs])

    merge_deps(bxrows, [b for _, b in modw_bufs])
    merge_deps(bxeT, [b for _, b in modw_bufs])
    merge_deps(bactT, [bg1, bsh2, bA2, bnwb])
    for _, b in W2B:
        merge_deps(b, [bwo, scb_t[1]] + [bb for _, bb in modb_bufs])
    tts = [(tmpA[:, 0:512], Buf("tt0")), (tmpA[:, 512:1024], Buf("tt1"))]
    lps = [(tmpB[:, 0:512], Buf("lp0")), (tmpB[:, 512:1024], Buf("lp1"))]
    t2s = [(xt[:, 0:512], Buf("t20")), (xt[:, 512:1024], Buf("t21"))]
    for (_, b), src_ in zip(tts + lps + t2s, [btmpA, btmpA, btmpB, btmpB, bxt, bxt]):
        merge_deps(b, [src_])
    C0 = float(LIMIT * ALPHA / (1.0 + np.exp(-LIMIT * ALPHA)))
    w1_issued = [0]

    def issue_w1(upto):
        while w1_issued[0] < min(upto, n_exp * 8):
            i = w1_issued[0]
            t, b = W1R[i % 3]
            kb.dma("pool", t.rearrange("p a b -> p (a b)"), w1[i // 8, i % 8, :, :], [dr], [b])
            w1_issued[0] += 1

    def issue_w2(ei):
        if ei < n_exp:
            t, b = W2B[ei % 2]
            src = w2[ei].rearrange("(fc p) d -> p fc d", p=128)
            for fc in range(8):
                kb.dma("pool", t[:, fc, :], src[:, fc, :], [dr], [b])

    nu = 0
    ny = 0
    ntp = 0
    issue_w2(0)
    for e in range(e_lo, e_hi):
        ei = e - e_lo
        issue_w2(ei + 1)
        b2b, bb2b = b2bs[ei % 2]
        kb.dma("sp", b2b[:], b2[e:e + 1, :].to_broadcast([128, D]), [dr], [bb2b])
        kb.dma("sp", xrows, Xs[e * ESTRIDE:e * ESTRIDE + CAP, :].rearrange("(i p) d -> p i d", p=128),
               [bXs], [bxrows])
        for i in range(8):
            pt, bpt = bank(6 + ntp % 2)
            ntp += 1
            ptb = pt.bitcast(BF16)
            for kc in range(8):
                kb.op("pe", lambda en, kc=kc, i=i: en.transpose(out=ptb[:, kc * 128:(kc + 1) * 128],
                                                                in_=xrows[:, i, kc * 128:(kc + 1) * 128],
                                                                identity=idb[:]), [bxrows, bidb], [bpt])
            kb.op("act", lambda en, i=i: en.copy(out=xeT[:, :, i * 128:(i + 1) * 128],
                                                 in_=ptb.rearrange("p (a b) -> p a b", a=8)), [bpt], [bxeT])
        for ft in range(8):
            gi = ei * 8 + ft
            issue_w1(gi + 2)
            wv, bwt = W1R[gi % 3]
            bcol = (e * 8 + ft) * 2
            for blk in range(CAP // 512):
                ts_ = slice(blk * 512, (blk + 1) * 512)
                pgl, bpgl = bank(0 + 2 * (nu % 2))
                pli, bpli = bank(1 + 2 * (nu % 2))
                for kc in range(8):
                    kb.op("pe", lambda en, kc=kc: en.matmul(pgl, wv[:, kc, 0:128], xeT[:, kc, ts_],
                                                            start=(kc == 0), stop=(kc == 7)), [bwt, bxeT], [bpgl])
                for kc in range(8):
                    kb.op("pe", lambda en, kc=kc: en.matmul(pli, wv[:, kc, 128:256], xeT[:, kc, ts_],
                                                            start=(kc == 0), stop=(kc == 7)), [bwt, bxeT], [bpli])
                tt, btt = tts[nu % 2]
                lp, blp = lps[nu % 2]
                t2, bt2 = t2s[nu % 2]
                kb.op("act", lambda en: en.activation(out=tt, in_=pgl, func=AF.Silu, scale=ALPHA,
                                                      bias=b1a[:, bcol:bcol + 1]), [bpgl, bb1a], [btt])
                kb.op("dve", lambda en: en.tensor_scalar(out=lp, in0=pli, scalar1=b1a[:, bcol + 1:bcol + 2],
                                                         scalar2=LIMIT + 1.0, op0=ALU.add, op1=ALU.min),
                      [bpli, bb1a], [blp])
                kb.op("dve", lambda en: en.tensor_scalar(out=t2, in0=tt, scalar1=C0, scalar2=1.0 / ALPHA,
                                                         op0=ALU.min, op1=ALU.mult), [btt], [bt2])
                kb.op("dve", lambda en: en.scalar_tensor_tensor(out=actT[:, ft, ts_], in0=lp, scalar=1.0 - LIMIT,
                                                                in1=t2, op0=ALU.max, op1=ALU.mult),
                      [blp, bt2], [bactT])
                nu += 1
        w2v, bw2 = W2B[ei % 2]
        for i in range(8):
            yr, byr = yrows[ny % 2]
            ny += 1
            for db in range(2):
                py, bpy = bank(4 + db)
                for fc in range(8):
                    kb.op("pe", lambda en, fc=fc, i=i, db=db: en.matmul(py, actT[:, fc, i * 128:(i + 1) * 128],
                                                                        w2v[:, fc, db * 512:(db + 1) * 512],
                                                                        start=(fc == 0), stop=(fc == 7)),
                          [bactT, bw2], [bpy])
                kb.op("dve", lambda en, db=db: en.tensor_tensor(out=yr[:, db * 512:(db + 1) * 512], in0=py,
                                                                in1=b2b[:, db * 512:(db + 1) * 512], op=ALU.add),
                      [bpy, bb2b], [byr])
            kb.dma("sp", Ys[e * ESTRIDE + i * 128:e * ESTRIDE + (i + 1) * 128, :], yr[:], [byr], [bYs])

    tf, btf = tts[0]
    for ti in range(NT):
        rows = slice(ti * 128, (ti + 1) * 128)
        if first:
            kb.dma("sp", yt[:], xo[rows, :], [bxo], [byt])
        else:
            kb.dma("sp", yt[:], xprev[rows, :], [dr], [byt])
        for k in range(4):
            gr, bgr = grows[k]
            _dma_ind(kb, gr[:], None, Ys[:, :], bass.IndirectOffsetOnAxis(ap=dsti[:, ti, k:k + 1], axis=0),
                     [bYs, bdsti], [bgr])
        g0, bg0 = grows[0]
        kb.op("dve", lambda en: en.tensor_scalar(out=g0[:], in0=g0[:], scalar1=gkall[:, ti, 0:1], scalar2=None,
                                                 op0=ALU.mult), [bg0, bgk], [bg0])
        for k in range(1, 4):
            gr, bgr = grows[k]
            kb.op("dve", lambda en, k=k, gr=gr: en.scalar_tensor_tensor(out=g0[:], in0=gr[:],
                                                                        scalar=gkall[:, ti, k:k + 1], in1=g0[:],
                                                                        op0=ALU.mult, op1=ALU.add),
                  [bgr, bgk, bg0], [bg0])
        kb.op("dve", lambda en: en.tensor_tensor(out=g0[:], in0=g0[:], in1=g2[:], op=ALU.mult), [bg0, bg2], [bg0])
        kb.op("dve", lambda en: en.tensor_tensor(out=yt[:], in0=g0[:], in1=yt[:], op=ALU.add), [bg0, byt], [byt])
        kb.dma("sp", xo[rows, :], yt[:], [byt], [bxo])
    kb.finish([bxo])
    return nc


def run_l4r(x, ycat, c, mod_w_l, mod_b_l, normw_l, w_out_l, rw_l, rb_l, w1r, b1r, w2_l, b2_l,
            splits=((0, 16), (16, 32))):
    i = np.arange(128)
    cst = np.ascontiguousarray(np.stack([np.eye(128, dtype=np.float32),
                                         (i[:, None] < i[None, :]).astype(np.float32),
                                         np.ones((128, 128), np.float32)], axis=1))
    xprev = None
    for (lo, hi) in splits:
        first = (lo == 0)
        nc = _get("l4r_%d_%d" % (lo, hi), lambda: build_l4r(lo, hi, first))
        w1s = np.ascontiguousarray(w1r[lo:hi])
        w2s = np.ascontiguousarray(w2_l[lo:hi])
        erow = np.stack([np.arange(NE) * ESTRIDE, ((np.arange(NE) >= lo) & (np.arange(NE) < hi))]).astype(np.float32)
        maps = []
        for cid in range(NCORES):
            m = {"x": core_tokens(x, cid), "ycat": core_tokens(ycat, cid), "cT": cT_of(c, cid // 4),
                 "mod_w": mod_w_l, "mod_b": mod_b_l.reshape(1, -1), "normw": normw_l.reshape(1, -1),
                 "w_out": w_out_l, "rw": rw_l, "rb": rb_l.reshape(1, -1), "w1": w1s, "b1": b1r, "w2": w2s,
                 "b2": b2_l, "cst": cst, "erow": erow}
            if not first:
                m["xprev"] = xprev[cid]
            maps.append(m)
        res = _run(nc, maps)
        xprev = [r["xo"] for r in res.results]
    return uncore_tokens(xprev, (D,))
```

```python
import numpy as np
import concourse.bass as bass
import concourse.mybir as mybir
from concourse.bass_utils import run_bass_kernel_spmd

F32 = mybir.dt.float32
BF16 = mybir.dt.bfloat16
U32 = mybir.dt.uint32
AF = mybir.ActivationFunctionType
ALU = mybir.AluOpType
AX = mybir.AxisListType

D = 1024
S = 8192
NB = 2
D_IN = 4176
EPS = 1e-6
NCORES = 8
TPC = 2048
NT = 16


class Buf:
    __slots__ = ("name", "w", "r", "dsem", "dcnt")

    def __init__(self, name):
        self.name = name
        self.w = None
        self.r = {}
        self.dsem = None
        self.dcnt = 0


class KB:
    def __init__(self, nc, self_sync=True):
        self.nc = nc
        self.eng = {"pe": nc.tensor, "dve": nc.vector, "act": nc.scalar,
                    "pool": nc.gpsimd, "sp": nc.sync}
        self.sems = {}
        self.cnt = {}
        self.known = {k: {} for k in self.eng}
        for k in self.eng:
            self.sems[k] = nc.alloc_semaphore("sem_" + k)
            self.cnt[k] = 0
        self.self_sync = self_sync
        self.nsem = 0
        self.n_ops = 0
        self.n_waits = 0
        self.nbuf = 0

    def sb(self, shape, dt=F32, name=None):
        self.nbuf += 1
        name = (name or "t") + "_%d" % self.nbuf
        return self.nc.alloc_sbuf_tensor(name, list(shape), dt), Buf(name)

    def ps(self, shape, dt=F32, name=None):
        self.nbuf += 1
        name = (name or "p") + "_%d" % self.nbuf
        return self.nc.alloc_psum_tensor(name, list(shape), dt), Buf(name)

    def _deps(self, reads, writes):
        d = {}

        def add(kv):
            if kv is None:
                return
            k, v = kv
            if d.get(k, 0) < v:
                d[k] = v
        for b in reads:
            add(b.w)
        for b in writes:
            add(b.w)
            for k, v in b.r.items():
                add((k, v))
        return d

    def _wait(self, en, deps):
        e = self.eng[en]
        kn = self.known[en]
        for k, v in deps.items():
            if k == en and (en == "pe" or not self.self_sync):
                continue
            if kn.get(k, 0) >= v:
                continue
            e.wait_ge(self.sems[k], v)
            kn[k] = v
            self.n_waits += 1

    def op(self, en, fn, reads=(), writes=()):
        deps = self._deps(reads, writes)
        self._wait(en, deps)
        ins = fn(self.eng[en])
        self.cnt[en] += 1
        v = self.cnt[en]
        ins.then_inc(self.sems[en], 1)
        for b in reads:
            if b.r.get(en, 0) < v:
                b.r[en] = v
        for b in writes:
            b.w = (en, v)
            b.r = {}
        self.n_ops += 1
        return ins

    def dma(self, en, out, in_, reads, writes, **kw):
        assert len(writes) == 1
        dst = writes[0]
        if dst.dsem is None:
            self.nsem += 1
            key = "d%d_%s" % (self.nsem, dst.name)
            dst.dsem = key
            self.sems[key] = self.nc.alloc_semaphore(key[:40])
        deps = self._deps(reads, writes)
        self._wait(en, deps)
        ins = self.eng[en].dma_start(out=out, in_=in_, **kw)
        dst.dcnt += 16
        ins.then_inc(self.sems[dst.dsem], 16)
        for b in reads:
            if b.r.get(dst.dsem, 0) < dst.dcnt:
                b.r[dst.dsem] = dst.dcnt
        dst.w = (dst.dsem, dst.dcnt)
        dst.r = {}
        self.n_ops += 1
        return ins

    def finish(self, bufs, en="sp"):
        d = {}
        for b in bufs:
            if b.w is not None:
                k, v = b.w
                d[k] = max(d.get(k, 0), v)
        self._wait(en, d)


def rview(t, off_bytes, shape, dt):
    esz = 2 if dt == BF16 else 4
    n = 1
    for s in shape[1:]:
        n *= s
    flat = t[:, :] if dt == F32 else t[:, :].bitcast(dt)
    e0 = off_bytes // esz
    v = flat[0:shape[0], e0:e0 + n]
    if len(shape) == 3:
        v = v.rearrange("p (a b) -> p a b", a=shape[1])
    return v


def merge_deps(dst, srcs):
    for s in srcs:
        for kv in ([s.w] if s.w else []) + list(s.r.items()):
            k, v = kv
            if dst.r.get(k, 0) < v:
                dst.r[k] = v


def _din(nc, name, shape, dt=F32):
    return nc.dram_tensor(name, list(shape), dt, kind="ExternalInput").ap()


def _dout(nc, name, shape, dt=F32):
    return nc.dram_tensor(name, list(shape), dt, kind="ExternalOutput").ap()


def emit_mod(kb, cT, mod_w, mod_b, groups, wbufs=None, bbufs=None, pss=None, scb_t=None, outs=None):
    nc = kb.nc
    dr = Buf("mod_dram")
    ct, bct = kb.sb([128, 8], F32, "ct")
    kb.dma("sp", ct[:], cT[:, :], [dr], [bct])
    sc, bsc = kb.sb([128, 8], F32, "sc")
    kb.op("act", lambda e: e.activation(out=sc[:], in_=ct[:], func=AF.Silu), [bct], [bsc])
    scb, bscb = scb_t if scb_t is not None else kb.sb([128, 8, 128], F32, "scb")
    kb.op("dve", lambda e: e.tensor_copy(out=scb[:], in_=sc[:, :].unsqueeze(2).to_broadcast([128, 8, 128])),
          [bsc], [bscb])
    mw = mod_w.rearrange("(kc p) n -> p kc n", p=128)
    if wbufs is None:
        wbufs = [kb.sb([128, 8, 512], F32, "modw") for _ in range(2)]
    if bbufs is None:
        bbufs = [kb.sb([128, 512], F32, "modb") for _ in range(2)]
    if pss is None:
        pss = [kb.ps([128, 512], F32, "modps") for _ in range(2)]
    res = {}
    it = 0
    for g in groups:
        mt, bmt = outs[g] if outs is not None else kb.sb([128, 1024], F32, "modrow")
        res[g] = (mt, bmt)
        for half in range(2):
            c0 = g * 1024 + half * 512
            wt, bw = wbufs[it % 2]
            bt, bb = bbufs[it % 2]
            pt, bp = pss[it % 2]
            kb.dma("sp", wt[:], mw[:, :, c0:c0 + 512], [dr], [bw])
            kb.dma("sp", bt[:], mod_b[0:1, c0:c0 + 512].to_broadcast([128, 512]), [dr], [bb])
            for kc in range(8):
                kb.op("pe", lambda e, kc=kc: e.matmul(pt[:], scb[:, kc, :], wt[:, kc, :],
                                                      start=(kc == 0), stop=(kc == 7)),
                      [bscb, bw], [bp])
            kb.op("dve", lambda e: e.tensor_tensor(out=mt[:, half * 512:(half + 1) * 512], in0=pt[:],
                                                   in1=bt[:], op=ALU.add), [bp, bb], [bmt])
            it += 1
    return res


NF1 = 2064
NB1 = 2112


def build_l1():
    nc = bass.Bass("TRN2", target_bir_lowering=False)
    x = _din(nc, "x", [TPC, D])
    cT = _din(nc, "cT", [128, 8])
    mod_w = _din(nc, "mod_w", [D, 6 * D])
    mod_b = _din(nc, "mod_b", [1, 6 * D])
    normw = _din(nc, "normw", [1, D])
    w_in = _din(nc, "w_in", [D, D_IN])
    qkw = _din(nc, "qkw", [1, 128])
    ident = _din(nc, "ident", [128, 128])
    of = _dout(nc, "of", [TPC, NF1])
    ob = _dout(nc, "ob", [TPC, NB1], BF16)
    kb = KB(nc)
    dr = Buf("dram_in")
    bof, bob = Buf("of"), Buf("ob")

    idb, bidb = kb.sb([128, 128], BF16, "identb")
    kb.dma("pool", idb[:], ident[:, :], [dr], [bidb])
    epst, beps = kb.sb([128, 1], F32, "eps")
    kb.op("dve", lambda e: e.memset(epst[:], EPS), [], [beps])
    nwb, bnwb = kb.sb([128, D], F32, "nwb")
    kb.dma("sp", nwb[:], normw[0:1, :].to_broadcast([128, D]), [dr], [bnwb])
    qkb, bqkb = kb.sb([128, 128], F32, "qkb")
    kb.dma("sp", qkb[:], qkw[0:1, :].to_broadcast([128, 128]), [dr], [bqkb])
    kb.op("dve", lambda e: e.tensor_scalar(out=qkb[:, 0:64], in0=qkb[:, 0:64], scalar1=0.125, scalar2=None,
                                           op0=ALU.mult), [bqkb], [bqkb])

    wb, bwb = kb.sb([128, 8, D_IN], BF16, "w_in")
    for kc in range(8):
        for pc in range(3):
            c0 = pc * 1392
            kb.dma("pool", wb[:, kc, c0:c0 + 1392], w_in[kc * 128:(kc + 1) * 128, c0:c0 + 1392], [dr], [bwb])

    mods = emit_mod(kb, cT, mod_w, mod_b, [0, 1])
    sh1, bsh1 = mods[0]
    sc1, bsc1 = mods[1]
    A1, bA1 = kb.sb([128, D], F32, "A1")
    kb.op("dve", lambda e: e.scalar_tensor_tensor(out=A1[:], in0=sc1[:], scalar=1.0, in1=nwb[:],
                                                  op0=ALU.add, op1=ALU.mult), [bsc1, bnwb], [bA1])

    xts = [kb.sb([128, D], F32, "xt") for _ in range(2)]
    junk, bjunk = kb.sb([128, D], BF16, "junk")
    tmp, btmp = kb.sb([128, D], F32, "tmp")
    hbs = [kb.sb([128, D], BF16, "hb") for _ in range(2)]
    pT, bpT = kb.ps([128, D], BF16, "pT")
    hTs = [kb.sb([128, 8, 128], BF16, "hT") for _ in range(2)]
    pps = [kb.ps([128, 512], F32, "pp") for _ in range(4)]
    projs = [kb.sb([128, D_IN], F32, "proj") for _ in range(2)]
    obts = [kb.sb([128, NB1], BF16, "obt") for _ in range(2)]
    sq, bsq = kb.sb([128, 1024], F32, "sq")
    st, bst = kb.sb([128, 24], F32, "stats")

    QO = 2056
    npp = 0
    for ti in range(NT):
        xt, bxt = xts[ti % 2]
        hb, bhb = hbs[ti % 2]
        hT, bhT = hTs[ti % 2]
        pj, bpj = projs[ti % 2]
        obt, bobt = obts[ti % 2]
        kb.dma("sp", xt[:], x[ti * 128:(ti + 1) * 128, :], [dr], [bxt])
        kb.op("act", lambda e: e.activation(out=junk[:], in_=xt[:], func=AF.Square, accum_out=st[:, 0:1]),
              [bxt], [bjunk, bst])
        kb.op("act", lambda e: e.activation(out=st[:, 1:2], in_=st[:, 0:1], func=AF.Sqrt, scale=1.0 / D,
                                            bias=epst[:]), [bst, beps], [bst])
        kb.op("dve", lambda e: e.reciprocal(out=st[:, 2:3], in_=st[:, 1:2]), [bst], [bst])
        kb.op("dve", lambda e: e.scalar_tensor_tensor(out=tmp[:], in0=xt[:], scalar=st[:, 2:3], in1=A1[:],
                                                      op0=ALU.mult, op1=ALU.mult), [bxt, bst, bA1], [btmp])
        kb.op("dve", lambda e: e.tensor_tensor(out=hb[:], in0=tmp[:], in1=sh1[:], op=ALU.add),
              [btmp, bsh1], [bhb])
        for kc in range(8):
            kb.op("pe", lambda e, kc=kc: e.transpose(out=pT[:, kc * 128:(kc + 1) * 128],
                                                     in_=hb[:, kc * 128:(kc + 1) * 128], identity=idb[:]),
                  [bhb, bidb], [bpT])
        kb.op("act", lambda e: e.copy(out=hT[:].rearrange("p a b -> p (a b)"), in_=pT[:]), [bpT], [bhT])
        for cb in range(9):
            c0 = cb * 512
            n = min(512, D_IN - c0)
            pp, bpp = pps[npp % 4]
            for kc in range(8):
                kb.op("pe", lambda e, kc=kc: e.matmul(pp[:, 0:n], hT[:, kc, :], wb[:, kc, c0:c0 + n],
                                                      start=(kc == 0), stop=(kc == 7)), [bhT, bwb], [bpp])
            if npp % 2 == 0:
                kb.op("act", lambda e: e.copy(out=pj[:, c0:c0 + n], in_=pp[:, 0:n]), [bpp], [bpj])
            else:
                kb.op("dve", lambda e: e.tensor_copy(out=pj[:, c0:c0 + n], in_=pp[:, 0:n]), [bpp], [bpj])
            npp += 1
        qk = pj[:, QO:QO + 1024]
        kb.op("dve", lambda e: e.tensor_tensor(out=sq[:], in0=qk, in1=qk, op=ALU.mult), [bpj], [bsq])
        kb.op("dve", lambda e: e.tensor_reduce(out=st[:, 4:20], in_=sq[:].rearrange("p (g d) -> p g d", g=16),
                                               axis=AX.X, op=ALU.add), [bsq], [bst])
        kb.op("act", lambda e: e.activation(out=st[:, 4:20], in_=st[:, 4:20], func=AF.Sqrt, scale=1.0 / 64,
                                            bias=epst[:]), [bst, beps], [bst])
        kb.op("dve", lambda e: e.reciprocal(out=st[:, 4:20], in_=st[:, 4:20]), [bst], [bst])
        kb.op("dve", lambda e: e.tensor_tensor(out=sq[:].rearrange("p (g d) -> p g d", g=16),
                                               in0=qk.rearrange("p (g d) -> p g d", g=16),
                                               in1=st[:, 4:20].unsqueeze(2).to_broadcast([128, 16, 64]),
                                               op=ALU.mult), [bpj, bst], [bsq])
        for half in range(2):
            kb.op("dve", lambda e, half=half: e.tensor_tensor(
                out=obt[:, half * 512:(half + 1) * 512].rearrange("p (g d) -> p g d", g=8),
                in0=sq[:, half * 512:(half + 1) * 512].rearrange("p (g d) -> p g d", g=8),
                in1=qkb[:, half * 64:(half + 1) * 64].unsqueeze(1).to_broadcast([128, 8, 64]),
                op=ALU.mult), [bsq, bqkb], [bobt])
        kb.op("act", lambda e: e.copy(out=obt[:, 1024:NB1], in_=pj[:, QO + 1024:QO + 1024 + 1088]), [bpj], [bobt])
        kb.op("dve", lambda e: e.tensor_scalar(out=pj[:, 4168:4176], in0=pj[:, 4168:4176],
                                               scalar1=float(8 ** -0.5 * 64 ** -0.5), scalar2=None,
                                               op0=ALU.mult), [bpj], [bpj])
        kb.dma("sp", of[ti * 128:(ti + 1) * 128, 0:2056], pj[:, 0:2056], [bpj], [bof])
        kb.dma("sp", of[ti * 128:(ti + 1) * 128, 2056:2064], pj[:, 4168:4176], [bpj], [bof])
        kb.dma("sp", ob[ti * 128:(ti + 1) * 128, :], obt[:], [bobt], [bob])
    kb.finish([bof, bob])
    return nc


def core_tokens(a, c):
    b, r = c // 4, c % 4
    t = a[b].reshape((64, 128) + a.shape[2:])[r::4]
    return np.ascontiguousarray(t.reshape((TPC,) + a.shape[2:]))


def uncore_tokens(parts, tail):
    out = np.empty((NB, 64, 128) + tuple(tail), parts[0].dtype)
    for c in range(NCORES):
        b, r = c // 4, c % 4
        out[b, r::4] = parts[c].reshape((16, 128) + tuple(tail))
    return out.reshape((NB, S) + tuple(tail))


_NC_CACHE = {}
_TRACE = False
_TIMES = []


def _run(nc, maps):
    if _TRACE:
        res = run_bass_kernel_spmd(nc, maps, core_ids=list(range(NCORES)), trace=True)
        _TIMES.append(res.exec_time_ns)
        print('exec_time_ns', res.exec_time_ns)
        return res
    return run_bass_kernel_spmd(nc, maps, core_ids=list(range(NCORES)))


def _get(name, fn):
    if name not in _NC_CACHE:
        _NC_CACHE[name] = fn()
    return _NC_CACHE[name]


def cT_of(c, b):
    return np.ascontiguousarray(c[b].reshape(8, 128).T)


def run_l1(x, c, mod_w_l, mod_b_l, normw_l, w_in_l, qw_l, kw_l):
    nc = _get("l1", build_l1)
    ident = np.eye(128, dtype=np.float32)
    qkw = np.concatenate([qw_l, kw_l]).reshape(1, 128).astype(np.float32)
    maps = []
    for cid in range(NCORES):
        maps.append({"x": core_tokens(x, cid), "cT": cT_of(c, cid // 4), "mod_w": mod_w_l,
                     "mod_b": mod_b_l.reshape(1, -1), "normw": normw_l.reshape(1, -1), "w_in": w_in_l,
                     "qkw": qkw, "ident": ident})
    res = _run(nc, maps)
    of = uncore_tokens([r["of"] for r in res.results], (NF1,))
    ob = uncore_tokens([r["ob"] for r in res.results], (NB1,))
    return of, ob


NE = 32
ALPHA = 1.702
LIMIT = 7.0


def build_l4(e_lo=0, e_hi=NE, first=True):
    n_exp = e_hi - e_lo
    do_final = True
    nc = bass.Bass("TRN2", target_bir_lowering=False)
    x = _din(nc, "x", [TPC, D])
    ycat = _din(nc, "ycat", [TPC, D])
    cT = _din(nc, "cT", [128, 8])
    mod_w = _din(nc, "mod_w", [D, 6 * D])
    mod_b = _din(nc, "mod_b", [1, 6 * D])
    normw = _din(nc, "normw", [1, D])
    w_out = _din(nc, "w_out", [D, D])
    rw = _din(nc, "rw", [D, NE])
    rb = _din(nc, "rb", [1, NE])
    w1 = _din(nc, "w1", [max(n_exp, 1), 8, 128, 8 * 256])
    b1 = _din(nc, "b1", [128, NE * 16])
    w2 = _din(nc, "w2", [max(n_exp, 1), 8, 128, 8 * 128])
    b2 = _din(nc, "b2", [NE, D])
    ident = _din(nc, "ident", [128, 128])
    xprev = None if first else _din(nc, "xprev", [TPC, D])
    xo = _dout(nc, "xo", [TPC, D])
    if first:
        hT_o = _dout(nc, "hT_o", [128, 8 * TPC], BF16)
        gT_o = _dout(nc, "gT_o", [NE, TPC])
        g2_o = _dout(nc, "g2_o", [128, D])
    else:
        hT_i = _din(nc, "hT_i", [128, 8 * TPC], BF16)
        gT_i = _din(nc, "gT_i", [NE, TPC])
        g2_i = _din(nc, "g2_i", [128, D])
    kb = KB(nc)
    dr = Buf("dram_in")
    bxo = Buf("xo")
    bho = Buf("handover")

    PS = []
    for i in range(4):
        t = nc.alloc_psum_tensor("PS%d" % i, [128, 1024], F32)
        PS.append((t, Buf("ps%da" % i), Buf("ps%db" % i)))

    def bank(i):
        t, ba, bb = PS[i // 2]
        return (t[:, 0:512], ba) if i % 2 == 0 else (t[:, 512:1024], bb)

    R_hT, bhT = kb.sb([128, 8192], F32, "R_hT")
    hT = rview(R_hT, 0, [128, 8, TPC], BF16)
    R_acc, bacc = kb.sb([128, 16384], F32, "R_acc")
    acc = rview(R_acc, 0, [128, 8, TPC], F32)
    wo = rview(R_acc, 0, [128, 8, D], BF16)
    bwo = Buf("wo")
    R_act, bactT = kb.sb([128, 8192], F32, "R_act")
    actT = rview(R_act, 0, [128, 8, TPC], BF16)
    modw_bufs = [(rview(R_act, i * 16384, [128, 8, 512], F32), Buf("modw%d" % i)) for i in range(2)]
    gateT, bgateT = kb.sb([NE, TPC], F32, "gateT")
    R_gsb, bgsb = kb.sb([128, TPC], F32, "gsb")
    gsb = R_gsb
    scb_t = (rview(R_gsb, 0, [128, 8, 128], F32), Buf("scb"))
    modb_bufs = [(rview(R_gsb, 4096 + i * 2048, [128, 512], F32), Buf("modb%d" % i)) for i in range(2)]
    R_W, _ = kb.sb([128, 4608], F32, "R_W")
    W1R = [(rview(R_W, i * 4096, [128, 8, 256], BF16), Buf("w1r%d" % i)) for i in range(3)]
    W2R = [(rview(R_W, 12288 + i * 2048, [128, 8, 128], BF16), Buf("w2r%d" % i)) for i in range(3)]
    g1, bg1 = rview(R_W, 0, [128, D], F32), Buf("g1")
    sh2, bsh2 = rview(R_W, 4096, [128, D], F32), Buf("sh2")
    A2, bA2 = rview(R_W, 8192, [128, D], F32), Buf("A2")
    nwb, bnwb = rview(R_W, 12288, [128, D], F32), Buf("nwb")
    g2, bg2 = kb.sb([128, D], F32, "g2")
    xt, bxt = kb.sb([128, D], F32, "xt")
    yt, byt = kb.sb([128, D], F32, "yt")
    tmpA, btmpA = kb.sb([128, D], F32, "tmpA")
    tmpB, btmpB = kb.sb([128, D], F32, "tmpB")
    R1, bR1 = kb.sb([128, D], F32, "R1")
    ybt = rview(R1, 0, [128, D], BF16)
    yT = rview(R1, 2048, [128, 8, 128], BF16)
    h32T = rview(R1, 0, [128, 8, 128], F32)
    R2, bR2 = kb.sb([128, D], F32, "R2")
    hb = rview(R2, 0, [128, D], BF16)
    junk = rview(R2, 2048, [128, D], BF16)

    idf, bidf = kb.sb([128, 128], F32, "identf")
    kb.dma("sp", idf[:], ident[:, :], [dr], [bidf])
    idb, bidb = kb.sb([128, 128], BF16, "identb")
    kb.op("dve", lambda e: e.tensor_copy(out=idb[:], in_=idf[:]), [bidf], [bidb])
    epst, beps = kb.sb([128, 1], F32, "eps")
    kb.op("dve", lambda e: e.memset(epst[:], EPS), [], [beps])
    kb.dma("sp", nwb, normw[0:1, :].to_broadcast([128, D]), [dr], [bnwb])
    rbb, brbb = kb.sb([128, NE], F32, "rbb")
    kb.dma("sp", rbb[:], rb[0:1, :].to_broadcast([128, NE]), [dr], [brbb])
    rwt, brwt = kb.sb([128, 8, NE], F32, "rwt")
    kb.dma("sp", rwt[:], rw.rearrange("(kc p) e -> p kc e", p=128), [dr], [brwt])
    b1a, bb1a = kb.sb([128, NE * 16], F32, "b1a")
    kb.dma("sp", b1a[:], b1[:, :], [dr], [bb1a])
    b1av = b1a[:].rearrange("p (g two) -> p g two", two=2)
    kb.op("dve", lambda e: e.tensor_scalar(out=b1av[:, :, 0:1], in0=b1av[:, :, 0:1], scalar1=ALPHA, scalar2=None,
                                           op0=ALU.mult), [bb1a], [bb1a])
    kb.op("dve", lambda e: e.tensor_scalar(out=b1av[:, :, 1:2], in0=b1av[:, :, 1:2], scalar1=1.0, scalar2=None,
                                           op0=ALU.add), [bb1a], [bb1a])
    b2t, bb2t = kb.sb([NE, D], F32, "b2t")
    kb.dma("sp", b2t[:], b2[:, :], [dr], [bb2t])
    selbs = [kb.sb([NE, 128], F32, "selb") for _ in range(2)]
    st, bst = kb.sb([128, 64], F32, "st")
    lg, blg = kb.sb([128, NE], F32, "lg")

    if first:
        sc2_out = (A2, bA2)
        mods = emit_mod(kb, cT, mod_w, mod_b, [2, 3, 4, 5], wbufs=modw_bufs, bbufs=modb_bufs,
                        pss=[bank(0), bank(1)], scb_t=scb_t,
                        outs={2: (g1, bg1), 3: (sh2, bsh2), 4: sc2_out, 5: (g2[:], bg2)})
        kb.op("dve", lambda e: e.scalar_tensor_tensor(out=A2, in0=A2, scalar=1.0, in1=nwb,
                                                      op0=ALU.add, op1=ALU.mult), [bA2, bnwb], [bA2])

        for kc in range(8):
            kb.dma("pool", wo[:, kc, :], w_out[kc * 128:(kc + 1) * 128, :], [dr], [bwo])
        for ti in range(NT):
            rows = slice(ti * 128, (ti + 1) * 128)
            kb.dma("sp", xt[:], x[rows, :], [dr], [bxt])
            kb.dma("sp", yt[:], ycat[rows, :], [dr], [byt])
            kb.op("act", lambda e: e.copy(out=ybt, in_=yt[:]), [byt], [bR1])
            pt, bpt = bank(0)
            ptb = pt.bitcast(BF16)
            for kc in range(8):
                kb.op("pe", lambda e, kc=kc: e.transpose(out=ptb[:, kc * 128:(kc + 1) * 128],
                                                         in_=ybt[:, kc * 128:(kc + 1) * 128], identity=idb[:]),
                      [bR1, bidb], [bpt])
            kb.op("act", lambda e: e.copy(out=yT.rearrange("p a b -> p (a b)"), in_=ptb), [bpt], [bR1])
            for half in range(2):
                pp, bpp = bank(2 + half)
                for kc in range(8):
                    kb.op("pe", lambda e, kc=kc: e.matmul(pp, yT[:, kc, :], wo[:, kc, half * 512:(half + 1) * 512],
                                                          start=(kc == 0), stop=(kc == 7)), [bR1, bwo], [bpp])
                cs = slice(half * 512, (half + 1) * 512)
                kb.op("dve", lambda e: e.tensor_tensor(out=tmpA[:, cs], in0=pp, in1=g1[:, cs], op=ALU.mult),
                      [bpp, bg1], [btmpA])
            kb.op("dve", lambda e: e.tensor_tensor(out=xt[:], in0=tmpA[:], in1=xt[:], op=ALU.add), [btmpA, bxt], [bxt])
            kb.dma("sp", xo[rows, :], xt[:], [bxt], [bxo])
            kb.op("act", lambda e: e.activation(out=junk, in_=xt[:], func=AF.Square, accum_out=st[:, 0:1]),
                  [bxt], [bR2, bst])
            kb.op("act", lambda e: e.activation(out=st[:, 1:2], in_=st[:, 0:1], func=AF.Sqrt, scale=1.0 / D,
                                                bias=epst[:]), [bst, beps], [bst])
            kb.op("dve", lambda e: e.reciprocal(out=st[:, 2:3], in_=st[:, 1:2]), [bst], [bst])
            kb.op("dve", lambda e: e.scalar_tensor_tensor(out=tmpA[:], in0=xt[:], scalar=st[:, 2:3], in1=A2,
                                                          op0=ALU.mult, op1=ALU.mult), [bxt, bst, bA2], [btmpA])
            kb.op("dve", lambda e: e.tensor_tensor(out=tmpB[:], in0=tmpA[:], in1=sh2, op=ALU.add),
                  [btmpA, bsh2], [btmpB])
            kb.op("act", lambda e: e.copy(out=hb, in_=tmpB[:]), [btmpB], [bR2])
            pt, bpt = bank(1)
            ptb = pt.bitcast(BF16)
            for kc in range(8):
                kb.op("pe", lambda e, kc=kc: e.transpose(out=ptb[:, kc * 128:(kc + 1) * 128],
                                                         in_=hb[:, kc * 128:(kc + 1) * 128], identity=idb[:]),
                      [bR2, bidb], [bpt])
            kb.op("act", lambda e: e.copy(out=hT[:, :, rows], in_=ptb.rearrange("p (a b) -> p a b", a=8)),
                  [bpt], [bhT])
            for half in range(2):
                pq, bpq = bank(4 + half)
                for k4 in range(4):
                    kc = half * 4 + k4
                    kb.op("pe", lambda e, kc=kc, k4=k4: e.transpose(out=pq[:, k4 * 128:(k4 + 1) * 128],
                                                                    in_=tmpB[:, kc * 128:(kc + 1) * 128],
                                                                    identity=idf[:]), [btmpB, bidf], [bpq])
                kb.op("dve", lambda e: e.tensor_copy(
                    out=h32T[:, half * 4:(half + 1) * 4, :], in_=pq.rearrange("p (a b) -> p a b", a=4)),
                    [bpq], [bR1])
            pr, bpr = bank(6)
            for kc in range(8):
                kb.op("pe", lambda e, kc=kc: e.matmul(pr[:, 0:NE], h32T[:, kc, :], rwt[:, kc, :],
                                                      start=(kc == 0), stop=(kc == 7)), [bR1, brwt], [bpr])
            kb.op("dve", lambda e: e.tensor_tensor(out=lg[:], in0=pr[:, 0:NE], in1=rbb[:], op=ALU.add),
                  [bpr, brbb], [blg])
            kb.op("dve", lambda e: e.max(out=st[:, 8:16], in_=lg[:]), [blg], [bst])
            kb.op("dve", lambda e: e.tensor_scalar(out=st[:, 16:17], in0=st[:, 8:9], scalar1=-1.0, scalar2=None,
                                                   op0=ALU.mult), [bst], [bst])
            kb.op("act", lambda e: e.activation(out=st[:, 20:24], in_=st[:, 8:12], func=AF.Exp, bias=st[:, 16:17],
                                                accum_out=st[:, 17:18]), [bst], [bst])
            kb.op("dve", lambda e: e.reciprocal(out=st[:, 18:19], in_=st[:, 17:18]), [bst], [bst])
            kb.op("act", lambda e: e.activation(out=st[:, 32:64], in_=lg[:], func=AF.Exp, bias=st[:, 16:17]),
                  [blg, bst], [bst])
            kb.op("dve", lambda e: e.tensor_scalar(out=lg[:], in0=lg[:], scalar1=st[:, 11:12], scalar2=None,
                                                   op0=ALU.is_ge), [blg, bst], [blg])
            kb.op("dve", lambda e: e.scalar_tensor_tensor(out=lg[:], in0=st[:, 32:64], scalar=st[:, 18:19], in1=lg[:],
                                                          op0=ALU.mult, op1=ALU.mult), [bst, blg], [blg])
            pg, bpg = bank(7)
            kb.op("pe", lambda e: e.transpose(out=pg[0:NE, 0:128], in_=lg[:], identity=idf[:]), [blg, bidf], [bpg])
            kb.op("act", lambda e: e.copy(out=gateT[:, rows], in_=pg[0:NE, 0:128]), [bpg], [bgateT])

        kb.dma("sp", hT_o[:, :], R_hT[:].bitcast(BF16), [bhT], [bho])
        kb.dma("sp", gT_o[:, :], gateT[:], [bgateT], [bho])
        kb.dma("sp", g2_o[:, :], g2[:], [bg2], [bho])
    else:
        kb.dma("sp", R_hT[:].bitcast(BF16), hT_i[:, :], [dr], [bhT])
        kb.dma("sp", gateT[:], gT_i[:, :], [dr], [bgateT])
        kb.dma("sp", g2[:], g2_i[:, :], [dr], [bg2])
    merge_deps(bacc, [bwo])
    merge_deps(bactT, [b for _, b in modw_bufs])
    merge_deps(bgsb, [scb_t[1]] + [b for _, b in modb_bufs])
    for _, b in W1R + W2R:
        merge_deps(b, [bg1, bsh2, bA2, bnwb])
    tts = [(tmpA[:, 0:512], Buf("tt0")), (tmpA[:, 512:1024], Buf("tt1"))]
    lps = [(tmpB[:, 0:512], Buf("lp0")), (tmpB[:, 512:1024], Buf("lp1"))]
    lgs = [(xt[:, 0:512], Buf("lg0")), (xt[:, 512:1024], Buf("lg1"))]
    for (_, b), src_ in zip(tts + lps + lgs, [btmpA, btmpA, btmpB, btmpB, bxt, bxt]):
        merge_deps(b, [src_])
    C0 = float(LIMIT * ALPHA / (1.0 + np.exp(-LIMIT * ALPHA)))

    w1_issued = [0]
    w2_issued = [0]

    def issue_w1(upto):
        while w1_issued[0] < min(upto, n_exp * 8):
            i = w1_issued[0]
            t, b = W1R[i % 3]
            kb.dma("pool", t.rearrange("p a b -> p (a b)"), w1[i // 8, i % 8, :, :], [dr], [b])
            w1_issued[0] += 1

    def issue_w2(upto):
        while w2_issued[0] < min(upto, n_exp * 8):
            i = w2_issued[0]
            t, b = W2R[i % 3]
            kb.dma("pool", t.rearrange("p a b -> p (a b)"), w2[i // 8, i % 8, :, :], [dr], [b])
            w2_issued[0] += 1

    nu = 0
    ny = 0
    for e in range(e_lo, e_hi):
        selb, bselb = selbs[e % 2]
        kb.op("dve", lambda en: en.tensor_copy(out=selb[:], in_=idf[0:NE, e:e + 1].to_broadcast([NE, 128])),
              [bidf], [bselb])
        for blk in range(4):
            pgb, bpgb = bank(6)
            kb.op("pe", lambda en, blk=blk: en.matmul(pgb, selb[:], gateT[:, blk * 512:(blk + 1) * 512],
                                                      start=True, stop=True), [bselb, bgateT], [bpgb])
            kb.op("act", lambda en, blk=blk: en.activation(out=gsb[:, blk * 512:(blk + 1) * 512], in_=pgb,
                                                           func=AF.Copy, scale=1.0 / ALPHA), [bpgb], [bgsb])
        for ft in range(8):
            gi = (e - e_lo) * 8 + ft
            issue_w1(gi + 2)
            wv, bwt = W1R[gi % 3]
            bcol = (e * 8 + ft) * 2
            for blk in range(4):
                ts_ = slice(blk * 512, (blk + 1) * 512)
                pgl, bpgl = bank(0 + 2 * (nu % 2))
                pli, bpli = bank(1 + 2 * (nu % 2))
                for kc in range(8):
                    kb.op("pe", lambda en, kc=kc: en.matmul(pgl, wv[:, kc, 0:128], hT[:, kc, ts_],
                                                            start=(kc == 0), stop=(kc == 7)), [bwt, bhT], [bpgl])
                for kc in range(8):
                    kb.op("pe", lambda en, kc=kc: en.matmul(pli, wv[:, kc, 128:256], hT[:, kc, ts_],
                                                            start=(kc == 0), stop=(kc == 7)), [bwt, bhT], [bpli])
                tt, btt = tts[nu % 2]
                lp, blp = lps[nu % 2]
                lgg, blgg = lgs[nu % 2]
                kb.op("act", lambda en: en.activation(out=tt, in_=pgl, func=AF.Silu, scale=ALPHA,
                                                      bias=b1a[:, bcol:bcol + 1]), [bpgl, bb1a], [btt])
                kb.op("dve", lambda en: en.tensor_scalar(out=lp, in0=pli, scalar1=b1a[:, bcol + 1:bcol + 2],
                                                         scalar2=LIMIT + 1.0, op0=ALU.add, op1=ALU.min),
                      [bpli, bb1a], [blp])
                kb.op("dve", lambda en: en.scalar_tensor_tensor(out=lgg, in0=lp, scalar=1.0 - LIMIT, in1=gsb[:, ts_],
                                                                op0=ALU.max, op1=ALU.mult), [blp, bgsb], [blgg])
                kb.op("dve", lambda en: en.scalar_tensor_tensor(out=actT[:, ft, ts_], in0=tt, scalar=C0, in1=lgg,
                                                                op0=ALU.min, op1=ALU.mult), [btt, blgg], [bactT])
                nu += 1
        for dt in range(8):
            gi = (e - e_lo) * 8 + dt
            issue_w2(gi + 2)
            wv, bwt = W2R[gi % 3]
            for blk in range(4):
                ts_ = slice(blk * 512, (blk + 1) * 512)
                py, bpy = bank(4 + (ny % 2))
                for fc in range(8):
                    kb.op("pe", lambda en, fc=fc: en.matmul(py, wv[:, fc, :], actT[:, fc, ts_],
                                                            start=(fc == 0), stop=(fc == 7)), [bwt, bactT], [bpy])
                if e == e_lo:
                    kb.op("dve", lambda en: en.tensor_copy(out=acc[:, dt, ts_], in_=py), [bpy], [bacc])
                else:
                    kb.op("dve", lambda en: en.tensor_tensor(out=acc[:, dt, ts_], in0=py, in1=acc[:, dt, ts_],
                                                             op=ALU.add), [bpy, bacc], [bacc])
                ny += 1

    tf, btf = tts[0]
    for ti in range(NT if do_final else 0):
        rows = slice(ti * 128, (ti + 1) * 128)
        if first:
            kb.dma("sp", yt[:], xo[rows, :], [bxo], [byt])
        else:
            kb.dma("sp", yt[:], xprev[rows, :], [dr], [byt])
        for half in range(2):
            po, bpo = bank(half)
            for d4 in range(4):
                dt = half * 4 + d4
                cs = slice(d4 * 128, (d4 + 1) * 128)
                if first:
                    kb.op("pe", lambda en, dt=dt, cs=cs: en.matmul(po[:, cs], gateT[:, rows],
                                                                  b2t[:, dt * 128:(dt + 1) * 128],
                                                                  start=True, stop=False), [bgateT, bb2t], [bpo])
                kb.op("pe", lambda en, dt=dt, cs=cs: en.matmul(po[:, cs], acc[:, dt, rows], idf[:],
                                                              start=(not first), stop=True), [bacc, bidf], [bpo])
            cs2 = slice(half * 512, (half + 1) * 512)
            kb.op("dve", lambda en: en.tensor_tensor(out=tf, in0=po, in1=g2[:, cs2], op=ALU.mult),
                  [bpo, bg2], [btf])
            kb.op("dve", lambda en: en.tensor_tensor(out=yt[:, cs2], in0=tf, in1=yt[:, cs2], op=ALU.add),
                  [btf, byt], [byt])
        kb.dma("sp", xo[rows, :], yt[:], [byt], [bxo])
    kb.finish([bxo, bho])
    return nc


def prep_l4_weights(w1_l, b1_l, w2_l, b2_l):
    w1r = w1_l.reshape(NE, 8, 128, 8, 128, 2)
    w1r = w1r.transpose(0, 3, 2, 1, 5, 4)
    w1r = np.ascontiguousarray(w1r).reshape(NE, 8, 128, 8 * 256)
    b1r = b1_l.reshape(NE, 8, 128, 2).transpose(2, 0, 1, 3)
    b1r = np.ascontiguousarray(b1r).reshape(128, NE * 16)
    w2r = w2_l.reshape(NE, 8, 128, 8, 128).transpose(0, 3, 2, 1, 4)
    w2r = np.ascontiguousarray(w2r).reshape(NE, 8, 128, 8 * 128)
    return w1r, b1r, w2r, np.ascontiguousarray(b2_l)


def run_l4(x, ycat, c, mod_w_l, mod_b_l, normw_l, w_out_l, rw_l, rb_l, w1r, b1r, w2r, b2_l, splits=((0, 16), (16, 32))):
    ident = np.eye(128, dtype=np.float32)
    xprev = None
    for (lo, hi) in splits:
        first = (lo == 0)
        nc = _get("l4_%d_%d" % (lo, hi), lambda: build_l4(lo, hi, first))
        w1s = np.ascontiguousarray(w1r[lo:hi])
        w2s = np.ascontiguousarray(w2r[lo:hi])
        maps = []
        for cid in range(NCORES):
            m = {"x": core_tokens(x, cid), "ycat": core_tokens(ycat, cid), "cT": cT_of(c, cid // 4),
                 "mod_w": mod_w_l, "mod_b": mod_b_l.reshape(1, -1), "normw": normw_l.reshape(1, -1),
                 "w_out": w_out_l, "rw": rw_l, "rb": rb_l.reshape(1, -1), "w1": w1s, "b1": b1r, "w2": w2s,
                 "b2": b2_l, "ident": ident}
            if not first:
                m["xprev"] = xprev[cid]
                m["hT_i"] = hand[cid]["hT_o"]
                m["gT_i"] = hand[cid]["gT_o"]
                m["g2_i"] = hand[cid]["g2_o"]
            maps.append(m)
        res = _run(nc, maps)
        xprev = [r["xo"] for r in res.results]
        if first:
            hand = res.results
    return uncore_tokens(xprev, (D,))


NIT = 16
KSEL = 256
NEGBIG = -30000.0
NREL = 1280


def build_l3(nj=16):
    nc = bass.Bass("TRN2", target_bir_lowering=False)
    qT = _din(nc, "qT", [128, 16, 4 * 128], BF16)
    kT = _din(nc, "kT", [128, 4 * S], BF16)
    v1 = _din(nc, "v1", [64, 128, 8 * 65], BF16)
    qiT = _din(nc, "qiT", [64, 16, 8 * 128], BF16)
    kiT = _din(nc, "kiT", [64, S], BF16)
    wi = _din(nc, "wi", [128, 128])
    pen = _din(nc, "pen", [128, 512])
    oh = _din(nc, "oh", [32, NREL])
    relb = _din(nc, "relb", [32, 8])
    negI4 = _din(nc, "negI4", [128, 512], BF16)
    ident = _din(nc, "ident", [128, 128])
    yb = _dout(nc, "yb", [TPC, 512])
    scr = nc.dram_tensor("scr", [8, NREL], BF16, kind="Internal").ap()
    kb = KB(nc)
    dr = Buf("dram_in")
    byb = Buf("yb")
    bscr = Buf("scr")

    PS = []
    for i in range(4):
        t = nc.alloc_psum_tensor("PS%d" % i, [128, 1024], F32)
        PS.append((t, Buf("ps%da" % i), Buf("ps%db" % i)))

    def bank(i):
        t, ba, bb = PS[i // 2]
        return (t[:, 0:512], ba) if i % 2 == 0 else (t[:, 512:1024], bb)

    kTt, bkT = kb.sb([128, 4 * S], BF16, "kT")
    for i in range(4):
        kb.dma("sp", kTt[:, i * S:(i + 1) * S], kT[:, i * S:(i + 1) * S], [dr], [bkT])
    kTv = kTt[:].rearrange("p (h s) -> p h s", h=4)
    kibs = [kb.sb([64, 512], BF16, "kib") for _ in range(3)]
    nkb = [0]
    wit, bwi = kb.sb([128, 128], F32, "wi")
    kb.dma("sp", wit[:], wi[:, :], [dr], [bwi])
    pent, bpen = kb.sb([128, 512], F32, "pen")
    kb.dma("sp", pent[:], pen[:, :], [dr], [bpen])
    n4, bn4 = kb.sb([128, 512], BF16, "negI4")
    kb.dma("sp", n4[:], negI4[:, :], [dr], [bn4])
    idf, bidf = kb.sb([128, 128], F32, "identf")
    kb.dma("sp", idf[:], ident[:, :], [dr], [bidf])
    idb, bidb = kb.sb([128, 128], BF16, "identb")
    kb.op("dve", lambda e: e.tensor_copy(out=idb[:], in_=idf[:]), [bidf], [bidb])
    zb, bzb = kb.sb([128, 260], BF16, "zeros")
    kb.op("dve", lambda e: e.memset(zb[:], 0.0), [], [bzb])

    scores = [kb.sb([128, S], F32, "score") for _ in range(2)]
    oht, boh = rview(scores[1][0], 0, [32, NREL], F32), Buf("oh")
    kb.dma("sp", oht, oh[:, :], [dr], [boh])
    rbt, brb = kb.sb([32, 8], F32, "relb")
    kb.dma("sp", rbt[:], relb[:, :], [dr], [brb])
    bv, bbv = rview(scores[1][0], 8192, [8, NREL], BF16), Buf("bvec")
    for i, (c0, n) in enumerate([(0, 512), (512, 512), (1024, 256)]):
        pb, bpb = bank(7)
        kb.op("pe", lambda e: e.matmul(pb[0:8, 0:n], rbt[:], oht[:, c0:c0 + n], start=True, stop=True),
              [brb, boh], [bpb])
        kb.op("dve", lambda e: e.tensor_copy(out=bv[:, c0:c0 + n], in_=pb[0:8, 0:n]), [bpb], [bbv])
    kb.dma("sp", scr[:, :], bv, [bbv], [bscr])
    merge_deps(scores[1][1], [boh, bbv])
    TU, bTU = kb.sb([128, 9, 8 * 128], BF16, "TU")
    for u in range(9):
        src = bass.AP(tensor=scr.tensor, offset=128 * u, ap=[[1, 128], [NREL, 8], [1, 128]])
        kb.dma("sp", TU[:, u, :].rearrange("p (h t) -> p h t", h=8), src, [bscr], [bTU])

    nots = [kb.sb([128, S], BF16, "notsel") for _ in range(2)]
    qts = [kb.sb([128, 512], BF16, "qTj") for _ in range(2)]
    qis = [kb.sb([64, 1024], BF16, "qiTj")] * 2
    dgs = [kb.sb([128, 1024], BF16, "Dg")] * 2
    rhs_ = [kb.sb([128, 512], BF16, "rh") for _ in range(4)]
    pts = [kb.sb([128, 512], BF16, "pt") for _ in range(2)]
    vts = [kb.sb([128, 520], BF16, "v1t") for _ in range(3)]
    outs = [kb.sb([128, 512], F32, "yo")] * 2
    st, bst = kb.sb([128, 32], F32, "st")

    nr = [0]
    nd = [0]
    nv = [0]
    nsc = [0]
    LAG = 3
    vts.append(kb.sb([128, 520], BF16, "v1t"))
    pts2 = [[pts[0], kb.sb([128, 512], BF16, "pt")], [pts[1], kb.sb([128, 512], BF16, "pt")]]

    def idx(j):
        qi_t, bqi = qis[j % 2]
        kb.dma("sp", qi_t[:], qiT[:, j, :], [dr], [bqi])
        dg, bdg = dgs[j % 2]
        for h in range(8):
            kb.op("pool", lambda e, h=h: e.tensor_scalar(out=dg[:, h * 128:(h + 1) * 128], in0=idf[:],
                                                         scalar1=wit[:, j * 8 + h:j * 8 + h + 1], scalar2=None,
                                                         op0=ALU.mult), [bidf, bwi], [bdg])
        score, bscore = scores[j % 2]
        steps = [(sb, h) for sb in range(j + 1) for h in range(8)]
        pend = []
        sbank = {}
        kibt = {}
        for s in range(len(steps) + LAG):
            if s < len(steps):
                sb, h = steps[s]
                if h == 0:
                    kibt[sb] = kibs[nkb[0] % 3]
                    nkb[0] += 1
                    kb.dma("sp", kibt[sb][0][:], kiT[:, sb * 512:(sb + 1) * 512], [dr], [kibt[sb][1]])
                kit, bki = kibt[sb]
                pd, bpd = bank(nd[0] % 4)
                nd[0] += 1
                kb.op("pe", lambda e, h=h, kit=kit: e.matmul(pd, qi_t[:, h * 128:(h + 1) * 128], kit[:],
                                                             start=True, stop=True), [bqi, bki], [bpd])
                rh, brh = rhs_[nr[0] % 4]
                nr[0] += 1
                kb.op("act", lambda e: e.activation(out=rh[:], in_=pd, func=AF.Relu), [bpd], [brh])
                pend.append((sb, h, rh, brh))
            if s - LAG >= 0:
                sb, h, rh, brh = pend[s - LAG]
                if h == 0:
                    sbank[sb] = bank(6 + nsc[0] % 2)
                    nsc[0] += 1
                ps, bps = sbank[sb]
                kb.op("pe", lambda e, h=h: e.matmul(ps, dg[:, h * 128:(h + 1) * 128], rh[:],
                                                    start=(h == 0), stop=(h == 7)), [bdg, brh], [bps])
                if h == 7:
                    kb.op("act", lambda e, sb=sb: e.copy(out=score[:, sb * 512:(sb + 1) * 512], in_=ps),
                          [bps], [bscore])

    def bis(j):
        n = 512 * (j + 1)
        ns, bns = nots[j % 2]
        score, bscore = scores[j % 2]
        sc = score[:, 0:n]
        kb.op("dve", lambda e: e.tensor_reduce(out=st[:, 0:1], in_=sc, axis=AX.X, op=ALU.min), [bscore], [bst])
        kb.op("dve", lambda e: e.tensor_tensor(out=score[:, n - 512:n], in0=score[:, n - 512:n], in1=pent[:],
                                               op=ALU.add), [bscore, bpen], [bscore])
        kb.op("dve", lambda e: e.tensor_reduce(out=st[:, 1:2], in_=sc, axis=AX.X, op=ALU.max), [bscore], [bst])
        kb.op("dve", lambda e: e.tensor_tensor(out=st[:, 2:3], in0=st[:, 1:2], in1=st[:, 0:1], op=ALU.subtract),
              [bst], [bst])
        kb.op("dve", lambda e: e.scalar_tensor_tensor(out=st[:, 3:4], in0=st[:, 2:3], scalar=-0.01, in1=st[:, 0:1],
                                                      op0=ALU.mult, op1=ALU.add), [bst], [bst])
        kb.op("dve", lambda e: e.tensor_scalar(out=st[:, 3:4], in0=st[:, 3:4], scalar1=-1e-6, scalar2=None,
                                               op0=ALU.add), [bst], [bst])
        kb.op("dve", lambda e: e.tensor_tensor(out=st[:, 4:5], in0=st[:, 1:2], in1=st[:, 3:4], op=ALU.subtract),
              [bst], [bst])
        kb.op("dve", lambda e: e.scalar_tensor_tensor(out=st[:, 5:6], in0=st[:, 4:5], scalar=0.5, in1=st[:, 3:4],
                                                      op0=ALU.mult, op1=ALU.add), [bst], [bst])
        kb.op("dve", lambda e: e.tensor_scalar(out=st[:, 6:7], in0=st[:, 4:5], scalar1=0.25, scalar2=None,
                                               op0=ALU.mult), [bst], [bst])
        for it in range(NIT):
            kb.op("dve", lambda e: e.tensor_scalar(out=ns[:, 0:n], in0=sc, scalar1=st[:, 5:6], scalar2=None,
                                                   op0=ALU.is_ge, op1=ALU.add, accum_out=st[:, 7:8]),
                  [bscore, bst], [bns, bst])
            kb.op("dve", lambda e: e.tensor_scalar(out=st[:, 8:9], in0=st[:, 7:8], scalar1=KSEL - 0.5, scalar2=2.0,
                                                   op0=ALU.is_ge, op1=ALU.mult), [bst], [bst])
            kb.op("dve", lambda e: e.scalar_tensor_tensor(out=st[:, 9:10], in0=st[:, 8:9], scalar=-1.0,
                                                          in1=st[:, 6:7], op0=ALU.add, op1=ALU.mult), [bst], [bst])
            kb.op("dve", lambda e: e.tensor_tensor(out=st[:, 5:6], in0=st[:, 5:6], in1=st[:, 9:10], op=ALU.add),
                  [bst], [bst])
            kb.op("dve", lambda e: e.tensor_scalar(out=st[:, 6:7], in0=st[:, 6:7], scalar1=0.5, scalar2=None,
                                                   op0=ALU.mult), [bst], [bst])
        kb.op("dve", lambda e: e.scalar_tensor_tensor(out=st[:, 10:11], in0=st[:, 6:7], scalar=-4.0, in1=st[:, 5:6],
                                                      op0=ALU.mult, op1=ALU.add), [bst], [bst])
        kb.op("dve", lambda e: e.tensor_scalar(out=ns[:, 0:n], in0=sc, scalar1=st[:, 10:11], scalar2=None,
                                               op0=ALU.is_lt), [bscore, bst], [bns])

    def att_main(j):
        ns, bns = nots[j % 2]
        qt, bqt = qts[j % 2]
        kb.dma("sp", qt[:], qT[:, j, :], [dr], [bqt])
        oacc = [bank(4), bank(5)]
        for g in range(2):
            oa, boa = oacc[g]
            kb.op("pe", lambda e: e.matmul(oa[:, 0:260], idb[:], zb[:], start=True, stop=False),
                  [bidb, bzb], [boa])
        ntile = 4 * j + 4
        vtl = {}
        for stl in range(ntile + 1):
            if stl < ntile:
                vt, bvt = vts[nv[0] % 4]
                nv[0] += 1
                vtl[stl] = (vt, bvt)
                kb.dma("sp", vt[:], v1[stl, :, :], [dr], [bvt])
                u = stl - 4 * j + 5
                for g in range(2):
                    lgp, blgp = bank(2 * g + stl % 2)
                    kb.op("pe", lambda e: e.matmul(lgp, ns[:, stl * 128:(stl + 1) * 128], n4[:], start=True,
                                                   stop=False), [bns, bn4], [blgp])
                    if u >= 0:
                        kb.op("pe", lambda e, u=u: e.matmul(lgp, idb[:], TU[:, u, g * 512:(g + 1) * 512],
                                                            start=False, stop=False), [bidb, bTU], [blgp])
                    for hq in range(4):
                        kb.op("pe", lambda e, hq=hq: e.matmul(lgp[:, hq * 128:(hq + 1) * 128],
                                                              kTv[g * 64:(g + 1) * 64, hq, stl * 128:(stl + 1) * 128],
                                                              qt[g * 64:(g + 1) * 64, hq * 128:(hq + 1) * 128],
                                                              start=False, stop=(hq == 3)), [bkT, bqt], [blgp])
                    pt, bpt = pts2[g][stl % 2]
                    kb.op("act", lambda e: e.activation(out=pt[:], in_=lgp, func=AF.Exp), [blgp], [bpt])
            if stl >= 1:
                sp_ = stl - 1
                vt, bvt = vtl[sp_]
                for g in range(2):
                    pt, bpt = pts2[g][sp_ % 2]
                    oa, boa = oacc[g]
                    for hq in range(4):
                        h = g * 4 + hq
                        kb.op("pe", lambda e, hq=hq, h=h: e.matmul(oa[:, hq * 65:(hq + 1) * 65],
                                                                   pt[:, hq * 128:(hq + 1) * 128],
                                                                   vt[:, h * 65:(h + 1) * 65],
                                                                   start=False, stop=(sp_ == ntile - 1)),
                              [bpt, bvt], [boa])

    def att_fin(j):
        oacc = [bank(4), bank(5)]
        yo, byo = outs[j % 2]
        for g in range(2):
            oa, boa = oacc[g]
            oav = oa[:, 0:260].rearrange("p (h c) -> p h c", h=4)
            kb.op("dve", lambda e: e.reciprocal(out=st[:, 16 + g * 4:20 + g * 4],
                                                in_=oav[:, :, 64:65].rearrange("p h c -> p (h c)")), [boa], [bst])
            kb.op("dve", lambda e: e.tensor_tensor(
                out=yo[:, g * 256:(g + 1) * 256].rearrange("p (h d) -> p h d", h=4), in0=oav[:, :, 0:64],
                in1=st[:, 16 + g * 4:20 + g * 4].unsqueeze(2).to_broadcast([128, 4, 64]), op=ALU.mult),
                [boa, bst], [byo])
        kb.dma("sp", yb[j * 128:(j + 1) * 128, :], yo[:], [byo], [byb])

    idx(0)
    if nj > 1:
        idx(1)
    bis(0)
    for j in range(nj):
        if j + 2 < nj:
            idx(j + 2)
        att_main(j)
        if j + 1 < nj:
            bis(j + 1)
        att_fin(j)
    kb.finish([byb])
    return nc


def t5_bucket_np(rel):
    nb = 16
    max_exact = 8
    side = np.where(rel > 0, nb, 0)
    n = np.abs(rel)
    nf = np.maximum(n, 1).astype(np.float32)
    large = max_exact + (np.log(nf / max_exact) / np.float32(np.log(1024 / max_exact)) * (nb - max_exact)).astype(np.int32)
    large = np.minimum(large, nb - 1)
    return side + np.where(n < max_exact, n, large)


def prep_l3(ob, of, cid):
    b, r = cid // 4, cid % 4
    bf = ob.dtype
    qsel = ob[b].reshape(64, 128, NB1)[r::4][:, ::-1]
    q = qsel[..., 0:512].reshape(16, 128, 2, 4, 64)
    qT = np.ascontiguousarray(q.transpose(2, 4, 0, 3, 1)).reshape(128, 16, 512)
    qi = qsel[..., 1536:2048].reshape(16, 128, 8, 64)
    qiT = np.ascontiguousarray(qi.transpose(3, 0, 2, 1)).reshape(64, 16, 1024)
    wsel = of[b].reshape(64, 128, NF1)[r::4][:, ::-1, 2056:2064]
    wi = np.ascontiguousarray(wsel.transpose(1, 0, 2)).reshape(128, 128).astype(np.float32)
    k = ob[b, :, 512:1024].reshape(S, 2, 4, 64)
    kT = np.ascontiguousarray(k.transpose(1, 3, 2, 0)).reshape(128, 4 * S)
    kiT = np.ascontiguousarray(ob[b, :, 2048:2112].T)
    v = ob[b, :, 1024:1536].reshape(64, 128, 8, 64)
    v1 = np.ones((64, 128, 8, 65), bf)
    v1[..., 0:64] = v
    v1 = v1.reshape(64, 128, 520)
    tq = np.arange(128)[:, None]
    sk = np.arange(512)[None, :]
    pen = np.where((sk // 64) <= 2 * r + (tq < 64), 0.0, -1e30).astype(np.float32)
    m = np.arange(NREL)
    rel = m - 767 - 128 * r
    bk = t5_bucket_np(rel)
    oh = np.zeros((32, NREL), np.float32)
    oh[bk, m] += 1.0
    oh[15, :] -= 1.0
    negI4 = np.tile(np.eye(128, dtype=np.float32) * NEGBIG, (1, 4)).astype(bf)
    return {"qT": qT, "kT": kT, "v1": v1, "qiT": qiT, "kiT": kiT, "wi": wi, "pen": pen, "oh": oh,
            "negI4": negI4, "ident": np.eye(128, dtype=np.float32)}


def run_l3(ob, of, rel_bias, nj=16):
    nc = _get("l3_%d" % nj, lambda: build_l3(nj))
    maps = []
    for cid in range(NCORES):
        m = prep_l3(ob, of, cid)
        m["relb"] = np.ascontiguousarray(rel_bias.astype(np.float32))
        maps.append(m)
    res = _run(nc, maps)
    parts = [r["yb"].reshape(16, 128, 512)[:, ::-1].reshape(TPC, 512) for r in res.results]
    return uncore_tokens(parts, (512,))


NCH = 64
PRE_STOP = 0
L2VAR = 0
POOL_ENG = "dve"


def build_l2(nch=NCH, stop=None):
    nc = bass.Bass("TRN2", target_bir_lowering=False)
    xin = _din(nc, "xin", [128, 3, S + 3])
    cw = _din(nc, "cw", [128, 12])
    zin = _din(nc, "zin", [128, NCH, 128])
    bcol = _din(nc, "bcol", [128, NCH])
    acol = _din(nc, "acol", [128, NCH])
    sc3 = _din(nc, "sc3", [1, 2])
    gnw = _din(nc, "gnw", [1, 128])
    cst = _din(nc, "cst", [128, 7, 128])
    ya = _dout(nc, "ya", [S, 128])
    kb = KB(nc)
    dr = Buf("dram_in")
    bya = Buf("ya")

    PSW = []
    for i in range(4):
        t = nc.alloc_psum_tensor("PS%d" % i, [128, 1024], F32)
        PSW.append(t)
    slots = []
    for bnk in range(6):
        t = PSW[bnk // 2]
        c0 = (bnk % 2) * 512
        slots.append((t[:, c0:c0 + 128], Buf("slot%d" % bnk)))
    wide = [(PSW[3][:, 0:512], Buf("wide0")), (PSW[3][:, 512:1024], Buf("wide1"))]
    nslot = [0]

    def slot():
        s = slots[nslot[0] % len(slots)]
        nslot[0] += 1
        return s

    ct, bct = kb.sb([128, 7, 128], F32, "cst")
    kb.dma("sp", ct[:], cst[:, :, :], [dr], [bct])
    ident, LT, ones, negones, penL, SM, sel127 = [ct[:, i, :] for i in range(7)]
    cwt, bcw = kb.sb([128, 12], F32, "cw")
    kb.dma("sp", cwt[:], cw[:, :], [dr], [bcw])
    gnb, bgnb = kb.sb([128, 128], F32, "gnw")
    kb.dma("sp", gnb[:], gnw[0:1, :].to_broadcast([128, 128]), [dr], [bgnb])
    s3, bs3 = kb.sb([128, 2], F32, "sc3")
    kb.dma("sp", s3[:], sc3[0:1, :].to_broadcast([128, 2]), [dr], [bs3])
    epst, beps = kb.sb([128, 3], F32, "eps")
    kb.op("dve", lambda e: e.memset(epst[:, 0:1], EPS), [], [beps])
    kb.op("dve", lambda e: e.memset(epst[:, 1:2], 128.0 * EPS), [], [beps])
    kb.op("dve", lambda e: e.memset(epst[:, 2:3], 1.0), [], [beps])

    cols, bcols = kb.sb([128, 10, NCH], F32, "cols")
    BETA, G, GC, EGC, BG, KD, EGL, NB_, TMP, TMP2 = range(10)
    kb.dma("sp", cols[:, BETA, :], bcol[:, :], [dr], [bcols])
    kb.dma("sp", cols[:, TMP, :], acol[:, :], [dr], [bcols])
    kb.op("act", lambda e: e.activation(out=cols[:, BETA, :], in_=cols[:, BETA, :], func=AF.Sigmoid), [bcols], [bcols])
    kb.op("act", lambda e: e.activation(out=cols[:, TMP, :], in_=cols[:, TMP, :], func=AF.Exp, bias=s3[:, 1:2]),
          [bcols, bs3], [bcols])
    kb.op("act", lambda e: e.activation(out=cols[:, TMP, :], in_=cols[:, TMP, :], func=AF.Ln, bias=epst[:, 2:3]),
          [bcols, beps], [bcols])
    kb.op("act", lambda e: e.activation(out=s3[:, 0:1], in_=s3[:, 0:1], func=AF.Exp), [bs3], [bs3])
    kb.op("dve", lambda e: e.tensor_scalar(out=cols[:, G, :], in0=cols[:, TMP, :], scalar1=s3[:, 0:1], scalar2=-1.0,
                                           op0=ALU.mult, op1=ALU.mult), [bcols, bs3], [bcols])
    pg, bpg = slot()
    kb.op("pe", lambda e: e.matmul(pg[:, 0:NCH], LT, cols[:, G, :], start=True, stop=True), [bct, bcols], [bpg])
    kb.op("dve", lambda e: e.tensor_copy(out=cols[:, GC, :], in_=pg[:, 0:NCH]), [bpg], [bcols])
    pg2, bpg2 = slot()
    kb.op("pe", lambda e: e.matmul(pg2[:, 0:NCH], sel127, cols[:, GC, :], start=True, stop=True), [bct, bcols], [bpg2])
    kb.op("dve", lambda e: e.tensor_copy(out=cols[:, TMP, :], in_=pg2[:, 0:NCH]), [bpg2], [bcols])
    kb.op("act", lambda e: e.activation(out=cols[:, EGL, :], in_=cols[:, TMP, :], func=AF.Exp), [bcols], [bcols])
    kb.op("dve", lambda e: e.tensor_tensor(out=cols[:, TMP2, :], in0=cols[:, TMP, :], in1=cols[:, GC, :], op=ALU.subtract),
          [bcols], [bcols])
    kb.op("act", lambda e: e.activation(out=cols[:, KD, :], in_=cols[:, TMP2, :], func=AF.Exp), [bcols], [bcols])
    kb.op("act", lambda e: e.activation(out=cols[:, EGC, :], in_=cols[:, GC, :], func=AF.Exp), [bcols], [bcols])
    kb.op("dve", lambda e: e.tensor_tensor(out=cols[:, BG, :], in0=cols[:, EGC, :], in1=cols[:, BETA, :], op=ALU.mult),
          [bcols], [bcols])
    kb.op("dve", lambda e: e.tensor_scalar(out=cols[:, NB_, :], in0=cols[:, BETA, :], scalar1=-1.0, scalar2=None,
                                           op0=ALU.mult), [bcols], [bcols])

    if stop == "cols":
        kb.dma("sp", ya[0:128, 0:NCH], cols[:, GC, :], [bcols], [bya])
        kb.finish([bya])
        return nc
    QT, bQT = kb.sb([128, S], F32, "QT")
    KT, bKT = kb.sb([128, S], F32, "KT")
    Vtok, bVtok = kb.sb([128, NCH, 128], F32, "Vtok")
    Ktok, bKtok = kb.sb([128, NCH, 128], F32, "Ktok")
    xbs = [kb.sb([128, 3, 515], F32, "xb") for _ in range(2)]
    u, bu = kb.sb([128, 3, 512], F32, "u")
    sqt, bsq = kb.sb([128, 512], F32, "sq")
    rs, brs = kb.sb([128, 512], F32, "rs")
    nblk = (nch * 128 + 511) // 512
    for blk in range(nblk):
        xb, bxb = xbs[blk % 2]
        kb.dma("sp", xb[:], xin[:, :, blk * 512:blk * 512 + 515], [dr], [bxb])
        for a in range(3):
            kb.op("dve", lambda e, a=a: e.tensor_scalar(out=u[:, a, :], in0=xb[:, a, 0:512],
                                                        scalar1=cwt[:, a * 4:a * 4 + 1], scalar2=None, op0=ALU.mult),
                  [bxb, bcw], [bu])
            for tap in range(1, 4):
                kb.op("dve", lambda e, a=a, tap=tap: e.scalar_tensor_tensor(
                    out=u[:, a, :], in0=xb[:, a, tap:tap + 512], scalar=cwt[:, a * 4 + tap:a * 4 + tap + 1],
                    in1=u[:, a, :], op0=ALU.mult, op1=ALU.add), [bxb, bcw, bu], [bu])
        kb.op("act", lambda e: e.activation(out=u[:].rearrange("p a n -> p (a n)"),
                                            in_=u[:].rearrange("p a n -> p (a n)"), func=AF.Silu), [bu], [bu])
        cs = slice(blk * 512, (blk + 1) * 512)
        for a, (dst, bdst, scl, epi) in enumerate([(QT, bQT, 128.0, 1), (KT, bKT, 1.0, 0)]):
            kb.op("dve", lambda e, a=a: e.tensor_tensor(out=sqt[:], in0=u[:, a, :], in1=u[:, a, :], op=ALU.mult),
                  [bu], [bsq])
            pw, bpw = wide[a]
            kb.op("pe", lambda e: e.matmul(pw, ones, sqt[:], start=True, stop=True), [bct, bsq], [bpw])
            kb.op("act", lambda e, scl=scl, epi=epi: e.activation(out=rs[:], in_=pw, func=AF.Sqrt, scale=scl,
                                                                  bias=epst[:, epi:epi + 1]), [bpw, beps], [brs])
            kb.op("dve", lambda e: e.reciprocal(out=rs[:], in_=rs[:]), [brs], [brs])
            kb.op("dve", lambda e, a=a, dst=dst: e.tensor_tensor(out=dst[:, cs], in0=u[:, a, :], in1=rs[:], op=ALU.mult),
                  [bu, brs], [bdst])
        for q4 in range(4):
            ch = blk * 4 + q4
            if ch >= nch:
                break
            pk, bpk = slot()
            kb.op("pe", lambda e, ch=ch: e.transpose(out=pk, in_=KT[:, ch * 128:(ch + 1) * 128], identity=ident),
                  [bKT, bct], [bpk])
            kb.op("act", lambda e, ch=ch: e.copy(out=Ktok[:, ch, :], in_=pk), [bpk], [bKtok])
            pv, bpv = slot()
            kb.op("pe", lambda e, q4=q4: e.transpose(out=pv, in_=u[:, 2, q4 * 128:(q4 + 1) * 128], identity=ident),
                  [bu, bct], [bpv])
            kb.op("act", lambda e, ch=ch: e.copy(out=Vtok[:, ch, :], in_=pv), [bpv], [bVtok])

    if stop == "prep":
        kb.dma("sp", ya[0:128, :], Ktok[:, 0, :], [bKtok], [bya])
        kb.dma("sp", ya[128:256, :], Vtok[:, 0, :], [bVtok], [bya])
        kb.dma("sp", ya[256:384, :], QT[:, 0:128], [bQT], [bya])
        kb.finish([bya])
        return nc
    RING = 4
    ring = [dict(wdT=kb.sb([128, 128], F32, "wdT"), uval=kb.sb([128, 128], F32, "uval"),
                 attnT=kb.sb([128, 128], F32, "attnT"), kdec=kb.sb([128, 128], F32, "kdec")) for _ in range(RING)]
    tmps = {}

    def tmp(name, k=2):
        if name not in tmps:
            tmps[name] = [kb.sb([128, 128], F32, name) for _ in range(k)]
            tmps[name + "_i"] = 0
        i = tmps[name + "_i"]
        tmps[name + "_i"] = i + 1
        return tmps[name][i % k]

    def pre(n):
        par = "_%d" % (n % 2)
        R = ring[n % RING]
        kt = KT[:, n * 128:(n + 1) * 128]
        qt = QT[:, n * 128:(n + 1) * 128]
        dg, bdg = tmp("diag" + par)
        kb.op("dve", lambda e: e.tensor_scalar(out=dg[:], in0=ident, scalar1=cols[:, GC, n:n + 1], scalar2=None,
                                               op0=ALU.mult), [bct, bcols], [bdg])
        pD, bpD = slot()
        kb.op("pe", lambda e: e.matmul(pD, dg[:], ones, start=True, stop=False), [bdg, bct], [bpD])
        kb.op("pe", lambda e: e.matmul(pD, negones, dg[:], start=False, stop=True), [bdg, bct], [bpD])
        Dl, bDl = tmp("Dl" + par)
        E, bE = tmp("E" + par)
        kb.op("dve", lambda e: e.tensor_tensor(out=Dl[:], in0=pD, in1=penL, op=ALU.min), [bpD, bct], [bDl])
        kb.op("act", lambda e: e.activation(out=E[:], in_=Dl[:], func=AF.Exp), [bDl], [bE])
        yield
        Es, bEs = tmp("Es" + par)
        kb.op(POOL_ENG, lambda e: e.tensor_tensor(out=Es[:], in0=E[:], in1=SM, op=ALU.mult), [bE, bct], [bEs])
        pA, bpA = slot()
        kb.op("pe", lambda e: e.matmul(pA, kt, kt, start=True, stop=True), [bKT], [bpA])
        Nm, bN = tmp("N" + par, 3)
        kb.op("dve", lambda e: e.scalar_tensor_tensor(out=Nm[:], in0=pA, scalar=cols[:, NB_, n:n + 1], in1=Es[:],
                                                      op0=ALU.mult, op1=ALU.mult), [bpA, bcols, bEs], [bN])
        yield
        pM, bpM = slot()
        kb.op("pe", lambda e: e.transpose(out=pM, in_=Nm[:], identity=ident), [bN, bct], [bpM])
        Mm, bM = tmp("M" + par, 3)
        kb.op("act", lambda e: e.copy(out=Mm[:], in_=pM), [bpM], [bM])
        P, bP = tmp("P" + par, 3)
        kb.op("dve", lambda e: e.tensor_tensor(out=P[:], in0=Mm[:], in1=ident, op=ALU.add), [bM, bct], [bP])
        yield
        pQK, bpQK = slot()
        kb.op("pe", lambda e: e.matmul(pQK, qt, kt, start=True, stop=True), [bQT, bKT], [bpQK])
        at, bat = tmp("attn" + par)
        kb.op("dve", lambda e: e.tensor_tensor(out=at[:], in0=pQK, in1=E[:], op=ALU.mult), [bpQK, bE], [bat])
        pAT, bpAT = slot()
        kb.op("pe", lambda e: e.transpose(out=pAT, in_=at[:], identity=ident), [bat, bct], [bpAT])
        aT, baT = R["attnT"]
        kb.op("act", lambda e: e.copy(out=aT[:], in_=pAT), [bpAT], [baT])
        yield
        for lev in range(1, 7):
            pN2, bpN2 = slot()
            kb.op("pe", lambda e: e.matmul(pN2, Mm[:], Nm[:], start=True, stop=True), [bM, bN], [bpN2])
            N2, bN2 = tmp("N" + par, 3)
            kb.op("act", lambda e: e.copy(out=N2[:], in_=pN2), [bpN2], [bN2])
            if lev < 6:
                pM2, bpM2 = slot()
                kb.op("pe", lambda e: e.matmul(pM2, Nm[:], Mm[:], start=True, stop=True), [bM, bN], [bpM2])
                M2, bM2 = tmp("M" + par, 3)
                kb.op("act", lambda e: e.copy(out=M2[:], in_=pM2), [bpM2], [bM2])
            yield
            pP, bpP = slot()
            kb.op("pe", lambda e: e.matmul(pP, N2[:], P[:], start=True, stop=True), [bN2, bP], [bpP])
            P2, bP2 = tmp("P" + par, 3)
            kb.op("dve", lambda e: e.tensor_tensor(out=P2[:], in0=pP, in1=P[:], op=ALU.add), [bpP, bP], [bP2])
            P, bP = P2, bP2
            yield
            Nm, bN = N2, bN2
            if lev < 6:
                Mm, bM = M2, bM2
        yield
        kbg, bkbg = tmp("kbg" + par)
        kb.op(POOL_ENG, lambda e: e.tensor_scalar(out=kbg[:], in0=Ktok[:, n, :], scalar1=cols[:, BG, n:n + 1],
                                                scalar2=None, op0=ALU.mult), [bKtok, bcols], [bkbg])
        vb, bvb = tmp("vb" + par)
        kb.op(POOL_ENG, lambda e: e.tensor_scalar(out=vb[:], in0=Vtok[:, n, :], scalar1=cols[:, BETA, n:n + 1],
                                                scalar2=None, op0=ALU.mult), [bVtok, bcols], [bvb])
        kd, bkd = R["kdec"]
        kb.op(POOL_ENG, lambda e: e.tensor_scalar(out=kd[:], in0=Ktok[:, n, :], scalar1=cols[:, KD, n:n + 1],
                                                scalar2=None, op0=ALU.mult), [bKtok, bcols], [bkd])
        pW, bpW = slot()
        kb.op("pe", lambda e: e.matmul(pW, kbg[:], P[:], start=True, stop=True), [bkbg, bP], [bpW])
        wd, bwd = R["wdT"]
        kb.op("act", lambda e: e.copy(out=wd[:], in_=pW), [bpW], [bwd])
        pU, bpU = slot()
        kb.op("pe", lambda e: e.matmul(pU, P[:], vb[:], start=True, stop=True), [bP, bvb], [bpU])
        uv, buv = R["uval"]
        kb.op("act", lambda e: e.copy(out=uv[:], in_=pU), [bpU], [buv])

    states = [kb.sb([128, 128], F32, "state") for _ in range(2)]
    kb.op("dve", lambda e: e.memset(states[0][0][:], 0.0), [], [states[0][1]])
    zts = [kb.sb([128, 128], F32, "zt") for _ in range(2)]
    st, bst = kb.sb([128, 8], F32, "st")
    junk, bjunk = kb.sb([128, 128], F32, "junk")

    def scan(n):
        R = ring[n % RING]
        wd, bwd = R["wdT"]
        uv, buv = R["uval"]
        aT, baT = R["attnT"]
        kd, bkd = R["kdec"]
        S0, bS0 = states[n % 2]
        S1, bS1 = states[(n + 1) % 2]
        zt, bzt = zts[n % 2]
        kb.dma("sp", zt[:], zin[:, n, :], [dr], [bzt])
        ppv, bppv = slot()
        kb.op("pe", lambda e: e.matmul(ppv, wd[:], S0[:], start=True, stop=True), [bwd, bS0], [bppv])
        po1, bpo1 = wide[0][0][:, 0:128], wide[0][1]
        kb.op("pe", lambda e: e.matmul(po1, QT[:, n * 128:(n + 1) * 128], S0[:], start=True, stop=True),
              [bQT, bS0], [bpo1])
        vn, bvn = tmp("vnew")
        kb.op("dve", lambda e: e.tensor_tensor(out=vn[:], in0=uv[:], in1=ppv, op=ALU.subtract), [buv, bppv], [bvn])
        yield
        psu, bpsu = slot()
        kb.op("pe", lambda e: e.matmul(psu, kd[:], vn[:], start=True, stop=True), [bkd, bvn], [bpsu])
        po2, bpo2 = slot()
        kb.op("pe", lambda e: e.matmul(po2, aT[:], vn[:], start=True, stop=True), [baT, bvn], [bpo2])
        kb.op("dve", lambda e: e.scalar_tensor_tensor(out=S1[:], in0=S0[:], scalar=cols[:, EGL, n:n + 1], in1=psu,
                                                      op0=ALU.mult, op1=ALU.add), [bS0, bcols, bpsu], [bS1])
        o2, bo2 = tmp("o2")
        kb.op("act", lambda e: e.copy(out=o2[:], in_=po2), [bpo2], [bo2])
        o, bo = tmp("o")
        kb.op("dve", lambda e: e.scalar_tensor_tensor(out=o[:], in0=po1, scalar=cols[:, EGC, n:n + 1], in1=o2[:],
                                                      op0=ALU.mult, op1=ALU.add), [bpo1, bcols, bo2], [bo])
        yield
        kb.op("act", lambda e: e.activation(out=junk[:], in_=o[:], func=AF.Square, accum_out=st[:, 0:1]),
              [bo], [bjunk, bst])
        kb.op("act", lambda e: e.activation(out=st[:, 1:2], in_=st[:, 0:1], func=AF.Ln, scale=1.0 / 128,
                                            bias=epst[:, 0:1]), [bst, beps], [bst])
        kb.op("act", lambda e: e.activation(out=st[:, 2:3], in_=st[:, 1:2], func=AF.Exp, scale=-0.5), [bst], [bst])
        sg, bsg = tmp("sg")
        kb.op("act", lambda e: e.activation(out=sg[:], in_=zt[:], func=AF.Exp, scale=-1.0), [bzt], [bsg])
        kb.op(POOL_ENG, lambda e: e.tensor_scalar(out=sg[:], in0=sg[:], scalar1=1.0, scalar2=None, op0=ALU.add),
              [bsg], [bsg])
        kb.op("dve", lambda e: e.reciprocal(out=sg[:], in_=sg[:]), [bsg], [bsg])
        kb.op(POOL_ENG, lambda e: e.tensor_tensor(out=sg[:], in0=sg[:], in1=zt[:], op=ALU.mult), [bsg, bzt], [bsg])
        yield
        t1, bt1 = tmp("t1")
        kb.op("dve", lambda e: e.scalar_tensor_tensor(out=t1[:], in0=o[:], scalar=st[:, 2:3], in1=gnb[:],
                                                      op0=ALU.mult, op1=ALU.mult), [bo, bst, bgnb], [bt1])
        yt_, byt_ = tmp("yout")
        kb.op("dve", lambda e: e.tensor_tensor(out=yt_[:], in0=t1[:], in1=sg[:], op=ALU.mult), [bt1, bsg], [byt_])
        kb.dma("sp", ya[n * 128:(n + 1) * 128, :], yt_[:], [byt_], [bya])

    def drive(gens):
        gens = list(gens)
        while gens:
            for g in list(gens):
                try:
                    next(g)
                except StopIteration:
                    gens.remove(g)

    def chain(*gs):
        for g in gs:
            yield from g

    if stop == "alloc":
        kb.dma("sp", ya[0:128, :], states[0][0][:], [states[0][1]], [bya])
        kb.finish([bya])
        return nc
    drive([pre(n) for n in range(min(2, nch))])
    for n in range(0, nch, 2):
        gens = [pre(m) for m in (n + 2, n + 3) if m < nch]
        gens.append(chain(*[scan(m) for m in (n, n + 1) if m < nch]))
        drive(gens)
    kb.finish([bya])
    return nc


def l2_consts():
    i = np.arange(128)
    ident = np.eye(128, dtype=np.float32)
    LT = (i[:, None] <= i[None, :]).astype(np.float32)
    ones = np.ones((128, 128), np.float32)
    penL = np.where(i[:, None] >= i[None, :], 0.0, -1e30).astype(np.float32)
    SM = (i[:, None] > i[None, :]).astype(np.float32)
    sel = np.zeros((128, 128), np.float32)
    sel[127, :] = 1.0
    return np.ascontiguousarray(np.stack([ident, LT, ones, -ones, penL, SM, sel], axis=1))


def run_l2(of, conv_w_l, a_log_l, dt_bias_l, gnw_l, nch=NCH, stop=None):
    nc = _get("l2_%d_%s" % (nch, stop), lambda: build_l2(nch, stop))
    cst = l2_consts()
    maps = []
    for cid in range(NCORES):
        b, g = cid // 4, cid % 4
        xs = []
        cws = []
        for a in range(3):
            cols_ = slice(a * 512 + g * 128, a * 512 + (g + 1) * 128)
            xa = np.zeros((128, S + 3), np.float32)
            xa[:, 3:] = of[b, :, cols_].T
            xs.append(xa)
            cws.append(conv_w_l[:, cols_].T)
        xin = np.ascontiguousarray(np.stack(xs, axis=1))
        cw = np.ascontiguousarray(np.concatenate(cws, axis=1)).astype(np.float32)
        z = of[b, :, 1536 + g * 128:1536 + (g + 1) * 128].reshape(NCH, 128, 128).transpose(1, 0, 2)
        bc = of[b, :, 2048 + g].reshape(NCH, 128).T
        ac = of[b, :, 2052 + g].reshape(NCH, 128).T
        maps.append({"xin": xin, "cw": cw, "zin": np.ascontiguousarray(z), "bcol": np.ascontiguousarray(bc),
                     "acol": np.ascontiguousarray(ac),
                     "sc3": np.array([[a_log_l[g], dt_bias_l[g]]], np.float32),
                     "gnw": gnw_l.reshape(1, 128).astype(np.float32), "cst": cst})
    res = _run(nc, maps)
    ya = np.zeros((NB, S, 512), np.float32)
    for cid in range(NCORES):
        b, g = cid // 4, cid % 4
        ya[b, :, g * 128:(g + 1) * 128] = res.results[cid]["ya"]
    return ya


def kernel(x, c, rel_bias, mod_w, mod_b, norm_mix_w, norm_ffn_w, w_in, conv_w, a_log, dt_bias,
           gdn_norm_w, q_norm_w, k_norm_w, w_out, router_w, router_b, w1, b1, w2, b2):
    f = lambda a: np.ascontiguousarray(np.asarray(a), dtype=np.float32)
    x = f(x)
    c = f(c)
    rel_bias = f(rel_bias)
    for l in range(2):
        of, ob = run_l1(x, c, f(mod_w[l]), f(mod_b[l]), f(norm_mix_w[l]), f(w_in[l]), f(q_norm_w[l]), f(k_norm_w[l]))
        ya = run_l2(of, f(conv_w[l]), f(a_log[l]), f(dt_bias[l]), f(gdn_norm_w[l]))
        yb = run_l3(ob, of, rel_bias)
        ycat = np.ascontiguousarray(np.concatenate([ya, yb], axis=-1))
        w1r, b1r, w2r, b2r = prep_l4_weights(f(w1[l]), f(b1[l]), f(w2[l]), f(b2[l]))
        x = run_l4(x, ycat, c, f(mod_w[l]), f(mod_b[l]), f(norm_ffn_w[l]), f(w_out[l]), f(router_w[l]),
                   f(router_b[l]), w1r, b1r, w2r, b2r)
    return x


CAP = 1024
ESTRIDE = CAP + 128
TRASH = NE * ESTRIDE


def _dma_ind(kb, out, out_off, in_, in_off, reads, writes):
    dst = writes[0]
    if dst.dsem is None:
        kb.nsem += 1
        key = "d%d_%s" % (kb.nsem, dst.name)
        dst.dsem = key
        kb.sems[key] = kb.nc.alloc_semaphore(key[:40])
    deps = kb._deps(reads, writes)
    kb._wait("pool", deps)
    ins = kb.nc.gpsimd.indirect_dma_start(out=out, out_offset=out_off, in_=in_, in_offset=in_off)
    dst.dcnt += 16
    ins.then_inc(kb.sems[dst.dsem], 16)
    for b in reads:
        if b.r.get(dst.dsem, 0) < dst.dcnt:
            b.r[dst.dsem] = dst.dcnt
    dst.w = (dst.dsem, dst.dcnt)
    dst.r = {}
    kb.n_ops += 1


def build_l4r(e_lo=0, e_hi=NE, first=True):
    n_exp = e_hi - e_lo
    nc = bass.Bass("TRN2", target_bir_lowering=False)
    x = _din(nc, "x", [TPC, D])
    ycat = _din(nc, "ycat", [TPC, D])
    cT = _din(nc, "cT", [128, 8])
    mod_w = _din(nc, "mod_w", [D, 6 * D])
    mod_b = _din(nc, "mod_b", [1, 6 * D])
    normw = _din(nc, "normw", [1, D])
    w_out = _din(nc, "w_out", [D, D])
    rw = _din(nc, "rw", [D, NE])
    rb = _din(nc, "rb", [1, NE])
    w1 = _din(nc, "w1", [n_exp, 8, 128, 8 * 256])
    b1 = _din(nc, "b1", [128, NE * 16])
    w2 = _din(nc, "w2", [n_exp, D, D])
    b2 = _din(nc, "b2", [NE, D])
    cst = _din(nc, "cst", [128, 3, 128])
    erow = _din(nc, "erow", [2, NE])
    xprev = None if first else _din(nc, "xprev", [TPC, D])
    xo = _dout(nc, "xo", [TPC, D])
    Xs = nc.dram_tensor("Xs", [TRASH + 128, D], BF16, kind="Internal").ap()
    Ys = nc.dram_tensor("Ys", [TRASH + 128, D], F32, kind="Internal").ap()
    kb = KB(nc)
    dr = Buf("dram_in")
    bxo = Buf("xo")
    bXs = Buf("Xs")
    bYs = Buf("Ys")

    PS = []
    for i in range(4):
        t = nc.alloc_psum_tensor("PS%d" % i, [128, 1024], F32)
        PS.append((t, Buf("ps%da" % i), Buf("ps%db" % i)))

    def bank(i):
        t, ba, bb = PS[i // 2]
        return (t[:, 0:512], ba) if i % 2 == 0 else (t[:, 512:1024], bb)

    R_x, _ = kb.sb([128, 8192], F32, "R_x")
    xrows = rview(R_x, 0, [128, 8, D], BF16)
    bxrows = Buf("xrows")
    xeT = rview(R_x, 16384, [128, 8, CAP], BF16)
    bxeT = Buf("xeT")
    modw_bufs = [(rview(R_x, i * 16384, [128, 8, 512], F32), Buf("modw%d" % i)) for i in range(2)]
    R_a, _ = kb.sb([128, 4096], F32, "R_a")
    actT = rview(R_a, 0, [128, 8, CAP], BF16)
    bactT = Buf("actT")
    g1, bg1 = rview(R_a, 0, [128, D], F32), Buf("g1")
    sh2, bsh2 = rview(R_a, 4096, [128, D], F32), Buf("sh2")
    A2, bA2 = rview(R_a, 8192, [128, D], F32), Buf("A2")
    nwb, bnwb = rview(R_a, 12288, [128, D], F32), Buf("nwb")
    R_w2, _ = kb.sb([128, 8192], F32, "R_w2")
    W2B = [(rview(R_w2, i * 16384, [128, 8, D], BF16), Buf("w2b%d" % i)) for i in range(2)]
    wo = rview(R_w2, 0, [128, 8, D], BF16)
    bwo = Buf("wo")
    scb_t = (rview(R_w2, 16384, [128, 8, 128], F32), Buf("scb"))
    modb_bufs = [(rview(R_w2, 20480 + i * 2048, [128, 512], F32), Buf("modb%d" % i)) for i in range(2)]
    R_W1, _ = kb.sb([128, 3072], F32, "R_W1")
    W1R = [(rview(R_W1, i * 4096, [128, 8, 256], BF16), Buf("w1r%d" % i)) for i in range(3)]
    g2, bg2 = kb.sb([128, D], F32, "g2")
    xt, bxt = kb.sb([128, D], F32, "xt")
    yt, byt = kb.sb([128, D], F32, "yt")
    tmpA, btmpA = kb.sb([128, D], F32, "tmpA")
    tmpB, btmpB = kb.sb([128, D], F32, "tmpB")
    R1, bR1 = kb.sb([128, D], F32, "R1")
    ybt = rview(R1, 0, [128, D], BF16)
    yT = rview(R1, 2048, [128, 8, 128], BF16)
    h32T = rview(R1, 0, [128, 8, 128], F32)
    R2, bR2 = kb.sb([128, D], F32, "R2")
    hb = rview(R2, 0, [128, D], BF16)
    junk = rview(R2, 2048, [128, D], BF16)
    yrows = [kb.sb([128, D], F32, "yrow") for _ in range(2)]
    b2bs = [kb.sb([128, D], F32, "b2bc") for _ in range(2)]
    grows = [kb.sb([128, D], F32, "grow") for _ in range(4)]

    ct, bct = kb.sb([128, 3, 128], F32, "cst")
    kb.dma("sp", ct[:], cst[:, :, :], [dr], [bct])
    idf, LTs, ones = ct[:, 0, :], ct[:, 1, :], ct[:, 2, :]
    bidf = bct
    idb, bidb = kb.sb([128, 128], BF16, "identb")
    kb.op("dve", lambda e: e.tensor_copy(out=idb[:], in_=idf), [bct], [bidb])
    er, ber = kb.sb([128, 2, NE], F32, "erow")
    kb.dma("sp", er[:, 0, :], erow[0:1, :].to_broadcast([128, NE]), [dr], [ber])
    kb.dma("sp", er[:, 1, :], erow[1:2, :].to_broadcast([128, NE]), [dr], [ber])
    epst, beps = kb.sb([128, 1], F32, "eps")
    kb.op("dve", lambda e: e.memset(epst[:], EPS), [], [beps])
    kb.dma("sp", nwb, normw[0:1, :].to_broadcast([128, D]), [dr], [bnwb])
    rbb, brbb = kb.sb([128, NE], F32, "rbb")
    kb.dma("sp", rbb[:], rb[0:1, :].to_broadcast([128, NE]), [dr], [brbb])
    rwt, brwt = kb.sb([128, 8, NE], F32, "rwt")
    kb.dma("sp", rwt[:], rw.rearrange("(kc p) e -> p kc e", p=128), [dr], [brwt])
    b1a, bb1a = kb.sb([128, NE * 16], F32, "b1a")
    kb.dma("sp", b1a[:], b1[:, :], [dr], [bb1a])
    b1av = b1a[:].rearrange("p (g two) -> p g two", two=2)
    kb.op("dve", lambda e: e.tensor_scalar(out=b1av[:, :, 0:1], in0=b1av[:, :, 0:1], scalar1=ALPHA, scalar2=None,
                                           op0=ALU.mult), [bb1a], [bb1a])
    kb.op("dve", lambda e: e.tensor_scalar(out=b1av[:, :, 1:2], in0=b1av[:, :, 1:2], scalar1=1.0, scalar2=None,
                                           op0=ALU.add), [bb1a], [bb1a])
    st, bst = kb.sb([128, 64], F32, "st")
    lg, blg = kb.sb([128, NE], F32, "lg")
    ohk, bohk = kb.sb([128, 4, NE], F32, "ohk")
    prod, bprod = kb.sb([128, 4, NE], F32, "prod")
    sel, bsel = kb.sb([128, NE], F32, "sel")
    pos, bpos = kb.sb([128, NE], F32, "pos")
    basebc, bbase = kb.sb([128, NE], F32, "basebc")
    kb.op("dve", lambda e: e.memset(basebc[:], 0.0), [], [bbase])
    gkall, bgk = kb.sb([128, NT, 4], F32, "gkall")
    dstf, bdstf = kb.sb([128, NT, 4], F32, "dstf")
    dsti, bdsti = kb.sb([128, NT, 4], U32, "dsti")

    kb.op("dve", lambda e: e.memset(tmpA[:], 0.0), [], [btmpA])
    zb16 = tmpA[:].bitcast(BF16)[:, 0:D]
    r0 = e_lo * ESTRIDE
    nz = (n_exp * ESTRIDE) // 128
    for i in range(nz):
        kb.dma("sp", Xs[r0 + i * 128:r0 + (i + 1) * 128, :], zb16, [btmpA], [bXs])
        kb.dma("sp", Ys[r0 + i * 128:r0 + (i + 1) * 128, :], tmpA[:], [btmpA], [bYs])
    kb.dma("sp", Ys[TRASH:TRASH + 128, :], tmpA[:], [btmpA], [bYs])
    kb.dma("sp", Xs[TRASH:TRASH + 128, :], zb16, [btmpA], [bXs])

    sc2_out = (A2, bA2)
    emit_mod(kb, cT, mod_w, mod_b, [2, 3, 4, 5], wbufs=modw_bufs, bbufs=modb_bufs,
             pss=[bank(0), bank(1)], scb_t=scb_t,
             outs={2: (g1, bg1), 3: (sh2, bsh2), 4: sc2_out, 5: (g2[:], bg2)})
    kb.op("dve", lambda e: e.scalar_tensor_tensor(out=A2, in0=A2, scalar=1.0, in1=nwb,
                                                  op0=ALU.add, op1=ALU.mult), [bA2, bnwb], [bA2])

    for kc in range(8):
        kb.dma("pool", wo[:, kc, :], w_out[kc * 128:(kc + 1) * 128, :], [dr], [bwo])
    for ti in range(NT):
        rows = slice(ti * 128, (ti + 1) * 128)
        kb.dma("sp", xt[:], x[rows, :], [dr], [bxt])
        kb.dma("sp", yt[:], ycat[rows, :], [dr], [byt])
        kb.op("act", lambda e: e.copy(out=ybt, in_=yt[:]), [byt], [bR1])
        pt, bpt = bank(0)
        ptb = pt.bitcast(BF16)
        for kc in range(8):
            kb.op("pe", lambda e, kc=kc: e.transpose(out=ptb[:, kc * 128:(kc + 1) * 128],
                                                     in_=ybt[:, kc * 128:(kc + 1) * 128], identity=idb[:]),
                  [bR1, bidb], [bpt])
        kb.op("act", lambda e: e.copy(out=yT.rearrange("p a b -> p (a b)"), in_=ptb), [bpt], [bR1])
        for half in range(2):
            pp, bpp = bank(2 + half)
            for kc in range(8):
                kb.op("pe", lambda e, kc=kc: e.matmul(pp, yT[:, kc, :], wo[:, kc, half * 512:(half + 1) * 512],
                                                      start=(kc == 0), stop=(kc == 7)), [bR1, bwo], [bpp])
            cs = slice(half * 512, (half + 1) * 512)
            kb.op("dve", lambda e: e.tensor_tensor(out=tmpA[:, cs], in0=pp, in1=g1[:, cs], op=ALU.mult),
                  [bpp, bg1], [btmpA])
        kb.op("dve", lambda e: e.tensor_tensor(out=xt[:], in0=tmpA[:], in1=xt[:], op=ALU.add), [btmpA, bxt], [bxt])
        kb.dma("sp", xo[rows, :], xt[:], [bxt], [bxo])
        kb.op("act", lambda e: e.activation(out=junk, in_=xt[:], func=AF.Square, accum_out=st[:, 0:1]),
              [bxt], [bR2, bst])
        kb.op("act", lambda e: e.activation(out=st[:, 1:2], in_=st[:, 0:1], func=AF.Sqrt, scale=1.0 / D,
                                            bias=epst[:]), [bst, beps], [bst])
        kb.op("dve", lambda e: e.reciprocal(out=st[:, 2:3], in_=st[:, 1:2]), [bst], [bst])
        kb.op("dve", lambda e: e.scalar_tensor_tensor(out=tmpA[:], in0=xt[:], scalar=st[:, 2:3], in1=A2,
                                                      op0=ALU.mult, op1=ALU.mult), [bxt, bst, bA2], [btmpA])
        kb.op("dve", lambda e: e.tensor_tensor(out=tmpB[:], in0=tmpA[:], in1=sh2, op=ALU.add),
              [btmpA, bsh2], [btmpB])
        kb.op("act", lambda e: e.copy(out=hb, in_=tmpB[:]), [btmpB], [bR2])
        for half in range(2):
            pq, bpq = bank(4 + half)
            for k4 in range(4):
                kc = half * 4 + k4
                kb.op("pe", lambda e, kc=kc, k4=k4: e.transpose(out=pq[:, k4 * 128:(k4 + 1) * 128],
                                                                in_=tmpB[:, kc * 128:(kc + 1) * 128],
                                                                identity=idf), [btmpB, bidf], [bpq])
            kb.op("dve", lambda e: e.tensor_copy(
                out=h32T[:, half * 4:(half + 1) * 4, :], in_=pq.rearrange("p (a b) -> p a b", a=4)),
                [bpq], [bR1])
        pr, bpr = bank(6)
        for kc in range(8):
            kb.op("pe", lambda e, kc=kc: e.matmul(pr[:, 0:NE], h32T[:, kc, :], rwt[:, kc, :],
                                                  start=(kc == 0), stop=(kc == 7)), [bR1, brwt], [bpr])
        kb.op("dve", lambda e: e.tensor_tensor(out=lg[:], in0=pr[:, 0:NE], in1=rbb[:], op=ALU.add),
              [bpr, brbb], [blg])
        kb.op("dve", lambda e: e.max(out=st[:, 8:16], in_=lg[:]), [blg], [bst])
        kb.op("dve", lambda e: e.tensor_scalar(out=st[:, 16:17], in0=st[:, 8:9], scalar1=-1.0, scalar2=None,
                                               op0=ALU.mult), [bst], [bst])
        kb.op("act", lambda e: e.activation(out=st[:, 20:24], in_=st[:, 8:12], func=AF.Exp, bias=st[:, 16:17],
                                            accum_out=st[:, 17:18]), [bst], [bst])
        kb.op("dve", lambda e: e.reciprocal(out=st[:, 18:19], in_=st[:, 17:18]), [bst], [bst])
        for k in range(4):
            kb.op("dve", lambda e, k=k: e.tensor_scalar(out=ohk[:, k, :], in0=lg[:], scalar1=st[:, 8 + k:9 + k],
                                                        scalar2=None, op0=ALU.is_equal), [blg, bst], [bohk])
        kb.op("dve", lambda e: e.tensor_scalar(out=sel[:], in0=lg[:], scalar1=st[:, 11:12], scalar2=None,
                                               op0=ALU.is_ge), [blg, bst], [bsel])
        ppf, bppf = bank(7)
        kb.op("pe", lambda e: e.matmul(ppf[:, 0:NE], LTs, sel[:], start=True, stop=True), [bct, bsel], [bppf])
        kb.op("dve", lambda e: e.tensor_tensor(out=pos[:], in0=ppf[:, 0:NE], in1=basebc[:], op=ALU.add),
              [bppf, bbase], [bpos])
        ppb, bppb = bank(1)
        kb.op("pe", lambda e: e.matmul(ppb[:, 0:NE], ones, sel[:], start=True, stop=True), [bct, bsel], [bppb])
        kb.op("dve", lambda e: e.tensor_tensor(out=basebc[:], in0=ppb[:, 0:NE], in1=basebc[:], op=ALU.add),
              [bppb, bbase], [bbase])
        kb.op("dve", lambda e: e.scalar_tensor_tensor(out=pos[:], in0=pos[:], scalar=float(CAP), in1=er[:, 0, :],
                                                      op0=ALU.min, op1=ALU.add), [bpos, ber], [bpos])
        kb.op("dve", lambda e: e.tensor_tensor(out=ohk[:], in0=ohk[:],
                                               in1=er[:, 1, :].unsqueeze(1).to_broadcast([128, 4, NE]),
                                               op=ALU.mult), [bohk, ber], [bohk])
        kb.op("dve", lambda e: e.tensor_tensor(out=prod[:], in0=ohk[:],
                                               in1=pos[:].unsqueeze(1).to_broadcast([128, 4, NE]),
                                               op=ALU.mult), [bohk, bpos], [bprod])
        kb.op("dve", lambda e: e.tensor_reduce(out=dstf[:, ti, :], in_=prod[:], axis=AX.X, op=ALU.add),
              [bprod], [bdstf])
        kb.op("dve", lambda e: e.tensor_reduce(out=st[:, 24:28], in_=ohk[:], axis=AX.X, op=ALU.add),
              [bohk], [bst])
        kb.op("dve", lambda e: e.tensor_scalar(out=st[:, 28:32], in0=st[:, 24:28], scalar1=-float(TRASH),
                                               scalar2=float(TRASH), op0=ALU.mult, op1=ALU.add), [bst], [bst])
        kb.op("dve", lambda e: e.tensor_tensor(out=dstf[:, ti, :], in0=dstf[:, ti, :], in1=st[:, 28:32], op=ALU.add),
              [bdstf, bst], [bdstf])
        kb.op("dve", lambda e: e.scalar_tensor_tensor(out=gkall[:, ti, :], in0=st[:, 20:24], scalar=st[:, 18:19],
                                                      in1=st[:, 24:28], op0=ALU.mult, op1=ALU.mult), [bst], [bgk])
        kb.op("dve", lambda e: e.tensor_copy(out=dsti[:, ti, :], in_=dstf[:, ti, :]), [bdstf], [bdsti])
        for k in range(4):
            _dma_ind(kb, Xs[:, :], bass.IndirectOffsetOnAxis(ap=dsti[:, ti, k:k + 1], axis=0), hb, None,
                     [bR2, bdsti], [bXs])

    merge_deps(bxrows, [b for _, b in modw_bufs])
    merge_deps(bxeT, [b for _, b in modw_bufs])
    merge_deps(bactT, [bg1, bsh2, bA2, bnwb])
    for _, b in W2B:
        merge_deps(b, [bwo, scb_t[1]] + [bb for _, bb in modb_bufs])
    tts = [(tmpA[:, 0:512], Buf("tt0")), (tmpA[:, 512:1024], Buf("tt1"))]
    lps = [(tmpB[:, 0:512], Buf("lp0")), (tmpB[:, 512:1024], Buf("lp1"))]
    t2s = [(xt[:, 0:512], Buf("t20")), (xt[:, 512:1024], Buf("t21"))]
    for (_, b), src_ in zip(tts + lps + t2s, [btmpA, btmpA, btmpB, btmpB, bxt, bxt]):
        merge_deps(b, [src_])
    C0 = float(LIMIT * ALPHA / (1.0 + np.exp(-LIMIT * ALPHA)))
    w1_issued = [0]

    def issue_w1(upto):
        while w1_issued[0] < min(upto, n_exp * 8):
            i = w1_issued[0]
            t, b = W1R[i % 3]
            kb.dma("pool", t.rearrange("p a b -> p (a b)"), w1[i // 8, i % 8, :, :], [dr], [b])
            w1_issued[0] += 1

    def issue_w2(ei):
        if ei < n_exp:
            t, b = W2B[ei % 2]
            src = w2[ei].rearrange("(fc p) d -> p fc d", p=128)
            for fc in range(8):
                kb.dma("pool", t[:, fc, :], src[:, fc, :], [dr], [b])

    nu = 0
    ny = 0
    ntp = 0
    issue_w2(0)
    for e in range(e_lo, e_hi):
        ei = e - e_lo
        issue_w2(ei + 1)
        b2b, bb2b = b2bs[ei % 2]
        kb.dma("sp", b2b[:], b2[e:e + 1, :].to_broadcast([128, D]), [dr], [bb2b])
        kb.dma("sp", xrows, Xs[e * ESTRIDE:e * ESTRIDE + CAP, :].rearrange("(i p) d -> p i d", p=128),
               [bXs], [bxrows])
        for i in range(8):
            pt, bpt = bank(6 + ntp % 2)
            ntp += 1
            ptb = pt.bitcast(BF16)
            for kc in range(8):
                kb.op("pe", lambda en, kc=kc, i=i: en.transpose(out=ptb[:, kc * 128:(kc + 1) * 128],
                                                                in_=xrows[:, i, kc * 128:(kc + 1) * 128],
                                                                identity=idb[:]), [bxrows, bidb], [bpt])
            kb.op("act", lambda en, i=i: en.copy(out=xeT[:, :, i * 128:(i + 1) * 128],
                                                 in_=ptb.rearrange("p (a b) -> p a b", a=8)), [bpt], [bxeT])
        for ft in range(8):
            gi = ei * 8 + ft
            issue_w1(gi + 2)
            wv, bwt = W1R[gi % 3]
            bcol = (e * 8 + ft) * 2
            for blk in range(CAP // 512):
                ts_ = slice(blk * 512, (blk + 1) * 512)
                pgl, bpgl = bank(0 + 2 * (nu % 2))
                pli, bpli = bank(1 + 2 * (nu % 2))
                for kc in range(8):
                    kb.op("pe", lambda en, kc=kc: en.matmul(pgl, wv[:, kc, 0:128], xeT[:, kc, ts_],
                                                            start=(kc == 0), stop=(kc == 7)), [bwt, bxeT], [bpgl])
                for kc in range(8):
                    kb.op("pe", lambda en, kc=kc: en.matmul(pli, wv[:, kc, 128:256], xeT[:, kc, ts_],
                                                            start=(kc == 0), stop=(kc == 7)), [bwt, bxeT], [bpli])
                tt, btt = tts[nu % 2]
                lp, blp = lps[nu % 2]
                t2, bt2 = t2s[nu % 2]
                kb.op("act", lambda en: en.activation(out=tt, in_=pgl, func=AF.Silu, scale=ALPHA,
                                                      bias=b1a[:, bcol:bcol + 1]), [bpgl, bb1a], [btt])
                kb.op("dve", lambda en: en.tensor_scalar(out=lp, in0=pli, scalar1=b1a[:, bcol + 1:bcol + 2],
                                                         scalar2=LIMIT + 1.0, op0=ALU.add, op1=ALU.min),
                      [bpli, bb1a], [blp])
                kb.op("dve", lambda en: en.tensor_scalar(out=t2, in0=tt, scalar1=C0, scalar2=1.0 / ALPHA,
                                                         op0=ALU.min, op1=ALU.mult), [btt], [bt2])
                kb.op("dve", lambda en: en.scalar_tensor_tensor(out=actT[:, ft, ts_], in0=lp, scalar=1.0 - LIMIT,
                                                                in1=t2, op0=ALU.max, op1=ALU.mult),
                      [blp, bt2], [bactT])
                nu += 1
        w2v, bw2 = W2B[ei % 2]
        for i in range(8):
            yr, byr = yrows[ny % 2]
            ny += 1
            for db in range(2):
                py, bpy = bank(4 + db)
                for fc in range(8):
                    kb.op("pe", lambda en, fc=fc, i=i, db=db: en.matmul(py, actT[:, fc, i * 128:(i + 1) * 128],
                                                                        w2v[:, fc, db * 512:(db + 1) * 512],
                                                                        start=(fc == 0), stop=(fc == 7)),
                          [bactT, bw2], [bpy])
                kb.op("dve", lambda en, db=db: en.tensor_tensor(out=yr[:, db * 512:(db + 1) * 512], in0=py,
                                                                in1=b2b[:, db * 512:(db + 1) * 512], op=ALU.add),
                      [bpy, bb2b], [byr])
            kb.dma("sp", Ys[e * ESTRIDE + i * 128:e * ESTRIDE + (i + 1) * 128, :], yr[:], [byr], [bYs])

    tf, btf = tts[0]
    for ti in range(NT):
        rows = slice(ti * 128, (ti + 1) * 128)
        if first:
            kb.dma("sp", yt[:], xo[rows, :], [bxo], [byt])
        else:
            kb.dma("sp", yt[:], xprev[rows, :], [dr], [byt])
        for k in range(4):
            gr, bgr = grows[k]
            _dma_ind(kb, gr[:], None, Ys[:, :], bass.IndirectOffsetOnAxis(ap=dsti[:, ti, k:k + 1], axis=0),
                     [bYs, bdsti], [bgr])
        g0, bg0 = grows[0]
        kb.op("dve", lambda en: en.tensor_scalar(out=g0[:], in0=g0[:], scalar1=gkall[:, ti, 0:1], scalar2=None,
                                                 op0=ALU.mult), [bg0, bgk], [bg0])
        for k in range(1, 4):
            gr, bgr = grows[k]
            kb.op("dve", lambda en, k=k, gr=gr: en.scalar_tensor_tensor(out=g0[:], in0=gr[:],
                                                                        scalar=gkall[:, ti, k:k + 1], in1=g0[:],
                                                                        op0=ALU.mult, op1=ALU.add),
                  [bgr, bgk, bg0], [bg0])
        kb.op("dve", lambda en: en.tensor_tensor(out=g0[:], in0=g0[:], in1=g2[:], op=ALU.mult), [bg0, bg2], [bg0])
        kb.op("dve", lambda en: en.tensor_tensor(out=yt[:], in0=g0[:], in1=yt[:], op=ALU.add), [bg0, byt], [byt])
        kb.dma("sp", xo[rows, :], yt[:], [byt], [bxo])
    kb.finish([bxo])
    return nc


def run_l4r(x, ycat, c, mod_w_l, mod_b_l, normw_l, w_out_l, rw_l, rb_l, w1r, b1r, w2_l, b2_l,
            splits=((0, 16), (16, 32))):
    i = np.arange(128)
    cst = np.ascontiguousarray(np.stack([np.eye(128, dtype=np.float32),
                                         (i[:, None] < i[None, :]).astype(np.float32),
                                         np.ones((128, 128), np.float32)], axis=1))
    xprev = None
    for (lo, hi) in splits:
        first = (lo == 0)
        nc = _get("l4r_%d_%d" % (lo, hi), lambda: build_l4r(lo, hi, first))
        w1s = np.ascontiguousarray(w1r[lo:hi])
        w2s = np.ascontiguousarray(w2_l[lo:hi])
        erow = np.stack([np.arange(NE) * ESTRIDE, ((np.arange(NE) >= lo) & (np.arange(NE) < hi))]).astype(np.float32)
        maps = []
        for cid in range(NCORES):
            m = {"x": core_tokens(x, cid), "ycat": core_tokens(ycat, cid), "cT": cT_of(c, cid // 4),
                 "mod_w": mod_w_l, "mod_b": mod_b_l.reshape(1, -1), "normw": normw_l.reshape(1, -1),
                 "w_out": w_out_l, "rw": rw_l, "rb": rb_l.reshape(1, -1), "w1": w1s, "b1": b1r, "w2": w2s,
                 "b2": b2_l, "cst": cst, "erow": erow}
            if not first:
                m["xprev"] = xprev[cid]
            maps.append(m)
        res = _run(nc, maps)
        xprev = [r["xo"] for r in res.results]
    return uncore_tokens(xprev, (D,))
```

```python
import numpy as np
import concourse.bass as bass
import concourse.mybir as mybir
from concourse.bass_utils import run_bass_kernel_spmd

F32 = mybir.dt.float32
BF16 = mybir.dt.bfloat16
U32 = mybir.dt.uint32
AF = mybir.ActivationFunctionType
ALU = mybir.AluOpType
AX = mybir.AxisListType

D = 1024
S = 8192
NB = 2
D_IN = 4176
EPS = 1e-6
NCORES = 8
TPC = 2048
NT = 16


class Buf:
    __slots__ = ("name", "w", "r", "dsem", "dcnt")

    def __init__(self, name):
        self.name = name
        self.w = None
        self.r = {}
        self.dsem = None
        self.dcnt = 0


class KB:
    def __init__(self, nc, self_sync=True):
        self.nc = nc
        self.eng = {"pe": nc.tensor, "dve": nc.vector, "act": nc.scalar,
                    "pool": nc.gpsimd, "sp": nc.sync}
        self.sems = {}
        self.cnt = {}
        self.known = {k: {} for k in self.eng}
        for k in self.eng:
            self.sems[k] = nc.alloc_semaphore("sem_" + k)
            self.cnt[k] = 0
        self.self_sync = self_sync
        self.nsem = 0
        self.n_ops = 0
        self.n_waits = 0
        self.nbuf = 0

    def sb(self, shape, dt=F32, name=None):
        self.nbuf += 1
        name = (name or "t") + "_%d" % self.nbuf
        return self.nc.alloc_sbuf_tensor(name, list(shape), dt), Buf(name)

    def ps(self, shape, dt=F32, name=None):
        self.nbuf += 1
        name = (name or "p") + "_%d" % self.nbuf
        return self.nc.alloc_psum_tensor(name, list(shape), dt), Buf(name)

    def _deps(self, reads, writes):
        d = {}

        def add(kv):
            if kv is None:
                return
            k, v = kv
            if d.get(k, 0) < v:
                d[k] = v
        for b in reads:
            add(b.w)
        for b in writes:
            add(b.w)
            for k, v in b.r.items():
                add((k, v))
        return d

    def _wait(self, en, deps):
        e = self.eng[en]
        kn = self.known[en]
        for k, v in deps.items():
            if k == en and (en == "pe" or not self.self_sync):
                continue
            if kn.get(k, 0) >= v:
                continue
            e.wait_ge(self.sems[k], v)
            kn[k] = v
            self.n_waits += 1

    def op(self, en, fn, reads=(), writes=()):
        deps = self._deps(reads, writes)
        self._wait(en, deps)
        ins = fn(self.eng[en])
        self.cnt[en] += 1
        v = self.cnt[en]
        ins.then_inc(self.sems[en], 1)
        for b in reads:
            if b.r.get(en, 0) < v:
                b.r[en] = v
        for b in writes:
            b.w = (en, v)
            b.r = {}
        self.n_ops += 1
        return ins

    def dma(self, en, out, in_, reads, writes, **kw):
        assert len(writes) == 1
        dst = writes[0]
        if dst.dsem is None:
            self.nsem += 1
            key = "d%d_%s" % (self.nsem, dst.name)
            dst.dsem = key
            self.sems[key] = self.nc.alloc_semaphore(key[:40])
        deps = self._deps(reads, writes)
        self._wait(en, deps)
        ins = self.eng[en].dma_start(out=out, in_=in_, **kw)
        dst.dcnt += 16
        ins.then_inc(self.sems[dst.dsem], 16)
        for b in reads:
            if b.r.get(dst.dsem, 0) < dst.dcnt:
                b.r[dst.dsem] = dst.dcnt
        dst.w = (dst.dsem, dst.dcnt)
        dst.r = {}
        self.n_ops += 1
        return ins

    def finish(self, bufs, en="sp"):
        d = {}
        for b in bufs:
            if b.w is not None:
                k, v = b.w
                d[k] = max(d.get(k, 0), v)
        self._wait(en, d)


def rview(t, off_bytes, shape, dt):
    esz = 2 if dt == BF16 else 4
    n = 1
    for s in shape[1:]:
        n *= s
    flat = t[:, :] if dt == F32 else t[:, :].bitcast(dt)
    e0 = off_bytes // esz
    v = flat[0:shape[0], e0:e0 + n]
    if len(shape) == 3:
        v = v.rearrange("p (a b) -> p a b", a=shape[1])
    return v


def merge_deps(dst, srcs):
    for s in srcs:
        for kv in ([s.w] if s.w else []) + list(s.r.items()):
            k, v = kv
            if dst.r.get(k, 0) < v:
                dst.r[k] = v


def _din(nc, name, shape, dt=F32):
    return nc.dram_tensor(name, list(shape), dt, kind="ExternalInput").ap()


def _dout(nc, name, shape, dt=F32):
    return nc.dram_tensor(name, list(shape), dt, kind="ExternalOutput").ap()


def emit_mod(kb, cT, mod_w, mod_b, groups, wbufs=None, bbufs=None, pss=None, scb_t=None, outs=None):
    nc = kb.nc
    dr = Buf("mod_dram")
    ct, bct = kb.sb([128, 8], F32, "ct")
    kb.dma("sp", ct[:], cT[:, :], [dr], [bct])
    sc, bsc = kb.sb([128, 8], F32, "sc")
    kb.op("act", lambda e: e.activation(out=sc[:], in_=ct[:], func=AF.Silu), [bct], [bsc])
    scb, bscb = scb_t if scb_t is not None else kb.sb([128, 8, 128], F32, "scb")
    kb.op("dve", lambda e: e.tensor_copy(out=scb[:], in_=sc[:, :].unsqueeze(2).to_broadcast([128, 8, 128])),
          [bsc], [bscb])
    mw = mod_w.rearrange("(kc p) n -> p kc n", p=128)
    if wbufs is None:
        wbufs = [kb.sb([128, 8, 512], F32, "modw") for _ in range(2)]
    if bbufs is None:
        bbufs = [kb.sb([128, 512], F32, "modb") for _ in range(2)]
    if pss is None:
        pss = [kb.ps([128, 512], F32, "modps") for _ in range(2)]
    res = {}
    it = 0
    for g in groups:
        mt, bmt = outs[g] if outs is not None else kb.sb([128, 1024], F32, "modrow")
        res[g] = (mt, bmt)
        for half in range(2):
            c0 = g * 1024 + half * 512
            wt, bw = wbufs[it % 2]
            bt, bb = bbufs[it % 2]
            pt, bp = pss[it % 2]
            kb.dma("sp", wt[:], mw[:, :, c0:c0 + 512], [dr], [bw])
            kb.dma("sp", bt[:], mod_b[0:1, c0:c0 + 512].to_broadcast([128, 512]), [dr], [bb])
            for kc in range(8):
                kb.op("pe", lambda e, kc=kc: e.matmul(pt[:], scb[:, kc, :], wt[:, kc, :],
                                                      start=(kc == 0), stop=(kc == 7)),
                      [bscb, bw], [bp])
            kb.op("dve", lambda e: e.tensor_tensor(out=mt[:, half * 512:(half + 1) * 512], in0=pt[:],
                                                   in1=bt[:], op=ALU.add), [bp, bb], [bmt])
            it += 1
    return res


NF1 = 2064
NB1 = 2112


def build_l1():
    nc = bass.Bass("TRN2", target_bir_lowering=False)
    x = _din(nc, "x", [TPC, D])
    cT = _din(nc, "cT", [128, 8])
    mod_w = _din(nc, "mod_w", [D, 6 * D])
    mod_b = _din(nc, "mod_b", [1, 6 * D])
    normw = _din(nc, "normw", [1, D])
    w_in = _din(nc, "w_in", [D, D_IN])
    qkw = _din(nc, "qkw", [1, 128])
    ident = _din(nc, "ident", [128, 128])
    of = _dout(nc, "of", [TPC, NF1])
    ob = _dout(nc, "ob", [TPC, NB1], BF16)
    kb = KB(nc)
    dr = Buf("dram_in")
    bof, bob = Buf("of"), Buf("ob")

    idb, bidb = kb.sb([128, 128], BF16, "identb")
    kb.dma("pool", idb[:], ident[:, :], [dr], [bidb])
    epst, beps = kb.sb([128, 1], F32, "eps")
    kb.op("dve", lambda e: e.memset(epst[:], EPS), [], [beps])
    nwb, bnwb = kb.sb([128, D], F32, "nwb")
    kb.dma("sp", nwb[:], normw[0:1, :].to_broadcast([128, D]), [dr], [bnwb])
    qkb, bqkb = kb.sb([128, 128], F32, "qkb")
    kb.dma("sp", qkb[:], qkw[0:1, :].to_broadcast([128, 128]), [dr], [bqkb])
    kb.op("dve", lambda e: e.tensor_scalar(out=qkb[:, 0:64], in0=qkb[:, 0:64], scalar1=0.125, scalar2=None,
                                           op0=ALU.mult), [bqkb], [bqkb])

    wb, bwb = kb.sb([128, 8, D_IN], BF16, "w_in")
    for kc in range(8):
        for pc in range(3):
            c0 = pc * 1392
            kb.dma("pool", wb[:, kc, c0:c0 + 1392], w_in[kc * 128:(kc + 1) * 128, c0:c0 + 1392], [dr], [bwb])

    mods = emit_mod(kb, cT, mod_w, mod_b, [0, 1])
    sh1, bsh1 = mods[0]
    sc1, bsc1 = mods[1]
    A1, bA1 = kb.sb([128, D], F32, "A1")
    kb.op("dve", lambda e: e.scalar_tensor_tensor(out=A1[:], in0=sc1[:], scalar=1.0, in1=nwb[:],
                                                  op0=ALU.add, op1=ALU.mult), [bsc1, bnwb], [bA1])

    xts = [kb.sb([128, D], F32, "xt") for _ in range(2)]
    junk, bjunk = kb.sb([128, D], BF16, "junk")
    tmp, btmp = kb.sb([128, D], F32, "tmp")
    hbs = [kb.sb([128, D], BF16, "hb") for _ in range(2)]
    pT, bpT = kb.ps([128, D], BF16, "pT")
    hTs = [kb.sb([128, 8, 128], BF16, "hT") for _ in range(2)]
    pps = [kb.ps([128, 512], F32, "pp") for _ in range(4)]
    projs = [kb.sb([128, D_IN], F32, "proj") for _ in range(2)]
    obts = [kb.sb([128, NB1], BF16, "obt") for _ in range(2)]
    sq, bsq = kb.sb([128, 1024], F32, "sq")
    st, bst = kb.sb([128, 24], F32, "stats")

    QO = 2056
    npp = [0]
    stA, bstA = kb.sb([128, 4], F32, "statsA")

    def stage_a(ti):
        xt, bxt = xts[ti % 2]
        hb, bhb = hbs[ti % 2]
        hT, bhT = hTs[ti % 2]
        kb.dma("sp", xt[:], x[ti * 128:(ti + 1) * 128, :], [dr], [bxt])
        kb.op("act", lambda e: e.activation(out=junk[:], in_=xt[:], func=AF.Square, accum_out=stA[:, 0:1]),
              [bxt], [bjunk, bstA])
        kb.op("act", lambda e: e.activation(out=stA[:, 1:2], in_=stA[:, 0:1], func=AF.Sqrt, scale=1.0 / D,
                                            bias=epst[:]), [bstA, beps], [bstA])
        kb.op("dve", lambda e: e.reciprocal(out=stA[:, 2:3], in_=stA[:, 1:2]), [bstA], [bstA])
        kb.op("dve", lambda e: e.scalar_tensor_tensor(out=tmp[:], in0=xt[:], scalar=stA[:, 2:3], in1=A1[:],
                                                      op0=ALU.mult, op1=ALU.mult), [bxt, bstA, bA1], [btmp])
        kb.op("dve", lambda e: e.tensor_tensor(out=hb[:], in0=tmp[:], in1=sh1[:], op=ALU.add),
              [btmp, bsh1], [bhb])
        for kc in range(8):
            kb.op("pe", lambda e, kc=kc: e.transpose(out=pT[:, kc * 128:(kc + 1) * 128],
                                                     in_=hb[:, kc * 128:(kc + 1) * 128], identity=idb[:]),
                  [bhb, bidb], [bpT])
        kb.op("act", lambda e: e.copy(out=hT[:].rearrange("p a b -> p (a b)"), in_=pT[:]), [bpT], [bhT])

    def stage_b(ti):
        hT, bhT = hTs[ti % 2]
        pj, bpj = projs[ti % 2]
        obt, bobt = obts[ti % 2]
        for cb in range(9):
            c0 = cb * 512
            n = min(512, D_IN - c0)
            pp, bpp = pps[npp[0] % 4]
            for kc in range(8):
                kb.op("pe", lambda e, kc=kc: e.matmul(pp[:, 0:n], hT[:, kc, :], wb[:, kc, c0:c0 + n],
                                                      start=(kc == 0), stop=(kc == 7)), [bhT, bwb], [bpp])
            if npp[0] % 2 == 0:
                kb.op("act", lambda e: e.copy(out=pj[:, c0:c0 + n], in_=pp[:, 0:n]), [bpp], [bpj])
            else:
                kb.op("dve", lambda e: e.tensor_copy(out=pj[:, c0:c0 + n], in_=pp[:, 0:n]), [bpp], [bpj])
            npp[0] += 1
        qk = pj[:, QO:QO + 1024]
        kb.op("dve", lambda e: e.tensor_tensor(out=sq[:], in0=qk, in1=qk, op=ALU.mult), [bpj], [bsq])
        kb.op("dve", lambda e: e.tensor_reduce(out=st[:, 4:20], in_=sq[:].rearrange("p (g d) -> p g d", g=16),
                                               axis=AX.X, op=ALU.add), [bsq], [bst])
        kb.op("act", lambda e: e.activation(out=st[:, 4:20], in_=st[:, 4:20], func=AF.Sqrt, scale=1.0 / 64,
                                            bias=epst[:]), [bst, beps], [bst])
        kb.op("dve", lambda e: e.reciprocal(out=st[:, 4:20], in_=st[:, 4:20]), [bst], [bst])
        kb.op("dve", lambda e: e.tensor_tensor(out=sq[:].rearrange("p (g d) -> p g d", g=16),
                                               in0=qk.rearrange("p (g d) -> p g d", g=16),
                                               in1=st[:, 4:20].unsqueeze(2).to_broadcast([128, 16, 64]),
                                               op=ALU.mult), [bpj, bst], [bsq])
        for half in range(2):
            kb.op("dve", lambda e, half=half: e.tensor_tensor(
                out=obt[:, half * 512:(half + 1) * 512].rearrange("p (g d) -> p g d", g=8),
                in0=sq[:, half * 512:(half + 1) * 512].rearrange("p (g d) -> p g d", g=8),
                in1=qkb[:, half * 64:(half + 1) * 64].unsqueeze(1).to_broadcast([128, 8, 64]),
                op=ALU.mult), [bsq, bqkb], [bobt])
        kb.op("act", lambda e: e.copy(out=obt[:, 1024:NB1], in_=pj[:, QO + 1024:QO + 1024 + 1088]), [bpj], [bobt])
        kb.op("dve", lambda e: e.tensor_scalar(out=pj[:, 4168:4176], in0=pj[:, 4168:4176],
                                               scalar1=float(8 ** -0.5 * 64 ** -0.5), scalar2=None,
                                               op0=ALU.mult), [bpj], [bpj])
        kb.dma("sp", of[ti * 128:(ti + 1) * 128, 0:2056], pj[:, 0:2056], [bpj], [bof])
        kb.dma("sp", of[ti * 128:(ti + 1) * 128, 2056:2064], pj[:, 4168:4176], [bpj], [bof])
        kb.dma("sp", ob[ti * 128:(ti + 1) * 128, :], obt[:], [bobt], [bob])

    stage_a(0)
    for ti in range(NT):
        if ti + 1 < NT:
            stage_a(ti + 1)
        stage_b(ti)
    kb.finish([bof, bob])
    return nc


def core_tokens(a, c):
    b, r = c // 4, c % 4
    t = a[b].reshape((64, 128) + a.shape[2:])[r::4]
    return np.ascontiguousarray(t.reshape((TPC,) + a.shape[2:]))


def uncore_tokens(parts, tail):
    out = np.empty((NB, 64, 128) + tuple(tail), parts[0].dtype)
    for c in range(NCORES):
        b, r = c // 4, c % 4
        out[b, r::4] = parts[c].reshape((16, 128) + tuple(tail))
    return out.reshape((NB, S) + tuple(tail))


_NC_CACHE = {}
_TRACE = False
_TIMES = []


def _run(nc, maps):
    if _TRACE:
        res = run_bass_kernel_spmd(nc, maps, core_ids=list(range(NCORES)), trace=True)
        _TIMES.append(res.exec_time_ns)
        print('exec_time_ns', res.exec_time_ns)
        return res
    return run_bass_kernel_spmd(nc, maps, core_ids=list(range(NCORES)))


def _get(name, fn):
    if name not in _NC_CACHE:
        _NC_CACHE[name] = fn()
    return _NC_CACHE[name]


def cT_of(c, b):
    return np.ascontiguousarray(c[b].reshape(8, 128).T)


def run_l1(x, c, mod_w_l, mod_b_l, normw_l, w_in_l, qw_l, kw_l):
    nc = _get("l1", build_l1)
    ident = np.eye(128, dtype=np.float32)
    qkw = np.concatenate([qw_l, kw_l]).reshape(1, 128).astype(np.float32)
    maps = []
    for cid in range(NCORES):
        maps.append({"x": core_tokens(x, cid), "cT": cT_of(c, cid // 4), "mod_w": mod_w_l,
                     "mod_b": mod_b_l.reshape(1, -1), "normw": normw_l.reshape(1, -1), "w_in": w_in_l,
                     "qkw": qkw, "ident": ident})
    res = _run(nc, maps)
    of = uncore_tokens([r["of"] for r in res.results], (NF1,))
    ob = uncore_tokens([r["ob"] for r in res.results], (NB1,))
    return of, ob


NE = 32
ALPHA = 1.702
LIMIT = 7.0


def build_l4(e_lo=0, e_hi=NE, first=True):
    n_exp = e_hi - e_lo
    do_final = True
    nc = bass.Bass("TRN2", target_bir_lowering=False)
    x = _din(nc, "x", [TPC, D])
    ycat = _din(nc, "ycat", [TPC, D])
    cT = _din(nc, "cT", [128, 8])
    mod_w = _din(nc, "mod_w", [D, 6 * D])
    mod_b = _din(nc, "mod_b", [1, 6 * D])
    normw = _din(nc, "normw", [1, D])
    w_out = _din(nc, "w_out", [D, D])
    rw = _din(nc, "rw", [D, NE])
    rb = _din(nc, "rb", [1, NE])
    w1 = _din(nc, "w1", [max(n_exp, 1), 8, 128, 8 * 256])
    b1 = _din(nc, "b1", [128, NE * 16])
    w2 = _din(nc, "w2", [max(n_exp, 1), 8, 128, 8 * 128])
    b2 = _din(nc, "b2", [NE, D])
    ident = _din(nc, "ident", [128, 128])
    xprev = None if first else _din(nc, "xprev", [TPC, D])
    xo = _dout(nc, "xo", [TPC, D])
    if first:
        hT_o = _dout(nc, "hT_o", [128, 8 * TPC], BF16)
        gT_o = _dout(nc, "gT_o", [NE, TPC])
        g2_o = _dout(nc, "g2_o", [128, D])
    else:
        hT_i = _din(nc, "hT_i", [128, 8 * TPC], BF16)
        gT_i = _din(nc, "gT_i", [NE, TPC])
        g2_i = _din(nc, "g2_i", [128, D])
    kb = KB(nc)
    dr = Buf("dram_in")
    bxo = Buf("xo")
    bho = Buf("handover")

    PS = []
    for i in range(4):
        t = nc.alloc_psum_tensor("PS%d" % i, [128, 1024], F32)
        PS.append((t, Buf("ps%da" % i), Buf("ps%db" % i)))

    def bank(i):
        t, ba, bb = PS[i // 2]
        return (t[:, 0:512], ba) if i % 2 == 0 else (t[:, 512:1024], bb)

    R_hT, bhT = kb.sb([128, 8192], F32, "R_hT")
    hT = rview(R_hT, 0, [128, 8, TPC], BF16)
    R_acc, bacc = kb.sb([128, 16384], F32, "R_acc")
    acc = rview(R_acc, 0, [128, 8, TPC], F32)
    wo = rview(R_acc, 0, [128, 8, D], BF16)
    bwo = Buf("wo")
    R_act, bactT = kb.sb([128, 8192], F32, "R_act")
    actT = rview(R_act, 0, [128, 8, TPC], BF16)
    modw_bufs = [(rview(R_act, i * 16384, [128, 8, 512], F32), Buf("modw%d" % i)) for i in range(2)]
    gateT, bgateT = kb.sb([NE, TPC], F32, "gateT")
    R_gsb, bgsb = kb.sb([128, TPC], F32, "gsb")
    gsb = R_gsb
    scb_t = (rview(R_gsb, 0, [128, 8, 128], F32), Buf("scb"))
    modb_bufs = [(rview(R_gsb, 4096 + i * 2048, [128, 512], F32), Buf("modb%d" % i)) for i in range(2)]
    R_W, _ = kb.sb([128, 4608], F32, "R_W")
    W1R = [(rview(R_W, i * 4096, [128, 8, 256], BF16), Buf("w1r%d" % i)) for i in range(3)]
    W2R = [(rview(R_W, 12288 + i * 2048, [128, 8, 128], BF16), Buf("w2r%d" % i)) for i in range(3)]
    g1, bg1 = rview(R_W, 0, [128, D], F32), Buf("g1")
    sh2, bsh2 = rview(R_W, 4096, [128, D], F32), Buf("sh2")
    A2, bA2 = rview(R_W, 8192, [128, D], F32), Buf("A2")
    nwb, bnwb = rview(R_W, 12288, [128, D], F32), Buf("nwb")
    g2, bg2 = kb.sb([128, D], F32, "g2")
    xt, bxt = kb.sb([128, D], F32, "xt")
    yt, byt = kb.sb([128, D], F32, "yt")
    tmpA, btmpA = kb.sb([128, D], F32, "tmpA")
    tmpB, btmpB = kb.sb([128, D], F32, "tmpB")
    R1, bR1 = kb.sb([128, D], F32, "R1")
    ybt = rview(R1, 0, [128, D], BF16)
    yT = rview(R1, 2048, [128, 8, 128], BF16)
    h32T = rview(R1, 0, [128, 8, 128], F32)
    R2, bR2 = kb.sb([128, D], F32, "R2")
    hb = rview(R2, 0, [128, D], BF16)
    junk = rview(R2, 2048, [128, D], BF16)

    idf, bidf = kb.sb([128, 128], F32, "identf")
    kb.dma("sp", idf[:], ident[:, :], [dr], [bidf])
    idb, bidb = kb.sb([128, 128], BF16, "identb")
    kb.op("dve", lambda e: e.tensor_copy(out=idb[:], in_=idf[:]), [bidf], [bidb])
    epst, beps = kb.sb([128, 1], F32, "eps")
    kb.op("dve", lambda e: e.memset(epst[:], EPS), [], [beps])
    kb.dma("sp", nwb, normw[0:1, :].to_broadcast([128, D]), [dr], [bnwb])
    rbb, brbb = kb.sb([128, NE], F32, "rbb")
    kb.dma("sp", rbb[:], rb[0:1, :].to_broadcast([128, NE]), [dr], [brbb])
    rwt, brwt = kb.sb([128, 8, NE], F32, "rwt")
    kb.dma("sp", rwt[:], rw.rearrange("(kc p) e -> p kc e", p=128), [dr], [brwt])
    b1a, bb1a = kb.sb([128, NE * 16], F32, "b1a")
    kb.dma("sp", b1a[:], b1[:, :], [dr], [bb1a])
    b1av = b1a[:].rearrange("p (g two) -> p g two", two=2)
    kb.op("dve", lambda e: e.tensor_scalar(out=b1av[:, :, 0:1], in0=b1av[:, :, 0:1], scalar1=ALPHA, scalar2=None,
                                           op0=ALU.mult), [bb1a], [bb1a])
    kb.op("dve", lambda e: e.tensor_scalar(out=b1av[:, :, 1:2], in0=b1av[:, :, 1:2], scalar1=1.0, scalar2=None,
                                           op0=ALU.add), [bb1a], [bb1a])
    b2t, bb2t = kb.sb([NE, D], F32, "b2t")
    kb.dma("sp", b2t[:], b2[:, :], [dr], [bb2t])
    selbs = [kb.sb([NE, 128], F32, "selb") for _ in range(2)]
    st, bst = kb.sb([128, 64], F32, "st")
    lg, blg = kb.sb([128, NE], F32, "lg")

    if first:
        sc2_out = (A2, bA2)
        mods = emit_mod(kb, cT, mod_w, mod_b, [2, 3, 4, 5], wbufs=modw_bufs, bbufs=modb_bufs,
                        pss=[bank(0), bank(1)], scb_t=scb_t,
                        outs={2: (g1, bg1), 3: (sh2, bsh2), 4: sc2_out, 5: (g2[:], bg2)})
        kb.op("dve", lambda e: e.scalar_tensor_tensor(out=A2, in0=A2, scalar=1.0, in1=nwb,
                                                      op0=ALU.add, op1=ALU.mult), [bA2, bnwb], [bA2])

        for kc in range(8):
            kb.dma("pool", wo[:, kc, :], w_out[kc * 128:(kc + 1) * 128, :], [dr], [bwo])
        for ti in range(NT):
            rows = slice(ti * 128, (ti + 1) * 128)
            kb.dma("sp", xt[:], x[rows, :], [dr], [bxt])
            kb.dma("sp", yt[:], ycat[rows, :], [dr], [byt])
            kb.op("act", lambda e: e.copy(out=ybt, in_=yt[:]), [byt], [bR1])
            pt, bpt = bank(0)
            ptb = pt.bitcast(BF16)
            for kc in range(8):
                kb.op("pe", lambda e, kc=kc: e.transpose(out=ptb[:, kc * 128:(kc + 1) * 128],
                                                         in_=ybt[:, kc * 128:(kc + 1) * 128], identity=idb[:]),
                      [bR1, bidb], [bpt])
            kb.op("act", lambda e: e.copy(out=yT.rearrange("p a b -> p (a b)"), in_=ptb), [bpt], [bR1])
            for half in range(2):
                pp, bpp = bank(2 + half)
                for kc in range(8):
                    kb.op("pe", lambda e, kc=kc: e.matmul(pp, yT[:, kc, :], wo[:, kc, half * 512:(half + 1) * 512],
                                                          start=(kc == 0), stop=(kc == 7)), [bR1, bwo], [bpp])
                cs = slice(half * 512, (half + 1) * 512)
                kb.op("dve", lambda e: e.tensor_tensor(out=tmpA[:, cs], in0=pp, in1=g1[:, cs], op=ALU.mult),
                      [bpp, bg1], [btmpA])
            kb.op("dve", lambda e: e.tensor_tensor(out=xt[:], in0=tmpA[:], in1=xt[:], op=ALU.add), [btmpA, bxt], [bxt])
            kb.dma("sp", xo[rows, :], xt[:], [bxt], [bxo])
            kb.op("act", lambda e: e.activation(out=junk, in_=xt[:], func=AF.Square, accum_out=st[:, 0:1]),
                  [bxt], [bR2, bst])
            kb.op("act", lambda e: e.activation(out=st[:, 1:2], in_=st[:, 0:1], func=AF.Sqrt, scale=1.0 / D,
                                                bias=epst[:]), [bst, beps], [bst])
            kb.op("dve", lambda e: e.reciprocal(out=st[:, 2:3], in_=st[:, 1:2]), [bst], [bst])
            kb.op("dve", lambda e: e.scalar_tensor_tensor(out=tmpA[:], in0=xt[:], scalar=st[:, 2:3], in1=A2,
                                                          op0=ALU.mult, op1=ALU.mult), [bxt, bst, bA2], [btmpA])
            kb.op("dve", lambda e: e.tensor_tensor(out=tmpB[:], in0=tmpA[:], in1=sh2, op=ALU.add),
                  [btmpA, bsh2], [btmpB])
            kb.op("act", lambda e: e.copy(out=hb, in_=tmpB[:]), [btmpB], [bR2])
            pt, bpt = bank(1)
            ptb = pt.bitcast(BF16)
            for kc in range(8):
                kb.op("pe", lambda e, kc=kc: e.transpose(out=ptb[:, kc * 128:(kc + 1) * 128],
                                                         in_=hb[:, kc * 128:(kc + 1) * 128], identity=idb[:]),
                      [bR2, bidb], [bpt])
            kb.op("act", lambda e: e.copy(out=hT[:, :, rows], in_=ptb.rearrange("p (a b) -> p a b", a=8)),
                  [bpt], [bhT])
            for half in range(2):
                pq, bpq = bank(4 + half)
                for k4 in range(4):
                    kc = half * 4 + k4
                    kb.op("pe", lambda e, kc=kc, k4=k4: e.transpose(out=pq[:, k4 * 128:(k4 + 1) * 128],
                                                                    in_=tmpB[:, kc * 128:(kc + 1) * 128],
                                                                    identity=idf[:]), [btmpB, bidf], [bpq])
                kb.op("dve", lambda e: e.tensor_copy(
                    out=h32T[:, half * 4:(half + 1) * 4, :], in_=pq.rearrange("p (a b) -> p a b", a=4)),
                    [bpq], [bR1])
            pr, bpr = bank(6)
            for kc in range(8):
                kb.op("pe", lambda e, kc=kc: e.matmul(pr[:, 0:NE], h32T[:, kc, :], rwt[:, kc, :],
                                                      start=(kc == 0), stop=(kc == 7)), [bR1, brwt], [bpr])
            kb.op("dve", lambda e: e.tensor_tensor(out=lg[:], in0=pr[:, 0:NE], in1=rbb[:], op=ALU.add),
                  [bpr, brbb], [blg])
            kb.op("dve", lambda e: e.max(out=st[:, 8:16], in_=lg[:]), [blg], [bst])
            kb.op("dve", lambda e: e.tensor_scalar(out=st[:, 16:17], in0=st[:, 8:9], scalar1=-1.0, scalar2=None,
                                                   op0=ALU.mult), [bst], [bst])
            kb.op("act", lambda e: e.activation(out=st[:, 20:24], in_=st[:, 8:12], func=AF.Exp, bias=st[:, 16:17],
                                                accum_out=st[:, 17:18]), [bst], [bst])
            kb.op("dve", lambda e: e.reciprocal(out=st[:, 18:19], in_=st[:, 17:18]), [bst], [bst])
            kb.op("act", lambda e: e.activation(out=st[:, 32:64], in_=lg[:], func=AF.Exp, bias=st[:, 16:17]),
                  [blg, bst], [bst])
            kb.op("dve", lambda e: e.tensor_scalar(out=lg[:], in0=lg[:], scalar1=st[:, 11:12], scalar2=None,
                                                   op0=ALU.is_ge), [blg, bst], [blg])
            kb.op("dve", lambda e: e.scalar_tensor_tensor(out=lg[:], in0=st[:, 32:64], scalar=st[:, 18:19], in1=lg[:],
                                                          op0=ALU.mult, op1=ALU.mult), [bst, blg], [blg])
            pg, bpg = bank(7)
            kb.op("pe", lambda e: e.transpose(out=pg[0:NE, 0:128], in_=lg[:], identity=idf[:]), [blg, bidf], [bpg])
            kb.op("act", lambda e: e.copy(out=gateT[:, rows], in_=pg[0:NE, 0:128]), [bpg], [bgateT])

        kb.dma("sp", hT_o[:, :], R_hT[:].bitcast(BF16), [bhT], [bho])
        kb.dma("sp", gT_o[:, :], gateT[:], [bgateT], [bho])
        kb.dma("sp", g2_o[:, :], g2[:], [bg2], [bho])
    else:
        kb.dma("sp", R_hT[:].bitcast(BF16), hT_i[:, :], [dr], [bhT])
        kb.dma("sp", gateT[:], gT_i[:, :], [dr], [bgateT])
        kb.dma("sp", g2[:], g2_i[:, :], [dr], [bg2])
    merge_deps(bacc, [bwo])
    merge_deps(bactT, [b for _, b in modw_bufs])
    merge_deps(bgsb, [scb_t[1]] + [b for _, b in modb_bufs])
    for _, b in W1R + W2R:
        merge_deps(b, [bg1, bsh2, bA2, bnwb])
    tts = [(tmpA[:, 0:512], Buf("tt0")), (tmpA[:, 512:1024], Buf("tt1"))]
    lps = [(tmpB[:, 0:512], Buf("lp0")), (tmpB[:, 512:1024], Buf("lp1"))]
    lgs = [(xt[:, 0:512], Buf("lg0")), (xt[:, 512:1024], Buf("lg1"))]
    for (_, b), src_ in zip(tts + lps + lgs, [btmpA, btmpA, btmpB, btmpB, bxt, bxt]):
        merge_deps(b, [src_])
    C0 = float(LIMIT * ALPHA / (1.0 + np.exp(-LIMIT * ALPHA)))

    w1_issued = [0]
    w2_issued = [0]

    def issue_w1(upto):
        while w1_issued[0] < min(upto, n_exp * 8):
            i = w1_issued[0]
            t, b = W1R[i % 3]
            kb.dma("pool", t.rearrange("p a b -> p (a b)"), w1[i // 8, i % 8, :, :], [dr], [b])
            w1_issued[0] += 1

    def issue_w2(upto):
        while w2_issued[0] < min(upto, n_exp * 8):
            i = w2_issued[0]
            t, b = W2R[i % 3]
            kb.dma("pool", t.rearrange("p a b -> p (a b)"), w2[i // 8, i % 8, :, :], [dr], [b])
            w2_issued[0] += 1

    nu = 0
    ny = 0
    for e in range(e_lo, e_hi):
        selb, bselb = selbs[e % 2]
        kb.op("dve", lambda en: en.tensor_copy(out=selb[:], in_=idf[0:NE, e:e + 1].to_broadcast([NE, 128])),
              [bidf], [bselb])
        for blk in range(4):
            pgb, bpgb = bank(6)
            kb.op("pe", lambda en, blk=blk: en.matmul(pgb, selb[:], gateT[:, blk * 512:(blk + 1) * 512],
                                                      start=True, stop=True), [bselb, bgateT], [bpgb])
            kb.op("act", lambda en, blk=blk: en.activation(out=gsb[:, blk * 512:(blk + 1) * 512], in_=pgb,
                                                           func=AF.Copy, scale=1.0 / ALPHA), [bpgb], [bgsb])
        for ft in range(8):
            gi = (e - e_lo) * 8 + ft
            issue_w1(gi + 2)
            wv, bwt = W1R[gi % 3]
            bcol = (e * 8 + ft) * 2
            for blk in range(4):
                ts_ = slice(blk * 512, (blk + 1) * 512)
                pgl, bpgl = bank(0 + 2 * (nu % 2))
                pli, bpli = bank(1 + 2 * (nu % 2))
                for kc in range(8):
                    kb.op("pe", lambda en, kc=kc: en.matmul(pgl, wv[:, kc, 0:128], hT[:, kc, ts_],
                                                            start=(kc == 0), stop=(kc == 7)), [bwt, bhT], [bpgl])
                for kc in range(8):
                    kb.op("pe", lambda en, kc=kc: en.matmul(pli, wv[:, kc, 128:256], hT[:, kc, ts_],
                                                            start=(kc == 0), stop=(kc == 7)), [bwt, bhT], [bpli])
                tt, btt = tts[nu % 2]
                lp, blp = lps[nu % 2]
                lgg, blgg = lgs[nu % 2]
                kb.op("act", lambda en: en.activation(out=tt, in_=pgl, func=AF.Silu, scale=ALPHA,
                                                      bias=b1a[:, bcol:bcol + 1]), [bpgl, bb1a], [btt])
                kb.op("dve", lambda en: en.tensor_scalar(out=lp, in0=pli, scalar1=b1a[:, bcol + 1:bcol + 2],
                                                         scalar2=LIMIT + 1.0, op0=ALU.add, op1=ALU.min),
                      [bpli, bb1a], [blp])
                kb.op("dve", lambda en: en.scalar_tensor_tensor(out=lgg, in0=lp, scalar=1.0 - LIMIT, in1=gsb[:, ts_],
                                                                op0=ALU.max, op1=ALU.mult), [blp, bgsb], [blgg])
                kb.op("dve", lambda en: en.scalar_tensor_tensor(out=actT[:, ft, ts_], in0=tt, scalar=C0, in1=lgg,
                                                                op0=ALU.min, op1=ALU.mult), [btt, blgg], [bactT])
                nu += 1
        for dt in range(8):
            gi = (e - e_lo) * 8 + dt
            issue_w2(gi + 2)
            wv, bwt = W2R[gi % 3]
            for blk in range(4):
                ts_ = slice(blk * 512, (blk + 1) * 512)
                py, bpy = bank(4 + (ny % 2))
                for fc in range(8):
                    kb.op("pe", lambda en, fc=fc: en.matmul(py, wv[:, fc, :], actT[:, fc, ts_],
                                                            start=(fc == 0), stop=(fc == 7)), [bwt, bactT], [bpy])
                if e == e_lo:
                    kb.op("dve", lambda en: en.tensor_copy(out=acc[:, dt, ts_], in_=py), [bpy], [bacc])
                else:
                    kb.op("dve", lambda en: en.tensor_tensor(out=acc[:, dt, ts_], in0=py, in1=acc[:, dt, ts_],
                                                             op=ALU.add), [bpy, bacc], [bacc])
                ny += 1

    tf, btf = tts[0]
    for ti in range(NT if do_final else 0):
        rows = slice(ti * 128, (ti + 1) * 128)
        if first:
            kb.dma("sp", yt[:], xo[rows, :], [bxo], [byt])
        else:
            kb.dma("sp", yt[:], xprev[rows, :], [dr], [byt])
        for half in range(2):
            po, bpo = bank(half)
            for d4 in range(4):
                dt = half * 4 + d4
                cs = slice(d4 * 128, (d4 + 1) * 128)
                if first:
                    kb.op("pe", lambda en, dt=dt, cs=cs: en.matmul(po[:, cs], gateT[:, rows],
                                                                  b2t[:, dt * 128:(dt + 1) * 128],
                                                                  start=True, stop=False), [bgateT, bb2t], [bpo])
                kb.op("pe", lambda en, dt=dt, cs=cs: en.matmul(po[:, cs], acc[:, dt, rows], idf[:],
                                                              start=(not first), stop=True), [bacc, bidf], [bpo])
            cs2 = slice(half * 512, (half + 1) * 512)
            kb.op("dve", lambda en: en.tensor_tensor(out=tf, in0=po, in1=g2[:, cs2], op=ALU.mult),
                  [bpo, bg2], [btf])
            kb.op("dve", lambda en: en.tensor_tensor(out=yt[:, cs2], in0=tf, in1=yt[:, cs2], op=ALU.add),
                  [btf, byt], [byt])
        kb.dma("sp", xo[rows, :], yt[:], [byt], [bxo])
    kb.finish([bxo, bho])
    return nc


def prep_l4_weights(w1_l, b1_l, w2_l, b2_l):
    w1r = w1_l.reshape(NE, 8, 128, 8, 128, 2)
    w1r = w1r.transpose(0, 3, 2, 1, 5, 4)
    w1r = np.ascontiguousarray(w1r).reshape(NE, 8, 128, 8 * 256)
    b1r = b1_l.reshape(NE, 8, 128, 2).transpose(2, 0, 1, 3)
    b1r = np.ascontiguousarray(b1r).reshape(128, NE * 16)
    w2r = w2_l.reshape(NE, 8, 128, 8, 128).transpose(0, 3, 2, 1, 4)
    w2r = np.ascontiguousarray(w2r).reshape(NE, 8, 128, 8 * 128)
    return w1r, b1r, w2r, np.ascontiguousarray(b2_l)


def run_l4(x, ycat, c, mod_w_l, mod_b_l, normw_l, w_out_l, rw_l, rb_l, w1r, b1r, w2r, b2_l, splits=((0, 16), (16, 32))):
    ident = np.eye(128, dtype=np.float32)
    xprev = None
    for (lo, hi) in splits:
        first = (lo == 0)
        nc = _get("l4_%d_%d" % (lo, hi), lambda: build_l4(lo, hi, first))
        w1s = np.ascontiguousarray(w1r[lo:hi])
        w2s = np.ascontiguousarray(w2r[lo:hi])
        maps = []
        for cid in range(NCORES):
            m = {"x": core_tokens(x, cid), "ycat": core_tokens(ycat, cid), "cT": cT_of(c, cid // 4),
                 "mod_w": mod_w_l, "mod_b": mod_b_l.reshape(1, -1), "normw": normw_l.reshape(1, -1),
                 "w_out": w_out_l, "rw": rw_l, "rb": rb_l.reshape(1, -1), "w1": w1s, "b1": b1r, "w2": w2s,
                 "b2": b2_l, "ident": ident}
            if not first:
                m["xprev"] = xprev[cid]
                m["hT_i"] = hand[cid]["hT_o"]
                m["gT_i"] = hand[cid]["gT_o"]
                m["g2_i"] = hand[cid]["g2_o"]
            maps.append(m)
        res = _run(nc, maps)
        xprev = [r["xo"] for r in res.results]
        if first:
            hand = res.results
    return uncore_tokens(xprev, (D,))


NIT = 16
KSEL = 256
NEGBIG = -30000.0
NREL = 1280


def build_l3(nj=16):
    nc = bass.Bass("TRN2", target_bir_lowering=False)
    qT = _din(nc, "qT", [128, 16, 4 * 128], BF16)
    kT = _din(nc, "kT", [128, 4 * S], BF16)
    v1 = _din(nc, "v1", [64, 128, 8 * 65], BF16)
    qiT = _din(nc, "qiT", [64, 16, 8 * 128], BF16)
    kiT = _din(nc, "kiT", [64, S], BF16)
    wi = _din(nc, "wi", [128, 128])
    pen = _din(nc, "pen", [128, 512])
    oh = _din(nc, "oh", [32, NREL])
    relb = _din(nc, "relb", [32, 8])
    negI4 = _din(nc, "negI4", [128, 512], BF16)
    ident = _din(nc, "ident", [128, 128])
    yb = _dout(nc, "yb", [TPC, 512])
    scr = nc.dram_tensor("scr", [8, NREL], BF16, kind="Internal").ap()
    kb = KB(nc)
    dr = Buf("dram_in")
    byb = Buf("yb")
    bscr = Buf("scr")

    PS = []
    for i in range(4):
        t = nc.alloc_psum_tensor("PS%d" % i, [128, 1024], F32)
        PS.append((t, Buf("ps%da" % i), Buf("ps%db" % i)))

    def bank(i):
        t, ba, bb = PS[i // 2]
        return (t[:, 0:512], ba) if i % 2 == 0 else (t[:, 512:1024], bb)

    kTt, bkT = kb.sb([128, 4 * S], BF16, "kT")
    for i in range(4):
        kb.dma("sp", kTt[:, i * S:(i + 1) * S], kT[:, i * S:(i + 1) * S], [dr], [bkT])
    kTv = kTt[:].rearrange("p (h s) -> p h s", h=4)
    kibs = [kb.sb([64, 512], BF16, "kib") for _ in range(3)]
    nkb = [0]
    wit, bwi = kb.sb([128, 128], F32, "wi")
    kb.dma("sp", wit[:], wi[:, :], [dr], [bwi])
    pent, bpen = kb.sb([128, 512], F32, "pen")
    kb.dma("sp", pent[:], pen[:, :], [dr], [bpen])
    n4, bn4 = kb.sb([128, 512], BF16, "negI4")
    kb.dma("sp", n4[:], negI4[:, :], [dr], [bn4])
    idf, bidf = kb.sb([128, 128], F32, "identf")
    kb.dma("sp", idf[:], ident[:, :], [dr], [bidf])
    idb, bidb = kb.sb([128, 128], BF16, "identb")
    kb.op("dve", lambda e: e.tensor_copy(out=idb[:], in_=idf[:]), [bidf], [bidb])
    zb, bzb = kb.sb([128, 260], BF16, "zeros")
    kb.op("dve", lambda e: e.memset(zb[:], 0.0), [], [bzb])

    scores = [kb.sb([128, S], F32, "score") for _ in range(2)]
    oht, boh = rview(scores[1][0], 0, [32, NREL], F32), Buf("oh")
    kb.dma("sp", oht, oh[:, :], [dr], [boh])
    rbt, brb = kb.sb([32, 8], F32, "relb")
    kb.dma("sp", rbt[:], relb[:, :], [dr], [brb])
    bv, bbv = rview(scores[1][0], 8192, [8, NREL], BF16), Buf("bvec")
    for i, (c0, n) in enumerate([(0, 512), (512, 512), (1024, 256)]):
        pb, bpb = bank(7)
        kb.op("pe", lambda e: e.matmul(pb[0:8, 0:n], rbt[:], oht[:, c0:c0 + n], start=True, stop=True),
              [brb, boh], [bpb])
        kb.op("dve", lambda e: e.tensor_copy(out=bv[:, c0:c0 + n], in_=pb[0:8, 0:n]), [bpb], [bbv])
    kb.dma("sp", scr[:, :], bv, [bbv], [bscr])
    merge_deps(scores[1][1], [boh, bbv])
    TU, bTU = kb.sb([128, 9, 8 * 128], BF16, "TU")
    for u in range(9):
        src = bass.AP(tensor=scr.tensor, offset=128 * u, ap=[[1, 128], [NREL, 8], [1, 128]])
        kb.dma("sp", TU[:, u, :].rearrange("p (h t) -> p h t", h=8), src, [bscr], [bTU])

    nots = [kb.sb([128, S], BF16, "notsel") for _ in range(2)]
    qts = [kb.sb([128, 512], BF16, "qTj") for _ in range(2)]
    qis = [kb.sb([64, 1024], BF16, "qiTj")] * 2
    dgs = [kb.sb([128, 1024], BF16, "Dg")] * 2
    rhs_ = [kb.sb([128, 512], BF16, "rh") for _ in range(4)]
    pts = [kb.sb([128, 512], BF16, "pt") for _ in range(2)]
    vts = [kb.sb([128, 520], BF16, "v1t") for _ in range(3)]
    outs = [kb.sb([128, 512], F32, "yo")] * 2
    st, bst = kb.sb([128, 32], F32, "st")

    nr = [0]
    nd = [0]
    nv = [0]
    nsc = [0]
    LAG = 3
    vts.append(kb.sb([128, 520], BF16, "v1t"))
    pts2 = [[pts[0], kb.sb([128, 512], BF16, "pt")], [pts[1], kb.sb([128, 512], BF16, "pt")]]

    def idx(j):
        qi_t, bqi = qis[j % 2]
        kb.dma("sp", qi_t[:], qiT[:, j, :], [dr], [bqi])
        dg, bdg = dgs[j % 2]
        for h in range(8):
            kb.op("pool", lambda e, h=h: e.tensor_scalar(out=dg[:, h * 128:(h + 1) * 128], in0=idf[:],
                                                         scalar1=wit[:, j * 8 + h:j * 8 + h + 1], scalar2=None,
                                                         op0=ALU.mult), [bidf, bwi], [bdg])
        score, bscore = scores[j % 2]
        steps = [(sb, h) for sb in range(j + 1) for h in range(8)]
        pend = []
        sbank = {}
        kibt = {}
        for s in range(len(steps) + LAG):
            if s < len(steps):
                sb, h = steps[s]
                if h == 0:
                    kibt[sb] = kibs[nkb[0] % 3]
                    nkb[0] += 1
                    kb.dma("sp", kibt[sb][0][:], kiT[:, sb * 512:(sb + 1) * 512], [dr], [kibt[sb][1]])
                kit, bki = kibt[sb]
                pd, bpd = bank(nd[0] % 4)
                nd[0] += 1
                kb.op("pe", lambda e, h=h, kit=kit: e.matmul(pd, qi_t[:, h * 128:(h + 1) * 128], kit[:],
                                                             start=True, stop=True), [bqi, bki], [bpd])
                rh, brh = rhs_[nr[0] % 4]
                nr[0] += 1
                kb.op("act", lambda e: e.activation(out=rh[:], in_=pd, func=AF.Relu), [bpd], [brh])
                pend.append((sb, h, rh, brh))
            if s - LAG >= 0:
                sb, h, rh, brh = pend[s - LAG]
                if h == 0:
                    sbank[sb] = bank(6 + nsc[0] % 2)
                    nsc[0] += 1
                ps, bps = sbank[sb]
                kb.op("pe", lambda e, h=h: e.matmul(ps, dg[:, h * 128:(h + 1) * 128], rh[:],
                                                    start=(h == 0), stop=(h == 7)), [bdg, brh], [bps])
                if h == 7:
                    kb.op("act", lambda e, sb=sb: e.copy(out=score[:, sb * 512:(sb + 1) * 512], in_=ps),
                          [bps], [bscore])

    def bis(j):
        n = 512 * (j + 1)
        ns, bns = nots[j % 2]
        score, bscore = scores[j % 2]
        sc = score[:, 0:n]
        kb.op("dve", lambda e: e.tensor_reduce(out=st[:, 0:1], in_=sc, axis=AX.X, op=ALU.min), [bscore], [bst])
        kb.op("dve", lambda e: e.tensor_tensor(out=score[:, n - 512:n], in0=score[:, n - 512:n], in1=pent[:],
                                               op=ALU.add), [bscore, bpen], [bscore])
        kb.op("dve", lambda e: e.tensor_reduce(out=st[:, 1:2], in_=sc, axis=AX.X, op=ALU.max), [bscore], [bst])
        kb.op("dve", lambda e: e.tensor_tensor(out=st[:, 2:3], in0=st[:, 1:2], in1=st[:, 0:1], op=ALU.subtract),
              [bst], [bst])
        kb.op("dve", lambda e: e.scalar_tensor_tensor(out=st[:, 3:4], in0=st[:, 2:3], scalar=-0.01, in1=st[:, 0:1],
                                                      op0=ALU.mult, op1=ALU.add), [bst], [bst])
        kb.op("dve", lambda e: e.tensor_scalar(out=st[:, 3:4], in0=st[:, 3:4], scalar1=-1e-6, scalar2=None,
                                               op0=ALU.add), [bst], [bst])
        kb.op("dve", lambda e: e.tensor_tensor(out=st[:, 4:5], in0=st[:, 1:2], in1=st[:, 3:4], op=ALU.subtract),
              [bst], [bst])
        kb.op("dve", lambda e: e.scalar_tensor_tensor(out=st[:, 5:6], in0=st[:, 4:5], scalar=0.5, in1=st[:, 3:4],
                                                      op0=ALU.mult, op1=ALU.add), [bst], [bst])
        kb.op("dve", lambda e: e.tensor_scalar(out=st[:, 6:7], in0=st[:, 4:5], scalar1=0.25, scalar2=None,
                                               op0=ALU.mult), [bst], [bst])
        for it in range(NIT):
            kb.op("dve", lambda e: e.tensor_scalar(out=ns[:, 0:n], in0=sc, scalar1=st[:, 5:6], scalar2=None,
                                                   op0=ALU.is_ge, op1=ALU.add, accum_out=st[:, 7:8]),
                  [bscore, bst], [bns, bst])
            kb.op("dve", lambda e: e.tensor_scalar(out=st[:, 8:9], in0=st[:, 7:8], scalar1=KSEL - 0.5, scalar2=2.0,
                                                   op0=ALU.is_ge, op1=ALU.mult), [bst], [bst])
            kb.op("dve", lambda e: e.scalar_tensor_tensor(out=st[:, 9:10], in0=st[:, 8:9], scalar=-1.0,
                                                          in1=st[:, 6:7], op0=ALU.add, op1=ALU.mult), [bst], [bst])
            kb.op("dve", lambda e: e.tensor_tensor(out=st[:, 5:6], in0=st[:, 5:6], in1=st[:, 9:10], op=ALU.add),
                  [bst], [bst])
            kb.op("dve", lambda e: e.tensor_scalar(out=st[:, 6:7], in0=st[:, 6:7], scalar1=0.5, scalar2=None,
                                                   op0=ALU.mult), [bst], [bst])
        kb.op("dve", lambda e: e.scalar_tensor_tensor(out=st[:, 10:11], in0=st[:, 6:7], scalar=-4.0, in1=st[:, 5:6],
                                                      op0=ALU.mult, op1=ALU.add), [bst], [bst])
        kb.op("dve", lambda e: e.tensor_scalar(out=ns[:, 0:n], in0=sc, scalar1=st[:, 10:11], scalar2=None,
                                               op0=ALU.is_lt), [bscore, bst], [bns])

    def att_main(j):
        ns, bns = nots[j % 2]
        qt, bqt = qts[j % 2]
        kb.dma("sp", qt[:], qT[:, j, :], [dr], [bqt])
        oacc = [bank(4), bank(5)]
        for g in range(2):
            oa, boa = oacc[g]
            kb.op("pe", lambda e: e.matmul(oa[:, 0:260], idb[:], zb[:], start=True, stop=False),
                  [bidb, bzb], [boa])
        ntile = 4 * j + 4
        vtl = {}
        for stl in range(ntile + 1):
            if stl < ntile:
                vt, bvt = vts[nv[0] % 4]
                nv[0] += 1
                vtl[stl] = (vt, bvt)
                kb.dma("sp", vt[:], v1[stl, :, :], [dr], [bvt])
                u = stl - 4 * j + 5
                for g in range(2):
                    lgp, blgp = bank(2 * g + stl % 2)
                    kb.op("pe", lambda e: e.matmul(lgp, ns[:, stl * 128:(stl + 1) * 128], n4[:], start=True,
                                                   stop=False), [bns, bn4], [blgp])
                    if u >= 0:
                        kb.op("pe", lambda e, u=u: e.matmul(lgp, idb[:], TU[:, u, g * 512:(g + 1) * 512],
                                                            start=False, stop=False), [bidb, bTU], [blgp])
                    for hq in range(4):
                        kb.op("pe", lambda e, hq=hq: e.matmul(lgp[:, hq * 128:(hq + 1) * 128],
                                                              kTv[g * 64:(g + 1) * 64, hq, stl * 128:(stl + 1) * 128],
                                                              qt[g * 64:(g + 1) * 64, hq * 128:(hq + 1) * 128],
                                                              start=False, stop=(hq == 3)), [bkT, bqt], [blgp])
                    pt, bpt = pts2[g][stl % 2]
                    kb.op("act", lambda e: e.activation(out=pt[:], in_=lgp, func=AF.Exp), [blgp], [bpt])
            if stl >= 1:
                sp_ = stl - 1
                vt, bvt = vtl[sp_]
                for g in range(2):
                    pt, bpt = pts2[g][sp_ % 2]
                    oa, boa = oacc[g]
                    for hq in range(4):
                        h = g * 4 + hq
                        kb.op("pe", lambda e, hq=hq, h=h: e.matmul(oa[:, hq * 65:(hq + 1) * 65],
                                                                   pt[:, hq * 128:(hq + 1) * 128],
                                                                   vt[:, h * 65:(h + 1) * 65],
                                                                   start=False, stop=(sp_ == ntile - 1 and hq == 3)),
                              [bpt, bvt], [boa])

    def att_fin(j):
        oacc = [bank(4), bank(5)]
        yo, byo = outs[j % 2]
        for g in range(2):
            oa, boa = oacc[g]
            oav = oa[:, 0:260].rearrange("p (h c) -> p h c", h=4)
            kb.op("dve", lambda e: e.reciprocal(out=st[:, 16 + g * 4:20 + g * 4],
                                                in_=oav[:, :, 64:65].rearrange("p h c -> p (h c)")), [boa], [bst])
            kb.op("dve", lambda e: e.tensor_tensor(
                out=yo[:, g * 256:(g + 1) * 256].rearrange("p (h d) -> p h d", h=4), in0=oav[:, :, 0:64],
                in1=st[:, 16 + g * 4:20 + g * 4].unsqueeze(2).to_broadcast([128, 4, 64]), op=ALU.mult),
                [boa, bst], [byo])
        kb.dma("sp", yb[j * 128:(j + 1) * 128, :], yo[:], [byo], [byb])

    idx(0)
    if nj > 1:
        idx(1)
    bis(0)
    for j in range(nj):
        if j + 2 < nj:
            idx(j + 2)
        att_main(j)
        if j + 1 < nj:
            bis(j + 1)
        att_fin(j)
    kb.finish([byb])
    return nc


def t5_bucket_np(rel):
    nb = 16
    max_exact = 8
    side = np.where(rel > 0, nb, 0)
    n = np.abs(rel)
    nf = np.maximum(n, 1).astype(np.float32)
    large = max_exact + (np.log(nf / max_exact) / np.float32(np.log(1024 / max_exact)) * (nb - max_exact)).astype(np.int32)
    large = np.minimum(large, nb - 1)
    return side + np.where(n < max_exact, n, large)


def prep_l3(ob, of, cid):
    b, r = cid // 4, cid % 4
    bf = ob.dtype
    qsel = ob[b].reshape(64, 128, NB1)[r::4][:, ::-1]
    q = qsel[..., 0:512].reshape(16, 128, 2, 4, 64)
    qT = np.ascontiguousarray(q.transpose(2, 4, 0, 3, 1)).reshape(128, 16, 512)
    qi = qsel[..., 1536:2048].reshape(16, 128, 8, 64)
    qiT = np.ascontiguousarray(qi.transpose(3, 0, 2, 1)).reshape(64, 16, 1024)
    wsel = of[b].reshape(64, 128, NF1)[r::4][:, ::-1, 2056:2064]
    wi = np.ascontiguousarray(wsel.transpose(1, 0, 2)).reshape(128, 128).astype(np.float32)
    k = ob[b, :, 512:1024].reshape(S, 2, 4, 64)
    kT = np.ascontiguousarray(k.transpose(1, 3, 2, 0)).reshape(128, 4 * S)
    kiT = np.ascontiguousarray(ob[b, :, 2048:2112].T)
    v = ob[b, :, 1024:1536].reshape(64, 128, 8, 64)
    v1 = np.ones((64, 128, 8, 65), bf)
    v1[..., 0:64] = v
    v1 = v1.reshape(64, 128, 520)
    tq = np.arange(128)[:, None]
    sk = np.arange(512)[None, :]
    pen = np.where((sk // 64) <= 2 * r + (tq < 64), 0.0, -1e30).astype(np.float32)
    m = np.arange(NREL)
    rel = m - 767 - 128 * r
    bk = t5_bucket_np(rel)
    oh = np.zeros((32, NREL), np.float32)
    oh[bk, m] += 1.0
    oh[15, :] -= 1.0
    negI4 = np.tile(np.eye(128, dtype=np.float32) * NEGBIG, (1, 4)).astype(bf)
    return {"qT": qT, "kT": kT, "v1": v1, "qiT": qiT, "kiT": kiT, "wi": wi, "pen": pen, "oh": oh,
            "negI4": negI4, "ident": np.eye(128, dtype=np.float32)}


def run_l3(ob, of, rel_bias, nj=16):
    nc = _get("l3_%d" % nj, lambda: build_l3(nj))
    maps = []
    for cid in range(NCORES):
        m = prep_l3(ob, of, cid)
        m["relb"] = np.ascontiguousarray(rel_bias.astype(np.float32))
        maps.append(m)
    res = _run(nc, maps)
    parts = [r["yb"].reshape(16, 128, 512)[:, ::-1].reshape(TPC, 512) for r in res.results]
    return uncore_tokens(parts, (512,))


NCH = 64
PRE_STOP = 0
L2VAR = 0
POOL_ENG = "dve"


def build_l2(nch=NCH, stop=None):
    nc = bass.Bass("TRN2", target_bir_lowering=False)
    xin = _din(nc, "xin", [128, 3, S + 3])
    cw = _din(nc, "cw", [128, 12])
    zin = _din(nc, "zin", [128, NCH, 128])
    bcol = _din(nc, "bcol", [128, NCH])
    acol = _din(nc, "acol", [128, NCH])
    sc3 = _din(nc, "sc3", [1, 2])
    gnw = _din(nc, "gnw", [1, 128])
    cst = _din(nc, "cst", [128, 7, 128])
    ya = _dout(nc, "ya", [S, 128])
    kb = KB(nc)
    dr = Buf("dram_in")
    bya = Buf("ya")

    PSW = []
    for i in range(4):
        t = nc.alloc_psum_tensor("PS%d" % i, [128, 1024], F32)
        PSW.append(t)
    slots = []
    for bnk in range(6):
        t = PSW[bnk // 2]
        c0 = (bnk % 2) * 512
        slots.append((t[:, c0:c0 + 128], Buf("slot%d" % bnk)))
    wide = [(PSW[3][:, 0:512], Buf("wide0")), (PSW[3][:, 512:1024], Buf("wide1"))]
    nslot = [0]

    def slot():
        s = slots[nslot[0] % len(slots)]
        nslot[0] += 1
        return s

    ct, bct = kb.sb([128, 7, 128], F32, "cst")
    kb.dma("sp", ct[:], cst[:, :, :], [dr], [bct])
    ident, LT, ones, negones, penL, SM, sel127 = [ct[:, i, :] for i in range(7)]
    cwt, bcw = kb.sb([128, 12], F32, "cw")
    kb.dma("sp", cwt[:], cw[:, :], [dr], [bcw])
    gnb, bgnb = kb.sb([128, 128], F32, "gnw")
    kb.dma("sp", gnb[:], gnw[0:1, :].to_broadcast([128, 128]), [dr], [bgnb])
    s3, bs3 = kb.sb([128, 2], F32, "sc3")
    kb.dma("sp", s3[:], sc3[0:1, :].to_broadcast([128, 2]), [dr], [bs3])
    epst, beps = kb.sb([128, 3], F32, "eps")
    kb.op("dve", lambda e: e.memset(epst[:, 0:1], EPS), [], [beps])
    kb.op("dve", lambda e: e.memset(epst[:, 1:2], 128.0 * EPS), [], [beps])
    kb.op("dve", lambda e: e.memset(epst[:, 2:3], 1.0), [], [beps])

    cols, bcols = kb.sb([128, 10, NCH], F32, "cols")
    BETA, G, GC, EGC, BG, KD, EGL, NB_, TMP, TMP2 = range(10)
    kb.dma("sp", cols[:, BETA, :], bcol[:, :], [dr], [bcols])
    kb.dma("sp", cols[:, TMP, :], acol[:, :], [dr], [bcols])
    kb.op("act", lambda e: e.activation(out=cols[:, BETA, :], in_=cols[:, BETA, :], func=AF.Sigmoid), [bcols], [bcols])
    kb.op("act", lambda e: e.activation(out=cols[:, TMP, :], in_=cols[:, TMP, :], func=AF.Exp, bias=s3[:, 1:2]),
          [bcols, bs3], [bcols])
    kb.op("act", lambda e: e.activation(out=cols[:, TMP, :], in_=cols[:, TMP, :], func=AF.Ln, bias=epst[:, 2:3]),
          [bcols, beps], [bcols])
    kb.op("act", lambda e: e.activation(out=s3[:, 0:1], in_=s3[:, 0:1], func=AF.Exp), [bs3], [bs3])
    kb.op("dve", lambda e: e.tensor_scalar(out=cols[:, G, :], in0=cols[:, TMP, :], scalar1=s3[:, 0:1], scalar2=-1.0,
                                           op0=ALU.mult, op1=ALU.mult), [bcols, bs3], [bcols])
    pg, bpg = slot()
    kb.op("pe", lambda e: e.matmul(pg[:, 0:NCH], LT, cols[:, G, :], start=True, stop=True), [bct, bcols], [bpg])
    kb.op("dve", lambda e: e.tensor_copy(out=cols[:, GC, :], in_=pg[:, 0:NCH]), [bpg], [bcols])
    pg2, bpg2 = slot()
    kb.op("pe", lambda e: e.matmul(pg2[:, 0:NCH], sel127, cols[:, GC, :], start=True, stop=True), [bct, bcols], [bpg2])
    kb.op("dve", lambda e: e.tensor_copy(out=cols[:, TMP, :], in_=pg2[:, 0:NCH]), [bpg2], [bcols])
    kb.op("act", lambda e: e.activation(out=cols[:, EGL, :], in_=cols[:, TMP, :], func=AF.Exp), [bcols], [bcols])
    kb.op("dve", lambda e: e.tensor_tensor(out=cols[:, TMP2, :], in0=cols[:, TMP, :], in1=cols[:, GC, :], op=ALU.subtract),
          [bcols], [bcols])
    kb.op("act", lambda e: e.activation(out=cols[:, KD, :], in_=cols[:, TMP2, :], func=AF.Exp), [bcols], [bcols])
    kb.op("act", lambda e: e.activation(out=cols[:, EGC, :], in_=cols[:, GC, :], func=AF.Exp), [bcols], [bcols])
    kb.op("dve", lambda e: e.tensor_tensor(out=cols[:, BG, :], in0=cols[:, EGC, :], in1=cols[:, BETA, :], op=ALU.mult),
          [bcols], [bcols])
    kb.op("dve", lambda e: e.tensor_scalar(out=cols[:, NB_, :], in0=cols[:, BETA, :], scalar1=-1.0, scalar2=None,
                                           op0=ALU.mult), [bcols], [bcols])

    if stop == "cols":
        kb.dma("sp", ya[0:128, 0:NCH], cols[:, GC, :], [bcols], [bya])
        kb.finish([bya])
        return nc
    QT, bQT = kb.sb([128, S], F32, "QT")
    KT, bKT = kb.sb([128, S], F32, "KT")
    Vtok, bVtok = kb.sb([128, NCH, 128], F32, "Vtok")
    Ktok, bKtok = kb.sb([128, NCH, 128], F32, "Ktok")
    xbs = [kb.sb([128, 3, 515], F32, "xb") for _ in range(2)]
    u, bu = kb.sb([128, 3, 512], F32, "u")
    sqt, bsq = kb.sb([128, 512], F32, "sq")
    rs, brs = kb.sb([128, 512], F32, "rs")
    nblk = (nch * 128 + 511) // 512
    for blk in range(nblk):
        xb, bxb = xbs[blk % 2]
        kb.dma("sp", xb[:], xin[:, :, blk * 512:blk * 512 + 515], [dr], [bxb])
        for a in range(3):
            kb.op("dve", lambda e, a=a: e.tensor_scalar(out=u[:, a, :], in0=xb[:, a, 0:512],
                                                        scalar1=cwt[:, a * 4:a * 4 + 1], scalar2=None, op0=ALU.mult),
                  [bxb, bcw], [bu])
            for tap in range(1, 4):
                kb.op("dve", lambda e, a=a, tap=tap: e.scalar_tensor_tensor(
                    out=u[:, a, :], in0=xb[:, a, tap:tap + 512], scalar=cwt[:, a * 4 + tap:a * 4 + tap + 1],
                    in1=u[:, a, :], op0=ALU.mult, op1=ALU.add), [bxb, bcw, bu], [bu])
        kb.op("act", lambda e: e.activation(out=u[:].rearrange("p a n -> p (a n)"),
                                            in_=u[:].rearrange("p a n -> p (a n)"), func=AF.Silu), [bu], [bu])
        cs = slice(blk * 512, (blk + 1) * 512)
        for a, (dst, bdst, scl, epi) in enumerate([(QT, bQT, 128.0, 1), (KT, bKT, 1.0, 0)]):
            kb.op("dve", lambda e, a=a: e.tensor_tensor(out=sqt[:], in0=u[:, a, :], in1=u[:, a, :], op=ALU.mult),
                  [bu], [bsq])
            pw, bpw = wide[a]
            kb.op("pe", lambda e: e.matmul(pw, ones, sqt[:], start=True, stop=True), [bct, bsq], [bpw])
            kb.op("act", lambda e, scl=scl, epi=epi: e.activation(out=rs[:], in_=pw, func=AF.Sqrt, scale=scl,
                                                                  bias=epst[:, epi:epi + 1]), [bpw, beps], [brs])
            kb.op("dve", lambda e: e.reciprocal(out=rs[:], in_=rs[:]), [brs], [brs])
            kb.op("dve", lambda e, a=a, dst=dst: e.tensor_tensor(out=dst[:, cs], in0=u[:, a, :], in1=rs[:], op=ALU.mult),
                  [bu, brs], [bdst])
        for q4 in range(4):
            ch = blk * 4 + q4
            if ch >= nch:
                break
            pk, bpk = slot()
            kb.op("pe", lambda e, ch=ch: e.transpose(out=pk, in_=KT[:, ch * 128:(ch + 1) * 128], identity=ident),
                  [bKT, bct], [bpk])
            kb.op("act", lambda e, ch=ch: e.copy(out=Ktok[:, ch, :], in_=pk), [bpk], [bKtok])
            pv, bpv = slot()
            kb.op("pe", lambda e, q4=q4: e.transpose(out=pv, in_=u[:, 2, q4 * 128:(q4 + 1) * 128], identity=ident),
                  [bu, bct], [bpv])
            kb.op("act", lambda e, ch=ch: e.copy(out=Vtok[:, ch, :], in_=pv), [bpv], [bVtok])

    if stop == "prep":
        kb.dma("sp", ya[0:128, :], Ktok[:, 0, :], [bKtok], [bya])
        kb.dma("sp", ya[128:256, :], Vtok[:, 0, :], [bVtok], [bya])
        kb.dma("sp", ya[256:384, :], QT[:, 0:128], [bQT], [bya])
        kb.finish([bya])
        return nc
    RING = 4
    ring = [dict(wdT=kb.sb([128, 128], F32, "wdT"), uval=kb.sb([128, 128], F32, "uval"),
                 attnT=kb.sb([128, 128], F32, "attnT"), kdec=kb.sb([128, 128], F32, "kdec")) for _ in range(RING)]
    tmps = {}

    def tmp(name, k=2):
        if name not in tmps:
            tmps[name] = [kb.sb([128, 128], F32, name) for _ in range(k)]
            tmps[name + "_i"] = 0
        i = tmps[name + "_i"]
        tmps[name + "_i"] = i + 1
        return tmps[name][i % k]

    def pre(n):
        par = "_%d" % (n % 2)
        R = ring[n % RING]
        kt = KT[:, n * 128:(n + 1) * 128]
        qt = QT[:, n * 128:(n + 1) * 128]
        dg, bdg = tmp("diag" + par)
        kb.op("dve", lambda e: e.tensor_scalar(out=dg[:], in0=ident, scalar1=cols[:, GC, n:n + 1], scalar2=None,
                                               op0=ALU.mult), [bct, bcols], [bdg])
        pD, bpD = slot()
        kb.op("pe", lambda e: e.matmul(pD, dg[:], ones, start=True, stop=False), [bdg, bct], [bpD])
        kb.op("pe", lambda e: e.matmul(pD, negones, dg[:], start=False, stop=True), [bdg, bct], [bpD])
        Dl, bDl = tmp("Dl" + par)
        E, bE = tmp("E" + par)
        kb.op("dve", lambda e: e.tensor_tensor(out=Dl[:], in0=pD, in1=penL, op=ALU.min), [bpD, bct], [bDl])
        kb.op("act", lambda e: e.activation(out=E[:], in_=Dl[:], func=AF.Exp), [bDl], [bE])
        yield
        Es, bEs = tmp("Es" + par)
        kb.op(POOL_ENG, lambda e: e.tensor_tensor(out=Es[:], in0=E[:], in1=SM, op=ALU.mult), [bE, bct], [bEs])
        pA, bpA = slot()
        kb.op("pe", lambda e: e.matmul(pA, kt, kt, start=True, stop=True), [bKT], [bpA])
        Nm, bN = tmp("N" + par, 3)
        kb.op("dve", lambda e: e.scalar_tensor_tensor(out=Nm[:], in0=pA, scalar=cols[:, NB_, n:n + 1], in1=Es[:],
                                                      op0=ALU.mult, op1=ALU.mult), [bpA, bcols, bEs], [bN])
        yield
        pM, bpM = slot()
        kb.op("pe", lambda e: e.transpose(out=pM, in_=Nm[:], identity=ident), [bN, bct], [bpM])
        Mm, bM = tmp("M" + par, 3)
        kb.op("act", lambda e: e.copy(out=Mm[:], in_=pM), [bpM], [bM])
        P, bP = tmp("P" + par, 3)
        kb.op("dve", lambda e: e.tensor_tensor(out=P[:], in0=Mm[:], in1=ident, op=ALU.add), [bM, bct], [bP])
        yield
        pQK, bpQK = slot()
        kb.op("pe", lambda e: e.matmul(pQK, qt, kt, start=True, stop=True), [bQT, bKT], [bpQK])
        at, bat = tmp("attn" + par)
        kb.op("dve", lambda e: e.tensor_tensor(out=at[:], in0=pQK, in1=E[:], op=ALU.mult), [bpQK, bE], [bat])
        pAT, bpAT = slot()
        kb.op("pe", lambda e: e.transpose(out=pAT, in_=at[:], identity=ident), [bat, bct], [bpAT])
        aT, baT = R["attnT"]
        kb.op("act", lambda e: e.copy(out=aT[:], in_=pAT), [bpAT], [baT])
        yield
        for lev in range(1, 7):
            pN2, bpN2 = slot()
            kb.op("pe", lambda e: e.matmul(pN2, Mm[:], Nm[:], start=True, stop=True), [bM, bN], [bpN2])
            N2, bN2 = tmp("N" + par, 3)
            kb.op("act", lambda e: e.copy(out=N2[:], in_=pN2), [bpN2], [bN2])
            if lev < 6:
                pM2, bpM2 = slot()
                kb.op("pe", lambda e: e.matmul(pM2, Nm[:], Mm[:], start=True, stop=True), [bM, bN], [bpM2])
                M2, bM2 = tmp("M" + par, 3)
                kb.op("act", lambda e: e.copy(out=M2[:], in_=pM2), [bpM2], [bM2])
            yield
            pP, bpP = slot()
            kb.op("pe", lambda e: e.matmul(pP, N2[:], P[:], start=True, stop=True), [bN2, bP], [bpP])
            P2, bP2 = tmp("P" + par, 3)
            kb.op("dve", lambda e: e.tensor_tensor(out=P2[:], in0=pP, in1=P[:], op=ALU.add), [bpP, bP], [bP2])
            P, bP = P2, bP2
            yield
            Nm, bN = N2, bN2
            if lev < 6:
                Mm, bM = M2, bM2
        yield
        kbg, bkbg = tmp("kbg" + par)
        kb.op(POOL_ENG, lambda e: e.tensor_scalar(out=kbg[:], in0=Ktok[:, n, :], scalar1=cols[:, BG, n:n + 1],
                                                scalar2=None, op0=ALU.mult), [bKtok, bcols], [bkbg])
        vb, bvb = tmp("vb" + par)
        kb.op(POOL_ENG, lambda e: e.tensor_scalar(out=vb[:], in0=Vtok[:, n, :], scalar1=cols[:, BETA, n:n + 1],
                                                scalar2=None, op0=ALU.mult), [bVtok, bcols], [bvb])
        kd, bkd = R["kdec"]
        kb.op(POOL_ENG, lambda e: e.tensor_scalar(out=kd[:], in0=Ktok[:, n, :], scalar1=cols[:, KD, n:n + 1],
                                                scalar2=None, op0=ALU.mult), [bKtok, bcols], [bkd])
        pW, bpW = slot()
        kb.op("pe", lambda e: e.matmul(pW, kbg[:], P[:], start=True, stop=True), [bkbg, bP], [bpW])
        wd, bwd = R["wdT"]
        kb.op("act", lambda e: e.copy(out=wd[:], in_=pW), [bpW], [bwd])
        pU, bpU = slot()
        kb.op("pe", lambda e: e.matmul(pU, P[:], vb[:], start=True, stop=True), [bP, bvb], [bpU])
        uv, buv = R["uval"]
        kb.op("act", lambda e: e.copy(out=uv[:], in_=pU), [bpU], [buv])

    states = [kb.sb([128, 128], F32, "state") for _ in range(2)]
    kb.op("dve", lambda e: e.memset(states[0][0][:], 0.0), [], [states[0][1]])
    zts = [kb.sb([128, 128], F32, "zt") for _ in range(2)]
    st, bst = kb.sb([128, 8], F32, "st")
    junk, bjunk = kb.sb([128, 128], F32, "junk")

    def scan(n):
        R = ring[n % RING]
        wd, bwd = R["wdT"]
        uv, buv = R["uval"]
        aT, baT = R["attnT"]
        kd, bkd = R["kdec"]
        S0, bS0 = states[n % 2]
        S1, bS1 = states[(n + 1) % 2]
        zt, bzt = zts[n % 2]
        kb.dma("sp", zt[:], zin[:, n, :], [dr], [bzt])
        ppv, bppv = slot()
        kb.op("pe", lambda e: e.matmul(ppv, wd[:], S0[:], start=True, stop=True), [bwd, bS0], [bppv])
        po1, bpo1 = wide[0][0][:, 0:128], wide[0][1]
        kb.op("pe", lambda e: e.matmul(po1, QT[:, n * 128:(n + 1) * 128], S0[:], start=True, stop=True),
              [bQT, bS0], [bpo1])
        vn, bvn = tmp("vnew")
        kb.op("dve", lambda e: e.tensor_tensor(out=vn[:], in0=uv[:], in1=ppv, op=ALU.subtract), [buv, bppv], [bvn])
        yield
        psu, bpsu = slot()
        kb.op("pe", lambda e: e.matmul(psu, kd[:], vn[:], start=True, stop=True), [bkd, bvn], [bpsu])
        po2, bpo2 = slot()
        kb.op("pe", lambda e: e.matmul(po2, aT[:], vn[:], start=True, stop=True), [baT, bvn], [bpo2])
        kb.op("dve", lambda e: e.scalar_tensor_tensor(out=S1[:], in0=S0[:], scalar=cols[:, EGL, n:n + 1], in1=psu,
                                                      op0=ALU.mult, op1=ALU.add), [bS0, bcols, bpsu], [bS1])
        o2, bo2 = tmp("o2")
        kb.op("act", lambda e: e.copy(out=o2[:], in_=po2), [bpo2], [bo2])
        o, bo = tmp("o")
        kb.op("dve", lambda e: e.scalar_tensor_tensor(out=o[:], in0=po1, scalar=cols[:, EGC, n:n + 1], in1=o2[:],
                                                      op0=ALU.mult, op1=ALU.add), [bpo1, bcols, bo2], [bo])
        yield
        kb.op("act", lambda e: e.activation(out=junk[:], in_=o[:], func=AF.Square, accum_out=st[:, 0:1]),
              [bo], [bjunk, bst])
        kb.op("act", lambda e: e.activation(out=st[:, 1:2], in_=st[:, 0:1], func=AF.Ln, scale=1.0 / 128,
                                            bias=epst[:, 0:1]), [bst, beps], [bst])
        kb.op("act", lambda e: e.activation(out=st[:, 2:3], in_=st[:, 1:2], func=AF.Exp, scale=-0.5), [bst], [bst])
        sg, bsg = tmp("sg")
        kb.op("act", lambda e: e.activation(out=sg[:], in_=zt[:], func=AF.Exp, scale=-1.0), [bzt], [bsg])
        kb.op(POOL_ENG, lambda e: e.tensor_scalar(out=sg[:], in0=sg[:], scalar1=1.0, scalar2=None, op0=ALU.add),
              [bsg], [bsg])
        kb.op("dve", lambda e: e.reciprocal(out=sg[:], in_=sg[:]), [bsg], [bsg])
        kb.op(POOL_ENG, lambda e: e.tensor_tensor(out=sg[:], in0=sg[:], in1=zt[:], op=ALU.mult), [bsg, bzt], [bsg])
        yield
        t1, bt1 = tmp("t1")
        kb.op("dve", lambda e: e.scalar_tensor_tensor(out=t1[:], in0=o[:], scalar=st[:, 2:3], in1=gnb[:],
                                                      op0=ALU.mult, op1=ALU.mult), [bo, bst, bgnb], [bt1])
        yt_, byt_ = tmp("yout")
        kb.op("dve", lambda e: e.tensor_tensor(out=yt_[:], in0=t1[:], in1=sg[:], op=ALU.mult), [bt1, bsg], [byt_])
        kb.dma("sp", ya[n * 128:(n + 1) * 128, :], yt_[:], [byt_], [bya])

    def drive(gens):
        gens = list(gens)
        while gens:
            for g in list(gens):
                try:
                    next(g)
                except StopIteration:
                    gens.remove(g)

    def chain(*gs):
        for g in gs:
            yield from g

    if stop == "alloc":
        kb.dma("sp", ya[0:128, :], states[0][0][:], [states[0][1]], [bya])
        kb.finish([bya])
        return nc
    drive([pre(n) for n in range(min(2, nch))])
    for n in range(0, nch, 2):
        gens = [pre(m) for m in (n + 2, n + 3) if m < nch]
        gens.append(chain(*[scan(m) for m in (n, n + 1) if m < nch]))
        drive(gens)
    kb.finish([bya])
    return nc


def l2_consts():
    i = np.arange(128)
    ident = np.eye(128, dtype=np.float32)
    LT = (i[:, None] <= i[None, :]).astype(np.float32)
    ones = np.ones((128, 128), np.float32)
    penL = np.where(i[:, None] >= i[None, :], 0.0, -1e30).astype(np.float32)
    SM = (i[:, None] > i[None, :]).astype(np.float32)
    sel = np.zeros((128, 128), np.float32)
    sel[127, :] = 1.0
    return np.ascontiguousarray(np.stack([ident, LT, ones, -ones, penL, SM, sel], axis=1))


def run_l2(of, conv_w_l, a_log_l, dt_bias_l, gnw_l, nch=NCH, stop=None):
    nc = _get("l2_%d_%s" % (nch, stop), lambda: build_l2(nch, stop))
    cst = l2_consts()
    maps = []
    for cid in range(NCORES):
        b, g = cid // 4, cid % 4
        xs = []
        cws = []
        for a in range(3):
            cols_ = slice(a * 512 + g * 128, a * 512 + (g + 1) * 128)
            xa = np.zeros((128, S + 3), np.float32)
            xa[:, 3:] = of[b, :, cols_].T
            xs.append(xa)
            cws.append(conv_w_l[:, cols_].T)
        xin = np.ascontiguousarray(np.stack(xs, axis=1))
        cw = np.ascontiguousarray(np.concatenate(cws, axis=1)).astype(np.float32)
        z = of[b, :, 1536 + g * 128:1536 + (g + 1) * 128].reshape(NCH, 128, 128).transpose(1, 0, 2)
        bc = of[b, :, 2048 + g].reshape(NCH, 128).T
        ac = of[b, :, 2052 + g].reshape(NCH, 128).T
        maps.append({"xin": xin, "cw": cw, "zin": np.ascontiguousarray(z), "bcol": np.ascontiguousarray(bc),
                     "acol": np.ascontiguousarray(ac),
                     "sc3": np.array([[a_log_l[g], dt_bias_l[g]]], np.float32),
                     "gnw": gnw_l.reshape(1, 128).astype(np.float32), "cst": cst})
    res = _run(nc, maps)
    ya = np.zeros((NB, S, 512), np.float32)
    for cid in range(NCORES):
        b, g = cid // 4, cid % 4
        ya[b, :, g * 128:(g + 1) * 128] = res.results[cid]["ya"]
    return ya


def kernel(x, c, rel_bias, mod_w, mod_b, norm_mix_w, norm_ffn_w, w_in, conv_w, a_log, dt_bias,
           gdn_norm_w, q_norm_w, k_norm_w, w_out, router_w, router_b, w1, b1, w2, b2):
    f = lambda a: np.ascontiguousarray(np.asarray(a), dtype=np.float32)
    x = f(x)
    c = f(c)
    rel_bias = f(rel_bias)
    for l in range(2):
        of, ob = run_l1(x, c, f(mod_w[l]), f(mod_b[l]), f(norm_mix_w[l]), f(w_in[l]), f(q_norm_w[l]), f(k_norm_w[l]))
        ya = run_l2(of, f(conv_w[l]), f(a_log[l]), f(dt_bias[l]), f(gdn_norm_w[l]))
        yb = run_l3(ob, of, rel_bias)
        ycat = np.ascontiguousarray(np.concatenate([ya, yb], axis=-1))
        w1r, b1r, w2r, b2r = prep_l4_weights(f(w1[l]), f(b1[l]), f(w2[l]), f(b2[l]))
        x = run_l4(x, ycat, c, f(mod_w[l]), f(mod_b[l]), f(norm_ffn_w[l]), f(w_out[l]), f(router_w[l]),
                   f(router_b[l]), w1r, b1r, w2r, b2r)
    return x


CAP = 1024
ESTRIDE = CAP + 128
TRASH = NE * ESTRIDE


def _dma_ind(kb, out, out_off, in_, in_off, reads, writes):
    dst = writes[0]
    if dst.dsem is None:
        kb.nsem += 1
        key = "d%d_%s" % (kb.nsem, dst.name)
        dst.dsem = key
        kb.sems[key] = kb.nc.alloc_semaphore(key[:40])
    deps = kb._deps(reads, writes)
    kb._wait("pool", deps)
    ins = kb.nc.gpsimd.indirect_dma_start(out=out, out_offset=out_off, in_=in_, in_offset=in_off)
    dst.dcnt += 16
    ins.then_inc(kb.sems[dst.dsem], 16)
    for b in reads:
        if b.r.get(dst.dsem, 0) < dst.dcnt:
            b.r[dst.dsem] = dst.dcnt
    dst.w = (dst.dsem, dst.dcnt)
    dst.r = {}
    kb.n_ops += 1


def build_l4r(e_lo=0, e_hi=NE, first=True):
    n_exp = e_hi - e_lo
    nc = bass.Bass("TRN2", target_bir_lowering=False)
    x = _din(nc, "x", [TPC, D])
    ycat = _din(nc, "ycat", [TPC, D])
    cT = _din(nc, "cT", [128, 8])
    mod_w = _din(nc, "mod_w", [D, 6 * D])
    mod_b = _din(nc, "mod_b", [1, 6 * D])
    normw = _din(nc, "normw", [1, D])
    w_out = _din(nc, "w_out", [D, D])
    rw = _din(nc, "rw", [D, NE])
    rb = _din(nc, "rb", [1, NE])
    w1 = _din(nc, "w1", [n_exp, 8, 128, 8 * 256])
    b1 = _din(nc, "b1", [128, NE * 16])
    w2 = _din(nc, "w2", [n_exp, D, D])
    b2 = _din(nc, "b2", [NE, D])
    cst = _din(nc, "cst", [128, 3, 128])
    erow = _din(nc, "erow", [2, NE])
    xprev = None if first else _din(nc, "xprev", [TPC, D])
    xo = _dout(nc, "xo", [TPC, D])
    Xs = nc.dram_tensor("Xs", [TRASH + 128, D], BF16, kind="Internal").ap()
    Ys = nc.dram_tensor("Ys", [TRASH + 128, D], F32, kind="Internal").ap()
    kb = KB(nc)
    dr = Buf("dram_in")
    bxo = Buf("xo")
    bXs = Buf("Xs")
    bYs = Buf("Ys")

    PS = []
    for i in range(4):
        t = nc.alloc_psum_tensor("PS%d" % i, [128, 1024], F32)
        PS.append((t, Buf("ps%da" % i), Buf("ps%db" % i)))

    def bank(i):
        t, ba, bb = PS[i // 2]
        return (t[:, 0:512], ba) if i % 2 == 0 else (t[:, 512:1024], bb)

    R_x, _ = kb.sb([128, 8192], F32, "R_x")
    xrows = rview(R_x, 0, [128, 8, D], BF16)
    bxrows = Buf("xrows")
    xeT = rview(R_x, 16384, [128, 8, CAP], BF16)
    bxeT = Buf("xeT")
    modw_bufs = [(rview(R_x, i * 16384, [128, 8, 512], F32), Buf("modw%d" % i)) for i in range(2)]
    R_a, _ = kb.sb([128, 4096], F32, "R_a")
    actT = rview(R_a, 0, [128, 8, CAP], BF16)
    bactT = Buf("actT")
    g1, bg1 = rview(R_a, 0, [128, D], F32), Buf("g1")
    sh2, bsh2 = rview(R_a, 4096, [128, D], F32), Buf("sh2")
    A2, bA2 = rview(R_a, 8192, [128, D], F32), Buf("A2")
    nwb, bnwb = rview(R_a, 12288, [128, D], F32), Buf("nwb")
    R_w2, _ = kb.sb([128, 8192], F32, "R_w2")
    W2B = [(rview(R_w2, i * 16384, [128, 8, D], BF16), Buf("w2b%d" % i)) for i in range(2)]
    wo = rview(R_w2, 0, [128, 8, D], BF16)
    bwo = Buf("wo")
    scb_t = (rview(R_w2, 16384, [128, 8, 128], F32), Buf("scb"))
    modb_bufs = [(rview(R_w2, 20480 + i * 2048, [128, 512], F32), Buf("modb%d" % i)) for i in range(2)]
    R_W1, _ = kb.sb([128, 3072], F32, "R_W1")
    W1R = [(rview(R_W1, i * 4096, [128, 8, 256], BF16), Buf("w1r%d" % i)) for i in range(3)]
    g2, bg2 = kb.sb([128, D], F32, "g2")
    xt, bxt = kb.sb([128, D], F32, "xt")
    yt, byt = kb.sb([128, D], F32, "yt")
    tmpA, btmpA = kb.sb([128, D], F32, "tmpA")
    tmpB, btmpB = kb.sb([128, D], F32, "tmpB")
    R1, bR1 = kb.sb([128, D], F32, "R1")
    ybt = rview(R1, 0, [128, D], BF16)
    yT = rview(R1, 2048, [128, 8, 128], BF16)
    h32T = rview(R1, 0, [128, 8, 128], F32)
    R2, bR2 = kb.sb([128, D], F32, "R2")
    hb = rview(R2, 0, [128, D], BF16)
    junk = rview(R2, 2048, [128, D], BF16)
    yrows = [kb.sb([128, D], F32, "yrow") for _ in range(2)]
    b2bs = [kb.sb([128, D], F32, "b2bc") for _ in range(2)]
    grows = [kb.sb([128, D], F32, "grow") for _ in range(4)]

    ct, bct = kb.sb([128, 3, 128], F32, "cst")
    kb.dma("sp", ct[:], cst[:, :, :], [dr], [bct])
    idf, LTs, ones = ct[:, 0, :], ct[:, 1, :], ct[:, 2, :]
    bidf = bct
    idb, bidb = kb.sb([128, 128], BF16, "identb")
    kb.op("dve", lambda e: e.tensor_copy(out=idb[:], in_=idf), [bct], [bidb])
    er, ber = kb.sb([128, 2, NE], F32, "erow")
    kb.dma("sp", er[:, 0, :], erow[0:1, :].to_broadcast([128, NE]), [dr], [ber])
    kb.dma("sp", er[:, 1, :], erow[1:2, :].to_broadcast([128, NE]), [dr], [ber])
    epst, beps = kb.sb([128, 1], F32, "eps")
    kb.op("dve", lambda e: e.memset(epst[:], EPS), [], [beps])
    kb.dma("sp", nwb, normw[0:1, :].to_broadcast([128, D]), [dr], [bnwb])
    rbb, brbb = kb.sb([128, NE], F32, "rbb")
    kb.dma("sp", rbb[:], rb[0:1, :].to_broadcast([128, NE]), [dr], [brbb])
    rwt, brwt = kb.sb([128, 8, NE], F32, "rwt")
    kb.dma("sp", rwt[:], rw.rearrange("(kc p) e -> p kc e", p=128), [dr], [brwt])
    b1a, bb1a = kb.sb([128, NE * 16], F32, "b1a")
    kb.dma("sp", b1a[:], b1[:, :], [dr], [bb1a])
    b1av = b1a[:].rearrange("p (g two) -> p g two", two=2)
    kb.op("dve", lambda e: e.tensor_scalar(out=b1av[:, :, 0:1], in0=b1av[:, :, 0:1], scalar1=ALPHA, scalar2=None,
                                           op0=ALU.mult), [bb1a], [bb1a])
    kb.op("dve", lambda e: e.tensor_scalar(out=b1av[:, :, 1:2], in0=b1av[:, :, 1:2], scalar1=1.0, scalar2=None,
                                           op0=ALU.add), [bb1a], [bb1a])
    st, bst = kb.sb([128, 64], F32, "st")
    lg, blg = kb.sb([128, NE], F32, "lg")
    ohk, bohk = kb.sb([128, 4, NE], F32, "ohk")
    prod, bprod = kb.sb([128, 4, NE], F32, "prod")
    sel, bsel = kb.sb([128, NE], F32, "sel")
    pos, bpos = kb.sb([128, NE], F32, "pos")
    basebc, bbase = kb.sb([128, NE], F32, "basebc")
    kb.op("dve", lambda e: e.memset(basebc[:], 0.0), [], [bbase])
    gkall, bgk = kb.sb([128, NT, 4], F32, "gkall")
    dstf, bdstf = kb.sb([128, NT, 4], F32, "dstf")
    dsti, bdsti = kb.sb([128, NT, 4], U32, "dsti")

    kb.op("dve", lambda e: e.memset(tmpA[:], 0.0), [], [btmpA])
    zb16 = tmpA[:].bitcast(BF16)[:, 0:D]
    r0 = e_lo * ESTRIDE
    nz = (n_exp * ESTRIDE) // 128
    for i in range(nz):
        kb.dma("sp", Xs[r0 + i * 128:r0 + (i + 1) * 128, :], zb16, [btmpA], [bXs])
        kb.dma("sp", Ys[r0 + i * 128:r0 + (i + 1) * 128, :], tmpA[:], [btmpA], [bYs])
    kb.dma("sp", Ys[TRASH:TRASH + 128, :], tmpA[:], [btmpA], [bYs])
    kb.dma("sp", Xs[TRASH:TRASH + 128, :], zb16, [btmpA], [bXs])

    sc2_out = (A2, bA2)
    emit_mod(kb, cT, mod_w, mod_b, [2, 3, 4, 5], wbufs=modw_bufs, bbufs=modb_bufs,
             pss=[bank(0), bank(1)], scb_t=scb_t,
             outs={2: (g1, bg1), 3: (sh2, bsh2), 4: sc2_out, 5: (g2[:], bg2)})
    kb.op("dve", lambda e: e.scalar_tensor_tensor(out=A2, in0=A2, scalar=1.0, in1=nwb,
                                                  op0=ALU.add, op1=ALU.mult), [bA2, bnwb], [bA2])

    for kc in range(8):
        kb.dma("pool", wo[:, kc, :], w_out[kc * 128:(kc + 1) * 128, :], [dr], [bwo])
    for ti in range(NT):
        rows = slice(ti * 128, (ti + 1) * 128)
        kb.dma("sp", xt[:], x[rows, :], [dr], [bxt])
        kb.dma("sp", yt[:], ycat[rows, :], [dr], [byt])
        kb.op("act", lambda e: e.copy(out=ybt, in_=yt[:]), [byt], [bR1])
        pt, bpt = bank(0)
        ptb = pt.bitcast(BF16)
        for kc in range(8):
            kb.op("pe", lambda e, kc=kc: e.transpose(out=ptb[:, kc * 128:(kc + 1) * 128],
                                                     in_=ybt[:, kc * 128:(kc + 1) * 128], identity=idb[:]),
                  [bR1, bidb], [bpt])
        kb.op("act", lambda e: e.copy(out=yT.rearrange("p a b -> p (a b)"), in_=ptb), [bpt], [bR1])
        for half in range(2):
            pp, bpp = bank(2 + half)
            for kc in range(8):
                kb.op("pe", lambda e, kc=kc: e.matmul(pp, yT[:, kc, :], wo[:, kc, half * 512:(half + 1) * 512],
                                                      start=(kc == 0), stop=(kc == 7)), [bR1, bwo], [bpp])
            cs = slice(half * 512, (half + 1) * 512)
            kb.op("dve", lambda e: e.tensor_tensor(out=tmpA[:, cs], in0=pp, in1=g1[:, cs], op=ALU.mult),
                  [bpp, bg1], [btmpA])
        kb.op("dve", lambda e: e.tensor_tensor(out=xt[:], in0=tmpA[:], in1=xt[:], op=ALU.add), [btmpA, bxt], [bxt])
        kb.dma("sp", xo[rows, :], xt[:], [bxt], [bxo])
        kb.op("act", lambda e: e.activation(out=junk, in_=xt[:], func=AF.Square, accum_out=st[:, 0:1]),
              [bxt], [bR2, bst])
        kb.op("act", lambda e: e.activation(out=st[:, 1:2], in_=st[:, 0:1], func=AF.Sqrt, scale=1.0 / D,
                                            bias=epst[:]), [bst, beps], [bst])
        kb.op("dve", lambda e: e.reciprocal(out=st[:, 2:3], in_=st[:, 1:2]), [bst], [bst])
        kb.op("dve", lambda e: e.scalar_tensor_tensor(out=tmpA[:], in0=xt[:], scalar=st[:, 2:3], in1=A2,
                                                      op0=ALU.mult, op1=ALU.mult), [bxt, bst, bA2], [btmpA])
        kb.op("dve", lambda e: e.tensor_tensor(out=tmpB[:], in0=tmpA[:], in1=sh2, op=ALU.add),
              [btmpA, bsh2], [btmpB])
        kb.op("act", lambda e: e.copy(out=hb, in_=tmpB[:]), [btmpB], [bR2])
        for half in range(2):
            pq, bpq = bank(4 + half)
            for k4 in range(4):
                kc = half * 4 + k4
                kb.op("pe", lambda e, kc=kc, k4=k4: e.transpose(out=pq[:, k4 * 128:(k4 + 1) * 128],
                                                                in_=tmpB[:, kc * 128:(kc + 1) * 128],
                                                                identity=idf), [btmpB, bidf], [bpq])
            kb.op("dve", lambda e: e.tensor_copy(
                out=h32T[:, half * 4:(half + 1) * 4, :], in_=pq.rearrange("p (a b) -> p a b", a=4)),
                [bpq], [bR1])
        pr, bpr = bank(6)
        for kc in range(8):
            kb.op("pe", lambda e, kc=kc: e.matmul(pr[:, 0:NE], h32T[:, kc, :], rwt[:, kc, :],
                                                  start=(kc == 0), stop=(kc == 7)), [bR1, brwt], [bpr])
        kb.op("dve", lambda e: e.tensor_tensor(out=lg[:], in0=pr[:, 0:NE], in1=rbb[:], op=ALU.add),
              [bpr, brbb], [blg])
        kb.op("dve", lambda e: e.max(out=st[:, 8:16], in_=lg[:]), [blg], [bst])
        kb.op("dve", lambda e: e.tensor_scalar(out=st[:, 16:17], in0=st[:, 8:9], scalar1=-1.0, scalar2=None,
                                               op0=ALU.mult), [bst], [bst])
        kb.op("act", lambda e: e.activation(out=st[:, 20:24], in_=st[:, 8:12], func=AF.Exp, bias=st[:, 16:17],
                                            accum_out=st[:, 17:18]), [bst], [bst])
        kb.op("dve", lambda e: e.reciprocal(out=st[:, 18:19], in_=st[:, 17:18]), [bst], [bst])
        for k in range(4):
            kb.op("dve", lambda e, k=k: e.tensor_scalar(out=ohk[:, k, :], in0=lg[:], scalar1=st[:, 8 + k:9 + k],
                                                        scalar2=None, op0=ALU.is_equal), [blg, bst], [bohk])
        kb.op("dve", lambda e: e.tensor_scalar(out=sel[:], in0=lg[:], scalar1=st[:, 11:12], scalar2=None,
                                               op0=ALU.is_ge), [blg, bst], [bsel])
        ppf, bppf = bank(7)
        kb.op("pe", lambda e: e.matmul(ppf[:, 0:NE], LTs, sel[:], start=True, stop=True), [bct, bsel], [bppf])
        kb.op("dve", lambda e: e.tensor_tensor(out=pos[:], in0=ppf[:, 0:NE], in1=basebc[:], op=ALU.add),
              [bppf, bbase], [bpos])
        ppb, bppb = bank(1)
        kb.op("pe", lambda e: e.matmul(ppb[:, 0:NE], ones, sel[:], start=True, stop=True), [bct, bsel], [bppb])
        kb.op("dve", lambda e: e.tensor_tensor(out=basebc[:], in0=ppb[:, 0:NE], in1=basebc[:], op=ALU.add),
              [bppb, bbase], [bbase])
        kb.op("dve", lambda e: e.scalar_tensor_tensor(out=pos[:], in0=pos[:], scalar=float(CAP), in1=er[:, 0, :],
                                                      op0=ALU.min, op1=ALU.add), [bpos, ber], [bpos])
        kb.op("dve", lambda e: e.tensor_tensor(out=ohk[:], in0=ohk[:],
                                               in1=er[:, 1, :].unsqueeze(1).to_broadcast([128, 4, NE]),
                                               op=ALU.mult), [bohk, ber], [bohk])
        kb.op("dve", lambda e: e.tensor_tensor(out=prod[:], in0=ohk[:],
                                               in1=pos[:].unsqueeze(1).to_broadcast([128, 4, NE]),
                                               op=ALU.mult), [bohk, bpos], [bprod])
        kb.op("dve", lambda e: e.tensor_reduce(out=dstf[:, ti, :], in_=prod[:], axis=AX.X, op=ALU.add),
              [bprod], [bdstf])
        kb.op("dve", lambda e: e.tensor_reduce(out=st[:, 24:28], in_=ohk[:], axis=AX.X, op=ALU.add),
              [bohk], [bst])
        kb.op("dve", lambda e: e.tensor_scalar(out=st[:, 28:32], in0=st[:, 24:28], scalar1=-float(TRASH),
                                               scalar2=float(TRASH), op0=ALU.mult, op1=ALU.add), [bst], [bst])
        kb.op("dve", lambda e: e.tensor_tensor(out=dstf[:, ti, :], in0=dstf[:, ti, :], in1=st[:, 28:32], op=ALU.add),
              [bdstf, bst], [bdstf])
        kb.op("dve", lambda e: e.scalar_tensor_tensor(out=gkall[:, ti, :], in0=st[:, 20:24], scalar=st[:, 18:19],
                                                      in1=st[:, 24:28], op0=ALU.mult, op1=ALU.mult), [bst], [bgk])
        kb.op("dve", lambda e: e.tensor_copy(out=dsti[:, ti, :], in_=dstf[:, ti, :]), [bdstf], [bdsti])
        for k in range(4):
            _dma_ind(kb, Xs[:, :], bass.IndirectOffsetOnAxis(ap=dsti[:, ti, k:k + 1], axis=0), hb, None,
                     [bR2, bdsti], [bXs])

    merge_deps(bxrows, [b for _, b in modw_bufs])
    merge_deps(bxeT, [b for _, b in modw_bufs])
    merge_deps(bactT, [bg1, bsh2, bA2, bnwb])
    for _, b in W2B:
        merge_deps(b, [bwo, scb_t[1]] + [bb for _, bb in modb_bufs])
    tts = [(tmpA[:, 0:512], Buf("tt0")), (tmpA[:, 512:1024], Buf("tt1"))]
    lps = [(tmpB[:, 0:512], Buf("lp0")), (tmpB[:, 512:1024], Buf("lp1"))]
    t2s = [(xt[:, 0:512], Buf("t20")), (xt[:, 512:1024], Buf("t21"))]
    for (_, b), src_ in zip(tts + lps + t2s, [btmpA, btmpA, btmpB, btmpB, bxt, bxt]):
        merge_deps(b, [src_])
    C0 = float(LIMIT * ALPHA / (1.0 + np.exp(-LIMIT * ALPHA)))
    w1_issued = [0]

    def issue_w1(upto):
        while w1_issued[0] < min(upto, n_exp * 8):
            i = w1_issued[0]
            t, b = W1R[i % 3]
            kb.dma("pool", t.rearrange("p a b -> p (a b)"), w1[i // 8, i % 8, :, :], [dr], [b])
            w1_issued[0] += 1

    def issue_w2(ei):
        if ei < n_exp:
            t, b = W2B[ei % 2]
            src = w2[ei].rearrange("(fc p) d -> p fc d", p=128)
            for fc in range(8):
                kb.dma("pool", t[:, fc, :], src[:, fc, :], [dr], [b])

    nu = 0
    ny = 0
    ntp = 0
    issue_w2(0)
    for e in range(e_lo, e_hi):
        ei = e - e_lo
        issue_w2(ei + 1)
        b2b, bb2b = b2bs[ei % 2]
        kb.dma("sp", b2b[:], b2[e:e + 1, :].to_broadcast([128, D]), [dr], [bb2b])
        kb.dma("sp", xrows, Xs[e * ESTRIDE:e * ESTRIDE + CAP, :].rearrange("(i p) d -> p i d", p=128),
               [bXs], [bxrows])
        for i in range(8):
            pt, bpt = bank(6 + ntp % 2)
            ntp += 1
            ptb = pt.bitcast(BF16)
            for kc in range(8):
                kb.op("pe", lambda en, kc=kc, i=i: en.transpose(out=ptb[:, kc * 128:(kc + 1) * 128],
                                                                in_=xrows[:, i, kc * 128:(kc + 1) * 128],
                                                                identity=idb[:]), [bxrows, bidb], [bpt])
            kb.op("act", lambda en, i=i: en.copy(out=xeT[:, :, i * 128:(i + 1) * 128],
                                                 in_=ptb.rearrange("p (a b) -> p a b", a=8)), [bpt], [bxeT])
        for ft in range(8):
            gi = ei * 8 + ft
            issue_w1(gi + 2)
            wv, bwt = W1R[gi % 3]
            bcol = (e * 8 + ft) * 2
            for blk in range(CAP // 512):
                ts_ = slice(blk * 512, (blk + 1) * 512)
                pgl, bpgl = bank(0 + 2 * (nu % 2))
                pli, bpli = bank(1 + 2 * (nu % 2))
                for kc in range(8):
                    kb.op("pe", lambda en, kc=kc: en.matmul(pgl, wv[:, kc, 0:128], xeT[:, kc, ts_],
                                                            start=(kc == 0), stop=(kc == 7)), [bwt, bxeT], [bpgl])
                for kc in range(8):
                    kb.op("pe", lambda en, kc=kc: en.matmul(pli, wv[:, kc, 128:256], xeT[:, kc, ts_],
                                                            start=(kc == 0), stop=(kc == 7)), [bwt, bxeT], [bpli])
                tt, btt = tts[nu % 2]
                lp, blp = lps[nu % 2]
                t2, bt2 = t2s[nu % 2]
                kb.op("act", lambda en: en.activation(out=tt, in_=pgl, func=AF.Silu, scale=ALPHA,
                                                      bias=b1a[:, bcol:bcol + 1]), [bpgl, bb1a], [btt])
                kb.op("dve", lambda en: en.tensor_scalar(out=lp, in0=pli, scalar1=b1a[:, bcol + 1:bcol + 2],
                                                         scalar2=LIMIT + 1.0, op0=ALU.add, op1=ALU.min),
                      [bpli, bb1a], [blp])
                kb.op("dve", lambda en: en.tensor_scalar(out=t2, in0=tt, scalar1=C0, scalar2=1.0 / ALPHA,
                                                         op0=ALU.min, op1=ALU.mult), [btt], [bt2])
                kb.op("dve", lambda en: en.scalar_tensor_tensor(out=actT[:, ft, ts_], in0=lp, scalar=1.0 - LIMIT,
                                                                in1=t2, op0=ALU.max, op1=ALU.mult),
                      [blp, bt2], [bactT])
                nu += 1
        w2v, bw2 = W2B[ei % 2]
        for i in range(8):
            yr, byr = yrows[ny % 2]
            ny += 1
            for db in range(2):
                py, bpy = bank(4 + db)
                for fc in range(8):
                    kb.op("pe", lambda en, fc=fc, i=i, db=db: en.matmul(py, actT[:, fc, i * 128:(i + 1) * 128],
                                                                        w2v[:, fc, db * 512:(db + 1) * 512],
                                                                        start=(fc == 0), stop=(fc == 7)),
                          [bactT, bw2], [bpy])
                kb.op("dve", lambda en, db=db: en.tensor_tensor(out=yr[:, db * 512:(db + 1) * 512], in0=py,
                                                                in1=b2b[:, db * 512:(db + 1) * 512], op=ALU.add),
                      [bpy, bb2b], [byr])
            kb.dma("sp", Ys[e * ESTRIDE + i * 128:e * ESTRIDE + (i + 1) * 128, :], yr[:], [byr], [bYs])

    tf, btf = tts[0]
    for ti in range(NT):
        rows = slice(ti * 128, (ti + 1) * 128)
        if first:
            kb.dma("sp", yt[:], xo[rows, :], [bxo], [byt])
        else:
            kb.dma("sp", yt[:], xprev[rows, :], [dr], [byt])
        for k in range(4):
            gr, bgr = grows[k]
            _dma_ind(kb, gr[:], None, Ys[:, :], bass.IndirectOffsetOnAxis(ap=dsti[:, ti, k:k + 1], axis=0),
                     [bYs, bdsti], [bgr])
        g0, bg0 = grows[0]
        kb.op("dve", lambda en: en.tensor_scalar(out=g0[:], in0=g0[:], scalar1=gkall[:, ti, 0:1], scalar2=None,
                                                 op0=ALU.mult), [bg0, bgk], [bg0])
        for k in range(1, 4):
            gr, bgr = grows[k]
            kb.op("dve", lambda en, k=k, gr=gr: en.scalar_tensor_tensor(out=g0[:], in0=gr[:],
                                                                        scalar=gkall[:, ti, k:k + 1], in1=g0[:],
                                                                        op0=ALU.mult, op1=ALU.add),
                  [bgr, bgk, bg0], [bg0])
        kb.op("dve", lambda en: en.tensor_tensor(out=g0[:], in0=g0[:], in1=g2[:], op=ALU.mult), [bg0, bg2], [bg0])
        kb.op("dve", lambda en: en.tensor_tensor(out=yt[:], in0=g0[:], in1=yt[:], op=ALU.add), [bg0, byt], [byt])
        kb.dma("sp", xo[rows, :], yt[:], [byt], [bxo])
    kb.finish([bxo])
    return nc


def run_l4r(x, ycat, c, mod_w_l, mod_b_l, normw_l, w_out_l, rw_l, rb_l, w1r, b1r, w2_l, b2_l,
            splits=((0, 16), (16, 32))):
    i = np.arange(128)
    cst = np.ascontiguousarray(np.stack([np.eye(128, dtype=np.float32),
                                         (i[:, None] < i[None, :]).astype(np.float32),
                                         np.ones((128, 128), np.float32)], axis=1))
    xprev = None
    for (lo, hi) in splits:
        first = (lo == 0)
        nc = _get("l4r_%d_%d" % (lo, hi), lambda: build_l4r(lo, hi, first))
        w1s = np.ascontiguousarray(w1r[lo:hi])
        w2s = np.ascontiguousarray(w2_l[lo:hi])
        erow = np.stack([np.arange(NE) * ESTRIDE, ((np.arange(NE) >= lo) & (np.arange(NE) < hi))]).astype(np.float32)
        maps = []
        for cid in range(NCORES):
            m = {"x": core_tokens(x, cid), "ycat": core_tokens(ycat, cid), "cT": cT_of(c, cid // 4),
                 "mod_w": mod_w_l, "mod_b": mod_b_l.reshape(1, -1), "normw": normw_l.reshape(1, -1),
                 "w_out": w_out_l, "rw": rw_l, "rb": rb_l.reshape(1, -1), "w1": w1s, "b1": b1r, "w2": w2s,
                 "b2": b2_l, "cst": cst, "erow": erow}
            if not first:
                m["xprev"] = xprev[cid]
            maps.append(m)
        res = _run(nc, maps)
        xprev = [r["xo"] for r in res.results]
    return uncore_tokens(xprev, (D,))
```

```python
import numpy as np
import concourse.bass as bass
import concourse.mybir as mybir
from concourse.bass_utils import run_bass_kernel_spmd

F32 = mybir.dt.float32
BF16 = mybir.dt.bfloat16
U32 = mybir.dt.uint32
AF = mybir.ActivationFunctionType
ALU = mybir.AluOpType
AX = mybir.AxisListType

D = 1024
S = 8192
NB = 2
D_IN = 4176
EPS = 1e-6
NCORES = 8
TPC = 2048
NT = 16


class Buf:
    __slots__ = ("name", "w", "r", "dsem", "dcnt")

    def __init__(self, name):
        self.name = name
        self.w = None
        self.r = {}
        self.dsem = None
        self.dcnt = 0


class KB:
    def __init__(self, nc, self_sync=True):
        self.nc = nc
        self.eng = {"pe": nc.tensor, "dve": nc.vector, "act": nc.scalar,
                    "pool": nc.gpsimd, "sp": nc.sync}
        self.sems = {}
        self.cnt = {}
        self.known = {k: {} for k in self.eng}
        for k in self.eng:
            self.sems[k] = nc.alloc_semaphore("sem_" + k)
            self.cnt[k] = 0
        self.self_sync = self_sync
        self.nsem = 0
        self.n_ops = 0
        self.n_waits = 0
        self.nbuf = 0

    def sb(self, shape, dt=F32, name=None):
        self.nbuf += 1
        name = (name or "t") + "_%d" % self.nbuf
        return self.nc.alloc_sbuf_tensor(name, list(shape), dt), Buf(name)

    def ps(self, shape, dt=F32, name=None):
        self.nbuf += 1
        name = (name or "p") + "_%d" % self.nbuf
        return self.nc.alloc_psum_tensor(name, list(shape), dt), Buf(name)

    def _deps(self, reads, writes):
        d = {}

        def add(kv):
            if kv is None:
                return
            k, v = kv
            if d.get(k, 0) < v:
                d[k] = v
        for b in reads:
            add(b.w)
        for b in writes:
            add(b.w)
            for k, v in b.r.items():
                add((k, v))
        return d

    def _wait(self, en, deps):
        e = self.eng[en]
        kn = self.known[en]
        for k, v in deps.items():
            if k == en and (en == "pe" or not self.self_sync):
                continue
            if kn.get(k, 0) >= v:
                continue
            e.wait_ge(self.sems[k], v)
            kn[k] = v
            self.n_waits += 1

    def op(self, en, fn, reads=(), writes=()):
        deps = self._deps(reads, writes)
        self._wait(en, deps)
        ins = fn(self.eng[en])
        self.cnt[en] += 1
        v = self.cnt[en]
        ins.then_inc(self.sems[en], 1)
        for b in reads:
            if b.r.get(en, 0) < v:
                b.r[en] = v
        for b in writes:
            b.w = (en, v)
            b.r = {}
        self.n_ops += 1
        return ins

    def dma(self, en, out, in_, reads, writes, **kw):
        assert len(writes) == 1
        dst = writes[0]
        if dst.dsem is None:
            self.nsem += 1
            key = "d%d_%s" % (self.nsem, dst.name)
            dst.dsem = key
            self.sems[key] = self.nc.alloc_semaphore(key[:40])
        deps = self._deps(reads, writes)
        self._wait(en, deps)
        ins = self.eng[en].dma_start(out=out, in_=in_, **kw)
        dst.dcnt += 16
        ins.then_inc(self.sems[dst.dsem], 16)
        for b in reads:
            if b.r.get(dst.dsem, 0) < dst.dcnt:
                b.r[dst.dsem] = dst.dcnt
        dst.w = (dst.dsem, dst.dcnt)
        dst.r = {}
        self.n_ops += 1
        return ins

    def finish(self, bufs, en="sp"):
        d = {}
        for b in bufs:
            if b.w is not None:
                k, v = b.w
                d[k] = max(d.get(k, 0), v)
        self._wait(en, d)


def rview(t, off_bytes, shape, dt):
    esz = 2 if dt == BF16 else 4
    n = 1
    for s in shape[1:]:
        n *= s
    flat = t[:, :] if dt == F32 else t[:, :].bitcast(dt)
    e0 = off_bytes // esz
    v = flat[0:shape[0], e0:e0 + n]
    if len(shape) == 3:
        v = v.rearrange("p (a b) -> p a b", a=shape[1])
    return v


def merge_deps(dst, srcs):
    for s in srcs:
        for kv in ([s.w] if s.w else []) + list(s.r.items()):
            k, v = kv
            if dst.r.get(k, 0) < v:
                dst.r[k] = v


def _din(nc, name, shape, dt=F32):
    return nc.dram_tensor(name, list(shape), dt, kind="ExternalInput").ap()


def _dout(nc, name, shape, dt=F32):
    return nc.dram_tensor(name, list(shape), dt, kind="ExternalOutput").ap()


def emit_mod(kb, cT, mod_w, mod_b, groups, wbufs=None, bbufs=None, pss=None, scb_t=None, outs=None):
    nc = kb.nc
    dr = Buf("mod_dram")
    ct, bct = kb.sb([128, 8], F32, "ct")
    kb.dma("sp", ct[:], cT[:, :], [dr], [bct])
    sc, bsc = kb.sb([128, 8], F32, "sc")
    kb.op("act", lambda e: e.activation(out=sc[:], in_=ct[:], func=AF.Silu), [bct], [bsc])
    scb, bscb = scb_t if scb_t is not None else kb.sb([128, 8, 128], F32, "scb")
    kb.op("dve", lambda e: e.tensor_copy(out=scb[:], in_=sc[:, :].unsqueeze(2).to_broadcast([128, 8, 128])),
          [bsc], [bscb])
    mw = mod_w.rearrange("(kc p) n -> p kc n", p=128)
    if wbufs is None:
        wbufs = [kb.sb([128, 8, 512], F32, "modw") for _ in range(2)]
    if bbufs is None:
        bbufs = [kb.sb([128, 512], F32, "modb") for _ in range(2)]
    if pss is None:
        pss = [kb.ps([128, 512], F32, "modps") for _ in range(2)]
    res = {}
    it = 0
    for g in groups:
        mt, bmt = outs[g] if outs is not None else kb.sb([128, 1024], F32, "modrow")
        res[g] = (mt, bmt)
        for half in range(2):
            c0 = g * 1024 + half * 512
            wt, bw = wbufs[it % 2]
            bt, bb = bbufs[it % 2]
            pt, bp = pss[it % 2]
            kb.dma("sp", wt[:], mw[:, :, c0:c0 + 512], [dr], [bw])
            kb.dma("sp", bt[:], mod_b[0:1, c0:c0 + 512].to_broadcast([128, 512]), [dr], [bb])
            for kc in range(8):
                kb.op("pe", lambda e, kc=kc: e.matmul(pt[:], scb[:, kc, :], wt[:, kc, :],
                                                      start=(kc == 0), stop=(kc == 7)),
                      [bscb, bw], [bp])
            kb.op("dve", lambda e: e.tensor_tensor(out=mt[:, half * 512:(half + 1) * 512], in0=pt[:],
                                                   in1=bt[:], op=ALU.add), [bp, bb], [bmt])
            it += 1
    return res


NF1 = 2064
NB1 = 2112


def build_l1():
    nc = bass.Bass("TRN2", target_bir_lowering=False)
    x = _din(nc, "x", [TPC, D])
    cT = _din(nc, "cT", [128, 8])
    mod_w = _din(nc, "mod_w", [D, 6 * D])
    mod_b = _din(nc, "mod_b", [1, 6 * D])
    normw = _din(nc, "normw", [1, D])
    w_in = _din(nc, "w_in", [D, D_IN])
    qkw = _din(nc, "qkw", [1, 128])
    ident = _din(nc, "ident", [128, 128])
    of = _dout(nc, "of", [TPC, NF1])
    ob = _dout(nc, "ob", [TPC, NB1], BF16)
    kb = KB(nc)
    dr = Buf("dram_in")
    bof, bob = Buf("of"), Buf("ob")

    idb, bidb = kb.sb([128, 128], BF16, "identb")
    kb.dma("pool", idb[:], ident[:, :], [dr], [bidb])
    epst, beps = kb.sb([128, 1], F32, "eps")
    kb.op("dve", lambda e: e.memset(epst[:], EPS), [], [beps])
    nwb, bnwb = kb.sb([128, D], F32, "nwb")
    kb.dma("sp", nwb[:], normw[0:1, :].to_broadcast([128, D]), [dr], [bnwb])
    qkb, bqkb = kb.sb([128, 128], F32, "qkb")
    kb.dma("sp", qkb[:], qkw[0:1, :].to_broadcast([128, 128]), [dr], [bqkb])
    kb.op("dve", lambda e: e.tensor_scalar(out=qkb[:, 0:64], in0=qkb[:, 0:64], scalar1=0.125, scalar2=None,
                                           op0=ALU.mult), [bqkb], [bqkb])

    wb, bwb = kb.sb([128, 8, D_IN], BF16, "w_in")
    for kc in range(8):
        for pc in range(3):
            c0 = pc * 1392
            kb.dma("pool", wb[:, kc, c0:c0 + 1392], w_in[kc * 128:(kc + 1) * 128, c0:c0 + 1392], [dr], [bwb])

    mods = emit_mod(kb, cT, mod_w, mod_b, [0, 1])
    sh1, bsh1 = mods[0]
    sc1, bsc1 = mods[1]
    A1, bA1 = kb.sb([128, D], F32, "A1")
    kb.op("dve", lambda e: e.scalar_tensor_tensor(out=A1[:], in0=sc1[:], scalar=1.0, in1=nwb[:],
                                                  op0=ALU.add, op1=ALU.mult), [bsc1, bnwb], [bA1])

    xts = [kb.sb([128, D], F32, "xt") for _ in range(2)]
    junk, bjunk = kb.sb([128, D], BF16, "junk")
    tmp, btmp = kb.sb([128, D], F32, "tmp")
    hbs = [kb.sb([128, D], BF16, "hb") for _ in range(2)]
    pT, bpT = kb.ps([128, D], BF16, "pT")
    hTs = [kb.sb([128, 8, 128], BF16, "hT") for _ in range(2)]
    pps = [kb.ps([128, 512], F32, "pp") for _ in range(4)]
    projs = [kb.sb([128, D_IN], F32, "proj") for _ in range(2)]
    obts = [kb.sb([128, NB1], BF16, "obt") for _ in range(2)]
    sq, bsq = kb.sb([128, 1024], F32, "sq")
    st, bst = kb.sb([128, 24], F32, "stats")

    QO = 2056
    npp = [0]
    stA, bstA = kb.sb([128, 4], F32, "statsA")

    def stage_a(ti):
        xt, bxt = xts[ti % 2]
        hb, bhb = hbs[ti % 2]
        hT, bhT = hTs[ti % 2]
        kb.dma("sp", xt[:], x[ti * 128:(ti + 1) * 128, :], [dr], [bxt])
        kb.op("act", lambda e: e.activation(out=junk[:], in_=xt[:], func=AF.Square, accum_out=stA[:, 0:1]),
              [bxt], [bjunk, bstA])
        kb.op("act", lambda e: e.activation(out=stA[:, 1:2], in_=stA[:, 0:1], func=AF.Sqrt, scale=1.0 / D,
                                            bias=epst[:]), [bstA, beps], [bstA])
        kb.op("dve", lambda e: e.reciprocal(out=stA[:, 2:3], in_=stA[:, 1:2]), [bstA], [bstA])
        kb.op("dve", lambda e: e.scalar_tensor_tensor(out=tmp[:], in0=xt[:], scalar=stA[:, 2:3], in1=A1[:],
                                                      op0=ALU.mult, op1=ALU.mult), [bxt, bstA, bA1], [btmp])
        kb.op("dve", lambda e: e.tensor_tensor(out=hb[:], in0=tmp[:], in1=sh1[:], op=ALU.add),
              [btmp, bsh1], [bhb])
        for kc in range(8):
            kb.op("pe", lambda e, kc=kc: e.transpose(out=pT[:, kc * 128:(kc + 1) * 128],
                                                     in_=hb[:, kc * 128:(kc + 1) * 128], identity=idb[:]),
                  [bhb, bidb], [bpT])
        kb.op("act", lambda e: e.copy(out=hT[:].rearrange("p a b -> p (a b)"), in_=pT[:]), [bpT], [bhT])

    def stage_b(ti):
        hT, bhT = hTs[ti % 2]
        pj, bpj = projs[ti % 2]
        obt, bobt = obts[ti % 2]
        for cb in range(9):
            c0 = cb * 512
            n = min(512, D_IN - c0)
            pp, bpp = pps[npp[0] % 4]
            for kc in range(8):
                kb.op("pe", lambda e, kc=kc: e.matmul(pp[:, 0:n], hT[:, kc, :], wb[:, kc, c0:c0 + n],
                                                      start=(kc == 0), stop=(kc == 7)), [bhT, bwb], [bpp])
            if npp[0] % 2 == 0:
                kb.op("act", lambda e: e.copy(out=pj[:, c0:c0 + n], in_=pp[:, 0:n]), [bpp], [bpj])
            else:
                kb.op("dve", lambda e: e.tensor_copy(out=pj[:, c0:c0 + n], in_=pp[:, 0:n]), [bpp], [bpj])
            npp[0] += 1
        qk = pj[:, QO:QO + 1024]
        kb.op("dve", lambda e: e.tensor_tensor(out=sq[:], in0=qk, in1=qk, op=ALU.mult), [bpj], [bsq])
        kb.op("dve", lambda e: e.tensor_reduce(out=st[:, 4:20], in_=sq[:].rearrange("p (g d) -> p g d", g=16),
                                               axis=AX.X, op=ALU.add), [bsq], [bst])
        kb.op("act", lambda e: e.activation(out=st[:, 4:20], in_=st[:, 4:20], func=AF.Sqrt, scale=1.0 / 64,
                                            bias=epst[:]), [bst, beps], [bst])
        kb.op("dve", lambda e: e.reciprocal(out=st[:, 4:20], in_=st[:, 4:20]), [bst], [bst])
        kb.op("dve", lambda e: e.tensor_tensor(out=sq[:].rearrange("p (g d) -> p g d", g=16),
                                               in0=qk.rearrange("p (g d) -> p g d", g=16),
                                               in1=st[:, 4:20].unsqueeze(2).to_broadcast([128, 16, 64]),
                                               op=ALU.mult), [bpj, bst], [bsq])
        for half in range(2):
            kb.op("dve", lambda e, half=half: e.tensor_tensor(
                out=obt[:, half * 512:(half + 1) * 512].rearrange("p (g d) -> p g d", g=8),
                in0=sq[:, half * 512:(half + 1) * 512].rearrange("p (g d) -> p g d", g=8),
                in1=qkb[:, half * 64:(half + 1) * 64].unsqueeze(1).to_broadcast([128, 8, 64]),
                op=ALU.mult), [bsq, bqkb], [bobt])
        kb.op("act", lambda e: e.copy(out=obt[:, 1024:NB1], in_=pj[:, QO + 1024:QO + 1024 + 1088]), [bpj], [bobt])
        kb.op("dve", lambda e: e.tensor_scalar(out=pj[:, 4168:4176], in0=pj[:, 4168:4176],
                                               scalar1=float(8 ** -0.5 * 64 ** -0.5), scalar2=None,
                                               op0=ALU.mult), [bpj], [bpj])
        kb.dma("sp", of[ti * 128:(ti + 1) * 128, 0:2056], pj[:, 0:2056], [bpj], [bof])
        kb.dma("sp", of[ti * 128:(ti + 1) * 128, 2056:2064], pj[:, 4168:4176], [bpj], [bof])
        kb.dma("sp", ob[ti * 128:(ti + 1) * 128, :], obt[:], [bobt], [bob])

    stage_a(0)
    for ti in range(NT):
        if ti + 1 < NT:
            stage_a(ti + 1)
        stage_b(ti)
    kb.finish([bof, bob])
    return nc


def core_tokens(a, c):
    b, r = c // 4, c % 4
    t = a[b].reshape((64, 128) + a.shape[2:])[r::4]
    return np.ascontiguousarray(t.reshape((TPC,) + a.shape[2:]))


def uncore_tokens(parts, tail):
    out = np.empty((NB, 64, 128) + tuple(tail), parts[0].dtype)
    for c in range(NCORES):
        b, r = c // 4, c % 4
        out[b, r::4] = parts[c].reshape((16, 128) + tuple(tail))
    return out.reshape((NB, S) + tuple(tail))


_NC_CACHE = {}
_TRACE = False
_TIMES = []


def _run(nc, maps):
    if _TRACE:
        res = run_bass_kernel_spmd(nc, maps, core_ids=list(range(NCORES)), trace=True)
        _TIMES.append(res.exec_time_ns)
        print('exec_time_ns', res.exec_time_ns)
        return res
    return run_bass_kernel_spmd(nc, maps, core_ids=list(range(NCORES)))


def _get(name, fn):
    if name not in _NC_CACHE:
        _NC_CACHE[name] = fn()
    return _NC_CACHE[name]


def cT_of(c, b):
    return np.ascontiguousarray(c[b].reshape(8, 128).T)


def run_l1(x, c, mod_w_l, mod_b_l, normw_l, w_in_l, qw_l, kw_l):
    nc = _get("l1", build_l1)
    ident = np.eye(128, dtype=np.float32)
    qkw = np.concatenate([qw_l, kw_l]).reshape(1, 128).astype(np.float32)
    maps = []
    for cid in range(NCORES):
        maps.append({"x": core_tokens(x, cid), "cT": cT_of(c, cid // 4), "mod_w": mod_w_l,
                     "mod_b": mod_b_l.reshape(1, -1), "normw": normw_l.reshape(1, -1), "w_in": w_in_l,
                     "qkw": qkw, "ident": ident})
    res = _run(nc, maps)
    of = uncore_tokens([r["of"] for r in res.results], (NF1,))
    ob = uncore_tokens([r["ob"] for r in res.results], (NB1,))
    return of, ob


NE = 32
ALPHA = 1.702
LIMIT = 7.0


def build_l4(e_lo=0, e_hi=NE, first=True):
    n_exp = e_hi - e_lo
    do_final = True
    nc = bass.Bass("TRN2", target_bir_lowering=False)
    x = _din(nc, "x", [TPC, D])
    ycat = _din(nc, "ycat", [TPC, D])
    cT = _din(nc, "cT", [128, 8])
    mod_w = _din(nc, "mod_w", [D, 6 * D])
    mod_b = _din(nc, "mod_b", [1, 6 * D])
    normw = _din(nc, "normw", [1, D])
    w_out = _din(nc, "w_out", [D, D])
    rw = _din(nc, "rw", [D, NE])
    rb = _din(nc, "rb", [1, NE])
    w1 = _din(nc, "w1", [max(n_exp, 1), 8, 128, 8 * 256])
    b1 = _din(nc, "b1", [128, NE * 16])
    w2 = _din(nc, "w2", [max(n_exp, 1), 8, 128, 8 * 128])
    b2 = _din(nc, "b2", [NE, D])
    ident = _din(nc, "ident", [128, 128])
    xprev = None if first else _din(nc, "xprev", [TPC, D])
    xo = _dout(nc, "xo", [TPC, D])
    if first:
        hT_o = _dout(nc, "hT_o", [128, 8 * TPC], BF16)
        gT_o = _dout(nc, "gT_o", [NE, TPC])
        g2_o = _dout(nc, "g2_o", [128, D])
    else:
        hT_i = _din(nc, "hT_i", [128, 8 * TPC], BF16)
        gT_i = _din(nc, "gT_i", [NE, TPC])
        g2_i = _din(nc, "g2_i", [128, D])
    kb = KB(nc)
    dr = Buf("dram_in")
    bxo = Buf("xo")
    bho = Buf("handover")

    PS = []
    for i in range(4):
        t = nc.alloc_psum_tensor("PS%d" % i, [128, 1024], F32)
        PS.append((t, Buf("ps%da" % i), Buf("ps%db" % i)))

    def bank(i):
        t, ba, bb = PS[i // 2]
        return (t[:, 0:512], ba) if i % 2 == 0 else (t[:, 512:1024], bb)

    R_hT, bhT = kb.sb([128, 8192], F32, "R_hT")
    hT = rview(R_hT, 0, [128, 8, TPC], BF16)
    R_acc, bacc = kb.sb([128, 16384], F32, "R_acc")
    acc = rview(R_acc, 0, [128, 8, TPC], F32)
    wo = rview(R_acc, 0, [128, 8, D], BF16)
    bwo = Buf("wo")
    R_act, bactT = kb.sb([128, 8192], F32, "R_act")
    actT = rview(R_act, 0, [128, 8, TPC], BF16)
    modw_bufs = [(rview(R_act, i * 16384, [128, 8, 512], F32), Buf("modw%d" % i)) for i in range(2)]
    gateT, bgateT = kb.sb([NE, TPC], F32, "gateT")
    R_gsb, bgsb = kb.sb([128, TPC], F32, "gsb")
    gsb = R_gsb
    scb_t = (rview(R_gsb, 0, [128, 8, 128], F32), Buf("scb"))
    modb_bufs = [(rview(R_gsb, 4096 + i * 2048, [128, 512], F32), Buf("modb%d" % i)) for i in range(2)]
    R_W, _ = kb.sb([128, 4608], F32, "R_W")
    W1R = [(rview(R_W, i * 4096, [128, 8, 256], BF16), Buf("w1r%d" % i)) for i in range(3)]
    W2R = [(rview(R_W, 12288 + i * 2048, [128, 8, 128], BF16), Buf("w2r%d" % i)) for i in range(3)]
    g1, bg1 = rview(R_W, 0, [128, D], F32), Buf("g1")
    sh2, bsh2 = rview(R_W, 4096, [128, D], F32), Buf("sh2")
    A2, bA2 = rview(R_W, 8192, [128, D], F32), Buf("A2")
    nwb, bnwb = rview(R_W, 12288, [128, D], F32), Buf("nwb")
    g2, bg2 = kb.sb([128, D], F32, "g2")
    xt, bxt = kb.sb([128, D], F32, "xt")
    yt, byt = kb.sb([128, D], F32, "yt")
    tmpA, btmpA = kb.sb([128, D], F32, "tmpA")
    tmpB, btmpB = kb.sb([128, D], F32, "tmpB")
    R1, bR1 = kb.sb([128, D], F32, "R1")
    ybt = rview(R1, 0, [128, D], BF16)
    yT = rview(R1, 2048, [128, 8, 128], BF16)
    h32T = rview(R1, 0, [128, 8, 128], F32)
    R2, bR2 = kb.sb([128, D], F32, "R2")
    hb = rview(R2, 0, [128, D], BF16)
    junk = rview(R2, 2048, [128, D], BF16)

    idf, bidf = kb.sb([128, 128], F32, "identf")
    kb.dma("sp", idf[:], ident[:, :], [dr], [bidf])
    idb, bidb = kb.sb([128, 128], BF16, "identb")
    kb.op("dve", lambda e: e.tensor_copy(out=idb[:], in_=idf[:]), [bidf], [bidb])
    epst, beps = kb.sb([128, 1], F32, "eps")
    kb.op("dve", lambda e: e.memset(epst[:], EPS), [], [beps])
    kb.dma("sp", nwb, normw[0:1, :].to_broadcast([128, D]), [dr], [bnwb])
    rbb, brbb = kb.sb([128, NE], F32, "rbb")
    kb.dma("sp", rbb[:], rb[0:1, :].to_broadcast([128, NE]), [dr], [brbb])
    rwt, brwt = kb.sb([128, 8, NE], F32, "rwt")
    kb.dma("sp", rwt[:], rw.rearrange("(kc p) e -> p kc e", p=128), [dr], [brwt])
    b1a, bb1a = kb.sb([128, NE * 16], F32, "b1a")
    kb.dma("sp", b1a[:], b1[:, :], [dr], [bb1a])
    b1av = b1a[:].rearrange("p (g two) -> p g two", two=2)
    kb.op("dve", lambda e: e.tensor_scalar(out=b1av[:, :, 0:1], in0=b1av[:, :, 0:1], scalar1=ALPHA, scalar2=None,
                                           op0=ALU.mult), [bb1a], [bb1a])
    kb.op("dve", lambda e: e.tensor_scalar(out=b1av[:, :, 1:2], in0=b1av[:, :, 1:2], scalar1=1.0, scalar2=None,
                                           op0=ALU.add), [bb1a], [bb1a])
    b2t, bb2t = kb.sb([NE, D], F32, "b2t")
    kb.dma("sp", b2t[:], b2[:, :], [dr], [bb2t])
    selbs = [kb.sb([NE, 128], F32, "selb") for _ in range(2)]
    st, bst = kb.sb([128, 64], F32, "st")
    lg, blg = kb.sb([128, NE], F32, "lg")

    if first:
        sc2_out = (A2, bA2)
        mods = emit_mod(kb, cT, mod_w, mod_b, [2, 3, 4, 5], wbufs=modw_bufs, bbufs=modb_bufs,
                        pss=[bank(0), bank(1)], scb_t=scb_t,
                        outs={2: (g1, bg1), 3: (sh2, bsh2), 4: sc2_out, 5: (g2[:], bg2)})
        kb.op("dve", lambda e: e.scalar_tensor_tensor(out=A2, in0=A2, scalar=1.0, in1=nwb,
                                                      op0=ALU.add, op1=ALU.mult), [bA2, bnwb], [bA2])

        for kc in range(8):
            kb.dma("pool", wo[:, kc, :], w_out[kc * 128:(kc + 1) * 128, :], [dr], [bwo])
        for ti in range(NT):
            rows = slice(ti * 128, (ti + 1) * 128)
            kb.dma("sp", xt[:], x[rows, :], [dr], [bxt])
            kb.dma("sp", yt[:], ycat[rows, :], [dr], [byt])
            kb.op("act", lambda e: e.copy(out=ybt, in_=yt[:]), [byt], [bR1])
            pt, bpt = bank(0)
            ptb = pt.bitcast(BF16)
            for kc in range(8):
                kb.op("pe", lambda e, kc=kc: e.transpose(out=ptb[:, kc * 128:(kc + 1) * 128],
                                                         in_=ybt[:, kc * 128:(kc + 1) * 128], identity=idb[:]),
                      [bR1, bidb], [bpt])
            kb.op("act", lambda e: e.copy(out=yT.rearrange("p a b -> p (a b)"), in_=ptb), [bpt], [bR1])
            for half in range(2):
                pp, bpp = bank(2 + half)
                for kc in range(8):
                    kb.op("pe", lambda e, kc=kc: e.matmul(pp, yT[:, kc, :], wo[:, kc, half * 512:(half + 1) * 512],
                                                          start=(kc == 0), stop=(kc == 7)), [bR1, bwo], [bpp])
                cs = slice(half * 512, (half + 1) * 512)
                kb.op("dve", lambda e: e.tensor_tensor(out=tmpA[:, cs], in0=pp, in1=g1[:, cs], op=ALU.mult),
                      [bpp, bg1], [btmpA])
            kb.op("dve", lambda e: e.tensor_tensor(out=xt[:], in0=tmpA[:], in1=xt[:], op=ALU.add), [btmpA, bxt], [bxt])
            kb.dma("sp", xo[rows, :], xt[:], [bxt], [bxo])
            kb.op("act", lambda e: e.activation(out=junk, in_=xt[:], func=AF.Square, accum_out=st[:, 0:1]),
                  [bxt], [bR2, bst])
            kb.op("act", lambda e: e.activation(out=st[:, 1:2], in_=st[:, 0:1], func=AF.Sqrt, scale=1.0 / D,
                                                bias=epst[:]), [bst, beps], [bst])
            kb.op("dve", lambda e: e.reciprocal(out=st[:, 2:3], in_=st[:, 1:2]), [bst], [bst])
            kb.op("dve", lambda e: e.scalar_tensor_tensor(out=tmpA[:], in0=xt[:], scalar=st[:, 2:3], in1=A2,
                                                          op0=ALU.mult, op1=ALU.mult), [bxt, bst, bA2], [btmpA])
            kb.op("dve", lambda e: e.tensor_tensor(out=tmpB[:], in0=tmpA[:], in1=sh2, op=ALU.add),
                  [btmpA, bsh2], [btmpB])
            kb.op("act", lambda e: e.copy(out=hb, in_=tmpB[:]), [btmpB], [bR2])
            pt, bpt = bank(1)
            ptb = pt.bitcast(BF16)
            for kc in range(8):
                kb.op("pe", lambda e, kc=kc: e.transpose(out=ptb[:, kc * 128:(kc + 1) * 128],
                                                         in_=hb[:, kc * 128:(kc + 1) * 128], identity=idb[:]),
                      [bR2, bidb], [bpt])
            kb.op("act", lambda e: e.copy(out=hT[:, :, rows], in_=ptb.rearrange("p (a b) -> p a b", a=8)),
                  [bpt], [bhT])
            for half in range(2):
                pq, bpq = bank(4 + half)
                for k4 in range(4):
                    kc = half * 4 + k4
                    kb.op("pe", lambda e, kc=kc, k4=k4: e.transpose(out=pq[:, k4 * 128:(k4 + 1) * 128],
                                                                    in_=tmpB[:, kc * 128:(kc + 1) * 128],
                                                                    identity=idf[:]), [btmpB, bidf], [bpq])
                kb.op("dve", lambda e: e.tensor_copy(
                    out=h32T[:, half * 4:(half + 1) * 4, :], in_=pq.rearrange("p (a b) -> p a b", a=4)),
                    [bpq], [bR1])
            pr, bpr = bank(6)
            for kc in range(8):
                kb.op("pe", lambda e, kc=kc: e.matmul(pr[:, 0:NE], h32T[:, kc, :], rwt[:, kc, :],
                                                      start=(kc == 0), stop=(kc == 7)), [bR1, brwt], [bpr])
            kb.op("dve", lambda e: e.tensor_tensor(out=lg[:], in0=pr[:, 0:NE], in1=rbb[:], op=ALU.add),
                  [bpr, brbb], [blg])
            kb.op("dve", lambda e: e.max(out=st[:, 8:16], in_=lg[:]), [blg], [bst])
            kb.op("dve", lambda e: e.tensor_scalar(out=st[:, 16:17], in0=st[:, 8:9], scalar1=-1.0, scalar2=None,
                                                   op0=ALU.mult), [bst], [bst])
            kb.op("act", lambda e: e.activation(out=st[:, 20:24], in_=st[:, 8:12], func=AF.Exp, bias=st[:, 16:17],
                                                accum_out=st[:, 17:18]), [bst], [bst])
            kb.op("dve", lambda e: e.reciprocal(out=st[:, 18:19], in_=st[:, 17:18]), [bst], [bst])
            kb.op("act", lambda e: e.activation(out=st[:, 32:64], in_=lg[:], func=AF.Exp, bias=st[:, 16:17]),
                  [blg, bst], [bst])
            kb.op("dve", lambda e: e.tensor_scalar(out=lg[:], in0=lg[:], scalar1=st[:, 11:12], scalar2=None,
                                                   op0=ALU.is_ge), [blg, bst], [blg])
            kb.op("dve", lambda e: e.scalar_tensor_tensor(out=lg[:], in0=st[:, 32:64], scalar=st[:, 18:19], in1=lg[:],
                                                          op0=ALU.mult, op1=ALU.mult), [bst, blg], [blg])
            pg, bpg = bank(7)
            kb.op("pe", lambda e: e.transpose(out=pg[0:NE, 0:128], in_=lg[:], identity=idf[:]), [blg, bidf], [bpg])
            kb.op("act", lambda e: e.copy(out=gateT[:, rows], in_=pg[0:NE, 0:128]), [bpg], [bgateT])

        kb.dma("sp", hT_o[:, :], R_hT[:].bitcast(BF16), [bhT], [bho])
        kb.dma("sp", gT_o[:, :], gateT[:], [bgateT], [bho])
        kb.dma("sp", g2_o[:, :], g2[:], [bg2], [bho])
    else:
        kb.dma("sp", R_hT[:].bitcast(BF16), hT_i[:, :], [dr], [bhT])
        kb.dma("sp", gateT[:], gT_i[:, :], [dr], [bgateT])
        kb.dma("sp", g2[:], g2_i[:, :], [dr], [bg2])
    merge_deps(bacc, [bwo])
    merge_deps(bactT, [b for _, b in modw_bufs])
    merge_deps(bgsb, [scb_t[1]] + [b for _, b in modb_bufs])
    for _, b in W1R + W2R:
        merge_deps(b, [bg1, bsh2, bA2, bnwb])
    tts = [(tmpA[:, 0:512], Buf("tt0")), (tmpA[:, 512:1024], Buf("tt1"))]
    lps = [(tmpB[:, 0:512], Buf("lp0")), (tmpB[:, 512:1024], Buf("lp1"))]
    lgs = [(xt[:, 0:512], Buf("lg0")), (xt[:, 512:1024], Buf("lg1"))]
    for (_, b), src_ in zip(tts + lps + lgs, [btmpA, btmpA, btmpB, btmpB, bxt, bxt]):
        merge_deps(b, [src_])
    C0 = float(LIMIT * ALPHA / (1.0 + np.exp(-LIMIT * ALPHA)))

    w1_issued = [0]
    w2_issued = [0]

    def issue_w1(upto):
        while w1_issued[0] < min(upto, n_exp * 8):
            i = w1_issued[0]
            t, b = W1R[i % 3]
            kb.dma("pool", t.rearrange("p a b -> p (a b)"), w1[i // 8, i % 8, :, :], [dr], [b])
            w1_issued[0] += 1

    def issue_w2(upto):
        while w2_issued[0] < min(upto, n_exp * 8):
            i = w2_issued[0]
            t, b = W2R[i % 3]
            kb.dma("pool", t.rearrange("p a b -> p (a b)"), w2[i // 8, i % 8, :, :], [dr], [b])
            w2_issued[0] += 1

    nu = 0
    ny = 0
    for e in range(e_lo, e_hi):
        selb, bselb = selbs[e % 2]
        kb.op("dve", lambda en: en.tensor_copy(out=selb[:], in_=idf[0:NE, e:e + 1].to_broadcast([NE, 128])),
              [bidf], [bselb])
        for blk in range(4):
            pgb, bpgb = bank(6)
            kb.op("pe", lambda en, blk=blk: en.matmul(pgb, selb[:], gateT[:, blk * 512:(blk + 1) * 512],
                                                      start=True, stop=True), [bselb, bgateT], [bpgb])
            kb.op("act", lambda en, blk=blk: en.activation(out=gsb[:, blk * 512:(blk + 1) * 512], in_=pgb,
                                                           func=AF.Copy, scale=1.0 / ALPHA), [bpgb], [bgsb])
        for ft in range(8):
            gi = (e - e_lo) * 8 + ft
            issue_w1(gi + 2)
            wv, bwt = W1R[gi % 3]
            bcol = (e * 8 + ft) * 2
            for blk in range(4):
                ts_ = slice(blk * 512, (blk + 1) * 512)
                pgl, bpgl = bank(0 + 2 * (nu % 2))
                pli, bpli = bank(1 + 2 * (nu % 2))
                for kc in range(8):
                    kb.op("pe", lambda en, kc=kc: en.matmul(pgl, wv[:, kc, 0:128], hT[:, kc, ts_],
                                                            start=(kc == 0), stop=(kc == 7)), [bwt, bhT], [bpgl])
                for kc in range(8):
                    kb.op("pe", lambda en, kc=kc: en.matmul(pli, wv[:, kc, 128:256], hT[:, kc, ts_],
                                                            start=(kc == 0), stop=(kc == 7)), [bwt, bhT], [bpli])
                tt, btt = tts[nu % 2]
                lp, blp = lps[nu % 2]
                lgg, blgg = lgs[nu % 2]
                kb.op("act", lambda en: en.activation(out=tt, in_=pgl, func=AF.Silu, scale=ALPHA,
                                                      bias=b1a[:, bcol:bcol + 1]), [bpgl, bb1a], [btt])
                kb.op("dve", lambda en: en.tensor_scalar(out=lp, in0=pli, scalar1=b1a[:, bcol + 1:bcol + 2],
                                                         scalar2=LIMIT + 1.0, op0=ALU.add, op1=ALU.min),
                      [bpli, bb1a], [blp])
                kb.op("dve", lambda en: en.scalar_tensor_tensor(out=lgg, in0=lp, scalar=1.0 - LIMIT, in1=gsb[:, ts_],
                                                                op0=ALU.max, op1=ALU.mult), [blp, bgsb], [blgg])
                kb.op("dve", lambda en: en.scalar_tensor_tensor(out=actT[:, ft, ts_], in0=tt, scalar=C0, in1=lgg,
                                                                op0=ALU.min, op1=ALU.mult), [btt, blgg], [bactT])
                nu += 1
        for dt in range(8):
            gi = (e - e_lo) * 8 + dt
            issue_w2(gi + 2)
            wv, bwt = W2R[gi % 3]
            for blk in range(4):
                ts_ = slice(blk * 512, (blk + 1) * 512)
                py, bpy = bank(4 + (ny % 2))
                for fc in range(8):
                    kb.op("pe", lambda en, fc=fc: en.matmul(py, wv[:, fc, :], actT[:, fc, ts_],
                                                            start=(fc == 0), stop=(fc == 7)), [bwt, bactT], [bpy])
                if e == e_lo:
                    kb.op("dve", lambda en: en.tensor_copy(out=acc[:, dt, ts_], in_=py), [bpy], [bacc])
                else:
                    kb.op("dve", lambda en: en.tensor_tensor(out=acc[:, dt, ts_], in0=py, in1=acc[:, dt, ts_],
                                                             op=ALU.add), [bpy, bacc], [bacc])
                ny += 1

    tf, btf = tts[0]
    for ti in range(NT if do_final else 0):
        rows = slice(ti * 128, (ti + 1) * 128)
        if first:
            kb.dma("sp", yt[:], xo[rows, :], [bxo], [byt])
        else:
            kb.dma("sp", yt[:], xprev[rows, :], [dr], [byt])
        for half in range(2):
            po, bpo = bank(half)
            for d4 in range(4):
                dt = half * 4 + d4
                cs = slice(d4 * 128, (d4 + 1) * 128)
                if first:
                    kb.op("pe", lambda en, dt=dt, cs=cs: en.matmul(po[:, cs], gateT[:, rows],
                                                                  b2t[:, dt * 128:(dt + 1) * 128],
                                                                  start=True, stop=False), [bgateT, bb2t], [bpo])
                kb.op("pe", lambda en, dt=dt, cs=cs: en.matmul(po[:, cs], acc[:, dt, rows], idf[:],
                                                              start=(not first), stop=True), [bacc, bidf], [bpo])
            cs2 = slice(half * 512, (half + 1) * 512)
            kb.op("dve", lambda en: en.tensor_tensor(out=tf, in0=po, in1=g2[:, cs2], op=ALU.mult),
                  [bpo, bg2], [btf])
            kb.op("dve", lambda en: en.tensor_tensor(out=yt[:, cs2], in0=tf, in1=yt[:, cs2], op=ALU.add),
                  [btf, byt], [byt])
        kb.dma("sp", xo[rows, :], yt[:], [byt], [bxo])
    kb.finish([bxo, bho])
    return nc


def prep_l4_weights(w1_l, b1_l, w2_l, b2_l):
    w1r = w1_l.reshape(NE, 8, 128, 8, 128, 2)
    w1r = w1r.transpose(0, 3, 2, 1, 5, 4)
    w1r = np.ascontiguousarray(w1r).reshape(NE, 8, 128, 8 * 256)
    b1r = b1_l.reshape(NE, 8, 128, 2).transpose(2, 0, 1, 3)
    b1r = np.ascontiguousarray(b1r).reshape(128, NE * 16)
    w2r = w2_l.reshape(NE, 8, 128, 8, 128).transpose(0, 3, 2, 1, 4)
    w2r = np.ascontiguousarray(w2r).reshape(NE, 8, 128, 8 * 128)
    return w1r, b1r, w2r, np.ascontiguousarray(b2_l)


def run_l4(x, ycat, c, mod_w_l, mod_b_l, normw_l, w_out_l, rw_l, rb_l, w1r, b1r, w2r, b2_l, splits=((0, 16), (16, 32))):
    ident = np.eye(128, dtype=np.float32)
    xprev = None
    for (lo, hi) in splits:
        first = (lo == 0)
        nc = _get("l4_%d_%d" % (lo, hi), lambda: build_l4(lo, hi, first))
        w1s = np.ascontiguousarray(w1r[lo:hi])
        w2s = np.ascontiguousarray(w2r[lo:hi])
        maps = []
        for cid in range(NCORES):
            m = {"x": core_tokens(x, cid), "ycat": core_tokens(ycat, cid), "cT": cT_of(c, cid // 4),
                 "mod_w": mod_w_l, "mod_b": mod_b_l.reshape(1, -1), "normw": normw_l.reshape(1, -1),
                 "w_out": w_out_l, "rw": rw_l, "rb": rb_l.reshape(1, -1), "w1": w1s, "b1": b1r, "w2": w2s,
                 "b2": b2_l, "ident": ident}
            if not first:
                m["xprev"] = xprev[cid]
                m["hT_i"] = hand[cid]["hT_o"]
                m["gT_i"] = hand[cid]["gT_o"]
                m["g2_i"] = hand[cid]["g2_o"]
            maps.append(m)
        res = _run(nc, maps)
        xprev = [r["xo"] for r in res.results]
        if first:
            hand = res.results
    return uncore_tokens(xprev, (D,))


NIT = 16
KSEL = 256
NEGBIG = -30000.0
NREL = 1280


def build_l3(nj=16):
    nc = bass.Bass("TRN2", target_bir_lowering=False)
    qT = _din(nc, "qT", [128, 16, 4 * 128], BF16)
    kT = _din(nc, "kT", [128, 4 * S], BF16)
    v1 = _din(nc, "v1", [64, 128, 8 * 65], BF16)
    qiT = _din(nc, "qiT", [128, 16, 4 * 128], BF16)
    kiT = _din(nc, "kiT", [64, S], BF16)
    wi = _din(nc, "wi", [128, 128])
    pen = _din(nc, "pen", [128, 512])
    oh = _din(nc, "oh", [32, NREL])
    relb = _din(nc, "relb", [32, 8])
    negI4 = _din(nc, "negI4", [128, 512], BF16)
    ident = _din(nc, "ident", [128, 128])
    yb = _dout(nc, "yb", [TPC, 512])
    scr = nc.dram_tensor("scr", [8, NREL], BF16, kind="Internal").ap()
    kb = KB(nc)
    dr = Buf("dram_in")
    byb = Buf("yb")
    bscr = Buf("scr")

    PS = []
    for i in range(4):
        t = nc.alloc_psum_tensor("PS%d" % i, [128, 1024], F32)
        PS.append((t, Buf("ps%da" % i), Buf("ps%db" % i)))

    def bank(i):
        t, ba, bb = PS[i // 2]
        return (t[:, 0:512], ba) if i % 2 == 0 else (t[:, 512:1024], bb)

    kTt, bkT = kb.sb([128, 4 * S], BF16, "kT")
    for i in range(4):
        kb.dma("sp", kTt[:, i * S:(i + 1) * S], kT[:, i * S:(i + 1) * S], [dr], [bkT])
    kTv = kTt[:].rearrange("p (h s) -> p h s", h=4)
    kibs = [kb.sb([128, 512], BF16, "kib") for _ in range(3)]
    nkb = [0]
    wit, bwi = kb.sb([128, 128], F32, "wi")
    kb.dma("sp", wit[:], wi[:, :], [dr], [bwi])
    pent, bpen = kb.sb([128, 512], F32, "pen")
    kb.dma("sp", pent[:], pen[:, :], [dr], [bpen])
    n4, bn4 = kb.sb([128, 512], BF16, "negI4")
    kb.dma("sp", n4[:], negI4[:, :], [dr], [bn4])
    idf, bidf = kb.sb([128, 128], F32, "identf")
    kb.dma("sp", idf[:], ident[:, :], [dr], [bidf])
    idb, bidb = kb.sb([128, 128], BF16, "identb")
    kb.op("dve", lambda e: e.tensor_copy(out=idb[:], in_=idf[:]), [bidf], [bidb])
    zb, bzb = kb.sb([128, 260], BF16, "zeros")
    kb.op("dve", lambda e: e.memset(zb[:], 0.0), [], [bzb])

    scores = [kb.sb([128, S], F32, "score") for _ in range(2)]
    oht, boh = rview(scores[1][0], 0, [32, NREL], F32), Buf("oh")
    kb.dma("sp", oht, oh[:, :], [dr], [boh])
    rbt, brb = kb.sb([32, 8], F32, "relb")
    kb.dma("sp", rbt[:], relb[:, :], [dr], [brb])
    bv, bbv = rview(scores[1][0], 8192, [8, NREL], BF16), Buf("bvec")
    for i, (c0, n) in enumerate([(0, 512), (512, 512), (1024, 256)]):
        pb, bpb = bank(7)
        kb.op("pe", lambda e: e.matmul(pb[0:8, 0:n], rbt[:], oht[:, c0:c0 + n], start=True, stop=True),
              [brb, boh], [bpb])
        kb.op("dve", lambda e: e.tensor_copy(out=bv[:, c0:c0 + n], in_=pb[0:8, 0:n]), [bpb], [bbv])
    kb.dma("sp", scr[:, :], bv, [bbv], [bscr])
    merge_deps(scores[1][1], [boh, bbv])
    TU, bTU = kb.sb([128, 9, 8 * 128], BF16, "TU")
    for u in range(9):
        src = bass.AP(tensor=scr.tensor, offset=128 * u, ap=[[1, 128], [NREL, 8], [1, 128]])
        kb.dma("sp", TU[:, u, :].rearrange("p (h t) -> p h t", h=8), src, [bscr], [bTU])

    nots = [kb.sb([128, S], BF16, "notsel") for _ in range(2)]
    qts = [kb.sb([128, 512], BF16, "qTj") for _ in range(2)]
    qis = [kb.sb([128, 512], BF16, "qiTj") for _ in range(2)]
    dgs = [kb.sb([128, 1024], BF16, "Dg")] * 2
    rhs_ = [kb.sb([128, 512], BF16, "rh") for _ in range(4)]
    pts = [kb.sb([128, 512], BF16, "pt") for _ in range(2)]
    vts = [kb.sb([128, 520], BF16, "v1t") for _ in range(3)]
    outs = [kb.sb([128, 512], F32, "yo")] * 2
    st, bst = kb.sb([128, 32], F32, "st")

    nr = [0]
    nd = [0]
    nv = [0]
    nsc = [0]
    LAG = 3
    vts.append(kb.sb([128, 520], BF16, "v1t"))
    pts2 = [[pts[0], kb.sb([128, 512], BF16, "pt")], [pts[1], kb.sb([128, 512], BF16, "pt")]]

    def idx(j):
        qi_t, bqi = qis[j % 2]
        kb.dma("sp", qi_t[:], qiT[:, j, :], [dr], [bqi])
        dg, bdg = dgs[j % 2]
        for h in range(8):
            kb.op("pool", lambda e, h=h: e.tensor_scalar(out=dg[:, h * 128:(h + 1) * 128], in0=idf[:],
                                                         scalar1=wit[:, j * 8 + h:j * 8 + h + 1], scalar2=None,
                                                         op0=ALU.mult), [bidf, bwi], [bdg])
        score, bscore = scores[j % 2]
        steps = [(sb, hq) for sb in range(j + 1) for hq in range(4)]
        pend = []
        sbank = {}
        kibt = {}
        LAGP = 1
        for s in range(len(steps) + LAGP):
            if s < len(steps):
                sb, hq = steps[s]
                if hq == 0:
                    kibt[sb] = kibs[nkb[0] % 3]
                    nkb[0] += 1
                    for hh in range(2):
                        kb.dma("sp", kibt[sb][0][hh * 64:(hh + 1) * 64, :], kiT[:, sb * 512:(sb + 1) * 512], [dr],
                               [kibt[sb][1]])
                kit, bki = kibt[sb]
                for hh in range(2):
                    pd, bpd = bank(nd[0] % 4)
                    nd[0] += 1
                    kb.op("pe", lambda e, hq=hq, hh=hh, kit=kit, pd=pd: e.matmul(
                        pd, qi_t[hh * 64:(hh + 1) * 64, hq * 128:(hq + 1) * 128], kit[hh * 64:(hh + 1) * 64, :],
                        start=True, stop=True), [bqi, bki], [bpd])
                    rh, brh = rhs_[nr[0] % 4]
                    nr[0] += 1
                    pend.append((sb, hh * 4 + hq, hq * 2 + hh, rh, brh, pd, bpd))
                for (sb_, h_, o_, rh, brh, pd, bpd) in pend[-2:]:
                    kb.op("act", lambda e, rh=rh, pd=pd: e.activation(out=rh[:], in_=pd, func=AF.Relu), [bpd], [brh])
            if s - LAGP >= 0:
                for (sb, h, o, rh, brh, pd, bpd) in pend[2 * (s - LAGP):2 * (s - LAGP) + 2]:
                    if o == 0:
                        sbank[sb] = bank(6 + nsc[0] % 2)
                        nsc[0] += 1
                    ps, bps = sbank[sb]
                    kb.op("pe", lambda e, h=h, rh=rh, ps=ps, o=o: e.matmul(ps, dg[:, h * 128:(h + 1) * 128], rh[:],
                                                                           start=(o == 0), stop=(o == 7)),
                          [bdg, brh], [bps])
                    if o == 7:
                        kb.op("act", lambda e, sb=sb, ps=ps: e.copy(out=score[:, sb * 512:(sb + 1) * 512], in_=ps),
                              [bps], [bscore])

    def bis(j):
        n = 512 * (j + 1)
        ns, bns = nots[j % 2]
        score, bscore = scores[j % 2]
        sc = score[:, 0:n]
        kb.op("dve", lambda e: e.tensor_reduce(out=st[:, 0:1], in_=sc, axis=AX.X, op=ALU.min), [bscore], [bst])
        kb.op("dve", lambda e: e.tensor_tensor(out=score[:, n - 512:n], in0=score[:, n - 512:n], in1=pent[:],
                                               op=ALU.add), [bscore, bpen], [bscore])
        kb.op("dve", lambda e: e.tensor_reduce(out=st[:, 1:2], in_=sc, axis=AX.X, op=ALU.max), [bscore], [bst])
        kb.op("dve", lambda e: e.tensor_tensor(out=st[:, 2:3], in0=st[:, 1:2], in1=st[:, 0:1], op=ALU.subtract),
              [bst], [bst])
        kb.op("dve", lambda e: e.scalar_tensor_tensor(out=st[:, 3:4], in0=st[:, 2:3], scalar=-0.01, in1=st[:, 0:1],
                                                      op0=ALU.mult, op1=ALU.add), [bst], [bst])
        kb.op("dve", lambda e: e.tensor_scalar(out=st[:, 3:4], in0=st[:, 3:4], scalar1=-1e-6, scalar2=None,
                                               op0=ALU.add), [bst], [bst])
        kb.op("dve", lambda e: e.tensor_tensor(out=st[:, 4:5], in0=st[:, 1:2], in1=st[:, 3:4], op=ALU.subtract),
              [bst], [bst])
        kb.op("dve", lambda e: e.scalar_tensor_tensor(out=st[:, 5:6], in0=st[:, 4:5], scalar=0.5, in1=st[:, 3:4],
                                                      op0=ALU.mult, op1=ALU.add), [bst], [bst])
        kb.op("dve", lambda e: e.tensor_scalar(out=st[:, 6:7], in0=st[:, 4:5], scalar1=0.25, scalar2=None,
                                               op0=ALU.mult), [bst], [bst])
        for it in range(NIT):
            kb.op("dve", lambda e: e.tensor_scalar(out=ns[:, 0:n], in0=sc, scalar1=st[:, 5:6], scalar2=None,
                                                   op0=ALU.is_ge, op1=ALU.add, accum_out=st[:, 7:8]),
                  [bscore, bst], [bns, bst])
            kb.op("dve", lambda e: e.tensor_scalar(out=st[:, 8:9], in0=st[:, 7:8], scalar1=KSEL - 0.5, scalar2=2.0,
                                                   op0=ALU.is_ge, op1=ALU.mult), [bst], [bst])
            kb.op("dve", lambda e: e.scalar_tensor_tensor(out=st[:, 9:10], in0=st[:, 8:9], scalar=-1.0,
                                                          in1=st[:, 6:7], op0=ALU.add, op1=ALU.mult), [bst], [bst])
            kb.op("dve", lambda e: e.tensor_tensor(out=st[:, 5:6], in0=st[:, 5:6], in1=st[:, 9:10], op=ALU.add),
                  [bst], [bst])
            kb.op("dve", lambda e: e.tensor_scalar(out=st[:, 6:7], in0=st[:, 6:7], scalar1=0.5, scalar2=None,
                                                   op0=ALU.mult), [bst], [bst])
        kb.op("dve", lambda e: e.scalar_tensor_tensor(out=st[:, 10:11], in0=st[:, 6:7], scalar=-4.0, in1=st[:, 5:6],
                                                      op0=ALU.mult, op1=ALU.add), [bst], [bst])
        kb.op("dve", lambda e: e.tensor_scalar(out=ns[:, 0:n], in0=sc, scalar1=st[:, 10:11], scalar2=None,
                                               op0=ALU.is_lt), [bscore, bst], [bns])

    def att_main(j):
        ns, bns = nots[j % 2]
        qt, bqt = qts[j % 2]
        kb.dma("sp", qt[:], qT[:, j, :], [dr], [bqt])
        oacc = [bank(4), bank(5)]
        for g in range(2):
            oa, boa = oacc[g]
            kb.op("pe", lambda e: e.matmul(oa[:, 0:260], idb[:], zb[:], start=True, stop=False),
                  [bidb, bzb], [boa])
        ntile = 4 * j + 4
        vtl = {}
        for stl in range(ntile + 1):
            if stl < ntile:
                vt, bvt = vts[nv[0] % 4]
                nv[0] += 1
                vtl[stl] = (vt, bvt)
                kb.dma("sp", vt[:], v1[stl, :, :], [dr], [bvt])
                u = stl - 4 * j + 5
                lgs_ = [bank(2 * g + stl % 2) for g in range(2)]
                for g in range(2):
                    lgp, blgp = lgs_[g]
                    kb.op("pe", lambda e, lgp=lgp: e.matmul(lgp, ns[:, stl * 128:(stl + 1) * 128], n4[:], start=True,
                                                            stop=False), [bns, bn4], [blgp])
                    if u >= 0:
                        kb.op("pe", lambda e, u=u, lgp=lgp, g=g: e.matmul(lgp, idb[:], TU[:, u, g * 512:(g + 1) * 512],
                                                                          start=False, stop=False), [bidb, bTU], [blgp])
                for hq in range(4):
                    for g in range(2):
                        lgp, blgp = lgs_[g]
                        kb.op("pe", lambda e, hq=hq, g=g, lgp=lgp: e.matmul(
                            lgp[:, hq * 128:(hq + 1) * 128],
                            kTv[g * 64:(g + 1) * 64, hq, stl * 128:(stl + 1) * 128],
                            qt[g * 64:(g + 1) * 64, hq * 128:(hq + 1) * 128],
                            start=False, stop=(hq == 3)), [bkT, bqt], [blgp])
                for g in range(2):
                    lgp, blgp = lgs_[g]
                    pt, bpt = pts2[g][stl % 2]
                    kb.op("act", lambda e, lgp=lgp, pt=pt: e.activation(out=pt[:], in_=lgp, func=AF.Exp), [blgp], [bpt])
            if stl >= 1:
                sp_ = stl - 1
                vt, bvt = vtl[sp_]
                for g in range(2):
                    pt, bpt = pts2[g][sp_ % 2]
                    oa, boa = oacc[g]
                    for hq in range(4):
                        h = g * 4 + hq
                        kb.op("pe", lambda e, hq=hq, h=h: e.matmul(oa[:, hq * 65:(hq + 1) * 65],
                                                                   pt[:, hq * 128:(hq + 1) * 128],
                                                                   vt[:, h * 65:(h + 1) * 65],
                                                                   start=False, stop=(sp_ == ntile - 1 and hq == 3)),
                              [bpt, bvt], [boa])

    def att_fin(j):
        oacc = [bank(4), bank(5)]
        yo, byo = outs[j % 2]
        for g in range(2):
            oa, boa = oacc[g]
            oav = oa[:, 0:260].rearrange("p (h c) -> p h c", h=4)
            kb.op("dve", lambda e: e.reciprocal(out=st[:, 16 + g * 4:20 + g * 4],
                                                in_=oav[:, :, 64:65].rearrange("p h c -> p (h c)")), [boa], [bst])
            kb.op("dve", lambda e: e.tensor_tensor(
                out=yo[:, g * 256:(g + 1) * 256].rearrange("p (h d) -> p h d", h=4), in0=oav[:, :, 0:64],
                in1=st[:, 16 + g * 4:20 + g * 4].unsqueeze(2).to_broadcast([128, 4, 64]), op=ALU.mult),
                [boa, bst], [byo])
        kb.dma("sp", yb[j * 128:(j + 1) * 128, :], yo[:], [byo], [byb])

    idx(0)
    if nj > 1:
        idx(1)
    bis(0)
    for j in range(nj):
        if j + 2 < nj:
            idx(j + 2)
        att_main(j)
        if j + 1 < nj:
            bis(j + 1)
        att_fin(j)
    kb.finish([byb])
    return nc


def t5_bucket_np(rel):
    nb = 16
    max_exact = 8
    side = np.where(rel > 0, nb, 0)
    n = np.abs(rel)
    nf = np.maximum(n, 1).astype(np.float32)
    large = max_exact + (np.log(nf / max_exact) / np.float32(np.log(1024 / max_exact)) * (nb - max_exact)).astype(np.int32)
    large = np.minimum(large, nb - 1)
    return side + np.where(n < max_exact, n, large)


def prep_l3(ob, of, cid):
    b, r = cid // 4, cid % 4
    bf = ob.dtype
    qsel = ob[b].reshape(64, 128, NB1)[r::4][:, ::-1]
    q = qsel[..., 0:512].reshape(16, 128, 2, 4, 64)
    qT = np.ascontiguousarray(q.transpose(2, 4, 0, 3, 1)).reshape(128, 16, 512)
    qi = qsel[..., 1536:2048].reshape(16, 128, 2, 4, 64)
    qiT = np.ascontiguousarray(qi.transpose(2, 4, 0, 3, 1)).reshape(128, 16, 512)
    wsel = of[b].reshape(64, 128, NF1)[r::4][:, ::-1, 2056:2064]
    wi = np.ascontiguousarray(wsel.transpose(1, 0, 2)).reshape(128, 128).astype(np.float32)
    k = ob[b, :, 512:1024].reshape(S, 2, 4, 64)
    kT = np.ascontiguousarray(k.transpose(1, 3, 2, 0)).reshape(128, 4 * S)
    kiT = np.ascontiguousarray(ob[b, :, 2048:2112].T)
    v = ob[b, :, 1024:1536].reshape(64, 128, 8, 64)
    v1 = np.ones((64, 128, 8, 65), bf)
    v1[..., 0:64] = v
    v1 = v1.reshape(64, 128, 520)
    tq = np.arange(128)[:, None]
    sk = np.arange(512)[None, :]
    pen = np.where((sk // 64) <= 2 * r + (tq < 64), 0.0, -1e30).astype(np.float32)
    m = np.arange(NREL)
    rel = m - 767 - 128 * r
    bk = t5_bucket_np(rel)
    oh = np.zeros((32, NREL), np.float32)
    oh[bk, m] += 1.0
    oh[15, :] -= 1.0
    negI4 = np.tile(np.eye(128, dtype=np.float32) * NEGBIG, (1, 4)).astype(bf)
    return {"qT": qT, "kT": kT, "v1": v1, "qiT": qiT, "kiT": kiT, "wi": wi, "pen": pen, "oh": oh,
            "negI4": negI4, "ident": np.eye(128, dtype=np.float32)}


def run_l3(ob, of, rel_bias, nj=16):
    nc = _get("l3_%d" % nj, lambda: build_l3(nj))
    maps = []
    for cid in range(NCORES):
        m = prep_l3(ob, of, cid)
        m["relb"] = np.ascontiguousarray(rel_bias.astype(np.float32))
        maps.append(m)
    res = _run(nc, maps)
    parts = [r["yb"].reshape(16, 128, 512)[:, ::-1].reshape(TPC, 512) for r in res.results]
    return uncore_tokens(parts, (512,))


NCH = 64
PRE_STOP = 0
L2VAR = 0
POOL_ENG = "dve"


def build_l2(nch=NCH, stop=None):
    nc = bass.Bass("TRN2", target_bir_lowering=False)
    xin = _din(nc, "xin", [128, 3, S + 3])
    cw = _din(nc, "cw", [128, 12])
    zin = _din(nc, "zin", [128, NCH, 128])
    bcol = _din(nc, "bcol", [128, NCH])
    acol = _din(nc, "acol", [128, NCH])
    sc3 = _din(nc, "sc3", [1, 2])
    gnw = _din(nc, "gnw", [1, 128])
    cst = _din(nc, "cst", [128, 7, 128])
    ya = _dout(nc, "ya", [S, 128])
    kb = KB(nc)
    dr = Buf("dram_in")
    bya = Buf("ya")

    PSW = []
    for i in range(4):
        t = nc.alloc_psum_tensor("PS%d" % i, [128, 1024], F32)
        PSW.append(t)
    slots = []
    for bnk in range(6):
        t = PSW[bnk // 2]
        c0 = (bnk % 2) * 512
        slots.append((t[:, c0:c0 + 128], Buf("slot%d" % bnk)))
    wide = [(PSW[3][:, 0:512], Buf("wide0")), (PSW[3][:, 512:1024], Buf("wide1"))]
    nslot = [0]

    def slot():
        s = slots[nslot[0] % len(slots)]
        nslot[0] += 1
        return s

    ct, bct = kb.sb([128, 7, 128], F32, "cst")
    kb.dma("sp", ct[:], cst[:, :, :], [dr], [bct])
    ident, LT, ones, negones, penL, SM, sel127 = [ct[:, i, :] for i in range(7)]
    cwt, bcw = kb.sb([128, 12], F32, "cw")
    kb.dma("sp", cwt[:], cw[:, :], [dr], [bcw])
    gnb, bgnb = kb.sb([128, 128], F32, "gnw")
    kb.dma("sp", gnb[:], gnw[0:1, :].to_broadcast([128, 128]), [dr], [bgnb])
    s3, bs3 = kb.sb([128, 2], F32, "sc3")
    kb.dma("sp", s3[:], sc3[0:1, :].to_broadcast([128, 2]), [dr], [bs3])
    epst, beps = kb.sb([128, 3], F32, "eps")
    kb.op("dve", lambda e: e.memset(epst[:, 0:1], EPS), [], [beps])
    kb.op("dve", lambda e: e.memset(epst[:, 1:2], 128.0 * EPS), [], [beps])
    kb.op("dve", lambda e: e.memset(epst[:, 2:3], 1.0), [], [beps])

    cols, bcols = kb.sb([128, 10, NCH], F32, "cols")
    BETA, G, GC, EGC, BG, KD, EGL, NB_, TMP, TMP2 = range(10)
    kb.dma("sp", cols[:, BETA, :], bcol[:, :], [dr], [bcols])
    kb.dma("sp", cols[:, TMP, :], acol[:, :], [dr], [bcols])
    kb.op("act", lambda e: e.activation(out=cols[:, BETA, :], in_=cols[:, BETA, :], func=AF.Sigmoid), [bcols], [bcols])
    kb.op("act", lambda e: e.activation(out=cols[:, TMP, :], in_=cols[:, TMP, :], func=AF.Exp, bias=s3[:, 1:2]),
          [bcols, bs3], [bcols])
    kb.op("act", lambda e: e.activation(out=cols[:, TMP, :], in_=cols[:, TMP, :], func=AF.Ln, bias=epst[:, 2:3]),
          [bcols, beps], [bcols])
    kb.op("act", lambda e: e.activation(out=s3[:, 0:1], in_=s3[:, 0:1], func=AF.Exp), [bs3], [bs3])
    kb.op("dve", lambda e: e.tensor_scalar(out=cols[:, G, :], in0=cols[:, TMP, :], scalar1=s3[:, 0:1], scalar2=-1.0,
                                           op0=ALU.mult, op1=ALU.mult), [bcols, bs3], [bcols])
    pg, bpg = slot()
    kb.op("pe", lambda e: e.matmul(pg[:, 0:NCH], LT, cols[:, G, :], start=True, stop=True), [bct, bcols], [bpg])
    kb.op("dve", lambda e: e.tensor_copy(out=cols[:, GC, :], in_=pg[:, 0:NCH]), [bpg], [bcols])
    pg2, bpg2 = slot()
    kb.op("pe", lambda e: e.matmul(pg2[:, 0:NCH], sel127, cols[:, GC, :], start=True, stop=True), [bct, bcols], [bpg2])
    kb.op("dve", lambda e: e.tensor_copy(out=cols[:, TMP, :], in_=pg2[:, 0:NCH]), [bpg2], [bcols])
    kb.op("act", lambda e: e.activation(out=cols[:, EGL, :], in_=cols[:, TMP, :], func=AF.Exp), [bcols], [bcols])
    kb.op("dve", lambda e: e.tensor_tensor(out=cols[:, TMP2, :], in0=cols[:, TMP, :], in1=cols[:, GC, :], op=ALU.subtract),
          [bcols], [bcols])
    kb.op("act", lambda e: e.activation(out=cols[:, KD, :], in_=cols[:, TMP2, :], func=AF.Exp), [bcols], [bcols])
    kb.op("act", lambda e: e.activation(out=cols[:, EGC, :], in_=cols[:, GC, :], func=AF.Exp), [bcols], [bcols])
    kb.op("dve", lambda e: e.tensor_tensor(out=cols[:, BG, :], in0=cols[:, EGC, :], in1=cols[:, BETA, :], op=ALU.mult),
          [bcols], [bcols])
    kb.op("dve", lambda e: e.tensor_scalar(out=cols[:, NB_, :], in0=cols[:, BETA, :], scalar1=-1.0, scalar2=None,
                                           op0=ALU.mult), [bcols], [bcols])

    if stop == "cols":
        kb.dma("sp", ya[0:128, 0:NCH], cols[:, GC, :], [bcols], [bya])
        kb.finish([bya])
        return nc
    QT, bQT = kb.sb([128, S], F32, "QT")
    KT, bKT = kb.sb([128, S], F32, "KT")
    Vtok, bVtok = kb.sb([128, NCH, 128], F32, "Vtok")
    Ktok, bKtok = kb.sb([128, NCH, 128], F32, "Ktok")
    xbs = [kb.sb([128, 3, 515], F32, "xb") for _ in range(2)]
    u, bu = kb.sb([128, 3, 512], F32, "u")
    sqt, bsq = kb.sb([128, 512], F32, "sq")
    rs, brs = kb.sb([128, 512], F32, "rs")
    nblk = (nch * 128 + 511) // 512
    for blk in range(nblk):
        xb, bxb = xbs[blk % 2]
        kb.dma("sp", xb[:], xin[:, :, blk * 512:blk * 512 + 515], [dr], [bxb])
        for a in range(3):
            kb.op("dve", lambda e, a=a: e.tensor_scalar(out=u[:, a, :], in0=xb[:, a, 0:512],
                                                        scalar1=cwt[:, a * 4:a * 4 + 1], scalar2=None, op0=ALU.mult),
                  [bxb, bcw], [bu])
            for tap in range(1, 4):
                kb.op("dve", lambda e, a=a, tap=tap: e.scalar_tensor_tensor(
                    out=u[:, a, :], in0=xb[:, a, tap:tap + 512], scalar=cwt[:, a * 4 + tap:a * 4 + tap + 1],
                    in1=u[:, a, :], op0=ALU.mult, op1=ALU.add), [bxb, bcw, bu], [bu])
        kb.op("act", lambda e: e.activation(out=u[:].rearrange("p a n -> p (a n)"),
                                            in_=u[:].rearrange("p a n -> p (a n)"), func=AF.Silu), [bu], [bu])
        cs = slice(blk * 512, (blk + 1) * 512)
        for a, (dst, bdst, scl, epi) in enumerate([(QT, bQT, 128.0, 1), (KT, bKT, 1.0, 0)]):
            kb.op("dve", lambda e, a=a: e.tensor_tensor(out=sqt[:], in0=u[:, a, :], in1=u[:, a, :], op=ALU.mult),
                  [bu], [bsq])
            pw, bpw = wide[a]
            kb.op("pe", lambda e: e.matmul(pw, ones, sqt[:], start=True, stop=True), [bct, bsq], [bpw])
            kb.op("act", lambda e, scl=scl, epi=epi: e.activation(out=rs[:], in_=pw, func=AF.Sqrt, scale=scl,
                                                                  bias=epst[:, epi:epi + 1]), [bpw, beps], [brs])
            kb.op("dve", lambda e: e.reciprocal(out=rs[:], in_=rs[:]), [brs], [brs])
            kb.op("dve", lambda e, a=a, dst=dst: e.tensor_tensor(out=dst[:, cs], in0=u[:, a, :], in1=rs[:], op=ALU.mult),
                  [bu, brs], [bdst])
        for q4 in range(4):
            ch = blk * 4 + q4
            if ch >= nch:
                break
            pk, bpk = slot()
            kb.op("pe", lambda e, ch=ch: e.transpose(out=pk, in_=KT[:, ch * 128:(ch + 1) * 128], identity=ident),
                  [bKT, bct], [bpk])
            kb.op("act", lambda e, ch=ch: e.copy(out=Ktok[:, ch, :], in_=pk), [bpk], [bKtok])
            pv, bpv = slot()
            kb.op("pe", lambda e, q4=q4: e.transpose(out=pv, in_=u[:, 2, q4 * 128:(q4 + 1) * 128], identity=ident),
                  [bu, bct], [bpv])
            kb.op("act", lambda e, ch=ch: e.copy(out=Vtok[:, ch, :], in_=pv), [bpv], [bVtok])

    if stop == "prep":
        kb.dma("sp", ya[0:128, :], Ktok[:, 0, :], [bKtok], [bya])
        kb.dma("sp", ya[128:256, :], Vtok[:, 0, :], [bVtok], [bya])
        kb.dma("sp", ya[256:384, :], QT[:, 0:128], [bQT], [bya])
        kb.finish([bya])
        return nc
    RING = 4
    ring = [dict(wdT=kb.sb([128, 128], F32, "wdT"), uval=kb.sb([128, 128], F32, "uval"),
                 attnT=kb.sb([128, 128], F32, "attnT"), kdec=kb.sb([128, 128], F32, "kdec")) for _ in range(RING)]
    tmps = {}

    def tmp(name, k=2):
        if name not in tmps:
            tmps[name] = [kb.sb([128, 128], F32, name) for _ in range(k)]
            tmps[name + "_i"] = 0
        i = tmps[name + "_i"]
        tmps[name + "_i"] = i + 1
        return tmps[name][i % k]

    def pre(n):
        par = "_%d" % (n % 2)
        R = ring[n % RING]
        kt = KT[:, n * 128:(n + 1) * 128]
        qt = QT[:, n * 128:(n + 1) * 128]
        dg, bdg = tmp("diag" + par)
        kb.op("dve", lambda e: e.tensor_scalar(out=dg[:], in0=ident, scalar1=cols[:, GC, n:n + 1], scalar2=None,
                                               op0=ALU.mult), [bct, bcols], [bdg])
        pD, bpD = slot()
        kb.op("pe", lambda e: e.matmul(pD, dg[:], ones, start=True, stop=False), [bdg, bct], [bpD])
        kb.op("pe", lambda e: e.matmul(pD, negones, dg[:], start=False, stop=True), [bdg, bct], [bpD])
        Dl, bDl = tmp("Dl" + par)
        E, bE = tmp("E" + par)
        kb.op("dve", lambda e: e.tensor_tensor(out=Dl[:], in0=pD, in1=penL, op=ALU.min), [bpD, bct], [bDl])
        kb.op("act", lambda e: e.activation(out=E[:], in_=Dl[:], func=AF.Exp), [bDl], [bE])
        yield
        Es, bEs = tmp("Es" + par)
        kb.op(POOL_ENG, lambda e: e.tensor_tensor(out=Es[:], in0=E[:], in1=SM, op=ALU.mult), [bE, bct], [bEs])
        pA, bpA = slot()
        kb.op("pe", lambda e: e.matmul(pA, kt, kt, start=True, stop=True), [bKT], [bpA])
        Nm, bN = tmp("N" + par, 3)
        kb.op("dve", lambda e: e.scalar_tensor_tensor(out=Nm[:], in0=pA, scalar=cols[:, NB_, n:n + 1], in1=Es[:],
                                                      op0=ALU.mult, op1=ALU.mult), [bpA, bcols, bEs], [bN])
        yield
        pM, bpM = slot()
        kb.op("pe", lambda e: e.transpose(out=pM, in_=Nm[:], identity=ident), [bN, bct], [bpM])
        Mm, bM = tmp("M" + par, 3)
        kb.op("act", lambda e: e.copy(out=Mm[:], in_=pM), [bpM], [bM])
        P, bP = tmp("P" + par, 3)
        kb.op("dve", lambda e: e.tensor_tensor(out=P[:], in0=Mm[:], in1=ident, op=ALU.add), [bM, bct], [bP])
        yield
        pQK, bpQK = slot()
        kb.op("pe", lambda e: e.matmul(pQK, qt, kt, start=True, stop=True), [bQT, bKT], [bpQK])
        at, bat = tmp("attn" + par)
        kb.op("dve", lambda e: e.tensor_tensor(out=at[:], in0=pQK, in1=E[:], op=ALU.mult), [bpQK, bE], [bat])
        pAT, bpAT = slot()
        kb.op("pe", lambda e: e.transpose(out=pAT, in_=at[:], identity=ident), [bat, bct], [bpAT])
        aT, baT = R["attnT"]
        kb.op("act", lambda e: e.copy(out=aT[:], in_=pAT), [bpAT], [baT])
        yield
        for lev in range(1, 7):
            pN2, bpN2 = slot()
            kb.op("pe", lambda e: e.matmul(pN2, Mm[:], Nm[:], start=True, stop=True), [bM, bN], [bpN2])
            N2, bN2 = tmp("N" + par, 3)
            kb.op("act", lambda e: e.copy(out=N2[:], in_=pN2), [bpN2], [bN2])
            if lev < 6:
                pM2, bpM2 = slot()
                kb.op("pe", lambda e: e.matmul(pM2, Nm[:], Mm[:], start=True, stop=True), [bM, bN], [bpM2])
                M2, bM2 = tmp("M" + par, 3)
                kb.op("act", lambda e: e.copy(out=M2[:], in_=pM2), [bpM2], [bM2])
            yield
            pP, bpP = slot()
            kb.op("pe", lambda e: e.matmul(pP, N2[:], P[:], start=True, stop=True), [bN2, bP], [bpP])
            P2, bP2 = tmp("P" + par, 3)
            kb.op("dve", lambda e: e.tensor_tensor(out=P2[:], in0=pP, in1=P[:], op=ALU.add), [bpP, bP], [bP2])
            P, bP = P2, bP2
            yield
            Nm, bN = N2, bN2
            if lev < 6:
                Mm, bM = M2, bM2
        yield
        kbg, bkbg = tmp("kbg" + par)
        kb.op(POOL_ENG, lambda e: e.tensor_scalar(out=kbg[:], in0=Ktok[:, n, :], scalar1=cols[:, BG, n:n + 1],
                                                scalar2=None, op0=ALU.mult), [bKtok, bcols], [bkbg])
        vb, bvb = tmp("vb" + par)
        kb.op(POOL_ENG, lambda e: e.tensor_scalar(out=vb[:], in0=Vtok[:, n, :], scalar1=cols[:, BETA, n:n + 1],
                                                scalar2=None, op0=ALU.mult), [bVtok, bcols], [bvb])
        kd, bkd = R["kdec"]
        kb.op(POOL_ENG, lambda e: e.tensor_scalar(out=kd[:], in0=Ktok[:, n, :], scalar1=cols[:, KD, n:n + 1],
                                                scalar2=None, op0=ALU.mult), [bKtok, bcols], [bkd])
        pW, bpW = slot()
        kb.op("pe", lambda e: e.matmul(pW, kbg[:], P[:], start=True, stop=True), [bkbg, bP], [bpW])
        wd, bwd = R["wdT"]
        kb.op("act", lambda e: e.copy(out=wd[:], in_=pW), [bpW], [bwd])
        pU, bpU = slot()
        kb.op("pe", lambda e: e.matmul(pU, P[:], vb[:], start=True, stop=True), [bP, bvb], [bpU])
        uv, buv = R["uval"]
        kb.op("act", lambda e: e.copy(out=uv[:], in_=pU), [bpU], [buv])

    states = [kb.sb([128, 128], F32, "state") for _ in range(2)]
    kb.op("dve", lambda e: e.memset(states[0][0][:], 0.0), [], [states[0][1]])
    zts = [kb.sb([128, 128], F32, "zt") for _ in range(2)]
    st, bst = kb.sb([128, 8], F32, "st")
    junk, bjunk = kb.sb([128, 128], F32, "junk")

    def scan(n):
        R = ring[n % RING]
        wd, bwd = R["wdT"]
        uv, buv = R["uval"]
        aT, baT = R["attnT"]
        kd, bkd = R["kdec"]
        S0, bS0 = states[n % 2]
        S1, bS1 = states[(n + 1) % 2]
        zt, bzt = zts[n % 2]
        kb.dma("sp", zt[:], zin[:, n, :], [dr], [bzt])
        ppv, bppv = slot()
        kb.op("pe", lambda e: e.matmul(ppv, wd[:], S0[:], start=True, stop=True), [bwd, bS0], [bppv])
        po1, bpo1 = wide[0][0][:, 0:128], wide[0][1]
        kb.op("pe", lambda e: e.matmul(po1, QT[:, n * 128:(n + 1) * 128], S0[:], start=True, stop=True),
              [bQT, bS0], [bpo1])
        vn, bvn = tmp("vnew")
        kb.op("dve", lambda e: e.tensor_tensor(out=vn[:], in0=uv[:], in1=ppv, op=ALU.subtract), [buv, bppv], [bvn])
        yield
        psu, bpsu = slot()
        kb.op("pe", lambda e: e.matmul(psu, kd[:], vn[:], start=True, stop=True), [bkd, bvn], [bpsu])
        po2, bpo2 = slot()
        kb.op("pe", lambda e: e.matmul(po2, aT[:], vn[:], start=True, stop=True), [baT, bvn], [bpo2])
        kb.op("dve", lambda e: e.scalar_tensor_tensor(out=S1[:], in0=S0[:], scalar=cols[:, EGL, n:n + 1], in1=psu,
                                                      op0=ALU.mult, op1=ALU.add), [bS0, bcols, bpsu], [bS1])
        o2, bo2 = tmp("o2")
        kb.op("act", lambda e: e.copy(out=o2[:], in_=po2), [bpo2], [bo2])
        o, bo = tmp("o")
        kb.op("dve", lambda e: e.scalar_tensor_tensor(out=o[:], in0=po1, scalar=cols[:, EGC, n:n + 1], in1=o2[:],
                                                      op0=ALU.mult, op1=ALU.add), [bpo1, bcols, bo2], [bo])
        yield
        kb.op("act", lambda e: e.activation(out=junk[:], in_=o[:], func=AF.Square, accum_out=st[:, 0:1]),
              [bo], [bjunk, bst])
        kb.op("act", lambda e: e.activation(out=st[:, 1:2], in_=st[:, 0:1], func=AF.Ln, scale=1.0 / 128,
                                            bias=epst[:, 0:1]), [bst, beps], [bst])
        kb.op("act", lambda e: e.activation(out=st[:, 2:3], in_=st[:, 1:2], func=AF.Exp, scale=-0.5), [bst], [bst])
        sg, bsg = tmp("sg")
        kb.op("act", lambda e: e.activation(out=sg[:], in_=zt[:], func=AF.Exp, scale=-1.0), [bzt], [bsg])
        kb.op(POOL_ENG, lambda e: e.tensor_scalar(out=sg[:], in0=sg[:], scalar1=1.0, scalar2=None, op0=ALU.add),
              [bsg], [bsg])
        kb.op("dve", lambda e: e.reciprocal(out=sg[:], in_=sg[:]), [bsg], [bsg])
        kb.op(POOL_ENG, lambda e: e.tensor_tensor(out=sg[:], in0=sg[:], in1=zt[:], op=ALU.mult), [bsg, bzt], [bsg])
        yield
        t1, bt1 = tmp("t1")
        kb.op("dve", lambda e: e.scalar_tensor_tensor(out=t1[:], in0=o[:], scalar=st[:, 2:3], in1=gnb[:],
                                                      op0=ALU.mult, op1=ALU.mult), [bo, bst, bgnb], [bt1])
        yt_, byt_ = tmp("yout")
        kb.op("dve", lambda e: e.tensor_tensor(out=yt_[:], in0=t1[:], in1=sg[:], op=ALU.mult), [bt1, bsg], [byt_])
        kb.dma("sp", ya[n * 128:(n + 1) * 128, :], yt_[:], [byt_], [bya])

    def drive(gens):
        gens = list(gens)
        while gens:
            for g in list(gens):
                try:
                    next(g)
                except StopIteration:
                    gens.remove(g)

    def chain(*gs):
        for g in gs:
            yield from g

    if stop == "alloc":
        kb.dma("sp", ya[0:128, :], states[0][0][:], [states[0][1]], [bya])
        kb.finish([bya])
        return nc
    drive([pre(n) for n in range(min(2, nch))])
    for n in range(0, nch, 2):
        gens = [pre(m) for m in (n + 2, n + 3) if m < nch]
        gens.append(chain(*[scan(m) for m in (n, n + 1) if m < nch]))
        drive(gens)
    kb.finish([bya])
    return nc


def l2_consts():
    i = np.arange(128)
    ident = np.eye(128, dtype=np.float32)
    LT = (i[:, None] <= i[None, :]).astype(np.float32)
    ones = np.ones((128, 128), np.float32)
    penL = np.where(i[:, None] >= i[None, :], 0.0, -1e30).astype(np.float32)
    SM = (i[:, None] > i[None, :]).astype(np.float32)
    sel = np.zeros((128, 128), np.float32)
    sel[127, :] = 1.0
    return np.ascontiguousarray(np.stack([ident, LT, ones, -ones, penL, SM, sel], axis=1))


def run_l2(of, conv_w_l, a_log_l, dt_bias_l, gnw_l, nch=NCH, stop=None):
    nc = _get("l2_%d_%s" % (nch, stop), lambda: build_l2(nch, stop))
    cst = l2_consts()
    maps = []
    for cid in range(NCORES):
        b, g = cid // 4, cid % 4
        xs = []
        cws = []
        for a in range(3):
            cols_ = slice(a * 512 + g * 128, a * 512 + (g + 1) * 128)
            xa = np.zeros((128, S + 3), np.float32)
            xa[:, 3:] = of[b, :, cols_].T
            xs.append(xa)
            cws.append(conv_w_l[:, cols_].T)
        xin = np.ascontiguousarray(np.stack(xs, axis=1))
        cw = np.ascontiguousarray(np.concatenate(cws, axis=1)).astype(np.float32)
        z = of[b, :, 1536 + g * 128:1536 + (g + 1) * 128].reshape(NCH, 128, 128).transpose(1, 0, 2)
        bc = of[b, :, 2048 + g].reshape(NCH, 128).T
        ac = of[b, :, 2052 + g].reshape(NCH, 128).T
        maps.append({"xin": xin, "cw": cw, "zin": np.ascontiguousarray(z), "bcol": np.ascontiguousarray(bc),
                     "acol": np.ascontiguousarray(ac),
                     "sc3": np.array([[a_log_l[g], dt_bias_l[g]]], np.float32),
                     "gnw": gnw_l.reshape(1, 128).astype(np.float32), "cst": cst})
    res = _run(nc, maps)
    ya = np.zeros((NB, S, 512), np.float32)
    for cid in range(NCORES):
        b, g = cid // 4, cid % 4
        ya[b, :, g * 128:(g + 1) * 128] = res.results[cid]["ya"]
    return ya


def kernel(x, c, rel_bias, mod_w, mod_b, norm_mix_w, norm_ffn_w, w_in, conv_w, a_log, dt_bias,
           gdn_norm_w, q_norm_w, k_norm_w, w_out, router_w, router_b, w1, b1, w2, b2):
    f = lambda a: np.ascontiguousarray(np.asarray(a), dtype=np.float32)
    x = f(x)
    c = f(c)
    rel_bias = f(rel_bias)
    for l in range(2):
        of, ob = run_l1(x, c, f(mod_w[l]), f(mod_b[l]), f(norm_mix_w[l]), f(w_in[l]), f(q_norm_w[l]), f(k_norm_w[l]))
        ya = run_l2(of, f(conv_w[l]), f(a_log[l]), f(dt_bias[l]), f(gdn_norm_w[l]))
        yb = run_l3(ob, of, rel_bias)
        ycat = np.ascontiguousarray(np.concatenate([ya, yb], axis=-1))
        w1r, b1r, w2r, b2r = prep_l4_weights(f(w1[l]), f(b1[l]), f(w2[l]), f(b2[l]))
        x = run_l4(x, ycat, c, f(mod_w[l]), f(mod_b[l]), f(norm_ffn_w[l]), f(w_out[l]), f(router_w[l]),
                   f(router_b[l]), w1r, b1r, w2r, b2r)
    return x


CAP = 1024
ESTRIDE = CAP + 128
TRASH = NE * ESTRIDE


def _dma_ind(kb, out, out_off, in_, in_off, reads, writes):
    dst = writes[0]
    if dst.dsem is None:
        kb.nsem += 1
        key = "d%d_%s" % (kb.nsem, dst.name)
        dst.dsem = key
        kb.sems[key] = kb.nc.alloc_semaphore(key[:40])
    deps = kb._deps(reads, writes)
    kb._wait("pool", deps)
    ins = kb.nc.gpsimd.indirect_dma_start(out=out, out_offset=out_off, in_=in_, in_offset=in_off)
    dst.dcnt += 16
    ins.then_inc(kb.sems[dst.dsem], 16)
    for b in reads:
        if b.r.get(dst.dsem, 0) < dst.dcnt:
            b.r[dst.dsem] = dst.dcnt
    dst.w = (dst.dsem, dst.dcnt)
    dst.r = {}
    kb.n_ops += 1


def build_l4r(e_lo=0, e_hi=NE, first=True):
    n_exp = e_hi - e_lo
    nc = bass.Bass("TRN2", target_bir_lowering=False)
    x = _din(nc, "x", [TPC, D])
    ycat = _din(nc, "ycat", [TPC, D])
    cT = _din(nc, "cT", [128, 8])
    mod_w = _din(nc, "mod_w", [D, 6 * D])
    mod_b = _din(nc, "mod_b", [1, 6 * D])
    normw = _din(nc, "normw", [1, D])
    w_out = _din(nc, "w_out", [D, D])
    rw = _din(nc, "rw", [D, NE])
    rb = _din(nc, "rb", [1, NE])
    w1 = _din(nc, "w1", [n_exp, 8, 128, 8 * 256])
    b1 = _din(nc, "b1", [128, NE * 16])
    w2 = _din(nc, "w2", [n_exp, D, D])
    b2 = _din(nc, "b2", [NE, D])
    cst = _din(nc, "cst", [128, 3, 128])
    erow = _din(nc, "erow", [2, NE])
    xprev = None if first else _din(nc, "xprev", [TPC, D])
    xo = _dout(nc, "xo", [TPC, D])
    Xs = nc.dram_tensor("Xs", [TRASH + 128, D], BF16, kind="Internal").ap()
    Ys = nc.dram_tensor("Ys", [TRASH + 128, D], F32, kind="Internal").ap()
    kb = KB(nc)
    dr = Buf("dram_in")
    bxo = Buf("xo")
    bXs = Buf("Xs")
    bYs = Buf("Ys")

    PS = []
    for i in range(4):
        t = nc.alloc_psum_tensor("PS%d" % i, [128, 1024], F32)
        PS.append((t, Buf("ps%da" % i), Buf("ps%db" % i)))

    def bank(i):
        t, ba, bb = PS[i // 2]
        return (t[:, 0:512], ba) if i % 2 == 0 else (t[:, 512:1024], bb)

    R_x, _ = kb.sb([128, 8192], F32, "R_x")
    xrows = rview(R_x, 0, [128, 8, D], BF16)
    bxrows = Buf("xrows")
    xeT = rview(R_x, 16384, [128, 8, CAP], BF16)
    bxeT = Buf("xeT")
    modw_bufs = [(rview(R_x, i * 16384, [128, 8, 512], F32), Buf("modw%d" % i)) for i in range(2)]
    R_a, _ = kb.sb([128, 4096], F32, "R_a")
    actT = rview(R_a, 0, [128, 8, CAP], BF16)
    bactT = Buf("actT")
    g1, bg1 = rview(R_a, 0, [128, D], F32), Buf("g1")
    sh2, bsh2 = rview(R_a, 4096, [128, D], F32), Buf("sh2")
    A2, bA2 = rview(R_a, 8192, [128, D], F32), Buf("A2")
    nwb, bnwb = rview(R_a, 12288, [128, D], F32), Buf("nwb")
    R_w2, _ = kb.sb([128, 8192], F32, "R_w2")
    W2B = [(rview(R_w2, i * 16384, [128, 8, D], BF16), Buf("w2b%d" % i)) for i in range(2)]
    wo = rview(R_w2, 0, [128, 8, D], BF16)
    bwo = Buf("wo")
    scb_t = (rview(R_w2, 16384, [128, 8, 128], F32), Buf("scb"))
    modb_bufs = [(rview(R_w2, 20480 + i * 2048, [128, 512], F32), Buf("modb%d" % i)) for i in range(2)]
    R_W1, _ = kb.sb([128, 3072], F32, "R_W1")
    W1R = [(rview(R_W1, i * 4096, [128, 8, 256], BF16), Buf("w1r%d" % i)) for i in range(3)]
    g2, bg2 = kb.sb([128, D], F32, "g2")
    xt, bxt = kb.sb([128, D], F32, "xt")
    yt, byt = kb.sb([128, D], F32, "yt")
    tmpA, btmpA = kb.sb([128, D], F32, "tmpA")
    tmpB, btmpB = kb.sb([128, D], F32, "tmpB")
    R1, bR1 = kb.sb([128, D], F32, "R1")
    ybt = rview(R1, 0, [128, D], BF16)
    yT = rview(R1, 2048, [128, 8, 128], BF16)
    h32T = rview(R1, 0, [128, 8, 128], F32)
    R2, bR2 = kb.sb([128, D], F32, "R2")
    hb = rview(R2, 0, [128, D], BF16)
    junk = rview(R2, 2048, [128, D], BF16)
    yrows = [kb.sb([128, D], F32, "yrow") for _ in range(2)]
    b2bs = [kb.sb([128, D], F32, "b2bc") for _ in range(2)]
    grows = [kb.sb([128, D], F32, "grow") for _ in range(4)]

    ct, bct = kb.sb([128, 3, 128], F32, "cst")
    kb.dma("sp", ct[:], cst[:, :, :], [dr], [bct])
    idf, LTs, ones = ct[:, 0, :], ct[:, 1, :], ct[:, 2, :]
    bidf = bct
    idb, bidb = kb.sb([128, 128], BF16, "identb")
    kb.op("dve", lambda e: e.tensor_copy(out=idb[:], in_=idf), [bct], [bidb])
    er, ber = kb.sb([128, 2, NE], F32, "erow")
    kb.dma("sp", er[:, 0, :], erow[0:1, :].to_broadcast([128, NE]), [dr], [ber])
    kb.dma("sp", er[:, 1, :], erow[1:2, :].to_broadcast([128, NE]), [dr], [ber])
    epst, beps = kb.sb([128, 1], F32, "eps")
    kb.op("dve", lambda e: e.memset(epst[:], EPS), [], [beps])
    kb.dma("sp", nwb, normw[0:1, :].to_broadcast([128, D]), [dr], [bnwb])
    rbb, brbb = kb.sb([128, NE], F32, "rbb")
    kb.dma("sp", rbb[:], rb[0:1, :].to_broadcast([128, NE]), [dr], [brbb])
    rwt, brwt = kb.sb([128, 8, NE], F32, "rwt")
    kb.dma("sp", rwt[:], rw.rearrange("(kc p) e -> p kc e", p=128), [dr], [brwt])
    b1a, bb1a = kb.sb([128, NE * 16], F32, "b1a")
    kb.dma("sp", b1a[:], b1[:, :], [dr], [bb1a])
    b1av = b1a[:].rearrange("p (g two) -> p g two", two=2)
    kb.op("dve", lambda e: e.tensor_scalar(out=b1av[:, :, 0:1], in0=b1av[:, :, 0:1], scalar1=ALPHA, scalar2=None,
                                           op0=ALU.mult), [bb1a], [bb1a])
    kb.op("dve", lambda e: e.tensor_scalar(out=b1av[:, :, 1:2], in0=b1av[:, :, 1:2], scalar1=1.0, scalar2=None,
                                           op0=ALU.add), [bb1a], [bb1a])
    st, bst = kb.sb([128, 64], F32, "st")
    lg, blg = kb.sb([128, NE], F32, "lg")
    ohk, bohk = kb.sb([128, 4, NE], F32, "ohk")
    prod, bprod = kb.sb([128, 4, NE], F32, "prod")
    sel, bsel = kb.sb([128, NE], F32, "sel")
    pos, bpos = kb.sb([128, NE], F32, "pos")
    basebc, bbase = kb.sb([128, NE], F32, "basebc")
    kb.op("dve", lambda e: e.memset(basebc[:], 0.0), [], [bbase])
    gkall, bgk = kb.sb([128, NT, 4], F32, "gkall")
    dstf, bdstf = kb.sb([128, NT, 4], F32, "dstf")
    dsti, bdsti = kb.sb([128, NT, 4], U32, "dsti")

    kb.op("dve", lambda e: e.memset(tmpA[:], 0.0), [], [btmpA])
    zb16 = tmpA[:].bitcast(BF16)[:, 0:D]
    r0 = e_lo * ESTRIDE
    nz = (n_exp * ESTRIDE) // 128
    for i in range(nz):
        kb.dma("sp", Xs[r0 + i * 128:r0 + (i + 1) * 128, :], zb16, [btmpA], [bXs])
        kb.dma("sp", Ys[r0 + i * 128:r0 + (i + 1) * 128, :], tmpA[:], [btmpA], [bYs])
    kb.dma("sp", Ys[TRASH:TRASH + 128, :], tmpA[:], [btmpA], [bYs])
    kb.dma("sp", Xs[TRASH:TRASH + 128, :], zb16, [btmpA], [bXs])

    sc2_out = (A2, bA2)
    emit_mod(kb, cT, mod_w, mod_b, [2, 3, 4, 5], wbufs=modw_bufs, bbufs=modb_bufs,
             pss=[bank(0), bank(1)], scb_t=scb_t,
             outs={2: (g1, bg1), 3: (sh2, bsh2), 4: sc2_out, 5: (g2[:], bg2)})
    kb.op("dve", lambda e: e.scalar_tensor_tensor(out=A2, in0=A2, scalar=1.0, in1=nwb,
                                                  op0=ALU.add, op1=ALU.mult), [bA2, bnwb], [bA2])

    for kc in range(8):
        kb.dma("pool", wo[:, kc, :], w_out[kc * 128:(kc + 1) * 128, :], [dr], [bwo])
    for ti in range(NT):
        rows = slice(ti * 128, (ti + 1) * 128)
        kb.dma("sp", xt[:], x[rows, :], [dr], [bxt])
        kb.dma("sp", yt[:], ycat[rows, :], [dr], [byt])
        kb.op("act", lambda e: e.copy(out=ybt, in_=yt[:]), [byt], [bR1])
        pt, bpt = bank(0)
        ptb = pt.bitcast(BF16)
        for kc in range(8):
            kb.op("pe", lambda e, kc=kc: e.transpose(out=ptb[:, kc * 128:(kc + 1) * 128],
                                                     in_=ybt[:, kc * 128:(kc + 1) * 128], identity=idb[:]),
                  [bR1, bidb], [bpt])
        kb.op("act", lambda e: e.copy(out=yT.rearrange("p a b -> p (a b)"), in_=ptb), [bpt], [bR1])
        for half in range(2):
            pp, bpp = bank(2 + half)
            for kc in range(8):
                kb.op("pe", lambda e, kc=kc: e.matmul(pp, yT[:, kc, :], wo[:, kc, half * 512:(half + 1) * 512],
                                                      start=(kc == 0), stop=(kc == 7)), [bR1, bwo], [bpp])
            cs = slice(half * 512, (half + 1) * 512)
            kb.op("dve", lambda e: e.tensor_tensor(out=tmpA[:, cs], in0=pp, in1=g1[:, cs], op=ALU.mult),
                  [bpp, bg1], [btmpA])
        kb.op("dve", lambda e: e.tensor_tensor(out=xt[:], in0=tmpA[:], in1=xt[:], op=ALU.add), [btmpA, bxt], [bxt])
        kb.dma("sp", xo[rows, :], xt[:], [bxt], [bxo])
        kb.op("act", lambda e: e.activation(out=junk, in_=xt[:], func=AF.Square, accum_out=st[:, 0:1]),
              [bxt], [bR2, bst])
        kb.op("act", lambda e: e.activation(out=st[:, 1:2], in_=st[:, 0:1], func=AF.Sqrt, scale=1.0 / D,
                                            bias=epst[:]), [bst, beps], [bst])
        kb.op("dve", lambda e: e.reciprocal(out=st[:, 2:3], in_=st[:, 1:2]), [bst], [bst])
        kb.op("dve", lambda e: e.scalar_tensor_tensor(out=tmpA[:], in0=xt[:], scalar=st[:, 2:3], in1=A2,
                                                      op0=ALU.mult, op1=ALU.mult), [bxt, bst, bA2], [btmpA])
        kb.op("dve", lambda e: e.tensor_tensor(out=tmpB[:], in0=tmpA[:], in1=sh2, op=ALU.add),
              [btmpA, bsh2], [btmpB])
        kb.op("act", lambda e: e.copy(out=hb, in_=tmpB[:]), [btmpB], [bR2])
        for half in range(2):
            pq, bpq = bank(4 + half)
            for k4 in range(4):
                kc = half * 4 + k4
                kb.op("pe", lambda e, kc=kc, k4=k4: e.transpose(out=pq[:, k4 * 128:(k4 + 1) * 128],
                                                                in_=tmpB[:, kc * 128:(kc + 1) * 128],
                                                                identity=idf), [btmpB, bidf], [bpq])
            kb.op("dve", lambda e: e.tensor_copy(
                out=h32T[:, half * 4:(half + 1) * 4, :], in_=pq.rearrange("p (a b) -> p a b", a=4)),
                [bpq], [bR1])
        pr, bpr = bank(6)
        for kc in range(8):
            kb.op("pe", lambda e, kc=kc: e.matmul(pr[:, 0:NE], h32T[:, kc, :], rwt[:, kc, :],
                                                  start=(kc == 0), stop=(kc == 7)), [bR1, brwt], [bpr])
        kb.op("dve", lambda e: e.tensor_tensor(out=lg[:], in0=pr[:, 0:NE], in1=rbb[:], op=ALU.add),
              [bpr, brbb], [blg])
        kb.op("dve", lambda e: e.max(out=st[:, 8:16], in_=lg[:]), [blg], [bst])
        kb.op("dve", lambda e: e.tensor_scalar(out=st[:, 16:17], in0=st[:, 8:9], scalar1=-1.0, scalar2=None,
                                               op0=ALU.mult), [bst], [bst])
        kb.op("act", lambda e: e.activation(out=st[:, 20:24], in_=st[:, 8:12], func=AF.Exp, bias=st[:, 16:17],
                                            accum_out=st[:, 17:18]), [bst], [bst])
        kb.op("dve", lambda e: e.reciprocal(out=st[:, 18:19], in_=st[:, 17:18]), [bst], [bst])
        for k in range(4):
            kb.op("dve", lambda e, k=k: e.tensor_scalar(out=ohk[:, k, :], in0=lg[:], scalar1=st[:, 8 + k:9 + k],
                                                        scalar2=None, op0=ALU.is_equal), [blg, bst], [bohk])
        kb.op("dve", lambda e: e.tensor_scalar(out=sel[:], in0=lg[:], scalar1=st[:, 11:12], scalar2=None,
                                               op0=ALU.is_ge), [blg, bst], [bsel])
        ppf, bppf = bank(7)
        kb.op("pe", lambda e: e.matmul(ppf[:, 0:NE], LTs, sel[:], start=True, stop=True), [bct, bsel], [bppf])
        kb.op("dve", lambda e: e.tensor_tensor(out=pos[:], in0=ppf[:, 0:NE], in1=basebc[:], op=ALU.add),
              [bppf, bbase], [bpos])
        ppb, bppb = bank(1)
        kb.op("pe", lambda e: e.matmul(ppb[:, 0:NE], ones, sel[:], start=True, stop=True), [bct, bsel], [bppb])
        kb.op("dve", lambda e: e.tensor_tensor(out=basebc[:], in0=ppb[:, 0:NE], in1=basebc[:], op=ALU.add),
              [bppb, bbase], [bbase])
        kb.op("dve", lambda e: e.scalar_tensor_tensor(out=pos[:], in0=pos[:], scalar=float(CAP), in1=er[:, 0, :],
                                                      op0=ALU.min, op1=ALU.add), [bpos, ber], [bpos])
        kb.op("dve", lambda e: e.tensor_tensor(out=ohk[:], in0=ohk[:],
                                               in1=er[:, 1, :].unsqueeze(1).to_broadcast([128, 4, NE]),
                                               op=ALU.mult), [bohk, ber], [bohk])
        kb.op("dve", lambda e: e.tensor_tensor(out=prod[:], in0=ohk[:],
                                               in1=pos[:].unsqueeze(1).to_broadcast([128, 4, NE]),
                                               op=ALU.mult), [bohk, bpos], [bprod])
        kb.op("dve", lambda e: e.tensor_reduce(out=dstf[:, ti, :], in_=prod[:], axis=AX.X, op=ALU.add),
              [bprod], [bdstf])
        kb.op("dve", lambda e: e.tensor_reduce(out=st[:, 24:28], in_=ohk[:], axis=AX.X, op=ALU.add),
              [bohk], [bst])
        kb.op("dve", lambda e: e.tensor_scalar(out=st[:, 28:32], in0=st[:, 24:28], scalar1=-float(TRASH),
                                               scalar2=float(TRASH), op0=ALU.mult, op1=ALU.add), [bst], [bst])
        kb.op("dve", lambda e: e.tensor_tensor(out=dstf[:, ti, :], in0=dstf[:, ti, :], in1=st[:, 28:32], op=ALU.add),
              [bdstf, bst], [bdstf])
        kb.op("dve", lambda e: e.scalar_tensor_tensor(out=gkall[:, ti, :], in0=st[:, 20:24], scalar=st[:, 18:19],
                                                      in1=st[:, 24:28], op0=ALU.mult, op1=ALU.mult), [bst], [bgk])
        kb.op("dve", lambda e: e.tensor_copy(out=dsti[:, ti, :], in_=dstf[:, ti, :]), [bdstf], [bdsti])
        for k in range(4):
            _dma_ind(kb, Xs[:, :], bass.IndirectOffsetOnAxis(ap=dsti[:, ti, k:k + 1], axis=0), hb, None,
                     [bR2, bdsti], [bXs])

    merge_deps(bxrows, [b for _, b in modw_bufs])
    merge_deps(bxeT, [b for _, b in modw_bufs])
    merge_deps(bactT, [bg1, bsh2, bA2, bnwb])
    for _, b in W2B:
        merge_deps(b, [bwo, scb_t[1]] + [bb for _, bb in modb_bufs])
    tts = [(tmpA[:, 0:512], Buf("tt0")), (tmpA[:, 512:1024], Buf("tt1"))]
    lps = [(tmpB[:, 0:512], Buf("lp0")), (tmpB[:, 512:1024], Buf("lp1"))]
    t2s = [(xt[:, 0:512], Buf("t20")), (xt[:, 512:1024], Buf("t21"))]
    for (_, b), src_ in zip(tts + lps + t2s, [btmpA, btmpA, btmpB, btmpB, bxt, bxt]):
        merge_deps(b, [src_])
    C0 = float(LIMIT * ALPHA / (1.0 + np.exp(-LIMIT * ALPHA)))
    w1_issued = [0]

    def issue_w1(upto):
        while w1_issued[0] < min(upto, n_exp * 8):
            i = w1_issued[0]
            t, b = W1R[i % 3]
            kb.dma("pool", t.rearrange("p a b -> p (a b)"), w1[i // 8, i % 8, :, :], [dr], [b])
            w1_issued[0] += 1

    def issue_w2(ei):
        if ei < n_exp:
            t, b = W2B[ei % 2]
            src = w2[ei].rearrange("(fc p) d -> p fc d", p=128)
            for fc in range(8):
                kb.dma("pool", t[:, fc, :], src[:, fc, :], [dr], [b])

    nu = 0
    ny = 0
    ntp = 0
    issue_w2(0)
    for e in range(e_lo, e_hi):
        ei = e - e_lo
        issue_w2(ei + 1)
        b2b, bb2b = b2bs[ei % 2]
        kb.dma("sp", b2b[:], b2[e:e + 1, :].to_broadcast([128, D]), [dr], [bb2b])
        kb.dma("sp", xrows, Xs[e * ESTRIDE:e * ESTRIDE + CAP, :].rearrange("(i p) d -> p i d", p=128),
               [bXs], [bxrows])
        for i in range(8):
            pt, bpt = bank(6 + ntp % 2)
            ntp += 1
            ptb = pt.bitcast(BF16)
            for kc in range(8):
                kb.op("pe", lambda en, kc=kc, i=i: en.transpose(out=ptb[:, kc * 128:(kc + 1) * 128],
                                                                in_=xrows[:, i, kc * 128:(kc + 1) * 128],
                                                                identity=idb[:]), [bxrows, bidb], [bpt])
            kb.op("act", lambda en, i=i: en.copy(out=xeT[:, :, i * 128:(i + 1) * 128],
                                                 in_=ptb.rearrange("p (a b) -> p a b", a=8)), [bpt], [bxeT])
        for ft in range(8):
            gi = ei * 8 + ft
            issue_w1(gi + 2)
            wv, bwt = W1R[gi % 3]
            bcol = (e * 8 + ft) * 2
            for blk in range(CAP // 512):
                ts_ = slice(blk * 512, (blk + 1) * 512)
                pgl, bpgl = bank(0 + 2 * (nu % 2))
                pli, bpli = bank(1 + 2 * (nu % 2))
                for kc in range(8):
                    kb.op("pe", lambda en, kc=kc: en.matmul(pgl, wv[:, kc, 0:128], xeT[:, kc, ts_],
                                                            start=(kc == 0), stop=(kc == 7)), [bwt, bxeT], [bpgl])
                for kc in range(8):
                    kb.op("pe", lambda en, kc=kc: en.matmul(pli, wv[:, kc, 128:256], xeT[:, kc, ts_],
                                                            start=(kc == 0), stop=(kc == 7)), [bwt, bxeT], [bpli])
                tt, btt = tts[nu % 2]
                lp, blp = lps[nu % 2]
                t2, bt2 = t2s[nu % 2]
                kb.op("act", lambda en: en.activation(out=tt, in_=pgl, func=AF.Silu, scale=ALPHA,
                                                      bias=b1a[:, bcol:bcol + 1]), [bpgl, bb1a], [btt])
                kb.op("dve", lambda en: en.tensor_scalar(out=lp, in0=pli, scalar1=b1a[:, bcol + 1:bcol + 2],
                                                         scalar2=LIMIT + 1.0, op0=ALU.add, op1=ALU.min),
                      [bpli, bb1a], [blp])
                kb.op("dve", lambda en: en.tensor_scalar(out=t2, in0=tt, scalar1=C0, scalar2=1.0 / ALPHA,
                                                         op0=ALU.min, op1=ALU.mult), [btt], [bt2])
                kb.op("dve", lambda en: en.scalar_tensor_tensor(out=actT[:, ft, ts_], in0=lp, scalar=1.0 - LIMIT,
                                                                in1=t2, op0=ALU.max, op1=ALU.mult),
                      [blp, bt2], [bactT])
                nu += 1
        w2v, bw2 = W2B[ei % 2]
        for i in range(8):
            yr, byr = yrows[ny % 2]
            ny += 1
            for db in range(2):
                py, bpy = bank(4 + db)
                for fc in range(8):
                    kb.op("pe", lambda en, fc=fc, i=i, db=db: en.matmul(py, actT[:, fc, i * 128:(i + 1) * 128],
                                                                        w2v[:, fc, db * 512:(db + 1) * 512],
                                                                        start=(fc == 0), stop=(fc == 7)),
                          [bactT, bw2], [bpy])
                kb.op("dve", lambda en, db=db: en.tensor_tensor(out=yr[:, db * 512:(db + 1) * 512], in0=py,
                                                                in1=b2b[:, db * 512:(db + 1) * 512], op=ALU.add),
                      [bpy, bb2b], [byr])
            kb.dma("sp", Ys[e * ESTRIDE + i * 128:e * ESTRIDE + (i + 1) * 128, :], yr[:], [byr], [bYs])

    tf, btf = tts[0]
    for ti in range(NT):
        rows = slice(ti * 128, (ti + 1) * 128)
        if first:
            kb.dma("sp", yt[:], xo[rows, :], [bxo], [byt])
        else:
            kb.dma("sp", yt[:], xprev[rows, :], [dr], [byt])
        for k in range(4):
            gr, bgr = grows[k]
            _dma_ind(kb, gr[:], None, Ys[:, :], bass.IndirectOffsetOnAxis(ap=dsti[:, ti, k:k + 1], axis=0),
                     [bYs, bdsti], [bgr])
        g0, bg0 = grows[0]
        kb.op("dve", lambda en: en.tensor_scalar(out=g0[:], in0=g0[:], scalar1=gkall[:, ti, 0:1], scalar2=None,
                                                 op0=ALU.mult), [bg0, bgk], [bg0])
        for k in range(1, 4):
            gr, bgr = grows[k]
            kb.op("dve", lambda en, k=k, gr=gr: en.scalar_tensor_tensor(out=g0[:], in0=gr[:],
                                                                        scalar=gkall[:, ti, k:k + 1], in1=g0[:],
                                                                        op0=ALU.mult, op1=ALU.add),
                  [bgr, bgk, bg0], [bg0])
        kb.op("dve", lambda en: en.tensor_tensor(out=g0[:], in0=g0[:], in1=g2[:], op=ALU.mult), [bg0, bg2], [bg0])
        kb.op("dve", lambda en: en.tensor_tensor(out=yt[:], in0=g0[:], in1=yt[:], op=ALU.add), [bg0, byt], [byt])
        kb.dma("sp", xo[rows, :], yt[:], [byt], [bxo])
    kb.finish([bxo])
    return nc


def run_l4r(x, ycat, c, mod_w_l, mod_b_l, normw_l, w_out_l, rw_l, rb_l, w1r, b1r, w2_l, b2_l,
            splits=((0, 16), (16, 32))):
    i = np.arange(128)
    cst = np.ascontiguousarray(np.stack([np.eye(128, dtype=np.float32),
                                         (i[:, None] < i[None, :]).astype(np.float32),
                                         np.ones((128, 128), np.float32)], axis=1))
    xprev = None
    for (lo, hi) in splits:
        first = (lo == 0)
        nc = _get("l4r_%d_%d" % (lo, hi), lambda: build_l4r(lo, hi, first))
        w1s = np.ascontiguousarray(w1r[lo:hi])
        w2s = np.ascontiguousarray(w2_l[lo:hi])
        erow = np.stack([np.arange(NE) * ESTRIDE, ((np.arange(NE) >= lo) & (np.arange(NE) < hi))]).astype(np.float32)
        maps = []
        for cid in range(NCORES):
            m = {"x": core_tokens(x, cid), "ycat": core_tokens(ycat, cid), "cT": cT_of(c, cid // 4),
                 "mod_w": mod_w_l, "mod_b": mod_b_l.reshape(1, -1), "normw": normw_l.reshape(1, -1),
                 "w_out": w_out_l, "rw": rw_l, "rb": rb_l.reshape(1, -1), "w1": w1s, "b1": b1r, "w2": w2s,
                 "b2": b2_l, "cst": cst, "erow": erow}
            if not first:
                m["xprev"] = xprev[cid]
            maps.append(m)
        res = _run(nc, maps)
        xprev = [r["xo"] for r in res.results]
    return uncore_tokens(xprev, (D,))
```

```python
import numpy as np
import concourse.bass as bass
import concourse.mybir as mybir
from concourse.bass_utils import run_bass_kernel_spmd

F32 = mybir.dt.float32
BF16 = mybir.dt.bfloat16
U32 = mybir.dt.uint32
AF = mybir.ActivationFunctionType
ALU = mybir.AluOpType
AX = mybir.AxisListType

D = 1024
S = 8192
NB = 2
D_IN = 4176
EPS = 1e-6
NCORES = 8
TPC = 2048
NT = 16


class Buf:
    __slots__ = ("name", "w", "r", "dsem", "dcnt")

    def __init__(self, name):
        self.name = name
        self.w = None
        self.r = {}
        self.dsem = None
        self.dcnt = 0


class KB:
    def __init__(self, nc, self_sync=True):
        self.nc = nc
        self.eng = {"pe": nc.tensor, "dve": nc.vector, "act": nc.scalar,
                    "pool": nc.gpsimd, "sp": nc.sync}
        self.sems = {}
        self.cnt = {}
        self.known = {k: {} for k in self.eng}
        for k in self.eng:
            self.sems[k] = nc.alloc_semaphore("sem_" + k)
            self.cnt[k] = 0
        self.self_sync = self_sync
        self.nsem = 0
        self.n_ops = 0
        self.n_waits = 0
        self.nbuf = 0

    def sb(self, shape, dt=F32, name=None):
        self.nbuf += 1
        name = (name or "t") + "_%d" % self.nbuf
        return self.nc.alloc_sbuf_tensor(name, list(shape), dt), Buf(name)

    def ps(self, shape, dt=F32, name=None):
        self.nbuf += 1
        name = (name or "p") + "_%d" % self.nbuf
        return self.nc.alloc_psum_tensor(name, list(shape), dt), Buf(name)

    def _deps(self, reads, writes):
        d = {}

        def add(kv):
            if kv is None:
                return
            k, v = kv
            if d.get(k, 0) < v:
                d[k] = v
        for b in reads:
            add(b.w)
        for b in writes:
            add(b.w)
            for k, v in b.r.items():
                add((k, v))
        return d

    def _wait(self, en, deps):
        e = self.eng[en]
        kn = self.known[en]
        for k, v in deps.items():
            if k == en and (en == "pe" or not self.self_sync):
                continue
            if kn.get(k, 0) >= v:
                continue
            e.wait_ge(self.sems[k], v)
            kn[k] = v
            self.n_waits += 1

    def op(self, en, fn, reads=(), writes=()):
        deps = self._deps(reads, writes)
        self._wait(en, deps)
        ins = fn(self.eng[en])
        self.cnt[en] += 1
        v = self.cnt[en]
        ins.then_inc(self.sems[en], 1)
        for b in reads:
            if b.r.get(en, 0) < v:
                b.r[en] = v
        for b in writes:
            b.w = (en, v)
            b.r = {}
        self.n_ops += 1
        return ins

    def dma(self, en, out, in_, reads, writes, **kw):
        assert len(writes) == 1
        dst = writes[0]
        if dst.dsem is None:
            self.nsem += 1
            key = "d%d_%s" % (self.nsem, dst.name)
            dst.dsem = key
            self.sems[key] = self.nc.alloc_semaphore(key[:40])
        deps = self._deps(reads, writes)
        self._wait(en, deps)
        ins = self.eng[en].dma_start(out=out, in_=in_, **kw)
        dst.dcnt += 16
        ins.then_inc(self.sems[dst.dsem], 16)
        for b in reads:
            if b.r.get(dst.dsem, 0) < dst.dcnt:
                b.r[dst.dsem] = dst.dcnt
        dst.w = (dst.dsem, dst.dcnt)
        dst.r = {}
        self.n_ops += 1
        return ins

    def finish(self, bufs, en="sp"):
        d = {}
        for b in bufs:
            if b.w is not None:
                k, v = b.w
                d[k] = max(d.get(k, 0), v)
        self._wait(en, d)


def rview(t, off_bytes, shape, dt):
    esz = 2 if dt == BF16 else 4
    n = 1
    for s in shape[1:]:
        n *= s
    flat = t[:, :] if dt == F32 else t[:, :].bitcast(dt)
    e0 = off_bytes // esz
    v = flat[0:shape[0], e0:e0 + n]
    if len(shape) == 3:
        v = v.rearrange("p (a b) -> p a b", a=shape[1])
    return v


def merge_deps(dst, srcs):
    for s in srcs:
        for kv in ([s.w] if s.w else []) + list(s.r.items()):
            k, v = kv
            if dst.r.get(k, 0) < v:
                dst.r[k] = v


def _din(nc, name, shape, dt=F32):
    return nc.dram_tensor(name, list(shape), dt, kind="ExternalInput").ap()


def _dout(nc, name, shape, dt=F32):
    return nc.dram_tensor(name, list(shape), dt, kind="ExternalOutput").ap()


def emit_mod(kb, cT, mod_w, mod_b, groups, wbufs=None, bbufs=None, pss=None, scb_t=None, outs=None):
    nc = kb.nc
    dr = Buf("mod_dram")
    ct, bct = kb.sb([128, 8], F32, "ct")
    kb.dma("sp", ct[:], cT[:, :], [dr], [bct])
    sc, bsc = kb.sb([128, 8], F32, "sc")
    kb.op("act", lambda e: e.activation(out=sc[:], in_=ct[:], func=AF.Silu), [bct], [bsc])
    scb, bscb = scb_t if scb_t is not None else kb.sb([128, 8, 128], F32, "scb")
    kb.op("dve", lambda e: e.tensor_copy(out=scb[:], in_=sc[:, :].unsqueeze(2).to_broadcast([128, 8, 128])),
          [bsc], [bscb])
    mw = mod_w.rearrange("(kc p) n -> p kc n", p=128)
    if wbufs is None:
        wbufs = [kb.sb([128, 8, 512], F32, "modw") for _ in range(2)]
    if bbufs is None:
        bbufs = [kb.sb([128, 512], F32, "modb") for _ in range(2)]
    if pss is None:
        pss = [kb.ps([128, 512], F32, "modps") for _ in range(2)]
    res = {}
    it = 0
    for g in groups:
        mt, bmt = outs[g] if outs is not None else kb.sb([128, 1024], F32, "modrow")
        res[g] = (mt, bmt)
        for half in range(2):
            c0 = g * 1024 + half * 512
            wt, bw = wbufs[it % 2]
            bt, bb = bbufs[it % 2]
            pt, bp = pss[it % 2]
            kb.dma("sp", wt[:], mw[:, :, c0:c0 + 512], [dr], [bw])
            kb.dma("sp", bt[:], mod_b[0:1, c0:c0 + 512].to_broadcast([128, 512]), [dr], [bb])
            for kc in range(8):
                kb.op("pe", lambda e, kc=kc: e.matmul(pt[:], scb[:, kc, :], wt[:, kc, :],
                                                      start=(kc == 0), stop=(kc == 7)),
                      [bscb, bw], [bp])
            kb.op("dve", lambda e: e.tensor_tensor(out=mt[:, half * 512:(half + 1) * 512], in0=pt[:],
                                                   in1=bt[:], op=ALU.add), [bp, bb], [bmt])
            it += 1
    return res


NF1 = 2064
NB1 = 2112


def build_l1():
    nc = bass.Bass("TRN2", target_bir_lowering=False)
    x = _din(nc, "x", [TPC, D])
    cT = _din(nc, "cT", [128, 8])
    mod_w = _din(nc, "mod_w", [D, 6 * D])
    mod_b = _din(nc, "mod_b", [1, 6 * D])
    normw = _din(nc, "normw", [1, D])
    w_in = _din(nc, "w_in", [D, D_IN])
    qkw = _din(nc, "qkw", [1, 128])
    ident = _din(nc, "ident", [128, 128])
    of = _dout(nc, "of", [TPC, NF1])
    ob = _dout(nc, "ob", [TPC, NB1], BF16)
    kb = KB(nc)
    dr = Buf("dram_in")
    bof, bob = Buf("of"), Buf("ob")

    idb, bidb = kb.sb([128, 128], BF16, "identb")
    kb.dma("pool", idb[:], ident[:, :], [dr], [bidb])
    epst, beps = kb.sb([128, 1], F32, "eps")
    kb.op("dve", lambda e: e.memset(epst[:], EPS), [], [beps])
    nwb, bnwb = kb.sb([128, D], F32, "nwb")
    kb.dma("sp", nwb[:], normw[0:1, :].to_broadcast([128, D]), [dr], [bnwb])
    qkb, bqkb = kb.sb([128, 128], F32, "qkb")
    kb.dma("sp", qkb[:], qkw[0:1, :].to_broadcast([128, 128]), [dr], [bqkb])
    kb.op("dve", lambda e: e.tensor_scalar(out=qkb[:, 0:64], in0=qkb[:, 0:64], scalar1=0.125, scalar2=None,
                                           op0=ALU.mult), [bqkb], [bqkb])

    wb, bwb = kb.sb([128, 8, D_IN], BF16, "w_in")
    bwbs = [Buf("w_in_p%d" % pc) for pc in range(3)]
    for pc in range(3):
        for kc in range(8):
            c0 = pc * 1392
            kb.dma("pool", wb[:, kc, c0:c0 + 1392], w_in[kc * 128:(kc + 1) * 128, c0:c0 + 1392], [dr], [bwbs[pc]])

    mods = emit_mod(kb, cT, mod_w, mod_b, [0, 1])
    sh1, bsh1 = mods[0]
    sc1, bsc1 = mods[1]
    A1, bA1 = kb.sb([128, D], F32, "A1")
    kb.op("dve", lambda e: e.scalar_tensor_tensor(out=A1[:], in0=sc1[:], scalar=1.0, in1=nwb[:],
                                                  op0=ALU.add, op1=ALU.mult), [bsc1, bnwb], [bA1])

    xts = [kb.sb([128, D], F32, "xt") for _ in range(2)]
    junk, bjunk = kb.sb([128, D], BF16, "junk")
    tmp, btmp = kb.sb([128, D], F32, "tmp")
    hbs = [kb.sb([128, D], BF16, "hb") for _ in range(2)]
    pT, bpT = kb.ps([128, D], BF16, "pT")
    hTs = [kb.sb([128, 8, 128], BF16, "hT") for _ in range(2)]
    pps = [kb.ps([128, 512], F32, "pp") for _ in range(4)]
    projs = [kb.sb([128, D_IN], F32, "proj") for _ in range(2)]
    obts = [kb.sb([128, NB1], BF16, "obt") for _ in range(2)]
    sq, bsq = kb.sb([128, 1024], F32, "sq")
    st, bst = kb.sb([128, 24], F32, "stats")

    QO = 2056
    npp = [0]
    stA, bstA = kb.sb([128, 4], F32, "statsA")

    def stage_a(ti):
        xt, bxt = xts[ti % 2]
        hb, bhb = hbs[ti % 2]
        hT, bhT = hTs[ti % 2]
        kb.dma("sp", xt[:], x[ti * 128:(ti + 1) * 128, :], [dr], [bxt])
        kb.op("act", lambda e: e.activation(out=junk[:], in_=xt[:], func=AF.Square, accum_out=stA[:, 0:1]),
              [bxt], [bjunk, bstA])
        kb.op("act", lambda e: e.activation(out=stA[:, 1:2], in_=stA[:, 0:1], func=AF.Sqrt, scale=1.0 / D,
                                            bias=epst[:]), [bstA, beps], [bstA])
        kb.op("dve", lambda e: e.reciprocal(out=stA[:, 2:3], in_=stA[:, 1:2]), [bstA], [bstA])
        kb.op("dve", lambda e: e.scalar_tensor_tensor(out=tmp[:], in0=xt[:], scalar=stA[:, 2:3], in1=A1[:],
                                                      op0=ALU.mult, op1=ALU.mult), [bxt, bstA, bA1], [btmp])
        kb.op("dve", lambda e: e.tensor_tensor(out=hb[:], in0=tmp[:], in1=sh1[:], op=ALU.add),
              [btmp, bsh1], [bhb])
        for kc in range(8):
            kb.op("pe", lambda e, kc=kc: e.transpose(out=pT[:, kc * 128:(kc + 1) * 128],
                                                     in_=hb[:, kc * 128:(kc + 1) * 128], identity=idb[:]),
                  [bhb, bidb], [bpT])
        kb.op("act", lambda e: e.copy(out=hT[:].rearrange("p a b -> p (a b)"), in_=pT[:]), [bpT], [bhT])

    def stage_b(ti):
        hT, bhT = hTs[ti % 2]
        pj, bpj = projs[ti % 2]
        obt, bobt = obts[ti % 2]
        for cb in range(9):
            c0 = cb * 512
            n = min(512, D_IN - c0)
            pp, bpp = pps[npp[0] % 4]
            for kc in range(8):
                kb.op("pe", lambda e, kc=kc: e.matmul(pp[:, 0:n], hT[:, kc, :], wb[:, kc, c0:c0 + n],
                                                      start=(kc == 0), stop=(kc == 7)),
                      [bhT] + [bwbs[pc_] for pc_ in range(c0 // 1392, min(2, (c0 + n - 1) // 1392) + 1)], [bpp])
            if npp[0] % 2 == 0:
                kb.op("act", lambda e: e.copy(out=pj[:, c0:c0 + n], in_=pp[:, 0:n]), [bpp], [bpj])
            else:
                kb.op("dve", lambda e: e.tensor_copy(out=pj[:, c0:c0 + n], in_=pp[:, 0:n]), [bpp], [bpj])
            npp[0] += 1
        qk = pj[:, QO:QO + 1024]
        kb.op("dve", lambda e: e.tensor_tensor(out=sq[:], in0=qk, in1=qk, op=ALU.mult), [bpj], [bsq])
        kb.op("dve", lambda e: e.tensor_reduce(out=st[:, 4:20], in_=sq[:].rearrange("p (g d) -> p g d", g=16),
                                               axis=AX.X, op=ALU.add), [bsq], [bst])
        kb.op("act", lambda e: e.activation(out=st[:, 4:20], in_=st[:, 4:20], func=AF.Sqrt, scale=1.0 / 64,
                                            bias=epst[:]), [bst, beps], [bst])
        kb.op("dve", lambda e: e.reciprocal(out=st[:, 4:20], in_=st[:, 4:20]), [bst], [bst])
        kb.op("dve", lambda e: e.tensor_tensor(out=sq[:].rearrange("p (g d) -> p g d", g=16),
                                               in0=qk.rearrange("p (g d) -> p g d", g=16),
                                               in1=st[:, 4:20].unsqueeze(2).to_broadcast([128, 16, 64]),
                                               op=ALU.mult), [bpj, bst], [bsq])
        for half in range(2):
            kb.op("dve", lambda e, half=half: e.tensor_tensor(
                out=obt[:, half * 512:(half + 1) * 512].rearrange("p (g d) -> p g d", g=8),
                in0=sq[:, half * 512:(half + 1) * 512].rearrange("p (g d) -> p g d", g=8),
                in1=qkb[:, half * 64:(half + 1) * 64].unsqueeze(1).to_broadcast([128, 8, 64]),
                op=ALU.mult), [bsq, bqkb], [bobt])
        kb.op("act", lambda e: e.copy(out=obt[:, 1024:NB1], in_=pj[:, QO + 1024:QO + 1024 + 1088]), [bpj], [bobt])
        kb.op("dve", lambda e: e.tensor_scalar(out=pj[:, 4168:4176], in0=pj[:, 4168:4176],
                                               scalar1=float(8 ** -0.5 * 64 ** -0.5), scalar2=None,
                                               op0=ALU.mult), [bpj], [bpj])
        kb.dma("sp", of[ti * 128:(ti + 1) * 128, 0:2056], pj[:, 0:2056], [bpj], [bof])
        kb.dma("sp", of[ti * 128:(ti + 1) * 128, 2056:2064], pj[:, 4168:4176], [bpj], [bof])
        kb.dma("sp", ob[ti * 128:(ti + 1) * 128, :], obt[:], [bobt], [bob])

    stage_a(0)
    for ti in range(NT):
        if ti + 1 < NT:
            stage_a(ti + 1)
        stage_b(ti)
    kb.finish([bof, bob])
    return nc


def core_tokens(a, c):
    b, r = c // 4, c % 4
    t = a[b].reshape((64, 128) + a.shape[2:])[r::4]
    return np.ascontiguousarray(t.reshape((TPC,) + a.shape[2:]))


def uncore_tokens(parts, tail):
    out = np.empty((NB, 64, 128) + tuple(tail), parts[0].dtype)
    for c in range(NCORES):
        b, r = c // 4, c % 4
        out[b, r::4] = parts[c].reshape((16, 128) + tuple(tail))
    return out.reshape((NB, S) + tuple(tail))


_NC_CACHE = {}
_TRACE = False
_TIMES = []


def _run(nc, maps):
    if _TRACE:
        res = run_bass_kernel_spmd(nc, maps, core_ids=list(range(NCORES)), trace=True)
        _TIMES.append(res.exec_time_ns)
        print('exec_time_ns', res.exec_time_ns)
        return res
    return run_bass_kernel_spmd(nc, maps, core_ids=list(range(NCORES)))


def _get(name, fn):
    if name not in _NC_CACHE:
        _NC_CACHE[name] = fn()
    return _NC_CACHE[name]


def cT_of(c, b):
    return np.ascontiguousarray(c[b].reshape(8, 128).T)


def run_l1(x, c, mod_w_l, mod_b_l, normw_l, w_in_l, qw_l, kw_l):
    nc = _get("l1", build_l1)
    ident = np.eye(128, dtype=np.float32)
    qkw = np.concatenate([qw_l, kw_l]).reshape(1, 128).astype(np.float32)
    maps = []
    for cid in range(NCORES):
        maps.append({"x": core_tokens(x, cid), "cT": cT_of(c, cid // 4), "mod_w": mod_w_l,
                     "mod_b": mod_b_l.reshape(1, -1), "normw": normw_l.reshape(1, -1), "w_in": w_in_l,
                     "qkw": qkw, "ident": ident})
    res = _run(nc, maps)
    of = uncore_tokens([r["of"] for r in res.results], (NF1,))
    ob = uncore_tokens([r["ob"] for r in res.results], (NB1,))
    return of, ob


NE = 32
ALPHA = 1.702
LIMIT = 7.0


def build_l4(e_lo=0, e_hi=NE, first=True):
    n_exp = e_hi - e_lo
    do_final = True
    nc = bass.Bass("TRN2", target_bir_lowering=False)
    x = _din(nc, "x", [TPC, D])
    ycat = _din(nc, "ycat", [TPC, D])
    cT = _din(nc, "cT", [128, 8])
    mod_w = _din(nc, "mod_w", [D, 6 * D])
    mod_b = _din(nc, "mod_b", [1, 6 * D])
    normw = _din(nc, "normw", [1, D])
    w_out = _din(nc, "w_out", [D, D])
    rw = _din(nc, "rw", [D, NE])
    rb = _din(nc, "rb", [1, NE])
    w1 = _din(nc, "w1", [max(n_exp, 1), 8, 128, 8 * 256])
    b1 = _din(nc, "b1", [128, NE * 16])
    w2 = _din(nc, "w2", [max(n_exp, 1), 8, 128, 8 * 128])
    b2 = _din(nc, "b2", [NE, D])
    ident = _din(nc, "ident", [128, 128])
    xprev = None if first else _din(nc, "xprev", [TPC, D])
    xo = _dout(nc, "xo", [TPC, D])
    if first:
        hT_o = _dout(nc, "hT_o", [128, 8 * TPC], BF16)
        gT_o = _dout(nc, "gT_o", [NE, TPC])
        g2_o = _dout(nc, "g2_o", [128, D])
    else:
        hT_i = _din(nc, "hT_i", [128, 8 * TPC], BF16)
        gT_i = _din(nc, "gT_i", [NE, TPC])
        g2_i = _din(nc, "g2_i", [128, D])
    kb = KB(nc)
    dr = Buf("dram_in")
    bxo = Buf("xo")
    bho = Buf("handover")

    PS = []
    for i in range(4):
        t = nc.alloc_psum_tensor("PS%d" % i, [128, 1024], F32)
        PS.append((t, Buf("ps%da" % i), Buf("ps%db" % i)))

    def bank(i):
        t, ba, bb = PS[i // 2]
        return (t[:, 0:512], ba) if i % 2 == 0 else (t[:, 512:1024], bb)

    R_hT, bhT = kb.sb([128, 8192], F32, "R_hT")
    hT = rview(R_hT, 0, [128, 8, TPC], BF16)
    R_acc, bacc = kb.sb([128, 16384], F32, "R_acc")
    acc = rview(R_acc, 0, [128, 8, TPC], F32)
    wo = rview(R_acc, 0, [128, 8, D], BF16)
    bwo = Buf("wo")
    R_act, bactT = kb.sb([128, 8192], F32, "R_act")
    actT = rview(R_act, 0, [128, 8, TPC], BF16)
    modw_bufs = [(rview(R_act, i * 16384, [128, 8, 512], F32), Buf("modw%d" % i)) for i in range(2)]
    gateT, bgateT = kb.sb([NE, TPC], F32, "gateT")
    R_gsb, bgsb = kb.sb([128, TPC], F32, "gsb")
    gsb = R_gsb
    scb_t = (rview(R_gsb, 0, [128, 8, 128], F32), Buf("scb"))
    modb_bufs = [(rview(R_gsb, 4096 + i * 2048, [128, 512], F32), Buf("modb%d" % i)) for i in range(2)]
    R_W, _ = kb.sb([128, 4608], F32, "R_W")
    W1R = [(rview(R_W, i * 4096, [128, 8, 256], BF16), Buf("w1r%d" % i)) for i in range(3)]
    W2R = [(rview(R_W, 12288 + i * 2048, [128, 8, 128], BF16), Buf("w2r%d" % i)) for i in range(3)]
    g1, bg1 = rview(R_W, 0, [128, D], F32), Buf("g1")
    sh2, bsh2 = rview(R_W, 4096, [128, D], F32), Buf("sh2")
    A2, bA2 = rview(R_W, 8192, [128, D], F32), Buf("A2")
    nwb, bnwb = rview(R_W, 12288, [128, D], F32), Buf("nwb")
    g2, bg2 = kb.sb([128, D], F32, "g2")
    xt, bxt = kb.sb([128, D], F32, "xt")
    yt, byt = kb.sb([128, D], F32, "yt")
    tmpA, btmpA = kb.sb([128, D], F32, "tmpA")
    tmpB, btmpB = kb.sb([128, D], F32, "tmpB")
    R1, bR1 = kb.sb([128, D], F32, "R1")
    ybt = rview(R1, 0, [128, D], BF16)
    yT = rview(R1, 2048, [128, 8, 128], BF16)
    h32T = rview(R1, 0, [128, 8, 128], F32)
    R2, bR2 = kb.sb([128, D], F32, "R2")
    hb = rview(R2, 0, [128, D], BF16)
    junk = rview(R2, 2048, [128, D], BF16)

    idf, bidf = kb.sb([128, 128], F32, "identf")
    kb.dma("sp", idf[:], ident[:, :], [dr], [bidf])
    idb, bidb = kb.sb([128, 128], BF16, "identb")
    kb.op("dve", lambda e: e.tensor_copy(out=idb[:], in_=idf[:]), [bidf], [bidb])
    epst, beps = kb.sb([128, 1], F32, "eps")
    kb.op("dve", lambda e: e.memset(epst[:], EPS), [], [beps])
    kb.dma("sp", nwb, normw[0:1, :].to_broadcast([128, D]), [dr], [bnwb])
    rbb, brbb = kb.sb([128, NE], F32, "rbb")
    kb.dma("sp", rbb[:], rb[0:1, :].to_broadcast([128, NE]), [dr], [brbb])
    rwt, brwt = kb.sb([128, 8, NE], F32, "rwt")
    kb.dma("sp", rwt[:], rw.rearrange("(kc p) e -> p kc e", p=128), [dr], [brwt])
    b1a, bb1a = kb.sb([128, NE * 16], F32, "b1a")
    kb.dma("sp", b1a[:], b1[:, :], [dr], [bb1a])
    b1av = b1a[:].rearrange("p (g two) -> p g two", two=2)
    kb.op("dve", lambda e: e.tensor_scalar(out=b1av[:, :, 0:1], in0=b1av[:, :, 0:1], scalar1=ALPHA, scalar2=None,
                                           op0=ALU.mult), [bb1a], [bb1a])
    kb.op("dve", lambda e: e.tensor_scalar(out=b1av[:, :, 1:2], in0=b1av[:, :, 1:2], scalar1=1.0, scalar2=None,
                                           op0=ALU.add), [bb1a], [bb1a])
    b2t, bb2t = kb.sb([NE, D], F32, "b2t")
    kb.dma("sp", b2t[:], b2[:, :], [dr], [bb2t])
    selbs = [kb.sb([NE, 128], F32, "selb") for _ in range(2)]
    st, bst = kb.sb([128, 64], F32, "st")
    lg, blg = kb.sb([128, NE], F32, "lg")

    if first:
        sc2_out = (A2, bA2)
        mods = emit_mod(kb, cT, mod_w, mod_b, [2, 3, 4, 5], wbufs=modw_bufs, bbufs=modb_bufs,
                        pss=[bank(0), bank(1)], scb_t=scb_t,
                        outs={2: (g1, bg1), 3: (sh2, bsh2), 4: sc2_out, 5: (g2[:], bg2)})
        kb.op("dve", lambda e: e.scalar_tensor_tensor(out=A2, in0=A2, scalar=1.0, in1=nwb,
                                                      op0=ALU.add, op1=ALU.mult), [bA2, bnwb], [bA2])

        for kc in range(8):
            kb.dma("pool", wo[:, kc, :], w_out[kc * 128:(kc + 1) * 128, :], [dr], [bwo])
        for ti in range(NT):
            rows = slice(ti * 128, (ti + 1) * 128)
            kb.dma("sp", xt[:], x[rows, :], [dr], [bxt])
            kb.dma("sp", yt[:], ycat[rows, :], [dr], [byt])
            kb.op("act", lambda e: e.copy(out=ybt, in_=yt[:]), [byt], [bR1])
            pt, bpt = bank(0)
            ptb = pt.bitcast(BF16)
            for kc in range(8):
                kb.op("pe", lambda e, kc=kc: e.transpose(out=ptb[:, kc * 128:(kc + 1) * 128],
                                                         in_=ybt[:, kc * 128:(kc + 1) * 128], identity=idb[:]),
                      [bR1, bidb], [bpt])
            kb.op("act", lambda e: e.copy(out=yT.rearrange("p a b -> p (a b)"), in_=ptb), [bpt], [bR1])
            for half in range(2):
                pp, bpp = bank(2 + half)
                for kc in range(8):
                    kb.op("pe", lambda e, kc=kc: e.matmul(pp, yT[:, kc, :], wo[:, kc, half * 512:(half + 1) * 512],
                                                          start=(kc == 0), stop=(kc == 7)), [bR1, bwo], [bpp])
                cs = slice(half * 512, (half + 1) * 512)
                kb.op("dve", lambda e: e.tensor_tensor(out=tmpA[:, cs], in0=pp, in1=g1[:, cs], op=ALU.mult),
                      [bpp, bg1], [btmpA])
            kb.op("dve", lambda e: e.tensor_tensor(out=xt[:], in0=tmpA[:], in1=xt[:], op=ALU.add), [btmpA, bxt], [bxt])
            kb.dma("sp", xo[rows, :], xt[:], [bxt], [bxo])
            kb.op("act", lambda e: e.activation(out=junk, in_=xt[:], func=AF.Square, accum_out=st[:, 0:1]),
                  [bxt], [bR2, bst])
            kb.op("act", lambda e: e.activation(out=st[:, 1:2], in_=st[:, 0:1], func=AF.Sqrt, scale=1.0 / D,
                                                bias=epst[:]), [bst, beps], [bst])
            kb.op("dve", lambda e: e.reciprocal(out=st[:, 2:3], in_=st[:, 1:2]), [bst], [bst])
            kb.op("dve", lambda e: e.scalar_tensor_tensor(out=tmpA[:], in0=xt[:], scalar=st[:, 2:3], in1=A2,
                                                          op0=ALU.mult, op1=ALU.mult), [bxt, bst, bA2], [btmpA])
            kb.op("dve", lambda e: e.tensor_tensor(out=tmpB[:], in0=tmpA[:], in1=sh2, op=ALU.add),
                  [btmpA, bsh2], [btmpB])
            kb.op("act", lambda e: e.copy(out=hb, in_=tmpB[:]), [btmpB], [bR2])
            pt, bpt = bank(1)
            ptb = pt.bitcast(BF16)
            for kc in range(8):
                kb.op("pe", lambda e, kc=kc: e.transpose(out=ptb[:, kc * 128:(kc + 1) * 128],
                                                         in_=hb[:, kc * 128:(kc + 1) * 128], identity=idb[:]),
                      [bR2, bidb], [bpt])
            kb.op("act", lambda e: e.copy(out=hT[:, :, rows], in_=ptb.rearrange("p (a b) -> p a b", a=8)),
                  [bpt], [bhT])
            for half in range(2):
                pq, bpq = bank(4 + half)
                for k4 in range(4):
                    kc = half * 4 + k4
                    kb.op("pe", lambda e, kc=kc, k4=k4: e.transpose(out=pq[:, k4 * 128:(k4 + 1) * 128],
                                                                    in_=tmpB[:, kc * 128:(kc + 1) * 128],
                                                                    identity=idf[:]), [btmpB, bidf], [bpq])
                kb.op("dve", lambda e: e.tensor_copy(
                    out=h32T[:, half * 4:(half + 1) * 4, :], in_=pq.rearrange("p (a b) -> p a b", a=4)),
                    [bpq], [bR1])
            pr, bpr = bank(6)
            for kc in range(8):
                kb.op("pe", lambda e, kc=kc: e.matmul(pr[:, 0:NE], h32T[:, kc, :], rwt[:, kc, :],
                                                      start=(kc == 0), stop=(kc == 7)), [bR1, brwt], [bpr])
            kb.op("dve", lambda e: e.tensor_tensor(out=lg[:], in0=pr[:, 0:NE], in1=rbb[:], op=ALU.add),
                  [bpr, brbb], [blg])
            kb.op("dve", lambda e: e.max(out=st[:, 8:16], in_=lg[:]), [blg], [bst])
            kb.op("dve", lambda e: e.tensor_scalar(out=st[:, 16:17], in0=st[:, 8:9], scalar1=-1.0, scalar2=None,
                                                   op0=ALU.mult), [bst], [bst])
            kb.op("act", lambda e: e.activation(out=st[:, 20:24], in_=st[:, 8:12], func=AF.Exp, bias=st[:, 16:17],
                                                accum_out=st[:, 17:18]), [bst], [bst])
            kb.op("dve", lambda e: e.reciprocal(out=st[:, 18:19], in_=st[:, 17:18]), [bst], [bst])
            kb.op("act", lambda e: e.activation(out=st[:, 32:64], in_=lg[:], func=AF.Exp, bias=st[:, 16:17]),
                  [blg, bst], [bst])
            kb.op("dve", lambda e: e.tensor_scalar(out=lg[:], in0=lg[:], scalar1=st[:, 11:12], scalar2=None,
                                                   op0=ALU.is_ge), [blg, bst], [blg])
            kb.op("dve", lambda e: e.scalar_tensor_tensor(out=lg[:], in0=st[:, 32:64], scalar=st[:, 18:19], in1=lg[:],
                                                          op0=ALU.mult, op1=ALU.mult), [bst, blg], [blg])
            pg, bpg = bank(7)
            kb.op("pe", lambda e: e.transpose(out=pg[0:NE, 0:128], in_=lg[:], identity=idf[:]), [blg, bidf], [bpg])
            kb.op("act", lambda e: e.copy(out=gateT[:, rows], in_=pg[0:NE, 0:128]), [bpg], [bgateT])

        kb.dma("sp", hT_o[:, :], R_hT[:].bitcast(BF16), [bhT], [bho])
        kb.dma("sp", gT_o[:, :], gateT[:], [bgateT], [bho])
        kb.dma("sp", g2_o[:, :], g2[:], [bg2], [bho])
    else:
        kb.dma("sp", R_hT[:].bitcast(BF16), hT_i[:, :], [dr], [bhT])
        kb.dma("sp", gateT[:], gT_i[:, :], [dr], [bgateT])
        kb.dma("sp", g2[:], g2_i[:, :], [dr], [bg2])
    merge_deps(bacc, [bwo])
    merge_deps(bactT, [b for _, b in modw_bufs])
    merge_deps(bgsb, [scb_t[1]] + [b for _, b in modb_bufs])
    for _, b in W1R + W2R:
        merge_deps(b, [bg1, bsh2, bA2, bnwb])
    tts = [(tmpA[:, 0:512], Buf("tt0")), (tmpA[:, 512:1024], Buf("tt1"))]
    lps = [(tmpB[:, 0:512], Buf("lp0")), (tmpB[:, 512:1024], Buf("lp1"))]
    lgs = [(xt[:, 0:512], Buf("lg0")), (xt[:, 512:1024], Buf("lg1"))]
    for (_, b), src_ in zip(tts + lps + lgs, [btmpA, btmpA, btmpB, btmpB, bxt, bxt]):
        merge_deps(b, [src_])
    C0 = float(LIMIT * ALPHA / (1.0 + np.exp(-LIMIT * ALPHA)))

    w1_issued = [0]
    w2_issued = [0]

    def issue_w1(upto):
        while w1_issued[0] < min(upto, n_exp * 8):
            i = w1_issued[0]
            t, b = W1R[i % 3]
            kb.dma("pool", t.rearrange("p a b -> p (a b)"), w1[i // 8, i % 8, :, :], [dr], [b])
            w1_issued[0] += 1

    def issue_w2(upto):
        while w2_issued[0] < min(upto, n_exp * 8):
            i = w2_issued[0]
            t, b = W2R[i % 3]
            kb.dma("pool", t.rearrange("p a b -> p (a b)"), w2[i // 8, i % 8, :, :], [dr], [b])
            w2_issued[0] += 1

    nu = 0
    ny = 0
    for e in range(e_lo, e_hi):
        selb, bselb = selbs[e % 2]
        kb.op("dve", lambda en: en.tensor_copy(out=selb[:], in_=idf[0:NE, e:e + 1].to_broadcast([NE, 128])),
              [bidf], [bselb])
        for blk in range(4):
            pgb, bpgb = bank(6)
            kb.op("pe", lambda en, blk=blk: en.matmul(pgb, selb[:], gateT[:, blk * 512:(blk + 1) * 512],
                                                      start=True, stop=True), [bselb, bgateT], [bpgb])
            kb.op("act", lambda en, blk=blk: en.activation(out=gsb[:, blk * 512:(blk + 1) * 512], in_=pgb,
                                                           func=AF.Copy, scale=1.0 / ALPHA), [bpgb], [bgsb])
        for ft in range(8):
            gi = (e - e_lo) * 8 + ft
            issue_w1(gi + 2)
            wv, bwt = W1R[gi % 3]
            bcol = (e * 8 + ft) * 2
            for blk in range(4):
                ts_ = slice(blk * 512, (blk + 1) * 512)
                pgl, bpgl = bank(0 + 2 * (nu % 2))
                pli, bpli = bank(1 + 2 * (nu % 2))
                for kc in range(8):
                    kb.op("pe", lambda en, kc=kc: en.matmul(pgl, wv[:, kc, 0:128], hT[:, kc, ts_],
                                                            start=(kc == 0), stop=(kc == 7)), [bwt, bhT], [bpgl])
                for kc in range(8):
                    kb.op("pe", lambda en, kc=kc: en.matmul(pli, wv[:, kc, 128:256], hT[:, kc, ts_],
                                                            start=(kc == 0), stop=(kc == 7)), [bwt, bhT], [bpli])
                tt, btt = tts[nu % 2]
                lp, blp = lps[nu % 2]
                lgg, blgg = lgs[nu % 2]
                kb.op("act", lambda en: en.activation(out=tt, in_=pgl, func=AF.Silu, scale=ALPHA,
                                                      bias=b1a[:, bcol:bcol + 1]), [bpgl, bb1a], [btt])
                kb.op("dve", lambda en: en.tensor_scalar(out=lp, in0=pli, scalar1=b1a[:, bcol + 1:bcol + 2],
                                                         scalar2=LIMIT + 1.0, op0=ALU.add, op1=ALU.min),
                      [bpli, bb1a], [blp])
                kb.op("dve", lambda en: en.scalar_tensor_tensor(out=lgg, in0=lp, scalar=1.0 - LIMIT, in1=gsb[:, ts_],
                                                                op0=ALU.max, op1=ALU.mult), [blp, bgsb], [blgg])
                kb.op("dve", lambda en: en.scalar_tensor_tensor(out=actT[:, ft, ts_], in0=tt, scalar=C0, in1=lgg,
                                                                op0=ALU.min, op1=ALU.mult), [btt, blgg], [bactT])
                nu += 1
        for dt in range(8):
            gi = (e - e_lo) * 8 + dt
            issue_w2(gi + 2)
            wv, bwt = W2R[gi % 3]
            for blk in range(4):
                ts_ = slice(blk * 512, (blk + 1) * 512)
                py, bpy = bank(4 + (ny % 2))
                for fc in range(8):
                    kb.op("pe", lambda en, fc=fc: en.matmul(py, wv[:, fc, :], actT[:, fc, ts_],
                                                            start=(fc == 0), stop=(fc == 7)), [bwt, bactT], [bpy])
                if e == e_lo:
                    kb.op("dve", lambda en: en.tensor_copy(out=acc[:, dt, ts_], in_=py), [bpy], [bacc])
                else:
                    kb.op("dve", lambda en: en.tensor_tensor(out=acc[:, dt, ts_], in0=py, in1=acc[:, dt, ts_],
                                                             op=ALU.add), [bpy, bacc], [bacc])
                ny += 1

    tf, btf = tts[0]
    for ti in range(NT if do_final else 0):
        rows = slice(ti * 128, (ti + 1) * 128)
        if first:
            kb.dma("sp", yt[:], xo[rows, :], [bxo], [byt])
        else:
            kb.dma("sp", yt[:], xprev[rows, :], [dr], [byt])
        for half in range(2):
            po, bpo = bank(half)
            for d4 in range(4):
                dt = half * 4 + d4
                cs = slice(d4 * 128, (d4 + 1) * 128)
                if first:
                    kb.op("pe", lambda en, dt=dt, cs=cs: en.matmul(po[:, cs], gateT[:, rows],
                                                                  b2t[:, dt * 128:(dt + 1) * 128],
                                                                  start=True, stop=False), [bgateT, bb2t], [bpo])
                kb.op("pe", lambda en, dt=dt, cs=cs: en.matmul(po[:, cs], acc[:, dt, rows], idf[:],
                                                              start=(not first), stop=True), [bacc, bidf], [bpo])
            cs2 = slice(half * 512, (half + 1) * 512)
            kb.op("dve", lambda en: en.tensor_tensor(out=tf, in0=po, in1=g2[:, cs2], op=ALU.mult),
                  [bpo, bg2], [btf])
            kb.op("dve", lambda en: en.tensor_tensor(out=yt[:, cs2], in0=tf, in1=yt[:, cs2], op=ALU.add),
                  [btf, byt], [byt])
        kb.dma("sp", xo[rows, :], yt[:], [byt], [bxo])
    kb.finish([bxo, bho])
    return nc


def prep_l4_weights(w1_l, b1_l, w2_l, b2_l):
    w1r = w1_l.reshape(NE, 8, 128, 8, 128, 2)
    w1r = w1r.transpose(0, 3, 2, 1, 5, 4)
    w1r = np.ascontiguousarray(w1r).reshape(NE, 8, 128, 8 * 256)
    b1r = b1_l.reshape(NE, 8, 128, 2).transpose(2, 0, 1, 3)
    b1r = np.ascontiguousarray(b1r).reshape(128, NE * 16)
    w2r = w2_l.reshape(NE, 8, 128, 8, 128).transpose(0, 3, 2, 1, 4)
    w2r = np.ascontiguousarray(w2r).reshape(NE, 8, 128, 8 * 128)
    return w1r, b1r, w2r, np.ascontiguousarray(b2_l)


def run_l4(x, ycat, c, mod_w_l, mod_b_l, normw_l, w_out_l, rw_l, rb_l, w1r, b1r, w2r, b2_l, splits=((0, 16), (16, 32))):
    ident = np.eye(128, dtype=np.float32)
    xprev = None
    for (lo, hi) in splits:
        first = (lo == 0)
        nc = _get("l4_%d_%d" % (lo, hi), lambda: build_l4(lo, hi, first))
        w1s = np.ascontiguousarray(w1r[lo:hi])
        w2s = np.ascontiguousarray(w2r[lo:hi])
        maps = []
        for cid in range(NCORES):
            m = {"x": core_tokens(x, cid), "ycat": core_tokens(ycat, cid), "cT": cT_of(c, cid // 4),
                 "mod_w": mod_w_l, "mod_b": mod_b_l.reshape(1, -1), "normw": normw_l.reshape(1, -1),
                 "w_out": w_out_l, "rw": rw_l, "rb": rb_l.reshape(1, -1), "w1": w1s, "b1": b1r, "w2": w2s,
                 "b2": b2_l, "ident": ident}
            if not first:
                m["xprev"] = xprev[cid]
                m["hT_i"] = hand[cid]["hT_o"]
                m["gT_i"] = hand[cid]["gT_o"]
                m["g2_i"] = hand[cid]["g2_o"]
            maps.append(m)
        res = _run(nc, maps)
        xprev = [r["xo"] for r in res.results]
        if first:
            hand = res.results
    return uncore_tokens(xprev, (D,))


NIT = 14
KSEL = 256
NEGBIG = -30000.0
NREL = 1280


def build_l3(nj=16):
    nc = bass.Bass("TRN2", target_bir_lowering=False)
    qT = _din(nc, "qT", [128, 16, 4 * 128], BF16)
    kT = _din(nc, "kT", [128, 4 * S], BF16)
    v1 = _din(nc, "v1", [64, 128, 8 * 65], BF16)
    qiT = _din(nc, "qiT", [128, 16, 4 * 128], BF16)
    kiT = _din(nc, "kiT", [64, S], BF16)
    wi = _din(nc, "wi", [128, 128])
    pen = _din(nc, "pen", [128, 512])
    oh = _din(nc, "oh", [32, NREL])
    relb = _din(nc, "relb", [32, 8])
    negI4 = _din(nc, "negI4", [128, 512], BF16)
    ident = _din(nc, "ident", [128, 128])
    yb = _dout(nc, "yb", [TPC, 512])
    scr = nc.dram_tensor("scr", [8, NREL], BF16, kind="Internal").ap()
    kb = KB(nc)
    dr = Buf("dram_in")
    byb = Buf("yb")
    bscr = Buf("scr")

    PS = []
    for i in range(4):
        t = nc.alloc_psum_tensor("PS%d" % i, [128, 1024], F32)
        PS.append((t, Buf("ps%da" % i), Buf("ps%db" % i)))

    def bank(i):
        t, ba, bb = PS[i // 2]
        return (t[:, 0:512], ba) if i % 2 == 0 else (t[:, 512:1024], bb)

    kTt, bkT = kb.sb([128, 4 * S], BF16, "kT")
    for i in range(4):
        kb.dma("sp", kTt[:, i * S:(i + 1) * S], kT[:, i * S:(i + 1) * S], [dr], [bkT])
    kTv = kTt[:].rearrange("p (h s) -> p h s", h=4)
    kibs = [kb.sb([128, 512], BF16, "kib") for _ in range(3)]
    nkb = [0]
    wit, bwi = kb.sb([128, 128], F32, "wi")
    kb.dma("sp", wit[:], wi[:, :], [dr], [bwi])
    pent, bpen = kb.sb([128, 512], F32, "pen")
    kb.dma("sp", pent[:], pen[:, :], [dr], [bpen])
    n4, bn4 = kb.sb([128, 512], BF16, "negI4")
    kb.dma("sp", n4[:], negI4[:, :], [dr], [bn4])
    idf, bidf = kb.sb([128, 128], F32, "identf")
    kb.dma("sp", idf[:], ident[:, :], [dr], [bidf])
    idb, bidb = kb.sb([128, 128], BF16, "identb")
    kb.op("dve", lambda e: e.tensor_copy(out=idb[:], in_=idf[:]), [bidf], [bidb])
    zb, bzb = kb.sb([128, 260], BF16, "zeros")
    kb.op("dve", lambda e: e.memset(zb[:], 0.0), [], [bzb])

    scores = [kb.sb([128, S], F32, "score") for _ in range(2)]
    oht, boh = rview(scores[1][0], 0, [32, NREL], F32), Buf("oh")
    kb.dma("sp", oht, oh[:, :], [dr], [boh])
    rbt, brb = kb.sb([32, 8], F32, "relb")
    kb.dma("sp", rbt[:], relb[:, :], [dr], [brb])
    bv, bbv = rview(scores[1][0], 8192, [8, NREL], BF16), Buf("bvec")
    for i, (c0, n) in enumerate([(0, 512), (512, 512), (1024, 256)]):
        pb, bpb = bank(7)
        kb.op("pe", lambda e: e.matmul(pb[0:8, 0:n], rbt[:], oht[:, c0:c0 + n], start=True, stop=True),
              [brb, boh], [bpb])
        kb.op("dve", lambda e: e.tensor_copy(out=bv[:, c0:c0 + n], in_=pb[0:8, 0:n]), [bpb], [bbv])
    kb.dma("sp", scr[:, :], bv, [bbv], [bscr])
    merge_deps(scores[1][1], [boh, bbv])
    TU, bTU = kb.sb([128, 9, 8 * 128], BF16, "TU")
    for u in range(9):
        src = bass.AP(tensor=scr.tensor, offset=128 * u, ap=[[1, 128], [NREL, 8], [1, 128]])
        kb.dma("sp", TU[:, u, :].rearrange("p (h t) -> p h t", h=8), src, [bscr], [bTU])

    nots = [kb.sb([128, S], BF16, "notsel") for _ in range(2)]
    qts = [kb.sb([128, 512], BF16, "qTj") for _ in range(2)]
    qis = [kb.sb([128, 512], BF16, "qiTj") for _ in range(2)]
    dgs = [kb.sb([128, 1024], BF16, "Dg")] * 2
    rhs_ = [kb.sb([128, 512], BF16, "rh") for _ in range(4)]
    pts = [kb.sb([128, 512], BF16, "pt") for _ in range(2)]
    vts = [kb.sb([128, 520], BF16, "v1t") for _ in range(3)]
    outs = [kb.sb([128, 512], F32, "yo")] * 2
    st, bst = kb.sb([128, 32], F32, "st")

    nr = [0]
    nd = [0]
    nv = [0]
    nsc = [0]
    LAG = 3
    vts.append(kb.sb([128, 520], BF16, "v1t"))
    pts2 = [[pts[0], kb.sb([128, 512], BF16, "pt")], [pts[1], kb.sb([128, 512], BF16, "pt")]]

    def idx(j):
        qi_t, bqi = qis[j % 2]
        kb.dma("sp", qi_t[:], qiT[:, j, :], [dr], [bqi])
        dg, bdg = dgs[j % 2]
        for h in range(8):
            kb.op("pool", lambda e, h=h: e.tensor_scalar(out=dg[:, h * 128:(h + 1) * 128], in0=idf[:],
                                                         scalar1=wit[:, j * 8 + h:j * 8 + h + 1], scalar2=None,
                                                         op0=ALU.mult), [bidf, bwi], [bdg])
        score, bscore = scores[j % 2]
        steps = [(sb, hq) for sb in range(j + 1) for hq in range(4)]
        pend = []
        sbank = {}
        kibt = {}
        LAGP = 1
        for s in range(len(steps) + LAGP):
            if s < len(steps):
                sb, hq = steps[s]
                if hq == 0:
                    kibt[sb] = kibs[nkb[0] % 3]
                    nkb[0] += 1
                    for hh in range(2):
                        kb.dma("sp", kibt[sb][0][hh * 64:(hh + 1) * 64, :], kiT[:, sb * 512:(sb + 1) * 512], [dr],
                               [kibt[sb][1]])
                kit, bki = kibt[sb]
                for hh in range(2):
                    pd, bpd = bank(nd[0] % 4)
                    nd[0] += 1
                    kb.op("pe", lambda e, hq=hq, hh=hh, kit=kit, pd=pd: e.matmul(
                        pd, qi_t[hh * 64:(hh + 1) * 64, hq * 128:(hq + 1) * 128], kit[hh * 64:(hh + 1) * 64, :],
                        start=True, stop=True), [bqi, bki], [bpd])
                    rh, brh = rhs_[nr[0] % 4]
                    nr[0] += 1
                    pend.append((sb, hh * 4 + hq, hq * 2 + hh, rh, brh, pd, bpd))
                for (sb_, h_, o_, rh, brh, pd, bpd) in pend[-2:]:
                    kb.op("act", lambda e, rh=rh, pd=pd: e.activation(out=rh[:], in_=pd, func=AF.Relu), [bpd], [brh])
            if s - LAGP >= 0:
                for (sb, h, o, rh, brh, pd, bpd) in pend[2 * (s - LAGP):2 * (s - LAGP) + 2]:
                    if o == 0:
                        sbank[sb] = bank(6 + nsc[0] % 2)
                        nsc[0] += 1
                    ps, bps = sbank[sb]
                    kb.op("pe", lambda e, h=h, rh=rh, ps=ps, o=o: e.matmul(ps, dg[:, h * 128:(h + 1) * 128], rh[:],
                                                                           start=(o == 0), stop=(o == 7)),
                          [bdg, brh], [bps])
                    if o == 7:
                        kb.op("act", lambda e, sb=sb, ps=ps: e.copy(out=score[:, sb * 512:(sb + 1) * 512], in_=ps),
                              [bps], [bscore])

    def bis(j):
        n = 512 * (j + 1)
        ns, bns = nots[j % 2]
        score, bscore = scores[j % 2]
        sc = score[:, 0:n]
        kb.op("dve", lambda e: e.tensor_reduce(out=st[:, 0:1], in_=sc, axis=AX.X, op=ALU.min), [bscore], [bst])
        kb.op("dve", lambda e: e.tensor_tensor(out=score[:, n - 512:n], in0=score[:, n - 512:n], in1=pent[:],
                                               op=ALU.add), [bscore, bpen], [bscore])
        kb.op("dve", lambda e: e.tensor_reduce(out=st[:, 1:2], in_=sc, axis=AX.X, op=ALU.max), [bscore], [bst])
        kb.op("dve", lambda e: e.tensor_tensor(out=st[:, 2:3], in0=st[:, 1:2], in1=st[:, 0:1], op=ALU.subtract),
              [bst], [bst])
        kb.op("dve", lambda e: e.scalar_tensor_tensor(out=st[:, 3:4], in0=st[:, 2:3], scalar=-0.01, in1=st[:, 0:1],
                                                      op0=ALU.mult, op1=ALU.add), [bst], [bst])
        kb.op("dve", lambda e: e.tensor_scalar(out=st[:, 3:4], in0=st[:, 3:4], scalar1=-1e-6, scalar2=None,
                                               op0=ALU.add), [bst], [bst])
        kb.op("dve", lambda e: e.tensor_tensor(out=st[:, 4:5], in0=st[:, 1:2], in1=st[:, 3:4], op=ALU.subtract),
              [bst], [bst])
        kb.op("dve", lambda e: e.scalar_tensor_tensor(out=st[:, 5:6], in0=st[:, 4:5], scalar=0.5, in1=st[:, 3:4],
                                                      op0=ALU.mult, op1=ALU.add), [bst], [bst])
        kb.op("dve", lambda e: e.tensor_scalar(out=st[:, 6:7], in0=st[:, 4:5], scalar1=0.25, scalar2=None,
                                               op0=ALU.mult), [bst], [bst])
        for it in range(NIT):
            kb.op("dve", lambda e: e.tensor_scalar(out=ns[:, 0:n], in0=sc, scalar1=st[:, 5:6], scalar2=None,
                                                   op0=ALU.is_ge, op1=ALU.add, accum_out=st[:, 7:8]),
                  [bscore, bst], [bns, bst])
            kb.op("dve", lambda e: e.tensor_scalar(out=st[:, 8:9], in0=st[:, 7:8], scalar1=KSEL - 0.5, scalar2=2.0,
                                                   op0=ALU.is_ge, op1=ALU.mult), [bst], [bst])
            kb.op("dve", lambda e: e.scalar_tensor_tensor(out=st[:, 9:10], in0=st[:, 8:9], scalar=-1.0,
                                                          in1=st[:, 6:7], op0=ALU.add, op1=ALU.mult), [bst], [bst])
            kb.op("dve", lambda e: e.tensor_tensor(out=st[:, 5:6], in0=st[:, 5:6], in1=st[:, 9:10], op=ALU.add),
                  [bst], [bst])
            kb.op("dve", lambda e: e.tensor_scalar(out=st[:, 6:7], in0=st[:, 6:7], scalar1=0.5, scalar2=None,
                                                   op0=ALU.mult), [bst], [bst])
        kb.op("dve", lambda e: e.scalar_tensor_tensor(out=st[:, 10:11], in0=st[:, 6:7], scalar=-4.0, in1=st[:, 5:6],
                                                      op0=ALU.mult, op1=ALU.add), [bst], [bst])
        kb.op("dve", lambda e: e.tensor_scalar(out=ns[:, 0:n], in0=sc, scalar1=st[:, 10:11], scalar2=None,
                                               op0=ALU.is_lt), [bscore, bst], [bns])

    def att_main(j):
        ns, bns = nots[j % 2]
        qt, bqt = qts[j % 2]
        kb.dma("sp", qt[:], qT[:, j, :], [dr], [bqt])
        oacc = [bank(4), bank(5)]
        for g in range(2):
            oa, boa = oacc[g]
            kb.op("pe", lambda e: e.matmul(oa[:, 0:260], idb[:], zb[:], start=True, stop=False),
                  [bidb, bzb], [boa])
        ntile = 4 * j + 4
        vtl = {}
        for stl in range(ntile + 1):
            if stl < ntile:
                vt, bvt = vts[nv[0] % 4]
                nv[0] += 1
                vtl[stl] = (vt, bvt)
                kb.dma("sp", vt[:], v1[stl, :, :], [dr], [bvt])
                u = stl - 4 * j + 5
                lgs_ = [bank(2 * g + stl % 2) for g in range(2)]
                for g in range(2):
                    lgp, blgp = lgs_[g]
                    kb.op("pe", lambda e, lgp=lgp: e.matmul(lgp, ns[:, stl * 128:(stl + 1) * 128], n4[:], start=True,
                                                            stop=False), [bns, bn4], [blgp])
                    if u >= 0:
                        kb.op("pe", lambda e, u=u, lgp=lgp, g=g: e.matmul(lgp, idb[:], TU[:, u, g * 512:(g + 1) * 512],
                                                                          start=False, stop=False), [bidb, bTU], [blgp])
                for hq in range(4):
                    for g in range(2):
                        lgp, blgp = lgs_[g]
                        kb.op("pe", lambda e, hq=hq, g=g, lgp=lgp: e.matmul(
                            lgp[:, hq * 128:(hq + 1) * 128],
                            kTv[g * 64:(g + 1) * 64, hq, stl * 128:(stl + 1) * 128],
                            qt[g * 64:(g + 1) * 64, hq * 128:(hq + 1) * 128],
                            start=False, stop=(hq == 3)), [bkT, bqt], [blgp])
                for g in range(2):
                    lgp, blgp = lgs_[g]
                    pt, bpt = pts2[g][stl % 2]
                    kb.op("act", lambda e, lgp=lgp, pt=pt: e.activation(out=pt[:], in_=lgp, func=AF.Exp), [blgp], [bpt])
            if stl >= 1:
                sp_ = stl - 1
                vt, bvt = vtl[sp_]
                for g in range(2):
                    pt, bpt = pts2[g][sp_ % 2]
                    oa, boa = oacc[g]
                    for hq in range(4):
                        h = g * 4 + hq
                        kb.op("pe", lambda e, hq=hq, h=h: e.matmul(oa[:, hq * 65:(hq + 1) * 65],
                                                                   pt[:, hq * 128:(hq + 1) * 128],
                                                                   vt[:, h * 65:(h + 1) * 65],
                                                                   start=False, stop=(sp_ == ntile - 1 and hq == 3)),
                              [bpt, bvt], [boa])

    def att_fin(j):
        oacc = [bank(4), bank(5)]
        yo, byo = outs[j % 2]
        for g in range(2):
            oa, boa = oacc[g]
            oav = oa[:, 0:260].rearrange("p (h c) -> p h c", h=4)
            kb.op("dve", lambda e: e.reciprocal(out=st[:, 16 + g * 4:20 + g * 4],
                                                in_=oav[:, :, 64:65].rearrange("p h c -> p (h c)")), [boa], [bst])
            kb.op("dve", lambda e: e.tensor_tensor(
                out=yo[:, g * 256:(g + 1) * 256].rearrange("p (h d) -> p h d", h=4), in0=oav[:, :, 0:64],
                in1=st[:, 16 + g * 4:20 + g * 4].unsqueeze(2).to_broadcast([128, 4, 64]), op=ALU.mult),
                [boa, bst], [byo])
        kb.dma("sp", yb[j * 128:(j + 1) * 128, :], yo[:], [byo], [byb])

    idx(0)
    if nj > 1:
        idx(1)
    bis(0)
    for j in range(nj):
        if j + 2 < nj:
            idx(j + 2)
        att_main(j)
        if j + 1 < nj:
            bis(j + 1)
        att_fin(j)
    kb.finish([byb])
    return nc


def t5_bucket_np(rel):
    nb = 16
    max_exact = 8
    side = np.where(rel > 0, nb, 0)
    n = np.abs(rel)
    nf = np.maximum(n, 1).astype(np.float32)
    large = max_exact + (np.log(nf / max_exact) / np.float32(np.log(1024 / max_exact)) * (nb - max_exact)).astype(np.int32)
    large = np.minimum(large, nb - 1)
    return side + np.where(n < max_exact, n, large)


def prep_l3(ob, of, cid):
    b, r = cid // 4, cid % 4
    bf = ob.dtype
    qsel = ob[b].reshape(64, 128, NB1)[r::4][:, ::-1]
    q = qsel[..., 0:512].reshape(16, 128, 2, 4, 64)
    qT = np.ascontiguousarray(q.transpose(2, 4, 0, 3, 1)).reshape(128, 16, 512)
    qi = qsel[..., 1536:2048].reshape(16, 128, 2, 4, 64)
    qiT = np.ascontiguousarray(qi.transpose(2, 4, 0, 3, 1)).reshape(128, 16, 512)
    wsel = of[b].reshape(64, 128, NF1)[r::4][:, ::-1, 2056:2064]
    wi = np.ascontiguousarray(wsel.transpose(1, 0, 2)).reshape(128, 128).astype(np.float32)
    k = ob[b, :, 512:1024].reshape(S, 2, 4, 64)
    kT = np.ascontiguousarray(k.transpose(1, 3, 2, 0)).reshape(128, 4 * S)
    kiT = np.ascontiguousarray(ob[b, :, 2048:2112].T)
    v = ob[b, :, 1024:1536].reshape(64, 128, 8, 64)
    v1 = np.ones((64, 128, 8, 65), bf)
    v1[..., 0:64] = v
    v1 = v1.reshape(64, 128, 520)
    tq = np.arange(128)[:, None]
    sk = np.arange(512)[None, :]
    pen = np.where((sk // 64) <= 2 * r + (tq < 64), 0.0, -1e30).astype(np.float32)
    m = np.arange(NREL)
    rel = m - 767 - 128 * r
    bk = t5_bucket_np(rel)
    oh = np.zeros((32, NREL), np.float32)
    oh[bk, m] += 1.0
    oh[15, :] -= 1.0
    negI4 = np.tile(np.eye(128, dtype=np.float32) * NEGBIG, (1, 4)).astype(bf)
    return {"qT": qT, "kT": kT, "v1": v1, "qiT": qiT, "kiT": kiT, "wi": wi, "pen": pen, "oh": oh,
            "negI4": negI4, "ident": np.eye(128, dtype=np.float32)}


def run_l3(ob, of, rel_bias, nj=16):
    nc = _get("l3_%d" % nj, lambda: build_l3(nj))
    maps = []
    for cid in range(NCORES):
        m = prep_l3(ob, of, cid)
        m["relb"] = np.ascontiguousarray(rel_bias.astype(np.float32))
        maps.append(m)
    res = _run(nc, maps)
    parts = [r["yb"].reshape(16, 128, 512)[:, ::-1].reshape(TPC, 512) for r in res.results]
    return uncore_tokens(parts, (512,))


NCH = 64
PRE_STOP = 0
L2VAR = 0
POOL_ENG = "dve"


def build_l2(nch=NCH, stop=None):
    nc = bass.Bass("TRN2", target_bir_lowering=False)
    xin = _din(nc, "xin", [128, 3, S + 3])
    cw = _din(nc, "cw", [128, 12])
    zin = _din(nc, "zin", [128, NCH, 128])
    bcol = _din(nc, "bcol", [128, NCH])
    acol = _din(nc, "acol", [128, NCH])
    sc3 = _din(nc, "sc3", [1, 2])
    gnw = _din(nc, "gnw", [1, 128])
    cst = _din(nc, "cst", [128, 7, 128])
    ya = _dout(nc, "ya", [S, 128])
    kb = KB(nc)
    dr = Buf("dram_in")
    bya = Buf("ya")

    PSW = []
    for i in range(4):
        t = nc.alloc_psum_tensor("PS%d" % i, [128, 1024], F32)
        PSW.append(t)
    slots = []
    for bnk in range(6):
        t = PSW[bnk // 2]
        c0 = (bnk % 2) * 512
        slots.append((t[:, c0:c0 + 128], Buf("slot%d" % bnk)))
    wide = [(PSW[3][:, 0:512], Buf("wide0")), (PSW[3][:, 512:1024], Buf("wide1"))]
    nslot = [0]

    def slot():
        s = slots[nslot[0] % len(slots)]
        nslot[0] += 1
        return s

    ct, bct = kb.sb([128, 7, 128], F32, "cst")
    kb.dma("sp", ct[:], cst[:, :, :], [dr], [bct])
    ident, LT, ones, negones, penL, SM, sel127 = [ct[:, i, :] for i in range(7)]
    cwt, bcw = kb.sb([128, 12], F32, "cw")
    kb.dma("sp", cwt[:], cw[:, :], [dr], [bcw])
    gnb, bgnb = kb.sb([128, 128], F32, "gnw")
    kb.dma("sp", gnb[:], gnw[0:1, :].to_broadcast([128, 128]), [dr], [bgnb])
    s3, bs3 = kb.sb([128, 2], F32, "sc3")
    kb.dma("sp", s3[:], sc3[0:1, :].to_broadcast([128, 2]), [dr], [bs3])
    epst, beps = kb.sb([128, 3], F32, "eps")
    kb.op("dve", lambda e: e.memset(epst[:, 0:1], EPS), [], [beps])
    kb.op("dve", lambda e: e.memset(epst[:, 1:2], 128.0 * EPS), [], [beps])
    kb.op("dve", lambda e: e.memset(epst[:, 2:3], 1.0), [], [beps])

    cols, bcols = kb.sb([128, 10, NCH], F32, "cols")
    BETA, G, GC, EGC, BG, KD, EGL, NB_, TMP, TMP2 = range(10)
    kb.dma("sp", cols[:, BETA, :], bcol[:, :], [dr], [bcols])
    kb.dma("sp", cols[:, TMP, :], acol[:, :], [dr], [bcols])
    kb.op("act", lambda e: e.activation(out=cols[:, BETA, :], in_=cols[:, BETA, :], func=AF.Sigmoid), [bcols], [bcols])
    kb.op("act", lambda e: e.activation(out=cols[:, TMP, :], in_=cols[:, TMP, :], func=AF.Exp, bias=s3[:, 1:2]),
          [bcols, bs3], [bcols])
    kb.op("act", lambda e: e.activation(out=cols[:, TMP, :], in_=cols[:, TMP, :], func=AF.Ln, bias=epst[:, 2:3]),
          [bcols, beps], [bcols])
    kb.op("act", lambda e: e.activation(out=s3[:, 0:1], in_=s3[:, 0:1], func=AF.Exp), [bs3], [bs3])
    kb.op("dve", lambda e: e.tensor_scalar(out=cols[:, G, :], in0=cols[:, TMP, :], scalar1=s3[:, 0:1], scalar2=-1.0,
                                           op0=ALU.mult, op1=ALU.mult), [bcols, bs3], [bcols])
    pg, bpg = slot()
    kb.op("pe", lambda e: e.matmul(pg[:, 0:NCH], LT, cols[:, G, :], start=True, stop=True), [bct, bcols], [bpg])
    kb.op("dve", lambda e: e.tensor_copy(out=cols[:, GC, :], in_=pg[:, 0:NCH]), [bpg], [bcols])
    pg2, bpg2 = slot()
    kb.op("pe", lambda e: e.matmul(pg2[:, 0:NCH], sel127, cols[:, GC, :], start=True, stop=True), [bct, bcols], [bpg2])
    kb.op("dve", lambda e: e.tensor_copy(out=cols[:, TMP, :], in_=pg2[:, 0:NCH]), [bpg2], [bcols])
    kb.op("act", lambda e: e.activation(out=cols[:, EGL, :], in_=cols[:, TMP, :], func=AF.Exp), [bcols], [bcols])
    kb.op("dve", lambda e: e.tensor_tensor(out=cols[:, TMP2, :], in0=cols[:, TMP, :], in1=cols[:, GC, :], op=ALU.subtract),
          [bcols], [bcols])
    kb.op("act", lambda e: e.activation(out=cols[:, KD, :], in_=cols[:, TMP2, :], func=AF.Exp), [bcols], [bcols])
    kb.op("act", lambda e: e.activation(out=cols[:, EGC, :], in_=cols[:, GC, :], func=AF.Exp), [bcols], [bcols])
    kb.op("dve", lambda e: e.tensor_tensor(out=cols[:, BG, :], in0=cols[:, EGC, :], in1=cols[:, BETA, :], op=ALU.mult),
          [bcols], [bcols])
    kb.op("dve", lambda e: e.tensor_scalar(out=cols[:, NB_, :], in0=cols[:, BETA, :], scalar1=-1.0, scalar2=None,
                                           op0=ALU.mult), [bcols], [bcols])

    if stop == "cols":
        kb.dma("sp", ya[0:128, 0:NCH], cols[:, GC, :], [bcols], [bya])
        kb.finish([bya])
        return nc
    QT, bQT = kb.sb([128, S], F32, "QT")
    KT, bKT = kb.sb([128, S], F32, "KT")
    Vtok, bVtok = kb.sb([128, NCH, 128], F32, "Vtok")
    Ktok, bKtok = kb.sb([128, NCH, 128], F32, "Ktok")
    xbs = [kb.sb([128, 3, 515], F32, "xb") for _ in range(2)]
    u, bu = kb.sb([128, 3, 512], F32, "u")
    sqt, bsq = kb.sb([128, 512], F32, "sq")
    rs, brs = kb.sb([128, 512], F32, "rs")
    nblk = (nch * 128 + 511) // 512
    for blk in range(nblk):
        xb, bxb = xbs[blk % 2]
        kb.dma("sp", xb[:], xin[:, :, blk * 512:blk * 512 + 515], [dr], [bxb])
        for a in range(3):
            kb.op("dve", lambda e, a=a: e.tensor_scalar(out=u[:, a, :], in0=xb[:, a, 0:512],
                                                        scalar1=cwt[:, a * 4:a * 4 + 1], scalar2=None, op0=ALU.mult),
                  [bxb, bcw], [bu])
            for tap in range(1, 4):
                kb.op("dve", lambda e, a=a, tap=tap: e.scalar_tensor_tensor(
                    out=u[:, a, :], in0=xb[:, a, tap:tap + 512], scalar=cwt[:, a * 4 + tap:a * 4 + tap + 1],
                    in1=u[:, a, :], op0=ALU.mult, op1=ALU.add), [bxb, bcw, bu], [bu])
        kb.op("act", lambda e: e.activation(out=u[:].rearrange("p a n -> p (a n)"),
                                            in_=u[:].rearrange("p a n -> p (a n)"), func=AF.Silu), [bu], [bu])
        cs = slice(blk * 512, (blk + 1) * 512)
        for a, (dst, bdst, scl, epi) in enumerate([(QT, bQT, 128.0, 1), (KT, bKT, 1.0, 0)]):
            kb.op("dve", lambda e, a=a: e.tensor_tensor(out=sqt[:], in0=u[:, a, :], in1=u[:, a, :], op=ALU.mult),
                  [bu], [bsq])
            pw, bpw = wide[a]
            kb.op("pe", lambda e: e.matmul(pw, ones, sqt[:], start=True, stop=True), [bct, bsq], [bpw])
            kb.op("act", lambda e, scl=scl, epi=epi: e.activation(out=rs[:], in_=pw, func=AF.Sqrt, scale=scl,
                                                                  bias=epst[:, epi:epi + 1]), [bpw, beps], [brs])
            kb.op("dve", lambda e: e.reciprocal(out=rs[:], in_=rs[:]), [brs], [brs])
            kb.op("dve", lambda e, a=a, dst=dst: e.tensor_tensor(out=dst[:, cs], in0=u[:, a, :], in1=rs[:], op=ALU.mult),
                  [bu, brs], [bdst])
        for q4 in range(4):
            ch = blk * 4 + q4
            if ch >= nch:
                break
            pk, bpk = slot()
            kb.op("pe", lambda e, ch=ch: e.transpose(out=pk, in_=KT[:, ch * 128:(ch + 1) * 128], identity=ident),
                  [bKT, bct], [bpk])
            kb.op("act", lambda e, ch=ch: e.copy(out=Ktok[:, ch, :], in_=pk), [bpk], [bKtok])
            pv, bpv = slot()
            kb.op("pe", lambda e, q4=q4: e.transpose(out=pv, in_=u[:, 2, q4 * 128:(q4 + 1) * 128], identity=ident),
                  [bu, bct], [bpv])
            kb.op("act", lambda e, ch=ch: e.copy(out=Vtok[:, ch, :], in_=pv), [bpv], [bVtok])

    if stop == "prep":
        kb.dma("sp", ya[0:128, :], Ktok[:, 0, :], [bKtok], [bya])
        kb.dma("sp", ya[128:256, :], Vtok[:, 0, :], [bVtok], [bya])
        kb.dma("sp", ya[256:384, :], QT[:, 0:128], [bQT], [bya])
        kb.finish([bya])
        return nc
    RING = 4
    ring = [dict(wdT=kb.sb([128, 128], F32, "wdT"), uval=kb.sb([128, 128], F32, "uval"),
                 attnT=kb.sb([128, 128], F32, "attnT"), kdec=kb.sb([128, 128], F32, "kdec")) for _ in range(RING)]
    tmps = {}

    def tmp(name, k=2):
        if name not in tmps:
            tmps[name] = [kb.sb([128, 128], F32, name) for _ in range(k)]
            tmps[name + "_i"] = 0
        i = tmps[name + "_i"]
        tmps[name + "_i"] = i + 1
        return tmps[name][i % k]

    def pre(n):
        par = "_%d" % (n % 2)
        R = ring[n % RING]
        kt = KT[:, n * 128:(n + 1) * 128]
        qt = QT[:, n * 128:(n + 1) * 128]
        dg, bdg = tmp("diag" + par)
        kb.op("dve", lambda e: e.tensor_scalar(out=dg[:], in0=ident, scalar1=cols[:, GC, n:n + 1], scalar2=None,
                                               op0=ALU.mult), [bct, bcols], [bdg])
        pD, bpD = slot()
        kb.op("pe", lambda e: e.matmul(pD, dg[:], ones, start=True, stop=False), [bdg, bct], [bpD])
        kb.op("pe", lambda e: e.matmul(pD, negones, dg[:], start=False, stop=True), [bdg, bct], [bpD])
        Dl, bDl = tmp("Dl" + par)
        E, bE = tmp("E" + par)
        kb.op("dve", lambda e: e.tensor_tensor(out=Dl[:], in0=pD, in1=penL, op=ALU.min), [bpD, bct], [bDl])
        kb.op("act", lambda e: e.activation(out=E[:], in_=Dl[:], func=AF.Exp), [bDl], [bE])
        yield
        Es, bEs = tmp("Es" + par)
        kb.op(POOL_ENG, lambda e: e.tensor_tensor(out=Es[:], in0=E[:], in1=SM, op=ALU.mult), [bE, bct], [bEs])
        pA, bpA = slot()
        kb.op("pe", lambda e: e.matmul(pA, kt, kt, start=True, stop=True), [bKT], [bpA])
        Nm, bN = tmp("N" + par, 3)
        kb.op("dve", lambda e: e.scalar_tensor_tensor(out=Nm[:], in0=pA, scalar=cols[:, NB_, n:n + 1], in1=Es[:],
                                                      op0=ALU.mult, op1=ALU.mult), [bpA, bcols, bEs], [bN])
        yield
        pM, bpM = slot()
        kb.op("pe", lambda e: e.transpose(out=pM, in_=Nm[:], identity=ident), [bN, bct], [bpM])
        Mm, bM = tmp("M" + par, 3)
        kb.op("act", lambda e: e.copy(out=Mm[:], in_=pM), [bpM], [bM])
        P, bP = tmp("P" + par, 3)
        kb.op("dve", lambda e: e.tensor_tensor(out=P[:], in0=Mm[:], in1=ident, op=ALU.add), [bM, bct], [bP])
        yield
        pQK, bpQK = slot()
        kb.op("pe", lambda e: e.matmul(pQK, qt, kt, start=True, stop=True), [bQT, bKT], [bpQK])
        at, bat = tmp("attn" + par)
        kb.op("dve", lambda e: e.tensor_tensor(out=at[:], in0=pQK, in1=E[:], op=ALU.mult), [bpQK, bE], [bat])
        pAT, bpAT = slot()
        kb.op("pe", lambda e: e.transpose(out=pAT, in_=at[:], identity=ident), [bat, bct], [bpAT])
        aT, baT = R["attnT"]
        kb.op("act", lambda e: e.copy(out=aT[:], in_=pAT), [bpAT], [baT])
        yield
        for lev in range(1, 7):
            pN2, bpN2 = slot()
            kb.op("pe", lambda e: e.matmul(pN2, Mm[:], Nm[:], start=True, stop=True), [bM, bN], [bpN2])
            N2, bN2 = tmp("N" + par, 3)
            kb.op("act", lambda e: e.copy(out=N2[:], in_=pN2), [bpN2], [bN2])
            if lev < 6:
                pM2, bpM2 = slot()
                kb.op("pe", lambda e: e.matmul(pM2, Nm[:], Mm[:], start=True, stop=True), [bM, bN], [bpM2])
                M2, bM2 = tmp("M" + par, 3)
                kb.op("act", lambda e: e.copy(out=M2[:], in_=pM2), [bpM2], [bM2])
            yield
            pP, bpP = slot()
            kb.op("pe", lambda e: e.matmul(pP, N2[:], P[:], start=True, stop=True), [bN2, bP], [bpP])
            P2, bP2 = tmp("P" + par, 3)
            kb.op("dve", lambda e: e.tensor_tensor(out=P2[:], in0=pP, in1=P[:], op=ALU.add), [bpP, bP], [bP2])
            P, bP = P2, bP2
            yield
            Nm, bN = N2, bN2
            if lev < 6:
                Mm, bM = M2, bM2
        yield
        kbg, bkbg = tmp("kbg" + par)
        kb.op(POOL_ENG, lambda e: e.tensor_scalar(out=kbg[:], in0=Ktok[:, n, :], scalar1=cols[:, BG, n:n + 1],
                                                scalar2=None, op0=ALU.mult), [bKtok, bcols], [bkbg])
        vb, bvb = tmp("vb" + par)
        kb.op(POOL_ENG, lambda e: e.tensor_scalar(out=vb[:], in0=Vtok[:, n, :], scalar1=cols[:, BETA, n:n + 1],
                                                scalar2=None, op0=ALU.mult), [bVtok, bcols], [bvb])
        kd, bkd = R["kdec"]
        kb.op(POOL_ENG, lambda e: e.tensor_scalar(out=kd[:], in0=Ktok[:, n, :], scalar1=cols[:, KD, n:n + 1],
                                                scalar2=None, op0=ALU.mult), [bKtok, bcols], [bkd])
        pW, bpW = slot()
        kb.op("pe", lambda e: e.matmul(pW, kbg[:], P[:], start=True, stop=True), [bkbg, bP], [bpW])
        wd, bwd = R["wdT"]
        kb.op("act", lambda e: e.copy(out=wd[:], in_=pW), [bpW], [bwd])
        pU, bpU = slot()
        kb.op("pe", lambda e: e.matmul(pU, P[:], vb[:], start=True, stop=True), [bP, bvb], [bpU])
        uv, buv = R["uval"]
        kb.op("act", lambda e: e.copy(out=uv[:], in_=pU), [bpU], [buv])

    states = [kb.sb([128, 128], F32, "state") for _ in range(2)]
    kb.op("dve", lambda e: e.memset(states[0][0][:], 0.0), [], [states[0][1]])
    zts = [kb.sb([128, 128], F32, "zt") for _ in range(2)]
    st, bst = kb.sb([128, 8], F32, "st")
    junk, bjunk = kb.sb([128, 128], F32, "junk")

    def scan(n):
        R = ring[n % RING]
        wd, bwd = R["wdT"]
        uv, buv = R["uval"]
        aT, baT = R["attnT"]
        kd, bkd = R["kdec"]
        S0, bS0 = states[n % 2]
        S1, bS1 = states[(n + 1) % 2]
        zt, bzt = zts[n % 2]
        kb.dma("sp", zt[:], zin[:, n, :], [dr], [bzt])
        ppv, bppv = slot()
        kb.op("pe", lambda e: e.matmul(ppv, wd[:], S0[:], start=True, stop=True), [bwd, bS0], [bppv])
        po1, bpo1 = wide[0][0][:, 0:128], wide[0][1]
        kb.op("pe", lambda e: e.matmul(po1, QT[:, n * 128:(n + 1) * 128], S0[:], start=True, stop=True),
              [bQT, bS0], [bpo1])
        vn, bvn = tmp("vnew")
        kb.op("dve", lambda e: e.tensor_tensor(out=vn[:], in0=uv[:], in1=ppv, op=ALU.subtract), [buv, bppv], [bvn])
        yield
        psu, bpsu = slot()
        kb.op("pe", lambda e: e.matmul(psu, kd[:], vn[:], start=True, stop=True), [bkd, bvn], [bpsu])
        po2, bpo2 = slot()
        kb.op("pe", lambda e: e.matmul(po2, aT[:], vn[:], start=True, stop=True), [baT, bvn], [bpo2])
        kb.op("dve", lambda e: e.scalar_tensor_tensor(out=S1[:], in0=S0[:], scalar=cols[:, EGL, n:n + 1], in1=psu,
                                                      op0=ALU.mult, op1=ALU.add), [bS0, bcols, bpsu], [bS1])
        o2, bo2 = tmp("o2")
        kb.op("act", lambda e: e.copy(out=o2[:], in_=po2), [bpo2], [bo2])
        o, bo = tmp("o")
        kb.op("dve", lambda e: e.scalar_tensor_tensor(out=o[:], in0=po1, scalar=cols[:, EGC, n:n + 1], in1=o2[:],
                                                      op0=ALU.mult, op1=ALU.add), [bpo1, bcols, bo2], [bo])
        yield
        kb.op("act", lambda e: e.activation(out=junk[:], in_=o[:], func=AF.Square, accum_out=st[:, 0:1]),
              [bo], [bjunk, bst])
        kb.op("act", lambda e: e.activation(out=st[:, 1:2], in_=st[:, 0:1], func=AF.Ln, scale=1.0 / 128,
                                            bias=epst[:, 0:1]), [bst, beps], [bst])
        kb.op("act", lambda e: e.activation(out=st[:, 2:3], in_=st[:, 1:2], func=AF.Exp, scale=-0.5), [bst], [bst])
        sg, bsg = tmp("sg")
        kb.op("act", lambda e: e.activation(out=sg[:], in_=zt[:], func=AF.Exp, scale=-1.0), [bzt], [bsg])
        kb.op(POOL_ENG, lambda e: e.tensor_scalar(out=sg[:], in0=sg[:], scalar1=1.0, scalar2=None, op0=ALU.add),
              [bsg], [bsg])
        kb.op("dve", lambda e: e.reciprocal(out=sg[:], in_=sg[:]), [bsg], [bsg])
        kb.op(POOL_ENG, lambda e: e.tensor_tensor(out=sg[:], in0=sg[:], in1=zt[:], op=ALU.mult), [bsg, bzt], [bsg])
        yield
        t1, bt1 = tmp("t1")
        kb.op("dve", lambda e: e.scalar_tensor_tensor(out=t1[:], in0=o[:], scalar=st[:, 2:3], in1=gnb[:],
                                                      op0=ALU.mult, op1=ALU.mult), [bo, bst, bgnb], [bt1])
        yt_, byt_ = tmp("yout")
        kb.op("dve", lambda e: e.tensor_tensor(out=yt_[:], in0=t1[:], in1=sg[:], op=ALU.mult), [bt1, bsg], [byt_])
        kb.dma("sp", ya[n * 128:(n + 1) * 128, :], yt_[:], [byt_], [bya])

    def drive(gens):
        gens = list(gens)
        while gens:
            for g in list(gens):
                try:
                    next(g)
                except StopIteration:
                    gens.remove(g)

    def chain(*gs):
        for g in gs:
            yield from g

    if stop == "alloc":
        kb.dma("sp", ya[0:128, :], states[0][0][:], [states[0][1]], [bya])
        kb.finish([bya])
        return nc
    drive([pre(n) for n in range(min(2, nch))])
    for n in range(0, nch, 2):
        gens = [pre(m) for m in (n + 2, n + 3) if m < nch]
        gens.append(chain(*[scan(m) for m in (n, n + 1) if m < nch]))
        drive(gens)
    kb.finish([bya])
    return nc


def l2_consts():
    i = np.arange(128)
    ident = np.eye(128, dtype=np.float32)
    LT = (i[:, None] <= i[None, :]).astype(np.float32)
    ones = np.ones((128, 128), np.float32)
    penL = np.where(i[:, None] >= i[None, :], 0.0, -1e30).astype(np.float32)
    SM = (i[:, None] > i[None, :]).astype(np.float32)
    sel = np.zeros((128, 128), np.float32)
    sel[127, :] = 1.0
    return np.ascontiguousarray(np.stack([ident, LT, ones, -ones, penL, SM, sel], axis=1))


def run_l2(of, conv_w_l, a_log_l, dt_bias_l, gnw_l, nch=NCH, stop=None):
    nc = _get("l2_%d_%s" % (nch, stop), lambda: build_l2(nch, stop))
    cst = l2_consts()
    maps = []
    for cid in range(NCORES):
        b, g = cid // 4, cid % 4
        xs = []
        cws = []
        for a in range(3):
            cols_ = slice(a * 512 + g * 128, a * 512 + (g + 1) * 128)
            xa = np.zeros((128, S + 3), np.float32)
            xa[:, 3:] = of[b, :, cols_].T
            xs.append(xa)
            cws.append(conv_w_l[:, cols_].T)
        xin = np.ascontiguousarray(np.stack(xs, axis=1))
        cw = np.ascontiguousarray(np.concatenate(cws, axis=1)).astype(np.float32)
        z = of[b, :, 1536 + g * 128:1536 + (g + 1) * 128].reshape(NCH, 128, 128).transpose(1, 0, 2)
        bc = of[b, :, 2048 + g].reshape(NCH, 128).T
        ac = of[b, :, 2052 + g].reshape(NCH, 128).T
        maps.append({"xin": xin, "cw": cw, "zin": np.ascontiguousarray(z), "bcol": np.ascontiguousarray(bc),
                     "acol": np.ascontiguousarray(ac),
                     "sc3": np.array([[a_log_l[g], dt_bias_l[g]]], np.float32),
                     "gnw": gnw_l.reshape(1, 128).astype(np.float32), "cst": cst})
    res = _run(nc, maps)
    ya = np.zeros((NB, S, 512), np.float32)
    for cid in range(NCORES):
        b, g = cid // 4, cid % 4
        ya[b, :, g * 128:(g + 1) * 128] = res.results[cid]["ya"]
    return ya


def kernel(x, c, rel_bias, mod_w, mod_b, norm_mix_w, norm_ffn_w, w_in, conv_w, a_log, dt_bias,
           gdn_norm_w, q_norm_w, k_norm_w, w_out, router_w, router_b, w1, b1, w2, b2):
    f = lambda a: np.ascontiguousarray(np.asarray(a), dtype=np.float32)
    x = f(x)
    c = f(c)
    rel_bias = f(rel_bias)
    for l in range(2):
        of, ob = run_l1(x, c, f(mod_w[l]), f(mod_b[l]), f(norm_mix_w[l]), f(w_in[l]), f(q_norm_w[l]), f(k_norm_w[l]))
        ya = run_l2(of, f(conv_w[l]), f(a_log[l]), f(dt_bias[l]), f(gdn_norm_w[l]))
        yb = run_l3(ob, of, rel_bias)
        ycat = np.ascontiguousarray(np.concatenate([ya, yb], axis=-1))
        w1r, b1r, w2r, b2r = prep_l4_weights(f(w1[l]), f(b1[l]), f(w2[l]), f(b2[l]))
        x = run_l4(x, ycat, c, f(mod_w[l]), f(mod_b[l]), f(norm_ffn_w[l]), f(w_out[l]), f(router_w[l]),
                   f(router_b[l]), w1r, b1r, w2r, b2r)
    return x


CAP = 1024
ESTRIDE = CAP + 128
TRASH = NE * ESTRIDE


def _dma_ind(kb, out, out_off, in_, in_off, reads, writes):
    dst = writes[0]
    if dst.dsem is None:
        kb.nsem += 1
        key = "d%d_%s" % (kb.nsem, dst.name)
        dst.dsem = key
        kb.sems[key] = kb.nc.alloc_semaphore(key[:40])
    deps = kb._deps(reads, writes)
    kb._wait("pool", deps)
    ins = kb.nc.gpsimd.indirect_dma_start(out=out, out_offset=out_off, in_=in_, in_offset=in_off)
    dst.dcnt += 16
    ins.then_inc(kb.sems[dst.dsem], 16)
    for b in reads:
        if b.r.get(dst.dsem, 0) < dst.dcnt:
            b.r[dst.dsem] = dst.dcnt
    dst.w = (dst.dsem, dst.dcnt)
    dst.r = {}
    kb.n_ops += 1


def build_l4r(e_lo=0, e_hi=NE, first=True):
    n_exp = e_hi - e_lo
    nc = bass.Bass("TRN2", target_bir_lowering=False)
    x = _din(nc, "x", [TPC, D])
    ycat = _din(nc, "ycat", [TPC, D])
    cT = _din(nc, "cT", [128, 8])
    mod_w = _din(nc, "mod_w", [D, 6 * D])
    mod_b = _din(nc, "mod_b", [1, 6 * D])
    normw = _din(nc, "normw", [1, D])
    w_out = _din(nc, "w_out", [D, D])
    rw = _din(nc, "rw", [D, NE])
    rb = _din(nc, "rb", [1, NE])
    w1 = _din(nc, "w1", [n_exp, 8, 128, 8 * 256])
    b1 = _din(nc, "b1", [128, NE * 16])
    w2 = _din(nc, "w2", [n_exp, D, D])
    b2 = _din(nc, "b2", [NE, D])
    cst = _din(nc, "cst", [128, 3, 128])
    erow = _din(nc, "erow", [2, NE])
    xprev = None if first else _din(nc, "xprev", [TPC, D])
    xo = _dout(nc, "xo", [TPC, D])
    Xs = nc.dram_tensor("Xs", [TRASH + 128, D], BF16, kind="Internal").ap()
    Ys = nc.dram_tensor("Ys", [TRASH + 128, D], F32, kind="Internal").ap()
    kb = KB(nc)
    dr = Buf("dram_in")
    bxo = Buf("xo")
    bXs = Buf("Xs")
    bYs = Buf("Ys")

    PS = []
    for i in range(4):
        t = nc.alloc_psum_tensor("PS%d" % i, [128, 1024], F32)
        PS.append((t, Buf("ps%da" % i), Buf("ps%db" % i)))

    def bank(i):
        t, ba, bb = PS[i // 2]
        return (t[:, 0:512], ba) if i % 2 == 0 else (t[:, 512:1024], bb)

    R_x, _ = kb.sb([128, 8192], F32, "R_x")
    xrows = rview(R_x, 0, [128, 8, D], BF16)
    bxrows = Buf("xrows")
    xeT = rview(R_x, 16384, [128, 8, CAP], BF16)
    bxeT = Buf("xeT")
    modw_bufs = [(rview(R_x, i * 16384, [128, 8, 512], F32), Buf("modw%d" % i)) for i in range(2)]
    R_a, _ = kb.sb([128, 4096], F32, "R_a")
    actT = rview(R_a, 0, [128, 8, CAP], BF16)
    bactT = Buf("actT")
    g1, bg1 = rview(R_a, 0, [128, D], F32), Buf("g1")
    sh2, bsh2 = rview(R_a, 4096, [128, D], F32), Buf("sh2")
    A2, bA2 = rview(R_a, 8192, [128, D], F32), Buf("A2")
    nwb, bnwb = rview(R_a, 12288, [128, D], F32), Buf("nwb")
    R_w2, _ = kb.sb([128, 8192], F32, "R_w2")
    W2B = [(rview(R_w2, i * 16384, [128, 8, D], BF16), Buf("w2b%d" % i)) for i in range(2)]
    wo = rview(R_w2, 0, [128, 8, D], BF16)
    bwo = Buf("wo")
    scb_t = (rview(R_w2, 16384, [128, 8, 128], F32), Buf("scb"))
    modb_bufs = [(rview(R_w2, 20480 + i * 2048, [128, 512], F32), Buf("modb%d" % i)) for i in range(2)]
    R_W1, _ = kb.sb([128, 3072], F32, "R_W1")
    W1R = [(rview(R_W1, i * 4096, [128, 8, 256], BF16), Buf("w1r%d" % i)) for i in range(3)]
    g2, bg2 = kb.sb([128, D], F32, "g2")
    xt, bxt = kb.sb([128, D], F32, "xt")
    yt, byt = kb.sb([128, D], F32, "yt")
    tmpA, btmpA = kb.sb([128, D], F32, "tmpA")
    tmpB, btmpB = kb.sb([128, D], F32, "tmpB")
    R1, bR1 = kb.sb([128, D], F32, "R1")
    ybt = rview(R1, 0, [128, D], BF16)
    yT = rview(R1, 2048, [128, 8, 128], BF16)
    h32T = rview(R1, 0, [128, 8, 128], F32)
    R2, bR2 = kb.sb([128, D], F32, "R2")
    hb = rview(R2, 0, [128, D], BF16)
    junk = rview(R2, 2048, [128, D], BF16)
    yrows = [kb.sb([128, D], F32, "yrow") for _ in range(2)]
    b2bs = [kb.sb([128, D], F32, "b2bc") for _ in range(2)]
    grows = [kb.sb([128, D], F32, "grow") for _ in range(4)]

    ct, bct = kb.sb([128, 3, 128], F32, "cst")
    kb.dma("sp", ct[:], cst[:, :, :], [dr], [bct])
    idf, LTs, ones = ct[:, 0, :], ct[:, 1, :], ct[:, 2, :]
    bidf = bct
    idb, bidb = kb.sb([128, 128], BF16, "identb")
    kb.op("dve", lambda e: e.tensor_copy(out=idb[:], in_=idf), [bct], [bidb])
    er, ber = kb.sb([128, 2, NE], F32, "erow")
    kb.dma("sp", er[:, 0, :], erow[0:1, :].to_broadcast([128, NE]), [dr], [ber])
    kb.dma("sp", er[:, 1, :], erow[1:2, :].to_broadcast([128, NE]), [dr], [ber])
    epst, beps = kb.sb([128, 1], F32, "eps")
    kb.op("dve", lambda e: e.memset(epst[:], EPS), [], [beps])
    kb.dma("sp", nwb, normw[0:1, :].to_broadcast([128, D]), [dr], [bnwb])
    rbb, brbb = kb.sb([128, NE], F32, "rbb")
    kb.dma("sp", rbb[:], rb[0:1, :].to_broadcast([128, NE]), [dr], [brbb])
    rwt, brwt = kb.sb([128, 8, NE], F32, "rwt")
    kb.dma("sp", rwt[:], rw.rearrange("(kc p) e -> p kc e", p=128), [dr], [brwt])
    b1a, bb1a = kb.sb([128, NE * 16], F32, "b1a")
    kb.dma("sp", b1a[:], b1[:, :], [dr], [bb1a])
    b1av = b1a[:].rearrange("p (g two) -> p g two", two=2)
    kb.op("dve", lambda e: e.tensor_scalar(out=b1av[:, :, 0:1], in0=b1av[:, :, 0:1], scalar1=ALPHA, scalar2=None,
                                           op0=ALU.mult), [bb1a], [bb1a])
    kb.op("dve", lambda e: e.tensor_scalar(out=b1av[:, :, 1:2], in0=b1av[:, :, 1:2], scalar1=1.0, scalar2=None,
                                           op0=ALU.add), [bb1a], [bb1a])
    st, bst = kb.sb([128, 64], F32, "st")
    lg, blg = kb.sb([128, NE], F32, "lg")
    ohk, bohk = kb.sb([128, 4, NE], F32, "ohk")
    prod, bprod = kb.sb([128, 4, NE], F32, "prod")
    sel, bsel = kb.sb([128, NE], F32, "sel")
    pos, bpos = kb.sb([128, NE], F32, "pos")
    basebc, bbase = kb.sb([128, NE], F32, "basebc")
    kb.op("dve", lambda e: e.memset(basebc[:], 0.0), [], [bbase])
    gkall, bgk = kb.sb([128, NT, 4], F32, "gkall")
    dstf, bdstf = kb.sb([128, NT, 4], F32, "dstf")
    dsti, bdsti = kb.sb([128, NT, 4], U32, "dsti")

    kb.op("dve", lambda e: e.memset(tmpA[:], 0.0), [], [btmpA])
    zb16 = tmpA[:].bitcast(BF16)[:, 0:D]
    r0 = e_lo * ESTRIDE
    nz = (n_exp * ESTRIDE) // 128
    for i in range(nz):
        kb.dma("sp", Xs[r0 + i * 128:r0 + (i + 1) * 128, :], zb16, [btmpA], [bXs])
        kb.dma("sp", Ys[r0 + i * 128:r0 + (i + 1) * 128, :], tmpA[:], [btmpA], [bYs])
    kb.dma("sp", Ys[TRASH:TRASH + 128, :], tmpA[:], [btmpA], [bYs])
    kb.dma("sp", Xs[TRASH:TRASH + 128, :], zb16, [btmpA], [bXs])

    sc2_out = (A2, bA2)
    emit_mod(kb, cT, mod_w, mod_b, [2, 3, 4, 5], wbufs=modw_bufs, bbufs=modb_bufs,
             pss=[bank(0), bank(1)], scb_t=scb_t,
             outs={2: (g1, bg1), 3: (sh2, bsh2), 4: sc2_out, 5: (g2[:], bg2)})
    kb.op("dve", lambda e: e.scalar_tensor_tensor(out=A2, in0=A2, scalar=1.0, in1=nwb,
                                                  op0=ALU.add, op1=ALU.mult), [bA2, bnwb], [bA2])

    for kc in range(8):
        kb.dma("pool", wo[:, kc, :], w_out[kc * 128:(kc + 1) * 128, :], [dr], [bwo])
    for ti in range(NT):
        rows = slice(ti * 128, (ti + 1) * 128)
        kb.dma("sp", xt[:], x[rows, :], [dr], [bxt])
        kb.dma("sp", yt[:], ycat[rows, :], [dr], [byt])
        kb.op("act", lambda e: e.copy(out=ybt, in_=yt[:]), [byt], [bR1])
        pt, bpt = bank(0)
        ptb = pt.bitcast(BF16)
        for kc in range(8):
            kb.op("pe", lambda e, kc=kc: e.transpose(out=ptb[:, kc * 128:(kc + 1) * 128],
                                                     in_=ybt[:, kc * 128:(kc + 1) * 128], identity=idb[:]),
                  [bR1, bidb], [bpt])
        kb.op("act", lambda e: e.copy(out=yT.rearrange("p a b -> p (a b)"), in_=ptb), [bpt], [bR1])
        for half in range(2):
            pp, bpp = bank(2 + half)
            for kc in range(8):
                kb.op("pe", lambda e, kc=kc: e.matmul(pp, yT[:, kc, :], wo[:, kc, half * 512:(half + 1) * 512],
                                                      start=(kc == 0), stop=(kc == 7)), [bR1, bwo], [bpp])
            cs = slice(half * 512, (half + 1) * 512)
            kb.op("dve", lambda e: e.tensor_tensor(out=tmpA[:, cs], in0=pp, in1=g1[:, cs], op=ALU.mult),
                  [bpp, bg1], [btmpA])
        kb.op("dve", lambda e: e.tensor_tensor(out=xt[:], in0=tmpA[:], in1=xt[:], op=ALU.add), [btmpA, bxt], [bxt])
        kb.dma("sp", xo[rows, :], xt[:], [bxt], [bxo])
        kb.op("act", lambda e: e.activation(out=junk, in_=xt[:], func=AF.Square, accum_out=st[:, 0:1]),
              [bxt], [bR2, bst])
        kb.op("act", lambda e: e.activation(out=st[:, 1:2], in_=st[:, 0:1], func=AF.Sqrt, scale=1.0 / D,
                                            bias=epst[:]), [bst, beps], [bst])
        kb.op("dve", lambda e: e.reciprocal(out=st[:, 2:3], in_=st[:, 1:2]), [bst], [bst])
        kb.op("dve", lambda e: e.scalar_tensor_tensor(out=tmpA[:], in0=xt[:], scalar=st[:, 2:3], in1=A2,
                                                      op0=ALU.mult, op1=ALU.mult), [bxt, bst, bA2], [btmpA])
        kb.op("dve", lambda e: e.tensor_tensor(out=tmpB[:], in0=tmpA[:], in1=sh2, op=ALU.add),
              [btmpA, bsh2], [btmpB])
        kb.op("act", lambda e: e.copy(out=hb, in_=tmpB[:]), [btmpB], [bR2])
        for half in range(2):
            pq, bpq = bank(4 + half)
            for k4 in range(4):
                kc = half * 4 + k4
                kb.op("pe", lambda e, kc=kc, k4=k4: e.transpose(out=pq[:, k4 * 128:(k4 + 1) * 128],
                                                                in_=tmpB[:, kc * 128:(kc + 1) * 128],
                                                                identity=idf), [btmpB, bidf], [bpq])
            kb.op("dve", lambda e: e.tensor_copy(
                out=h32T[:, half * 4:(half + 1) * 4, :], in_=pq.rearrange("p (a b) -> p a b", a=4)),
                [bpq], [bR1])
        pr, bpr = bank(6)
        for kc in range(8):
            kb.op("pe", lambda e, kc=kc: e.matmul(pr[:, 0:NE], h32T[:, kc, :], rwt[:, kc, :],
                                                  start=(kc == 0), stop=(kc == 7)), [bR1, brwt], [bpr])
        kb.op("dve", lambda e: e.tensor_tensor(out=lg[:], in0=pr[:, 0:NE], in1=rbb[:], op=ALU.add),
              [bpr, brbb], [blg])
        kb.op("dve", lambda e: e.max(out=st[:, 8:16], in_=lg[:]), [blg], [bst])
        kb.op("dve", lambda e: e.tensor_scalar(out=st[:, 16:17], in0=st[:, 8:9], scalar1=-1.0, scalar2=None,
                                               op0=ALU.mult), [bst], [bst])
        kb.op("act", lambda e: e.activation(out=st[:, 20:24], in_=st[:, 8:12], func=AF.Exp, bias=st[:, 16:17],
                                            accum_out=st[:, 17:18]), [bst], [bst])
        kb.op("dve", lambda e: e.reciprocal(out=st[:, 18:19], in_=st[:, 17:18]), [bst], [bst])
        for k in range(4):
            kb.op("dve", lambda e, k=k: e.tensor_scalar(out=ohk[:, k, :], in0=lg[:], scalar1=st[:, 8 + k:9 + k],
                                                        scalar2=None, op0=ALU.is_equal), [blg, bst], [bohk])
        kb.op("dve", lambda e: e.tensor_scalar(out=sel[:], in0=lg[:], scalar1=st[:, 11:12], scalar2=None,
                                               op0=ALU.is_ge), [blg, bst], [bsel])
        ppf, bppf = bank(7)
        kb.op("pe", lambda e: e.matmul(ppf[:, 0:NE], LTs, sel[:], start=True, stop=True), [bct, bsel], [bppf])
        kb.op("dve", lambda e: e.tensor_tensor(out=pos[:], in0=ppf[:, 0:NE], in1=basebc[:], op=ALU.add),
              [bppf, bbase], [bpos])
        ppb, bppb = bank(1)
        kb.op("pe", lambda e: e.matmul(ppb[:, 0:NE], ones, sel[:], start=True, stop=True), [bct, bsel], [bppb])
        kb.op("dve", lambda e: e.tensor_tensor(out=basebc[:], in0=ppb[:, 0:NE], in1=basebc[:], op=ALU.add),
              [bppb, bbase], [bbase])
        kb.op("dve", lambda e: e.scalar_tensor_tensor(out=pos[:], in0=pos[:], scalar=float(CAP), in1=er[:, 0, :],
                                                      op0=ALU.min, op1=ALU.add), [bpos, ber], [bpos])
        kb.op("dve", lambda e: e.tensor_tensor(out=ohk[:], in0=ohk[:],
                                               in1=er[:, 1, :].unsqueeze(1).to_broadcast([128, 4, NE]),
                                               op=ALU.mult), [bohk, ber], [bohk])
        kb.op("dve", lambda e: e.tensor_tensor(out=prod[:], in0=ohk[:],
                                               in1=pos[:].unsqueeze(1).to_broadcast([128, 4, NE]),
                                               op=ALU.mult), [bohk, bpos], [bprod])
        kb.op("dve", lambda e: e.tensor_reduce(out=dstf[:, ti, :], in_=prod[:], axis=AX.X, op=ALU.add),
              [bprod], [bdstf])
        kb.op("dve", lambda e: e.tensor_reduce(out=st[:, 24:28], in_=ohk[:], axis=AX.X, op=ALU.add),
              [bohk], [bst])
        kb.op("dve", lambda e: e.tensor_scalar(out=st[:, 28:32], in0=st[:, 24:28], scalar1=-float(TRASH),
                                               scalar2=float(TRASH), op0=ALU.mult, op1=ALU.add), [bst], [bst])
        kb.op("dve", lambda e: e.tensor_tensor(out=dstf[:, ti, :], in0=dstf[:, ti, :], in1=st[:, 28:32], op=ALU.add),
              [bdstf, bst], [bdstf])
        kb.op("dve", lambda e: e.scalar_tensor_tensor(out=gkall[:, ti, :], in0=st[:, 20:24], scalar=st[:, 18:19],
                                                      in1=st[:, 24:28], op0=ALU.mult, op1=ALU.mult), [bst], [bgk])
        kb.op("dve", lambda e: e.tensor_copy(out=dsti[:, ti, :], in_=dstf[:, ti, :]), [bdstf], [bdsti])
        for k in range(4):
            _dma_ind(kb, Xs[:, :], bass.IndirectOffsetOnAxis(ap=dsti[:, ti, k:k + 1], axis=0), hb, None,
                     [bR2, bdsti], [bXs])

    merge_deps(bxrows, [b for _, b in modw_bufs])
    merge_deps(bxeT, [b for _, b in modw_bufs])
    merge_deps(bactT, [bg1, bsh2, bA2, bnwb])
    for _, b in W2B:
        merge_deps(b, [bwo, scb_t[1]] + [bb for _, bb in modb_bufs])
    tts = [(tmpA[:, 0:512], Buf("tt0")), (tmpA[:, 512:1024], Buf("tt1"))]
    lps = [(tmpB[:, 0:512], Buf("lp0")), (tmpB[:, 512:1024], Buf("lp1"))]
    t2s = [(xt[:, 0:512], Buf("t20")), (xt[:, 512:1024], Buf("t21"))]
    for (_, b), src_ in zip(tts + lps + t2s, [btmpA, btmpA, btmpB, btmpB, bxt, bxt]):
        merge_deps(b, [src_])
    C0 = float(LIMIT * ALPHA / (1.0 + np.exp(-LIMIT * ALPHA)))
    w1_issued = [0]

    def issue_w1(upto):
        while w1_issued[0] < min(upto, n_exp * 8):
            i = w1_issued[0]
            t, b = W1R[i % 3]
            kb.dma("pool", t.rearrange("p a b -> p (a b)"), w1[i // 8, i % 8, :, :], [dr], [b])
            w1_issued[0] += 1

    def issue_w2(ei):
        if ei < n_exp:
            t, b = W2B[ei % 2]
            src = w2[ei].rearrange("(fc p) d -> p fc d", p=128)
            for fc in range(8):
                kb.dma("pool", t[:, fc, :], src[:, fc, :], [dr], [b])

    nu = 0
    ny = 0
    ntp = 0
    issue_w2(0)
    for e in range(e_lo, e_hi):
        ei = e - e_lo
        issue_w2(ei + 1)
        b2b, bb2b = b2bs[ei % 2]
        kb.dma("sp", b2b[:], b2[e:e + 1, :].to_broadcast([128, D]), [dr], [bb2b])
        kb.dma("sp", xrows, Xs[e * ESTRIDE:e * ESTRIDE + CAP, :].rearrange("(i p) d -> p i d", p=128),
               [bXs], [bxrows])
        for i in range(8):
            pt, bpt = bank(6 + ntp % 2)
            ntp += 1
            ptb = pt.bitcast(BF16)
            for kc in range(8):
                kb.op("pe", lambda en, kc=kc, i=i: en.transpose(out=ptb[:, kc * 128:(kc + 1) * 128],
                                                                in_=xrows[:, i, kc * 128:(kc + 1) * 128],
                                                                identity=idb[:]), [bxrows, bidb], [bpt])
            kb.op("act", lambda en, i=i: en.copy(out=xeT[:, :, i * 128:(i + 1) * 128],
                                                 in_=ptb.rearrange("p (a b) -> p a b", a=8)), [bpt], [bxeT])
        for ft in range(8):
            gi = ei * 8 + ft
            issue_w1(gi + 2)
            wv, bwt = W1R[gi % 3]
            bcol = (e * 8 + ft) * 2
            for blk in range(CAP // 512):
                ts_ = slice(blk * 512, (blk + 1) * 512)
                pgl, bpgl = bank(0 + 2 * (nu % 2))
                pli, bpli = bank(1 + 2 * (nu % 2))
                for kc in range(8):
                    kb.op("pe", lambda en, kc=kc: en.matmul(pgl, wv[:, kc, 0:128], xeT[:, kc, ts_],
                                                            start=(kc == 0), stop=(kc == 7)), [bwt, bxeT], [bpgl])
                for kc in range(8):
                    kb.op("pe", lambda en, kc=kc: en.matmul(pli, wv[:, kc, 128:256], xeT[:, kc, ts_],
                                                            start=(kc == 0), stop=(kc == 7)), [bwt, bxeT], [bpli])
                tt, btt = tts[nu % 2]
                lp, blp = lps[nu % 2]
                t2, bt2 = t2s[nu % 2]
                kb.op("act", lambda en: en.activation(out=tt, in_=pgl, func=AF.Silu, scale=ALPHA,
                                                      bias=b1a[:, bcol:bcol + 1]), [bpgl, bb1a], [btt])
                kb.op("dve", lambda en: en.tensor_scalar(out=lp, in0=pli, scalar1=b1a[:, bcol + 1:bcol + 2],
                                                         scalar2=LIMIT + 1.0, op0=ALU.add, op1=ALU.min),
                      [bpli, bb1a], [blp])
                kb.op("dve", lambda en: en.tensor_scalar(out=t2, in0=tt, scalar1=C0, scalar2=1.0 / ALPHA,
                                                         op0=ALU.min, op1=ALU.mult), [btt], [bt2])
                kb.op("dve", lambda en: en.scalar_tensor_tensor(out=actT[:, ft, ts_], in0=lp, scalar=1.0 - LIMIT,
                                                                in1=t2, op0=ALU.max, op1=ALU.mult),
                      [blp, bt2], [bactT])
                nu += 1
        w2v, bw2 = W2B[ei % 2]
        for i in range(8):
            yr, byr = yrows[ny % 2]
            ny += 1
            for db in range(2):
                py, bpy = bank(4 + db)
                for fc in range(8):
                    kb.op("pe", lambda en, fc=fc, i=i, db=db: en.matmul(py, actT[:, fc, i * 128:(i + 1) * 128],
                                                                        w2v[:, fc, db * 512:(db + 1) * 512],
                                                                        start=(fc == 0), stop=(fc == 7)),
                          [bactT, bw2], [bpy])
                kb.op("dve", lambda en, db=db: en.tensor_tensor(out=yr[:, db * 512:(db + 1) * 512], in0=py,
                                                                in1=b2b[:, db * 512:(db + 1) * 512], op=ALU.add),
                      [bpy, bb2b], [byr])
            kb.dma("sp", Ys[e * ESTRIDE + i * 128:e * ESTRIDE + (i + 1) * 128, :], yr[:], [byr], [bYs])

    tf, btf = tts[0]
    for ti in range(NT):
        rows = slice(ti * 128, (ti + 1) * 128)
        if first:
            kb.dma("sp", yt[:], xo[rows, :], [bxo], [byt])
        else:
            kb.dma("sp", yt[:], xprev[rows, :], [dr], [byt])
        for k in range(4):
            gr, bgr = grows[k]
            _dma_ind(kb, gr[:], None, Ys[:, :], bass.IndirectOffsetOnAxis(ap=dsti[:, ti, k:k + 1], axis=0),
                     [bYs, bdsti], [bgr])
        g0, bg0 = grows[0]
        kb.op("dve", lambda en: en.tensor_scalar(out=g0[:], in0=g0[:], scalar1=gkall[:, ti, 0:1], scalar2=None,
                                                 op0=ALU.mult), [bg0, bgk], [bg0])
        for k in range(1, 4):
            gr, bgr = grows[k]
            kb.op("dve", lambda en, k=k, gr=gr: en.scalar_tensor_tensor(out=g0[:], in0=gr[:],
                                                                        scalar=gkall[:, ti, k:k + 1], in1=g0[:],
                                                                        op0=ALU.mult, op1=ALU.add),
                  [bgr, bgk, bg0], [bg0])
        kb.op("dve", lambda en: en.tensor_tensor(out=g0[:], in0=g0[:], in1=g2[:], op=ALU.mult), [bg0, bg2], [bg0])
        kb.op("dve", lambda en: en.tensor_tensor(out=yt[:], in0=g0[:], in1=yt[:], op=ALU.add), [bg0, byt], [byt])
        kb.dma("sp", xo[rows, :], yt[:], [byt], [bxo])
    kb.finish([bxo])
    return nc


def run_l4r(x, ycat, c, mod_w_l, mod_b_l, normw_l, w_out_l, rw_l, rb_l, w1r, b1r, w2_l, b2_l,
            splits=((0, 16), (16, 32))):
    i = np.arange(128)
    cst = np.ascontiguousarray(np.stack([np.eye(128, dtype=np.float32),
                                         (i[:, None] < i[None, :]).astype(np.float32),
                                         np.ones((128, 128), np.float32)], axis=1))
    xprev = None
    for (lo, hi) in splits:
        first = (lo == 0)
        nc = _get("l4r_%d_%d" % (lo, hi), lambda: build_l4r(lo, hi, first))
        w1s = np.ascontiguousarray(w1r[lo:hi])
        w2s = np.ascontiguousarray(w2_l[lo:hi])
        erow = np.stack([np.arange(NE) * ESTRIDE, ((np.arange(NE) >= lo) & (np.arange(NE) < hi))]).astype(np.float32)
        maps = []
        for cid in range(NCORES):
            m = {"x": core_tokens(x, cid), "ycat": core_tokens(ycat, cid), "cT": cT_of(c, cid // 4),
                 "mod_w": mod_w_l, "mod_b": mod_b_l.reshape(1, -1), "normw": normw_l.reshape(1, -1),
                 "w_out": w_out_l, "rw": rw_l, "rb": rb_l.reshape(1, -1), "w1": w1s, "b1": b1r, "w2": w2s,
                 "b2": b2_l, "cst": cst, "erow": erow}
            if not first:
                m["xprev"] = xprev[cid]
            maps.append(m)
        res = _run(nc, maps)
        xprev = [r["xo"] for r in res.results]
    return uncore_tokens(xprev, (D,))
```
